# Optimizing a Trainium2 kernel written in Bass

```python
import math
import jax
import jax.numpy as jnp
from jax import lax
import numpy as np

D_MODEL = 1024
BATCH = 2
SEQ = 8192
DEPTH = 1

CTX_LEN = 256
GRID_W = 64
POS_BASE = 10000.0
NORM_EPS = 1e-6
N_ADA = 6

HY_WIDTH = 512
HY_ORDER = 2
HY_PROJ = (HY_ORDER + 1) * HY_WIDTH
HY_SHORT = 3
HY_EMB = 33
HY_BANDS = (HY_EMB - 1) // 2
HY_FILTER_HIDDEN = 64
HY_N_FILTERS = HY_ORDER * 2 * HY_WIDTH
HY_DECAY_TARGET = 1e-2
HY_FAST_PCT = 0.3
HY_SLOW_PCT = 1.5
HY_MIN_DECAY = math.log(HY_DECAY_TARGET) / HY_SLOW_PCT
HY_MAX_DECAY = math.log(HY_DECAY_TARGET) / HY_FAST_PCT

HG_HEADS = 4
HG_DK = 128
HG_DV = 128
HG_KW = HG_HEADS * HG_DK
HG_WIDTH = HG_HEADS * HG_DV
HG_CHUNK = 64

IN_SIZES = (HY_PROJ, HG_KW, HG_WIDTH, HG_KW, HG_KW, HG_WIDTH, D_MODEL, D_MODEL)
IN_COLS = sum(IN_SIZES)
IN_SPLITS = tuple(int(v) for v in np.cumsum(IN_SIZES)[:-1])

N_EXPERTS = 256
TOP_K = 8
N_GROUPS = 8
TOPK_GROUPS = 4
EXPERT_HIDDEN = 256
SHARED_HIDDEN = 256
ROUTED_SCALE = 2.5
MOE_BLOCK = 128

kernel_name = 'hyena_hgrn2_moe_prefix_dit_block'


def rmsnorm(x, g):
    xf = x.astype(jnp.float32)
    y = xf * lax.rsqrt(jnp.mean(xf * xf, axis=-1, keepdims=True) + NORM_EPS)
    return (y * g.astype(jnp.float32)).astype(x.dtype)


def modulate(h, shift, scale):
    return h * (1 + scale) + shift


def grid_pos_embed(rows, cols, dim):
    quarter = dim // 4
    omega = 1.0 / (POS_BASE ** (jnp.arange(quarter, dtype=jnp.float32) / quarter))
    ang_r = jnp.arange(rows, dtype=jnp.float32)[:, None] * omega
    ang_c = jnp.arange(cols, dtype=jnp.float32)[:, None] * omega
    emb_r = jnp.concatenate([jnp.sin(ang_r), jnp.cos(ang_r)], axis=-1)
    emb_c = jnp.concatenate([jnp.sin(ang_c), jnp.cos(ang_c)], axis=-1)
    emb = jnp.concatenate([jnp.broadcast_to(emb_r[:, None], (rows, cols, dim // 2)),
                           jnp.broadcast_to(emb_c[None], (rows, cols, dim // 2))], axis=-1)
    return emb.reshape(rows * cols, dim)


def short_conv(u, w, b):
    L = u.shape[1]
    half = HY_SHORT // 2
    up = jnp.pad(u, ((0, 0), (half, half), (0, 0)))
    return sum(up[:, j:j + L] * w[j] for j in range(HY_SHORT)) + b


def hyena_filters(L, w1, b1, w2, b2, w3, b3, w4, freq):
    f32 = lambda a: a.astype(jnp.float32)
    t = jnp.arange(L, dtype=jnp.float32)
    tn = t / max(L - 1, 1)
    ang = (2 * math.pi / L) * t[:, None] * jnp.linspace(1e-4, HY_BANDS - 1, HY_BANDS, dtype=jnp.float32)
    z = jnp.concatenate([tn[:, None], jnp.cos(ang), -jnp.sin(ang)], axis=-1)
    fr = f32(freq)
    a = jnp.sin(fr * (z @ f32(w1) + f32(b1)))
    a = jnp.sin(fr * (a @ f32(w2) + f32(b2)))
    a = jnp.sin(fr * (a @ f32(w3) + f32(b3)))
    h = (a @ f32(w4)).reshape(L, HY_ORDER, 2, HY_WIDTH)
    deltas = jnp.abs(jnp.linspace(HY_MIN_DECAY, HY_MAX_DECAY, HY_WIDTH, dtype=jnp.float32))
    return h * jnp.exp(-tn[:, None] * deltas)[:, None, None, :]


def long_conv(u, h_fwd, h_bwd, skip):
    L = u.shape[1]
    kern = jnp.concatenate([h_fwd, jnp.zeros_like(h_fwd[:1]), h_bwd[:0:-1]], axis=0)
    kern = kern / jnp.sum(jnp.abs(kern), axis=0, keepdims=True)
    uf = u.astype(jnp.float32)
    spec = jnp.fft.rfft(uf, n=2 * L, axis=1) * jnp.fft.rfft(kern, n=2 * L, axis=0)[None]
    y = jnp.fft.irfft(spec, n=2 * L, axis=1)[:, :L]
    return (y + uf * skip.astype(jnp.float32)).astype(u.dtype)


def hyena_branch(p_hy, conv_w, conv_b, filt, skip):
    u = short_conv(p_hy, conv_w, conv_b)
    v, x1, x2 = jnp.split(u, HY_ORDER + 1, axis=-1)
    z = x1 * long_conv(v, filt[:, 0, 0], filt[:, 0, 1], skip[0])
    return x2 * long_conv(z, filt[:, 1, 0], filt[:, 1, 1], skip[1])


def hgrn2_chunk_scan(q, k, v, logf, s0):
    B, L, H, DK = q.shape
    DV = v.shape[-1]
    C = HG_CHUNK
    n = L // C
    q, k, v, logf = [a.reshape(B, n, C, H, a.shape[-1]) for a in (q, k, v, logf)]
    b = jnp.cumsum(logf, axis=2)
    b_end = b[:, :, -1]
    b_ref = b[:, :, C // 2 - 1][:, :, None]
    qd = q * jnp.exp(b - b_ref)
    kd = k * jnp.exp(b_ref - b)
    tri = jnp.tril(jnp.ones((C, C), dtype=bool))
    att = jnp.where(tri, jnp.einsum('bnthd,bnshd->bnhts', qd, kd), 0.0)
    o_intra = jnp.einsum('bnhts,bnshe->bnthe', att, v)
    delta = jnp.einsum('bnshd,bnshe->bnhde', k * jnp.exp(b_end[:, :, None] - b), v)
    decay = jnp.exp(b_end)

    def step(S, xs):
        dec, dlt = xs
        return dec[..., None] * S + dlt, S

    s_fin, s_start = lax.scan(step, s0, (jnp.moveaxis(decay, 1, 0), jnp.moveaxis(delta, 1, 0)))
    o_inter = jnp.einsum('bnthd,nbhde->bnthe', q * jnp.exp(b), s_start)
    return (o_intra + o_inter).reshape(B, L, H, DV), s_fin


def hgrn2_branch(q_raw, i_raw, ff_raw, fb_raw, g_raw, lb_f, lb_b, norm_g, s0_f, s0_b):
    B, L, _ = q_raw.shape
    f32 = lambda a: a.astype(jnp.float32)
    q = jax.nn.silu(f32(q_raw)).reshape(B, L, HG_HEADS, HG_DK)
    v = f32(i_raw).reshape(B, L, HG_HEADS, HG_DV)

    def gates(f_raw, lb):
        f = (lb + (1 - lb) * jax.nn.sigmoid(f32(f_raw))).reshape(B, L, HG_HEADS, HG_DK)
        return 1 - f, jnp.log(f)

    k_f, lf_f = gates(ff_raw, lb_f)
    k_b, lf_b = gates(fb_raw, lb_b)
    rev = lambda a: jnp.flip(a, axis=1)
    o_f, s_f = hgrn2_chunk_scan(q, k_f, v, lf_f, s0_f)
    o_b, s_b = hgrn2_chunk_scan(rev(q), rev(k_b), rev(v), rev(lf_b), s0_b)
    o = o_f + rev(o_b)
    o = o * lax.rsqrt(jnp.mean(o * o, axis=-1, keepdims=True) + NORM_EPS) * f32(norm_g)
    o = o.reshape(B, L, HG_WIDTH) * jax.nn.silu(f32(g_raw))
    return o.astype(q_raw.dtype), s_f, s_b


def merge_branches(y_hy, y_hg, gate_hy, gate_hg, w_hy_out, w_hg_out, w_out):
    m = jax.nn.sigmoid(gate_hy) * (y_hy @ w_hy_out) + jax.nn.sigmoid(gate_hg) * (y_hg @ w_hg_out)
    return m @ w_out


def swiglu(t, wg, wu, wd):
    return (jax.nn.silu(t @ wg) * (t @ wu)) @ wd


def moe_ffn(h, router_w, router_bias, w_gate, w_up, w_down, sh_gate, sh_up, sh_down):
    Bh, Lh, D = h.shape
    t = h.reshape(-1, D)
    T = t.shape[0]
    scores = jax.nn.sigmoid(t.astype(jnp.float32) @ router_w.astype(jnp.float32))
    biased = scores + router_bias.astype(jnp.float32)
    grp_score = lax.top_k(biased.reshape(T, N_GROUPS, -1), 2)[0].sum(-1)
    _, gidx = lax.top_k(grp_score, TOPK_GROUPS)
    gmask = jnp.any(gidx[..., None] == jnp.arange(N_GROUPS), axis=1)
    emask = jnp.repeat(gmask, N_EXPERTS // N_GROUPS, axis=1)
    _, eidx = lax.top_k(jnp.where(emask, biased, -jnp.inf), TOP_K)
    wsel = jnp.take_along_axis(scores, eidx, axis=1)
    wsel = wsel / jnp.sum(wsel, axis=-1, keepdims=True) * ROUTED_SCALE

    A = T * TOP_K
    eid = eidx.reshape(-1)
    tok = jnp.repeat(jnp.arange(T, dtype=jnp.int32), TOP_K)
    order = jnp.argsort(eid)
    eid_s = eid[order]
    counts = jnp.bincount(eid, length=N_EXPERTS)
    padded = (counts + MOE_BLOCK - 1) // MOE_BLOCK * MOE_BLOCK
    starts = jnp.cumsum(counts) - counts
    pad_ends = jnp.cumsum(padded)
    pad_starts = pad_ends - padded
    dest = pad_starts[eid_s] + jnp.arange(A) - starts[eid_s]
    n_blocks = (A + N_EXPERTS * (MOE_BLOCK - 1) + MOE_BLOCK - 1) // MOE_BLOCK
    P = n_blocks * MOE_BLOCK
    tok_buf = jnp.full((P,), T, dtype=jnp.int32).at[dest].set(tok[order])
    w_buf = jnp.zeros((P,), jnp.float32).at[dest].set(wsel.reshape(-1)[order])
    block_e = jnp.minimum(jnp.searchsorted(pad_ends, jnp.arange(n_blocks) * MOE_BLOCK, side='right'),
                          N_EXPERTS - 1)
    t_pad = jnp.concatenate([t, jnp.zeros((1, D), t.dtype)], axis=0)

    def block_fn(args):
        e, toks, wb = args
        xb = t_pad[toks]
        return swiglu(xb, w_gate[e], w_up[e], w_down[e]) * wb[:, None].astype(xb.dtype)

    y = lax.map(block_fn, (block_e, tok_buf.reshape(n_blocks, MOE_BLOCK), w_buf.reshape(n_blocks, MOE_BLOCK)))
    routed = jax.ops.segment_sum(y.reshape(P, D), tok_buf, num_segments=T + 1)[:T]
    out = routed + swiglu(t, sh_gate, sh_up, sh_down)
    return out.reshape(Bh, Lh, D)


def setup_inputs(seed: int = 0) -> dict:
    key = jax.random.key(seed)
    ks = iter(jax.random.split(key, 40))
    nrm = lambda shape, scale: jax.random.normal(next(ks), shape, jnp.float32) * scale
    D = D_MODEL
    NL = DEPTH
    F = EXPERT_HIDDEN
    FS = SHARED_HIDDEN
    HF = HY_FILTER_HIDDEN
    return {
        'x': nrm((BATCH, SEQ, D), 1.0),
        'c': nrm((BATCH, D), 1.0),
        'ctx': nrm((BATCH, CTX_LEN, D), 1.0),
        'c_ctx': nrm((D,), 1.0),
        'norm1_g': 1.0 + nrm((NL, D), 0.02),
        'norm2_g': 1.0 + nrm((NL, D), 0.02),
        'ada_w': nrm((NL, D, N_ADA * D), 0.5 * D ** -0.5),
        'ada_b': nrm((NL, N_ADA * D), 0.02),
        'w_in': nrm((NL, D, IN_COLS), D ** -0.5),
        'hy_conv_w': nrm((NL, HY_SHORT, HY_PROJ), HY_SHORT ** -0.5),
        'hy_conv_b': nrm((NL, HY_PROJ), 0.02),
        'hy_f_w1': nrm((NL, HY_EMB, HF), HY_EMB ** -0.5),
        'hy_f_b1': nrm((NL, HF), 0.02),
        'hy_f_w2': nrm((NL, HF, HF), HF ** -0.5),
        'hy_f_b2': nrm((NL, HF), 0.02),
        'hy_f_w3': nrm((NL, HF, HF), HF ** -0.5),
        'hy_f_b3': nrm((NL, HF), 0.02),
        'hy_f_w4': nrm((NL, HF, HY_N_FILTERS), HF ** -0.5),
        'hy_f_freq': 1.0 + nrm((NL, HF), 0.02),
        'hy_skip': nrm((NL, HY_ORDER, HY_WIDTH), 1.0),
        'hg_lb_logits': nrm((NL + 1, 2, HG_KW), 0.1),
        'hg_norm_g': 1.0 + nrm((NL, HG_DV), 0.02),
        'w_hy_out': nrm((NL, HY_WIDTH, D), HY_WIDTH ** -0.5),
        'w_hg_out': nrm((NL, HG_WIDTH, D), HG_WIDTH ** -0.5),
        'w_out': nrm((NL, D, D), D ** -0.5),
        'router_w': nrm((NL, D, N_EXPERTS), D ** -0.5),
        'router_bias': nrm((NL, N_EXPERTS), 0.01),
        'exp_w_gate': nrm((NL, N_EXPERTS, D, F), D ** -0.5),
        'exp_w_up': nrm((NL, N_EXPERTS, D, F), D ** -0.5),
        'exp_w_down': nrm((NL, N_EXPERTS, F, D), F ** -0.5),
        'sh_w_gate': nrm((NL, D, FS), D ** -0.5),
        'sh_w_up': nrm((NL, D, FS), D ** -0.5),
        'sh_w_down': nrm((NL, FS, D), FS ** -0.5),
        'final_g': 1.0 + nrm((D,), 0.02),
    }


def reference(x, c, ctx, c_ctx, norm1_g, norm2_g, ada_w, ada_b, w_in, hy_conv_w, hy_conv_b,
              hy_f_w1, hy_f_b1, hy_f_w2, hy_f_b2, hy_f_w3, hy_f_b3, hy_f_w4, hy_f_freq, hy_skip,
              hg_lb_logits, hg_norm_g, w_hy_out, w_hg_out, w_out, router_w, router_bias,
              exp_w_gate, exp_w_up, exp_w_down, sh_w_gate, sh_w_up, sh_w_down, final_g):
    B, S, D = x.shape
    rows = S // GRID_W
    x = x + grid_pos_embed(rows, GRID_W, D).astype(x.dtype)
    xc = ctx
    n_ctx = xc.shape[1]
    s_lat = jax.nn.silu(c)
    s_ctx = jax.nn.silu(c_ctx)
    lbs = jnp.cumsum(jax.nn.softmax(hg_lb_logits.astype(jnp.float32), axis=0), axis=0)
    zero_state = jnp.zeros((B, HG_HEADS, HG_DK, HG_DV), jnp.float32)

    for l in range(DEPTH):
        last = l == DEPTH - 1
        sh1, sc1, g1, sh2, sc2, g2 = jnp.split((s_lat @ ada_w[l] + ada_b[l])[:, None, :], N_ADA, axis=-1)
        csh1, csc1, cg1, csh2, csc2, cg2 = jnp.split(s_ctx @ ada_w[l] + ada_b[l], N_ADA, axis=-1)
        filt_w = (hy_f_w1[l], hy_f_b1[l], hy_f_w2[l], hy_f_b2[l], hy_f_w3[l], hy_f_b3[l], hy_f_w4[l], hy_f_freq[l])

        p = jnp.split(modulate(rmsnorm(x, norm1_g[l]), sh1, sc1) @ w_in[l], IN_SPLITS, axis=-1)
        pc = jnp.split(modulate(rmsnorm(xc, norm1_g[l]), csh1, csc1) @ w_in[l], IN_SPLITS, axis=-1)
        lb_f, lb_b = lbs[l, 0], lbs[l, 1]
        hg_c, st_f, st_b = hgrn2_branch(*pc[1:6], lb_f, lb_b, hg_norm_g[l], zero_state, zero_state)
        hg_x, _, _ = hgrn2_branch(*p[1:6], lb_f, lb_b, hg_norm_g[l], st_f, st_b)
        hy_x = hyena_branch(p[0], hy_conv_w[l], hy_conv_b[l], hyena_filters(S, *filt_w), hy_skip[l])
        x = x + g1 * merge_branches(hy_x, hg_x, p[6], p[7], w_hy_out[l], w_hg_out[l], w_out[l])
        if not last:
            hy_c = hyena_branch(pc[0], hy_conv_w[l], hy_conv_b[l], hyena_filters(n_ctx, *filt_w), hy_skip[l])
            xc = xc + cg1 * merge_branches(hy_c, hg_c, pc[6], pc[7], w_hy_out[l], w_hg_out[l], w_out[l])

        moe_w = (router_w[l], router_bias[l], exp_w_gate[l], exp_w_up[l], exp_w_down[l],
                 sh_w_gate[l], sh_w_up[l], sh_w_down[l])
        h2 = modulate(rmsnorm(x, norm2_g[l]), sh2, sc2)
        if last:
            x = x + g2 * moe_ffn(h2, *moe_w)
        else:
            hc2 = modulate(rmsnorm(xc, norm2_g[l]), csh2, csc2)
            y = moe_ffn(jnp.concatenate([hc2, h2], axis=1), *moe_w)
            xc = xc + cg2 * y[:, :n_ctx]
            x = x + g2 * y[:, n_ctx:]

    return rmsnorm(x, final_g)
```

```python
import math
import numpy as np
from contextlib import ExitStack, contextmanager
import concourse.bass as bass
import concourse.mybir as mybir
from concourse.bass_utils import run_bass_kernel_spmd

F32 = mybir.dt.float32
I32 = mybir.dt.int32
U32 = mybir.dt.uint32
ALU = mybir.AluOpType
AF = mybir.ActivationFunctionType
AX = mybir.AxisListType

N_DMA_SEMS = 24
D = 1024
L = 8192
NCTX = 256
LT = L + NCTX
OWN = 2048
NE = 256
NBLK = 383
EPS = 1e-6
TWO_PI = 2.0 * math.pi


class KB:
    def __init__(self, nc, es):
        self.nc = nc
        self.engs = {'pe': nc.tensor, 'act': nc.scalar, 'dve': nc.vector, 'pool': nc.gpsimd, 'sp': nc.sync}
        self.sems = {}
        self.cnt = {}
        for e in self.engs:
            self.sems[e] = es.enter_context(nc.semaphore("s_" + e))
            self.cnt[e] = 0
        for i in range(N_DMA_SEMS):
            self.sems['d%d' % i] = es.enter_context(nc.semaphore("s_d%d" % i))
            self.cnt['d%d' % i] = 0
        self.dnext = 0
        self.waited = {e: {} for e in self.engs}
        self.res = {}
        self.ninstr = 0

    def _need(self, eng, toks):
        best = {}
        for t in toks:
            if t is None:
                continue
            sk, v = t
            if sk == eng and eng == 'pe':
                continue
            if best.get(sk, 0) < v:
                best[sk] = v
        for sk, v in best.items():
            if self.waited[eng].get(sk, 0) >= v:
                continue
            self.engs[eng].wait_ge(self.sems[sk], v)
            self.waited[eng][sk] = v

    def _deps(self, r, w):
        toks = []
        for k in r:
            st = self.res.get(k)
            if st is not None:
                toks.append(st[0])
        for k in w:
            st = self.res.get(k)
            if st is not None:
                toks.append(st[0])
                toks.extend(st[1])
        return toks

    def _commit(self, tok, r, w):
        for k in r:
            st = self.res.setdefault(k, [None, []])
            st[1].append(tok)
            if len(st[1]) > 32:
                best = {}
                for sk, v in st[1]:
                    if best.get(sk, 0) < v:
                        best[sk] = v
                st[1] = list(best.items())
        for k in w:
            self.res[k] = [tok, []]

    def op(self, eng, fn, r=(), w=()):
        self._need(eng, self._deps(r, w))
        ins = fn(self.engs[eng])
        self.cnt[eng] += 1
        ins.then_inc(self.sems[eng], 1)
        self._commit((eng, self.cnt[eng]), r, w)
        self.ninstr += 1

    def dma(self, q, fn, r=(), w=()):
        i = self.dnext
        self.dnext = (self.dnext + 1) % N_DMA_SEMS
        sk = 'd%d' % i
        toks = self._deps(r, w)
        if self.cnt[sk] > 0:
            toks.append((sk, self.cnt[sk]))
        self._need(q, toks)
        ins = fn(self.engs[q])
        self.cnt[sk] += 16
        ins.then_inc(self.sems[sk], 16)
        self._commit((sk, self.cnt[sk]), r, w)
        self.ninstr += 1

    def barrier(self):
        toks = [(sk, v) for sk, v in self.cnt.items() if v > 0]
        for e in self.engs:
            self._need(e, toks)

    def finish(self, eng):
        toks = []
        for st in self.res.values():
            toks.append(st[0])
            toks.extend(st[1])
        self._need(eng, toks)


_CONST = None


def host_consts():
    global _CONST
    if _CONST is not None:
        return _CONST
    c = {}
    quarter = D // 4
    omega = (1.0 / (np.float32(10000.0) ** (np.arange(quarter, dtype=np.float32) / np.float32(quarter)))).astype(np.float32)
    rows, cols = L // 64, 64
    ang_r = (np.arange(rows, dtype=np.float32)[:, None] * omega).astype(np.float32)
    ang_c = (np.arange(cols, dtype=np.float32)[:, None] * omega).astype(np.float32)
    emb_r = np.concatenate([np.sin(ang_r), np.cos(ang_r)], -1)
    emb_c = np.concatenate([np.sin(ang_c), np.cos(ang_c)], -1)
    emb = np.concatenate([np.broadcast_to(emb_r[:, None], (rows, cols, D // 2)),
                          np.broadcast_to(emb_c[None], (rows, cols, D // 2))], -1)
    c['POS'] = np.ascontiguousarray(emb.reshape(L, D).astype(np.float32))
    c['IDENT'] = np.eye(128, dtype=np.float32)
    si = np.arange(128)[:, None]; ti = np.arange(128)[None, :]
    same = (si // 64) == (ti // 64)
    c['TRIF'] = (same & (si <= ti)).astype(np.float32)
    c['TRIB'] = (same & (si >= ti)).astype(np.float32)
    c['ONES'] = np.ones((128, 128), np.float32)
    c['STRI'] = (si < ti).astype(np.float32)
    e1 = np.arange(128)[:, None]; e2 = np.arange(256)[None, :]
    c['SLT'] = np.concatenate([(e1 < e2), (e1 + 128 < e2)], 1).astype(np.float32)
    c['BLK128'] = np.broadcast_to((np.arange(NBLK, dtype=np.float32) * 128.0)[None, :], (128, NBLK)).copy()
    c['PIDX'] = np.arange(128, dtype=np.float32).reshape(128, 1).copy()
    NN = 16384
    a = np.arange(128, dtype=np.float64)
    ang = 2.0 * np.pi * np.outer(a, a) / 128.0
    Fre = np.cos(ang); Fim = -np.sin(ang)
    f32 = lambda v: np.ascontiguousarray(v.astype(np.float32))
    c['FA'] = f32(np.concatenate([Fre, Fim], 1)); c['FAH'] = f32(np.concatenate([Fre, Fim], 1)[64:128])
    c['FRE'] = f32(Fre); c['FIM'] = f32(Fim); c['NFIM'] = f32(-Fim)
    c['CA'] = f32(np.concatenate([Fre, -Fim], 1)); c['CB'] = f32(np.concatenate([Fim, Fre], 1))
    c['FREN'] = f32(Fre[:, :64] / NN); c['FIMN'] = f32(Fim[:, :64] / NN)
    angT = 2.0 * np.pi * np.outer(a, a) / NN
    c['TRE2'] = f32(np.concatenate([np.cos(angT), np.cos(angT)], 1)); c['TIM2'] = f32(np.concatenate([-np.sin(angT), -np.sin(angT)], 1))
    bands = np.linspace(1e-4, 15.0, 16, dtype=np.float32)
    def zfeat(t):
        t = t.astype(np.float32)
        tn = (t / np.float32(L - 1)).astype(np.float32)
        an = (np.float32(2 * math.pi / L) * t[:, None] * bands).astype(np.float32)
        return np.concatenate([tn[:, None], np.cos(an), -np.sin(an)], -1).astype(np.float32), tn
    zf, tnf = zfeat(np.arange(L)); zb, tnb = zfeat(L - np.arange(L))
    c['ZT'] = np.ascontiguousarray(np.concatenate([zf, zb], 1).T)
    lo_ = math.log(1e-2) / 1.5; hi_ = math.log(1e-2) / 0.3
    deltas = np.abs(np.linspace(lo_, hi_, 512, dtype=np.float32))
    decf = np.exp(-tnf[:, None] * deltas).astype(np.float32)
    decb = np.exp(-tnb[:, None] * deltas).astype(np.float32); decb[0] = 0.0
    c['DEC'] = np.ascontiguousarray(np.stack([decf.reshape(64, 128, 512).transpose(0, 2, 1), decb.reshape(64, 128, 512).transpose(0, 2, 1)]))
    _CONST = c
    return c


class Prog:
    def __init__(self, debug=None):
        self.debug = debug or ()
        self.nc = bass.Bass("TRN2", target_bir_lowering=False)
        self.ins = {}
        self.outs = {}

    def inp(self, name, shape, dt=F32):
        t = self.nc.dram_tensor(name, list(shape), dt, kind="ExternalInput").ap()
        self.ins[name] = t
        return t

    def scratch(self, name, shape, dt=F32):
        if name in self.debug:
            t = self.nc.dram_tensor(name, list(shape), dt, kind="ExternalOutput").ap()
            self.outs[name] = t
        else:
            t = self.nc.dram_tensor(name, list(shape), dt, kind="Internal").ap()
        return t


def build(stages=99, debug=None, dummy_mix=False):
    pg = Prog(debug)
    nc = pg.nc
    X = pg.inp("x", [L, D]); CTX = pg.inp("ctx", [NCTX, D]); XOWN = pg.inp("xown", [OWN, D]); POSOWN = pg.inp("posown", [OWN, D])
    CV = pg.inp("c", [D]); CCTX = pg.inp("c_ctx", [D])
    N1G = pg.inp("norm1_g", [D]); N2G = pg.inp("norm2_g", [D])
    ADAW = pg.inp("ada_w", [D, 6 * D]); ADAB = pg.inp("ada_b", [6 * D])
    WIN = pg.inp("w_in", [D, 6144])
    POS = pg.inp("POS", [L, D]); IDENT = pg.inp("IDENT", [128, 128])
    WHY = pg.inp("w_hy_out", [512, D]); WHG = pg.inp("w_hg_out", [512, D]); WOUT = pg.inp("w_out", [D, D])
    SHG = pg.inp("sh_w_gate", [D, 256]); SHU = pg.inp("sh_w_up", [D, 256]); SHD = pg.inp("sh_w_down", [256, D])
    FING = pg.inp("final_g", [D]); OWNIDX = pg.inp("OWNIDX", [128, 4], I32)
    CONVW = pg.inp("hy_conv_w", [3, 1536]); CONVB = pg.inp("hy_conv_b", [1536])
    TRIFd = pg.inp("TRIF", [128, 128]); TRIBd = pg.inp("TRIB", [128, 128]); ONESd = pg.inp("ONES", [128, 128])
    LBL = pg.inp("hg_lb_logits", [2, 1024]); HGNG = pg.inp("hg_norm_g", [128])
    STRId = pg.inp("STRI", [128, 128]); SLTd = pg.inp("SLT", [128, 512]); BLKd = pg.inp("BLK128", [128, NBLK]); PIDXd = pg.inp("PIDX", [128, 1])
    RW = pg.inp("router_w", [D, NE]); RB = pg.inp("router_bias", [NE])
    EWGU = pg.inp("EWGU", [NE * 128, 4096]); EWD = pg.inp("EWD", [NE * 128, 2048])
    FAd = pg.inp("FA", [128, 256]); FAHd = pg.inp("FAH", [64, 256]); FREd = pg.inp("FRE", [128, 128]); FIMd = pg.inp("FIM", [128, 128]); NFIMd = pg.inp("NFIM", [128, 128])
    CAd = pg.inp("CA", [128, 256]); CBd = pg.inp("CB", [128, 256]); FRENd = pg.inp("FREN", [128, 64]); FIMNd = pg.inp("FIMN", [128, 64])
    TRE2d = pg.inp("TRE2", [128, 256]); TIM2d = pg.inp("TIM2", [128, 256]); ZTd = pg.inp("ZT", [66, L]); DECd = pg.inp("DEC", [2, 64, 512, 128])
    FW1 = pg.inp("hy_f_w1", [33, 64]); FB1 = pg.inp("hy_f_b1", [64]); FW2 = pg.inp("hy_f_w2", [64, 64]); FB2 = pg.inp("hy_f_b2", [64])
    FW3 = pg.inp("hy_f_w3", [64, 64]); FB3 = pg.inp("hy_f_b3", [64]); FW4 = pg.inp("hy_f_w4", [64, 2048]); FFQ = pg.inp("hy_f_freq", [64])
    HSKIP = pg.inp("hy_skip", [1024])
    HSPEC = pg.scratch("HSPEC", [1024, 128, 2, 128])
    OUT = pg.nc.dram_tensor("out", [OWN, D], F32, kind="ExternalOutput").ap()
    pg.outs["out"] = OUT
    XNT = pg.scratch("XNT", [D, LT]); XNTOWN = pg.scratch("XNTOWN", [D, OWN])
    MODROW = pg.scratch("MODROW", [2, 6 * D])
    HYRAW = pg.scratch("HYRAW", [1536, L])
    QT = pg.scratch("QT", [512, L]); GT = pg.scratch("GT", [512, L])
    IFF = pg.scratch("IFF", [LT, 1536])
    SGT = pg.scratch("SGT", [2048, OWN])
    HYC = pg.scratch("HYC", [1536, L])
    YHYT = pg.scratch("YHYT", [512, L]); YHGT = pg.scratch("YHGT", [512, L])
    YOWN = pg.scratch("YOWN", [1024, OWN])
    X1D = pg.scratch("X1D", [OWN, D]); H2T = pg.scratch("H2T", [D, OWN])
    SGA = pg.scratch("SGA", [256, OWN]); SUA = pg.scratch("SUA", [256, OWN]); ACTT = pg.scratch("ACTT", [256, OWN])
    SHOUT = pg.scratch("SHOUT", [OWN, D]); ROUTED = pg.scratch("ROUTED", [OWN, D])
    H2 = pg.scratch("H2", [OWN, D]); SCORES = pg.scratch("SCORES", [OWN, NE])
    XS = pg.scratch("XS", [NBLK * 128, D]); YS = pg.scratch("YS", [NBLK * 128, D])

    with ExitStack() as es:
        kb = KB(nc, es)
        sbt = lambda stack, name, shape, dt=F32: stack.enter_context(nc.sbuf_tensor(name, list(shape), dt))
        PA = es.enter_context(nc.psum_tensor("PA", [128, 2048], F32))
        PB = es.enter_context(nc.psum_tensor("PB", [128, 2048], F32))
        banks = [(PA[:, 512 * i:512 * (i + 1)], "PA%d" % i) for i in range(4)] + \
                [(PB[:, 512 * i:512 * (i + 1)], "PB%d" % i) for i in range(4)]
        RB_OWN = nc.gpsimd.alloc_register("bc_own"); nc.gpsimd.reg_mov(RB_OWN, 2047)
        RB_XS = nc.gpsimd.alloc_register("bc_xs"); nc.gpsimd.reg_mov(RB_XS, NBLK * 128 - 1)
        RB_W = nc.gpsimd.alloc_register("bc_w"); nc.gpsimd.reg_mov(RB_W, NE * 128 - 1)
        ident = sbt(es, "ident", [128, 128])
        kb.dma('sp', lambda e: e.dma_start(out=ident[:], in_=IDENT), w=['ident'])
        modT = sbt(es, "modT", [128, 48, 2])
        a1T = sbt(es, "a1T", [128, 8, 2]); sh1T = sbt(es, "sh1T", [128, 8, 2]); g2T = sbt(es, "g2T", [128, 8])
        @contextmanager
        def stage():
            with ExitStack() as st_:
                yield st_
                kb.barrier()

        def dbg(name, ap, shape):
            if name in pg.debug:
                t = nc.dram_tensor("D_" + name, list(shape), F32, kind="ExternalOutput").ap()
                pg.outs["D_" + name] = t
                kb.dma('pool', lambda e: e.dma_start(out=t, in_=ap), r=[name], w=['DBG' + name])

        with stage() as s0:
            sT = sbt(s0, "sT", [128, 8, 2]); vraw = sbt(s0, "vraw", [80, 128]); VT = sbt(s0, "VT", [128, 80])
            kb.dma('sp', lambda e: e.dma_start(out=vraw[0:8, :], in_=CV.rearrange("(q p) -> q p", p=128)), w=['vraw'])
            kb.dma('sp', lambda e: e.dma_start(out=vraw[8:16, :], in_=CCTX.rearrange("(q p) -> q p", p=128)), r=['vraw'], w=['vraw'])
            kb.dma('sp', lambda e: e.dma_start(out=vraw[16:24, :], in_=N1G.rearrange("(q p) -> q p", p=128)), r=['vraw'], w=['vraw'])
            kb.dma('sp', lambda e: e.dma_start(out=vraw[24:32, :], in_=N2G.rearrange("(q p) -> q p", p=128)), r=['vraw'], w=['vraw'])
            kb.dma('sp', lambda e: e.dma_start(out=vraw[32:80, :], in_=ADAB.rearrange("(q p) -> q p", p=128)), r=['vraw'], w=['vraw'])
            ps, psk = banks[0]
            kb.op('pe', lambda e: e.transpose(out=ps[:, 128:208], in_=vraw[:], identity=ident[0:80, 0:80]), r=['vraw', 'ident'], w=[psk])
            kb.op('dve', lambda e: e.tensor_copy(out=VT[:], in_=ps[:, 128:208]), r=[psk], w=['VT'])
            abT = VT[:, 32:80]; g1T = VT[:, 16:24]
            for r_ in range(2):
                kb.op('act', lambda e: e.activation(out=sT[:, :, r_], in_=VT[:, 8 * r_:8 * r_ + 8], func=AF.Silu), r=['VT', 'sT'], w=['sT'])
            kb.op('dve', lambda e: e.tensor_copy(out=g2T[:], in_=VT[:, 24:32]), r=['VT'], w=['g2T'])
            wbufs = [sbt(s0, "adaw%d" % i, [128, 8, 768]) for i in range(2)]
            mrow = sbt(s0, "mrow", [2, 6144]); brow = sbt(s0, "brow", [2, 6144])
            for r_ in range(2):
                kb.dma('sp', lambda e: e.dma_start(out=brow[r_:r_ + 1, :], in_=ADAB.rearrange("(o n) -> o n", o=1)), r=['brow'], w=['brow'])
            for cb in range(8):
                wb = wbufs[cb % 2]; wk = "adaw%d" % (cb % 2)
                kb.dma('sp', lambda e: e.dma_start(out=wb[:], in_=ADAW[:, cb * 768:(cb + 1) * 768].rearrange("(k p) c -> p k c", p=128)), w=[wk])
                for hh in range(2):
                    pr, prk = banks[1 + (2 * cb + hh) % 3]
                    for kk in range(8):
                        kb.op('pe', lambda e: e.matmul(pr[0:2, 0:384], lhsT=sT[:, kk, :], rhs=wb[:, kk, hh * 384:(hh + 1) * 384],
                                                       start=(kk == 0), stop=(kk == 7)), r=[wk, 'sT'], w=[prk])
                    c0 = cb * 768 + hh * 384
                    kb.op('dve', lambda e: e.tensor_tensor(out=mrow[:, c0:c0 + 384], in0=pr[0:2, 0:384], in1=brow[:, c0:c0 + 384], op=ALU.add),
                          r=[prk, 'brow'], w=['mrow'])
            kb.dma('pool', lambda e: e.dma_start(out=MODROW, in_=mrow[:]), r=['mrow'], w=['MODROW'])
            mq = sbt(s0, "mq", [96, 128])
            kb.dma('sp', lambda e: e.dma_start(out=mq[:], in_=MODROW.rearrange("r (q p) -> (r q) p", p=128)), r=['MODROW'], w=['mq'])
            kb.op('pe', lambda e: e.transpose(out=ps[:, 256:352], in_=mq[:], identity=ident[0:96, 0:96]), r=['mq', 'ident'], w=[psk])
            for r_ in range(2):
                kb.op('dve', lambda e: e.tensor_copy(out=modT[:, :, r_], in_=ps[:, 256 + 48 * r_:256 + 48 * r_ + 48]), r=[psk, 'modT'], w=['modT'])
            kb.op('dve', lambda e: e.tensor_scalar(out=a1T[:], in0=modT[:, 8:16, :], scalar1=1.0, scalar2=None, op0=ALU.add),
                  r=['modT'], w=['a1T'])
            for r_ in range(2):
                kb.op('dve', lambda e: e.tensor_tensor(out=a1T[:, :, r_], in0=a1T[:, :, r_], in1=g1T, op=ALU.mult),
                      r=['a1T', 'VT'], w=['a1T'])
            kb.op('dve', lambda e: e.tensor_copy(out=sh1T[:], in_=modT[:, 0:8, :]), r=['modT'], w=['sh1T'])

        dbg('modT', modT[:], [128, 48, 2]); dbg('a1T', a1T[:], [128, 8, 2])
        def norm_to_xt(stack, srcs, XT, a_ap, b_ap, tag, xtk):
            xin = [sbt(stack, "%s_xin%d" % (tag, i), [128, 1024]) for i in range(2)]
            pin = [sbt(stack, "%s_pin%d" % (tag, i), [128, 1024]) for i in range(2)]
            junk = sbt(stack, "%s_junk" % tag, [128, 1024])
            st = [sbt(stack, "%s_st%d" % (tag, i), [128, 4]) for i in range(2)]
            xo = [sbt(stack, "%s_xo%d" % (tag, i), [128, 8, 128]) for i in range(2)]
            for i, (xap, pap, c0) in enumerate(srcs):
                b = i % 2
                xk, pk, sk, ok = "%s_xin%d" % (tag, b), "%s_pin%d" % (tag, b), "%s_st%d" % (tag, b), "%s_xo%d" % (tag, b)
                kb.dma('sp', lambda e: e.dma_start(out=xin[b][:], in_=xap), w=[xk])
                if pap is not None:
                    kb.dma('sp', lambda e: e.dma_start(out=pin[b][:], in_=pap), w=[pk])
                    kb.op('pool', lambda e: e.tensor_tensor(out=xin[b][:], in0=xin[b][:], in1=pin[b][:], op=ALU.add), r=[xk, pk], w=[xk])
                kb.op('act', lambda e: e.activation(out=junk[:], in_=xin[b][:], func=AF.Square, accum_out=st[b][:, 0:1]),
                      r=[xk], w=[tag + '_junk', sk])
                kb.op('dve', lambda e: e.tensor_scalar(out=st[b][:, 1:2], in0=st[b][:, 0:1], scalar1=1.0 / D, scalar2=EPS, op0=ALU.mult, op1=ALU.add),
                      r=[sk], w=[sk])
                kb.op('act', lambda e: e.activation(out=st[b][:, 2:3], in_=st[b][:, 1:2], func=AF.Sqrt), r=[sk], w=[sk])
                kb.op('dve', lambda e: e.reciprocal(out=st[b][:, 3:4], in_=st[b][:, 2:3]), r=[sk], w=[sk])
                kb.op('dve', lambda e: e.tensor_scalar(out=xin[b][:], in0=xin[b][:], scalar1=st[b][:, 3:4], scalar2=None, op0=ALU.mult),
                      r=[xk, sk], w=[xk])
                for h in range(2):
                    pb, pbk = banks[(2 * i + h) % 4]
                    for kk in range(4):
                        k8 = h * 4 + kk
                        kb.op('pe', lambda e: e.transpose(out=pb[:, kk * 128:(kk + 1) * 128], in_=xin[b][:, k8 * 128:(k8 + 1) * 128], identity=ident[:]),
                              r=[xk, 'ident'], w=[pbk])
                    for kk in range(4):
                        k8 = h * 4 + kk
                        kb.op('act', lambda e: e.activation(out=xo[b][:, k8, :], in_=pb[:, kk * 128:(kk + 1) * 128], func=AF.Identity,
                                                            bias=b_ap[:, k8:k8 + 1], scale=a_ap[:, k8:k8 + 1]),
                              r=[pbk, 'a1T', 'sh1T', 'a2T'], w=[ok])
                kb.dma('pool', lambda e: e.dma_start(out=XT[:, c0:c0 + 128].rearrange("(k p) t -> p k t", p=128), in_=xo[b][:]), r=[ok], w=[xtk])

        if stages >= 1:
            with stage() as s1:
                srcs = [(X[i * 128:(i + 1) * 128, :], POS[i * 128:(i + 1) * 128, :], i * 128) for i in range(L // 128)]
                norm_to_xt(s1, srcs, XNT, a1T[:, :, 0], sh1T[:, :, 0], "n1", 'XNT')
            with stage() as s1:
                srcs = [(CTX[i * 128:(i + 1) * 128, :], None, L + i * 128) for i in range(NCTX // 128)]
                norm_to_xt(s1, srcs, XNT, a1T[:, :, 1], sh1T[:, :, 1], "n1c", 'XNT')
            with stage() as s1:
                srcs = [(XOWN[i * 128:(i + 1) * 128, :], POSOWN[i * 128:(i + 1) * 128, :], i * 128) for i in range(OWN // 128)]
                norm_to_xt(s1, srcs, XNTOWN, a1T[:, :, 0], sh1T[:, :, 0], "n1o", 'XNTOWN')

        cp_toggle = [0]

        def evac(out_ap, in_ap, func, r, w):
            if func is None:
                cp_toggle[0] ^= 1
                if cp_toggle[0]:
                    kb.op('dve', lambda e: e.tensor_copy(out=out_ap, in_=in_ap), r=r, w=w)
                else:
                    kb.op('act', lambda e: e.copy(out=out_ap, in_=in_ap), r=r, w=w)
            else:
                kb.op('act', lambda e: e.activation(out=out_ap, in_=in_ap, func=func), r=r, w=w)

        def gemm_group(tag, Wsrc, kch, ncols, XT, xtkey, tblocks, jobs):
            with stage() as st:
                Wsb = sbt(st, tag + "_W", [128, kch, ncols])
                kb.dma('sp', lambda e: e.dma_start(out=Wsb[:], in_=Wsrc.rearrange("(k p) c -> p k c", p=128)), w=[tag + '_W'])
                Xb = [sbt(st, "%s_X%d" % (tag, i), [128, kch, 512]) for i in range(2)]
                stg = [sbt(st, "%s_s%d" % (tag, i), [128, 512]) for i in range(4)]
                si = 0
                bi = 0
                for ti, (t0, tn) in enumerate(tblocks):
                    xb = Xb[ti % 2]; xk = "%s_X%d" % (tag, ti % 2)
                    kb.dma('sp', lambda e: e.dma_start(out=xb[:, :, 0:tn], in_=XT[:, t0:t0 + tn].rearrange("(k p) t -> p k t", p=128)),
                           r=[xtkey], w=[xk])
                    for jb in jobs:
                        if jb.get('tsel') is not None and not jb['tsel'](t0):
                            continue
                        c0, cn, tofs = jb['c0'], jb['cn'], jb.get('tofs', 0)
                        if jb['mode'] == 'fm':
                            for m in range(cn // 128):
                                pb, pbk = banks[bi % 8]; bi += 1
                                for kk in range(kch):
                                    kb.op('pe', lambda e: e.matmul(pb[:, 0:tn], lhsT=Wsb[:, kk, c0 + m * 128:c0 + (m + 1) * 128], rhs=xb[:, kk, 0:tn],
                                                                   start=(kk == 0), stop=(kk == kch - 1)), r=[tag + '_W', xk], w=[pbk])
                                sg = stg[si % 4]; sgk = "%s_s%d" % (tag, si % 4); si += 1
                                evac(sg[:, 0:tn], pb[:, 0:tn], jb['func'], [pbk], [sgk])
                                orow = jb.get('oc0', 0) + m * 128
                                kb.dma('pool', lambda e: e.dma_start(out=jb['out'][orow:orow + 128, t0 - tofs:t0 - tofs + tn], in_=sg[:, 0:tn]),
                                       r=[sgk], w=[jb['okey']])
                        else:
                            for tt in range(tn // 128):
                                pb, pbk = banks[bi % 8]; bi += 1
                                for kk in range(kch):
                                    kb.op('pe', lambda e: e.matmul(pb[:, 0:cn], lhsT=xb[:, kk, tt * 128:(tt + 1) * 128], rhs=Wsb[:, kk, c0:c0 + cn],
                                                                   start=(kk == 0), stop=(kk == kch - 1)), r=[tag + '_W', xk], w=[pbk])
                                sg = stg[si % 4]; sgk = "%s_s%d" % (tag, si % 4); si += 1
                                evac(sg[:, 0:cn], pb[:, 0:cn], jb['func'], [pbk], [sgk])
                                tr = t0 - tofs + tt * 128
                                oc0 = jb.get('oc0', 0)
                                kb.dma('pool', lambda e: e.dma_start(out=jb['out'][tr:tr + 128, oc0:oc0 + cn], in_=sg[:, 0:cn]),
                                       r=[sgk], w=[jb['okey']])

        if stages >= 2:
            lat_blocks = [(t, 512) for t in range(0, L, 512)]
            all_blocks = lat_blocks + [(L, 256)]
            gemm_group("g1", WIN[:, 0:1024], 8, 1024, XNT, 'XNT', lat_blocks,
                       [dict(c0=0, cn=1024, mode='fm', func=None, out=HYRAW, okey='HYRAW', oc0=0)])
            gemm_group("g2", WIN[:, 1024:2048], 8, 1024, XNT, 'XNT', lat_blocks,
                       [dict(c0=0, cn=512, mode='fm', func=None, out=HYRAW, okey='HYRAW', oc0=1024),
                        dict(c0=512, cn=512, mode='fm', func=AF.Silu, out=QT, okey='QT', oc0=0)])
        if stages >= 3:
            gemm_group("g3", WIN[:, 2048:3072], 8, 1024, XNT, 'XNT', all_blocks,
                       [dict(c0=0, cn=512, mode='tm', func=None, out=IFF, okey='IFF', oc0=0),
                        dict(c0=512, cn=512, mode='tm', func=None, out=IFF, okey='IFF', oc0=512)])
            gemm_group("g4", WIN[:, 3072:4096], 8, 1024, XNT, 'XNT', all_blocks,
                       [dict(c0=0, cn=512, mode='tm', func=None, out=IFF, okey='IFF', oc0=1024),
                        dict(c0=512, cn=512, mode='fm', func=AF.Silu, out=GT, okey='GT', oc0=0, tsel=lambda t0: t0 < L)])

        if stages >= 3:
            own_blocks = [(t, 512) for t in range(0, OWN, 512)]
            for gi in range(2):
                gemm_group("g%d" % (5 + gi), WIN[:, 4096 + 1024 * gi:5120 + 1024 * gi], 8, 1024, XNTOWN, 'XNTOWN', own_blocks,
                           [dict(c0=0, cn=1024, mode='fm', func=AF.Sigmoid, out=SGT, okey='SGT', oc0=1024 * gi)])

        if stages >= 4:
            with stage() as st:
                cwT = sbt(st, "cwT", [128, 12, 4]); craw = sbt(st, "craw", [48, 128])
                for j3 in range(3):
                    kb.dma('sp', lambda e: e.dma_start(out=craw[12 * j3:12 * j3 + 12, :], in_=CONVW[j3].rearrange("(q p) -> q p", p=128)), r=['craw'], w=['craw'])
                kb.dma('sp', lambda e: e.dma_start(out=craw[36:48, :], in_=CONVB.rearrange("(q p) -> q p", p=128)), r=['craw'], w=['craw'])
                pb, pbk = banks[0]
                kb.op('pe', lambda e: e.transpose(out=pb[:, 0:48], in_=craw[:], identity=ident[0:48, 0:48]), r=['craw', 'ident'], w=[pbk])
                kb.op('dve', lambda e: e.tensor_copy(out=cwT[:], in_=pb[:, 0:48].rearrange("p (j q) -> p q j", j=4)), r=[pbk], w=['cwT'])
                uin = [sbt(st, "uin%d" % i, [128, L + 2]) for i in range(2)]
                uo = [sbt(st, "uo%d" % i, [128, L]) for i in range(2)]
                for i in range(2):
                    kb.op('pool', lambda e: e.memset(uin[i][:, 0:1], 0.0), w=['uin%d' % i])
                    kb.op('pool', lambda e: e.memset(uin[i][:, L + 1:L + 2], 0.0), r=['uin%d' % i], w=['uin%d' % i])
                for q in range(12):
                    b2 = q % 2; uk = 'uin%d' % b2; ok = 'uo%d' % b2
                    kb.dma('sp', lambda e: e.dma_start(out=uin[b2][:, 1:L + 1], in_=HYRAW[q * 128:(q + 1) * 128, :]), r=['HYRAW', uk], w=[uk])
                    kb.op('dve', lambda e: e.tensor_scalar(out=uo[b2][:], in0=uin[b2][:, 0:L], scalar1=cwT[:, q, 0:1], scalar2=cwT[:, q, 3:4],
                                                           op0=ALU.mult, op1=ALU.add), r=[uk, 'cwT'], w=[ok])
                    kb.op('dve', lambda e: e.scalar_tensor_tensor(out=uo[b2][:], in0=uin[b2][:, 1:L + 1], scalar=cwT[:, q, 1:2], in1=uo[b2][:],
                                                                  op0=ALU.mult, op1=ALU.add), r=[uk, 'cwT', ok], w=[ok])
                    kb.op('dve', lambda e: e.scalar_tensor_tensor(out=uo[b2][:], in0=uin[b2][:, 2:L + 2], scalar=cwT[:, q, 2:3], in1=uo[b2][:],
                                                                  op0=ALU.mult, op1=ALU.add), r=[uk, 'cwT', ok], w=[ok])
                    kb.dma('pool', lambda e: e.dma_start(out=HYC[q * 128:(q + 1) * 128, :], in_=uo[b2][:]), r=[ok], w=['HYC'])

        if stages >= 5 and not dummy_mix:
          with stage() as st:
            tri = {0: sbt(st, "trif", [128, 128]), 1: sbt(st, "trib", [128, 128])}
            ones = sbt(st, "ones", [128, 128])
            kb.dma('sp', lambda e: e.dma_start(out=tri[0][:], in_=TRIFd), w=['tri'])
            kb.dma('sp', lambda e: e.dma_start(out=tri[1][:], in_=TRIBd), r=['tri'], w=['tri'])
            kb.dma('sp', lambda e: e.dma_start(out=ones[:], in_=ONESd), w=['ones'])
            l0 = sbt(st, "l0", [128, 1024]); l1 = sbt(st, "l1", [128, 1024]); oml = sbt(st, "oml", [128, 1024])
            kb.dma('sp', lambda e: e.dma_start(out=l0[:], in_=LBL[0:1, :].to_broadcast([128, 1024])), w=['l0'])
            kb.dma('sp', lambda e: e.dma_start(out=l1[:], in_=LBL[1:2, :].to_broadcast([128, 1024])), w=['l1'])
            kb.op('dve', lambda e: e.tensor_tensor(out=l0[:], in0=l0[:], in1=l1[:], op=ALU.subtract), r=['l0', 'l1'], w=['l0'])
            kb.op('act', lambda e: e.activation(out=l0[:], in_=l0[:], func=AF.Sigmoid), r=['l0'], w=['l0'])
            kb.op('dve', lambda e: e.tensor_scalar(out=oml[:], in0=l0[:], scalar1=-1.0, scalar2=1.0, op0=ALU.mult, op1=ALU.add), r=['l0'], w=['oml'])
            ngT = sbt(st, "ngT", [128, 1])
            with nc.allow_non_contiguous_dma(reason="128-element vector to partitions"):
                kb.dma('sp', lambda e: e.dma_start(out=ngT[:], in_=HGNG.rearrange("(p o) -> p o", o=1)), w=['ngT'])
            osum = sbt(st, "osum", [128, L])
            LB8 = sbt(st, "LB8", [128, 8, 128]); OML8 = sbt(st, "OML8", [128, 8, 128])
            vseg = [sbt(st, "vseg%d" % i, [128, 8, 128]) for i in range(2)]
            fseg = [sbt(st, "fseg%d" % i, [128, 8, 128]) for i in range(2)]
            lseg = [sbt(st, "lseg%d" % i, [128, 8, 128]) for i in range(2)]
            kseg = [sbt(st, "kseg%d" % i, [128, 8, 128]) for i in range(2)]
            qseg = [sbt(st, "qseg%d" % i, [128, 1024]) for i in range(2)]
            R3 = 3
            ekb = [sbt(st, "ekb%d" % i, [128, 128]) for i in range(R3)]
            kd = [sbt(st, "kd%d" % i, [128, 128]) for i in range(R3)]
            ebT = [sbt(st, "ebT%d" % i, [128, 128]) for i in range(R3)]
            qdT = [sbt(st, "qdT%d" % i, [128, 128]) for i in range(R3)]
            kdT = [sbt(st, "kdT%d" % i, [128, 128]) for i in range(R3)]
            attm = [sbt(st, "attm%d" % i, [128, 128]) for i in range(R3)]
            Sr = [sbt(st, "S%d" % i, [128, 128]) for i in range(4)]
            Se = [sbt(st, "Se%d" % i, [128, 128]) for i in range(2)]
            pbi = [0]

            def nb():
                b_ = banks[pbi[0] % 8]; pbi[0] += 1
                return b_

            for h in range(4):
                for dr in range(2):
                    T = tri[dr]
                    fcol = 512 + 512 * dr + h * 128
                    for n in range(8):
                        kb.op('pool', lambda e: e.tensor_copy(out=LB8[:, n, :], in_=l0[:, dr * 512 + h * 128:dr * 512 + (h + 1) * 128]), r=['l0', 'LB8'], w=['LB8'])
                        kb.op('pool', lambda e: e.tensor_copy(out=OML8[:, n, :], in_=oml[:, dr * 512 + h * 128:dr * 512 + (h + 1) * 128]), r=['oml', 'OML8'], w=['OML8'])
                    si_ = 0
                    kb.op('pool', lambda e: e.memset(Sr[0][:], 0.0), w=['S0'])
                    segs = [(L, 2, False)] + [(sg * 1024, 8, True) for sg in (range(8) if dr == 0 else range(7, -1, -1))]
                    tcount = 0
                    for sgi, (r0, nt, lat) in enumerate(segs):
                        b2 = sgi % 2
                        vk, fk, lk, kk_, qk = 'vseg%d' % b2, 'fseg%d' % b2, 'lseg%d' % b2, 'kseg%d' % b2, 'qseg%d' % b2
                        kb.dma('sp', lambda e: e.dma_start(out=vseg[b2][:, 0:nt, :], in_=IFF[r0:r0 + nt * 128, h * 128:(h + 1) * 128].rearrange("(n p) c -> p n c", p=128)),
                               r=['IFF'], w=[vk])
                        kb.dma('sp', lambda e: e.dma_start(out=fseg[b2][:, 0:nt, :], in_=IFF[r0:r0 + nt * 128, fcol:fcol + 128].rearrange("(n p) c -> p n c", p=128)),
                               r=['IFF'], w=[fk])
                        if lat:
                            kb.dma('sp', lambda e: e.dma_start(out=qseg[b2][:], in_=QT[h * 128:(h + 1) * 128, r0:r0 + 1024]), r=['QT'], w=[qk])
                        kb.op('act', lambda e: e.activation(out=fseg[b2][:, 0:nt, :], in_=fseg[b2][:, 0:nt, :], func=AF.Sigmoid), r=[fk], w=[fk])
                        kb.op('dve', lambda e: e.tensor_tensor(out=fseg[b2][:, 0:nt, :], in0=fseg[b2][:, 0:nt, :], in1=OML8[:, 0:nt, :], op=ALU.mult), r=[fk, 'OML8'], w=[fk])
                        kb.op('dve', lambda e: e.tensor_tensor(out=fseg[b2][:, 0:nt, :], in0=fseg[b2][:, 0:nt, :], in1=LB8[:, 0:nt, :], op=ALU.add), r=[fk, 'LB8'], w=[fk])
                        kb.op('act', lambda e: e.activation(out=lseg[b2][:, 0:nt, :], in_=fseg[b2][:, 0:nt, :], func=AF.Ln), r=[fk], w=[lk])
                        kb.op('dve', lambda e: e.tensor_scalar(out=kseg[b2][:, 0:nt, :], in0=fseg[b2][:, 0:nt, :], scalar1=-1.0, scalar2=1.0, op0=ALU.mult, op1=ALU.add),
                              r=[fk], w=[kk_])
                        order = range(nt) if dr == 0 else range(nt - 1, -1, -1)
                        for n in order:
                            r3 = tcount % R3; tcount += 1
                            ek, kdk, ebk, qdk, ktk, amk = 'ekb%d' % r3, 'kd%d' % r3, 'ebT%d' % r3, 'qdT%d' % r3, 'kdT%d' % r3, 'attm%d' % r3
                            p1, p1k = nb()
                            kb.op('pe', lambda e: e.matmul(p1[:, 0:128], lhsT=T[:], rhs=lseg[b2][:, n, :], start=True, stop=True), r=['tri', lk], w=[p1k])
                            kb.op('act', lambda e: e.activation(out=ekb[r3][:], in_=p1[:, 0:128], func=AF.Exp, scale=-1.0), r=[p1k], w=[ek])
                            kb.op('dve', lambda e: e.tensor_tensor(out=kd[r3][:], in0=kseg[b2][:, n, :], in1=ekb[r3][:], op=ALU.mult), r=[kk_, ek], w=[kdk])
                            p2, p2k = nb()
                            kb.op('pe', lambda e: e.matmul(p2[:, 0:128], lhsT=lseg[b2][:, n, :], rhs=T[:], start=True, stop=True), r=['tri', lk], w=[p2k])
                            kb.op('act', lambda e: e.activation(out=ebT[r3][:], in_=p2[:, 0:128], func=AF.Exp), r=[p2k], w=[ebk])
                            if lat:
                                kb.op('dve', lambda e: e.tensor_tensor(out=qdT[r3][:], in0=qseg[b2][:, n * 128:(n + 1) * 128], in1=ebT[r3][:], op=ALU.mult), r=[qk, ebk], w=[qdk])
                                p3, p3k = nb()
                                kb.op('pe', lambda e: e.transpose(out=p3[:, 0:128], in_=kd[r3][:], identity=ident[:]), r=[kdk, 'ident'], w=[p3k])
                                kb.op('act', lambda e: e.copy(out=kdT[r3][:], in_=p3[:, 0:128]), r=[p3k], w=[ktk])
                                p4, p4k = nb()
                                kb.op('pe', lambda e: e.matmul(p4[:, 0:128], lhsT=kdT[r3][:], rhs=qdT[r3][:], start=True, stop=True), r=[ktk, qdk], w=[p4k])
                                kb.op('dve', lambda e: e.tensor_tensor(out=attm[r3][:], in0=p4[:, 0:128], in1=T[:], op=ALU.mult), r=[p4k, 'tri'], w=[amk])
                                po, pok = nb()
                                kb.op('pe', lambda e: e.matmul(po[:, 0:128], lhsT=vseg[b2][:, n, :], rhs=attm[r3][:], start=True, stop=False), r=[vk, amk], w=[pok])
                            halves = [(0, 64, 63), (64, 128, 127)] if dr == 0 else [(64, 128, 64), (0, 64, 0)]
                            for (c0, c1, ce) in halves:
                                Sc = Sr[si_ % 4]; Sck = 'S%d' % (si_ % 4)
                                Sn = Sr[(si_ + 1) % 4]; Snk = 'S%d' % ((si_ + 1) % 4)
                                sew = Se[si_ % 2]; sek = 'Se%d' % (si_ % 2)
                                si_ += 1
                                if lat:
                                    kb.op('pe', lambda e: e.matmul(po[:, c0:c1], lhsT=Sc[:], rhs=qdT[r3][:, c0:c1], start=False, stop=True), r=[Sck, qdk], w=[pok])
                                pd, pdk = nb()
                                kb.op('pe', lambda e: e.matmul(pd[:, 0:128], lhsT=kd[r3][c0:c1, :], rhs=vseg[b2][c0:c1, n, :], start=True, stop=True), r=[kdk, vk], w=[pdk])
                                kb.op('act', lambda e: e.activation(out=sew[:], in_=Sc[:], func=AF.Copy, scale=ebT[r3][:, ce:ce + 1]), r=[Sck, ebk], w=[sek])
                                kb.op('dve', lambda e: e.scalar_tensor_tensor(out=Sn[:], in0=pd[:, 0:128], scalar=ebT[r3][:, ce:ce + 1], in1=sew[:], op0=ALU.mult, op1=ALU.add),
                                      r=[pdk, ebk, sek], w=[Snk])
                            if lat:
                                cols = slice(r0 + n * 128, r0 + (n + 1) * 128)
                                if dr == 0:
                                    kb.op('act', lambda e: e.copy(out=osum[:, cols], in_=po[:, 0:128]), r=[pok], w=['osum%d' % (r0 // 1024)])
                                else:
                                    kb.op('dve', lambda e: e.tensor_tensor(out=osum[:, cols], in0=po[:, 0:128], in1=osum[:, cols], op=ALU.add),
                                          r=[pok, 'osum%d' % (r0 // 1024)], w=['osum%d' % (r0 // 1024)])
                    if si_ % 4 != 0:
                        pass
                    kb.res.pop('unused', None)
                sq = [sbt(st, "hsq%d_%d" % (h, i), [128, 512]) for i in range(2)] if h == 0 else sq
                gt_ = [sbt(st, "hgt%d_%d" % (h, i), [128, 512]) for i in range(2)] if h == 0 else gt_
                rs = [sbt(st, "hrs%d_%d" % (h, i), [128, 512]) for i in range(2)] if h == 0 else rs
                for blk in range(L // 512):
                    b2 = blk % 2; cols = slice(blk * 512, (blk + 1) * 512)
                    sqk, gk, rk = 'hsq%d' % b2, 'hgt%d' % b2, 'hrs%d' % b2
                    ok_ = 'osum%d' % (blk // 2)
                    kb.dma('sp', lambda e: e.dma_start(out=gt_[b2][:], in_=GT[h * 128:(h + 1) * 128, cols]), r=['GT'], w=[gk])
                    kb.op('act', lambda e: e.activation(out=sq[b2][:], in_=osum[:, cols], func=AF.Square), r=[ok_], w=[sqk])
                    pb, pbk = nb()
                    kb.op('pe', lambda e: e.matmul(pb[:, :], lhsT=ones[:], rhs=sq[b2][:], start=True, stop=True), r=['ones', sqk], w=[pbk])
                    kb.op('dve', lambda e: e.tensor_scalar(out=rs[b2][:], in0=pb[:, :], scalar1=1.0 / 128, scalar2=EPS, op0=ALU.mult, op1=ALU.add), r=[pbk], w=[rk])
                    kb.op('act', lambda e: e.activation(out=rs[b2][:], in_=rs[b2][:], func=AF.Sqrt), r=[rk], w=[rk])
                    kb.op('dve', lambda e: e.reciprocal(out=rs[b2][:], in_=rs[b2][:]), r=[rk], w=[rk])
                    kb.op('dve', lambda e: e.scalar_tensor_tensor(out=sq[b2][:], in0=osum[:, cols], scalar=ngT[:, 0:1], in1=rs[b2][:], op0=ALU.mult, op1=ALU.mult),
                          r=[ok_, 'ngT', rk, sqk], w=[sqk])
                    kb.op('pool', lambda e: e.tensor_tensor(out=sq[b2][:], in0=sq[b2][:], in1=gt_[b2][:], op=ALU.mult), r=[sqk, gk], w=[sqk])
                    kb.dma('pool', lambda e: e.dma_start(out=YHGT[h * 128:(h + 1) * 128, cols], in_=sq[b2][:]), r=[sqk], w=['YHGT'])

        if stages >= 5 and not dummy_mix:
          with stage() as st:
            cst = {}
            for nm, src, shp in (("FA", FAd, [128, 256]), ("FAH", FAHd, [64, 256]), ("FRE", FREd, [128, 128]), ("FIM", FIMd, [128, 128]), ("NFIM", NFIMd, [128, 128]),
                                 ("CA", CAd, [128, 256]), ("CB", CBd, [128, 256]), ("FREN", FRENd, [128, 64]), ("FIMN", FIMNd, [128, 64]),
                                 ("TRE2", TRE2d, [128, 256]), ("TIM2", TIM2d, [128, 256]), ("ONESH", ONESd, [128, 128])):
                cst[nm] = sbt(st, "c_" + nm, shp)
                kb.dma('sp', lambda e: e.dma_start(out=cst[nm][:], in_=src), w=['hconst'])
            skb = sbt(st, "skb", [128, 1024])
            kb.dma('sp', lambda e: e.dma_start(out=skb[:], in_=HSKIP.rearrange("(o n) -> o n", o=1).to_broadcast([128, 1024])), w=['skb'])
            tw = [sbt(st, "tw%d" % i, [128, 256]) for i in range(4)]
            hbi = [0]

            def hb():
                b_ = banks[hbi[0] % 8]; hbi[0] += 1
                return b_

            def twiddle(ps, psk, Bt, bkey, c0, conj):
                A = ps[:, :].rearrange("p (c r f) -> p c r f", c=2, r=2)
                Are, Aim = A[:, :, 0, :], A[:, :, 1, :]
                T2r = cst["TRE2"][:].rearrange("p (c f) -> p c f", c=2); T2i = cst["TIM2"][:].rearrange("p (c f) -> p c f", c=2)
                t = [x[:].rearrange("p (c f) -> p c f", c=2) for x in tw]
                kb.op('dve', lambda e: e.tensor_tensor(out=t[0], in0=Are, in1=T2r, op=ALU.mult), r=[psk, 'hconst', 'tw0'], w=['tw0'])
                kb.op('dve', lambda e: e.tensor_tensor(out=t[1], in0=Aim, in1=T2i, op=ALU.mult), r=[psk, 'hconst', 'tw1'], w=['tw1'])
                kb.op('dve', lambda e: e.tensor_tensor(out=t[2], in0=Are, in1=T2i, op=ALU.mult), r=[psk, 'hconst', 'tw2'], w=['tw2'])
                kb.op('dve', lambda e: e.tensor_tensor(out=t[3], in0=Aim, in1=T2r, op=ALU.mult), r=[psk, 'hconst', 'tw3'], w=['tw3'])
                if not conj:
                    kb.op('pool', lambda e: e.tensor_tensor(out=Bt[:, c0:c0 + 2, 0, :], in0=t[0], in1=t[1], op=ALU.subtract), r=['tw0', 'tw1', bkey], w=[bkey])
                    kb.op('pool', lambda e: e.tensor_tensor(out=Bt[:, c0:c0 + 2, 1, :], in0=t[2], in1=t[3], op=ALU.add), r=['tw2', 'tw3', bkey], w=[bkey])
                else:
                    kb.op('pool', lambda e: e.tensor_tensor(out=Bt[:, c0:c0 + 2, 0, :], in0=t[0], in1=t[1], op=ALU.add), r=['tw0', 'tw1', bkey], w=[bkey])
                    kb.op('pool', lambda e: e.tensor_tensor(out=Bt[:, c0:c0 + 2, 1, :], in0=t[3], in1=t[2], op=ALU.subtract), r=['tw2', 'tw3', bkey], w=[bkey])

            def stage2(Bt, bkey, c4):
                pr, prk = hb(); pi_, pik = hb()
                Bre, Bim = Bt[:, c4:c4 + 4, 0, :], Bt[:, c4:c4 + 4, 1, :]
                kb.op('pe', lambda e: e.matmul(pr[:, :], lhsT=cst["FRE"][:], rhs=Bre, start=True, stop=False), r=['hconst', bkey], w=[prk])
                kb.op('pe', lambda e: e.matmul(pr[:, :], lhsT=cst["NFIM"][:], rhs=Bim, start=False, stop=True), r=['hconst', bkey], w=[prk])
                kb.op('pe', lambda e: e.matmul(pi_[:, :], lhsT=cst["FIM"][:], rhs=Bre, start=True, stop=False), r=['hconst', bkey], w=[pik])
                kb.op('pe', lambda e: e.matmul(pi_[:, :], lhsT=cst["FRE"][:], rhs=Bim, start=False, stop=True), r=['hconst', bkey], w=[pik])
                return pr, prk, pi_, pik

            B16 = sbt(st, "B16", [128, 16, 2, 128])
            with stage() as sf:
                a3 = sbt(sf, "a3", [128, L])
                w4sb = sbt(sf, "w4sb", [128, 2048])
                kb.dma('sp', lambda e: e.dma_start(out=w4sb[0:64, :], in_=FW4), w=['w4sb'])
                kb.dma('sp', lambda e: e.dma_start(out=w4sb[64:128, :], in_=FW4), r=['w4sb'], w=['w4sb'])
                with stage() as sm:
                    zts = sbt(sm, "zts", [66, L]); bufB = sbt(sm, "bufB", [128, L])
                    kb.dma('sp', lambda e: e.dma_start(out=zts[:], in_=ZTd), w=['zts'])
                    wl = [sbt(sm, "w1bd", [66, 128]), sbt(sm, "w2bd", [128, 128]), sbt(sm, "w3bd", [128, 128])]
                    for i_, (wt, src, kin) in enumerate(zip(wl, (FW1, FW2, FW3), (33, 64, 64))):
                        kb.op('pool', lambda e: e.memset(wt[:], 0.0), w=['wbd%d' % i_])
                        kb.dma('sp', lambda e: e.dma_start(out=wt[0:kin, 0:64], in_=src), r=['wbd%d' % i_], w=['wbd%d' % i_])
                        kb.dma('sp', lambda e: e.dma_start(out=wt[kin:2 * kin, 64:128], in_=src), r=['wbd%d' % i_], w=['wbd%d' % i_])
                    fqb = sbt(sm, "fqb", [128, 4])
                    with nc.allow_non_contiguous_dma(reason="64-element vectors onto partitions"):
                        for j_, src in enumerate((FFQ, FB1, FB2, FB3)):
                            for hh in range(2):
                                kb.dma('sp', lambda e: e.dma_start(out=fqb[64 * hh:64 * hh + 64, j_:j_ + 1], in_=src.rearrange("(p o) -> p o", o=1)), r=['fqb'], w=['fqb'])
                    kb.op('dve', lambda e: e.tensor_scalar(out=fqb[:, 0:1], in0=fqb[:, 0:1], scalar1=1.0 / TWO_PI, scalar2=None, op0=ALU.mult), r=['fqb'], w=['fqb'])
                    kb.op('dve', lambda e: e.tensor_scalar(out=fqb[:, 1:4], in0=fqb[:, 1:4], scalar1=fqb[:, 0:1], scalar2=None, op0=ALU.mult), r=['fqb'], w=['fqb'])
                    uu = sbt(sm, "uu", [128, 2048]); ui = sbt(sm, "ui", [128, 2048], I32); uf = sbt(sm, "uf", [128, 2048])
                    srcs = [(zts, 66, 'zts'), (bufB, 128, 'bufB'), (a3, 128, 'a3')]
                    dsts = [(bufB, 'bufB'), (a3, 'a3'), (bufB, 'bufB')]
                    for l_ in range(3):
                        src_t, kdim, srck = srcs[l_]; dst_t, dstk = dsts[l_]
                        for ch in range(4):
                            Pq, pkeys = (PA, ["PA0", "PA1", "PA2", "PA3"]) if ch % 2 == 0 else (PB, ["PB0", "PB1", "PB2", "PB3"])
                            for q in range(4):
                                cs = slice(ch * 2048 + q * 512, ch * 2048 + (q + 1) * 512)
                                kb.op('pe', lambda e: e.matmul(Pq[:, q * 512:(q + 1) * 512], lhsT=wl[l_][0:kdim, :], rhs=src_t[0:kdim, cs], start=True, stop=True),
                                      r=['wbd%d' % l_, srck], w=[pkeys[q]])
                            kb.op('act', lambda e: e.activation(out=uu[:], in_=Pq[:, :], func=AF.Identity, bias=fqb[:, 1 + l_:2 + l_], scale=fqb[:, 0:1]), r=pkeys + ['fqb'], w=['uu'])
                            kb.op('dve', lambda e: e.tensor_copy(out=ui[:], in_=uu[:]), r=['uu'], w=['ui'])
                            kb.op('dve', lambda e: e.tensor_copy(out=uf[:], in_=ui[:]), r=['ui'], w=['uf'])
                            kb.op('pool', lambda e: e.tensor_tensor(out=uu[:], in0=uu[:], in1=uf[:], op=ALU.subtract), r=['uu', 'uf'], w=['uu'])
                            kb.op('dve', lambda e: e.scalar_tensor_tensor(out=uf[:], in0=uu[:], scalar=0.5, in1=uu[:], op0=ALU.is_gt, op1=ALU.subtract), r=['uu', 'uf'], w=['uf'])
                            kb.op('dve', lambda e: e.scalar_tensor_tensor(out=uu[:], in0=uf[:], scalar=0.5, in1=uf[:], op0=ALU.is_gt, op1=ALU.subtract), r=['uu', 'uf'], w=['uu'])
                            kb.op('act', lambda e: e.activation(out=dst_t[:, ch * 2048:(ch + 1) * 2048], in_=uu[:], func=AF.Sin, scale=6.283185), r=['uu', dstk], w=[dstk])
                    kb.op('pool', lambda e: e.tensor_copy(out=a3[:], in_=bufB[:]), r=['bufB', 'a3'], w=['a3'])
                Kt = [sbt(sf, "Kt%d" % i, [64, 64, 128]) for i in range(2)]
                dec = sbt(sf, "dec", [64, 64, 128]); rab = sbt(sf, "rab", [64, 2, 64]); rn = sbt(sf, "rn", [128, 64]); Hst = sbt(sf, "Hst", [128, 16, 2, 128])
                for o in range(2):
                    for cg in range(8):
                        for hh in range(2):
                            col0 = o * 1024 + hh * 512 + cg * 64
                            for nbk in range(16):
                                ps, psk = hb()
                                for j_ in range(8):
                                    n2 = nbk * 8 + j_
                                    kb.op('pe', lambda e: e.matmul(ps[0:64, j_ * 64:(j_ + 1) * 64], lhsT=a3[64 * hh:64 * hh + 64, n2:L:128], rhs=w4sb[64 * hh:64 * hh + 64, col0:col0 + 64],
                                                                   start=True, stop=True), r=['a3', 'w4sb'], w=[psk])
                                evac(Kt[hh][:, :, nbk * 8:(nbk + 1) * 8], ps[0:64, :].rearrange("p (n c) -> p c n", c=64), None, [psk, 'Kt%d' % hh], ['Kt%d' % hh])
                            kb.dma('sp', lambda e: e.dma_start(out=dec[:], in_=DECd[hh, :, cg * 64:(cg + 1) * 64, :]), r=['dec'], w=['dec'])
                            kb.op('pool', lambda e: e.tensor_tensor(out=Kt[hh][:], in0=Kt[hh][:], in1=dec[:], op=ALU.mult), r=['Kt%d' % hh, 'dec'], w=['Kt%d' % hh])
                            kb.op('dve', lambda e: e.tensor_reduce(out=rab[:, hh, :], in_=Kt[hh][:], axis=AX.X, op=ALU.add, apply_absolute_value=True), r=['Kt%d' % hh, 'rab'], w=['rab'])
                        kb.op('dve', lambda e: e.tensor_tensor(out=rab[:, 0, :], in0=rab[:, 0, :], in1=rab[:, 1, :], op=ALU.add), r=['rab'], w=['rab'])
                        ps, psk = hb()
                        kb.op('pe', lambda e: e.matmul(ps[:, 0:64], lhsT=cst["ONESH"][0:64, :], rhs=rab[:, 0, :], start=True, stop=True), r=['hconst', 'rab'], w=[psk])
                        kb.op('dve', lambda e: e.reciprocal(out=rn[:], in_=ps[:, 0:64]), r=[psk], w=['rn'])
                        for sb4 in range(4):
                            for c2 in range(8):
                                ps, psk = hb()
                                for j_ in range(2):
                                    cc = sb4 * 16 + c2 * 2 + j_
                                    kb.op('pe', lambda e: e.matmul(ps[:, j_ * 256:(j_ + 1) * 256], lhsT=Kt[0][:, cc, :], rhs=cst["FA"][0:64, :], start=True, stop=False), r=['Kt0', 'hconst'], w=[psk])
                                    kb.op('pe', lambda e: e.matmul(ps[:, j_ * 256:(j_ + 1) * 256], lhsT=Kt[1][:, cc, :], rhs=cst["FAH"][:], start=False, stop=True), r=['Kt1', 'hconst'], w=[psk])
                                twiddle(ps, psk, B16, 'B16', c2 * 2, False)
                            for c4 in range(0, 16, 4):
                                pr, prk, pi_, pik = stage2(B16, 'B16', c4)
                                for j_ in range(4):
                                    cc = sb4 * 16 + c4 + j_; gc = cg * 64 + cc
                                    kb.op('dve', lambda e: e.tensor_scalar(out=Hst[:, c4 + j_, 0, :], in0=pr[:, j_ * 128:(j_ + 1) * 128], scalar1=rn[:, cc:cc + 1], scalar2=skb[:, o * 512 + gc:o * 512 + gc + 1],
                                                                           op0=ALU.mult, op1=ALU.add), r=[prk, 'rn', 'skb', 'Hst'], w=['Hst'])
                                    kb.op('act', lambda e: e.activation(out=Hst[:, c4 + j_, 1, :], in_=pi_[:, j_ * 128:(j_ + 1) * 128], func=AF.Copy, scale=rn[:, cc:cc + 1]), r=[pik, 'rn', 'Hst'], w=['Hst'])
                            g0 = o * 512 + cg * 64 + sb4 * 16
                            kb.dma('sp', lambda e: e.dma_start(out=HSPEC[g0:g0 + 16].rearrange("c k r f -> k c r f"), in_=Hst[:]), r=['Hst'], w=['HSPEC'])
            with stage() as sc:
                v16 = sbt(sc, "v16", [64, 16, 128]); x116 = sbt(sc, "x116", [64, 16, 128]); x216 = sbt(sc, "x216", [64, 16, 128]); z16 = sbt(sc, "z16", [64, 16, 128])
                y16 = sbt(sc, "y16", [64, 16, 128])
                H1 = sbt(sc, "H1", [128, 16, 2, 128]); H2s = sbt(sc, "H2s", [128, 16, 2, 128]); Y16 = sbt(sc, "Y16", [128, 16, 2, 128]); G16 = sbt(sc, "G16", [128, 16, 2, 128])
                hm = [sbt(sc, "hm%d" % i, [128, 4, 128]) for i in range(4)]

                def conv16(Din, dkey, Hs, hkey, Xmul, xkey, Out, okey):
                    for c2 in range(8):
                        ps, psk = hb()
                        for j_ in range(2):
                            kb.op('pe', lambda e: e.matmul(ps[:, j_ * 256:(j_ + 1) * 256], lhsT=Din[:, c2 * 2 + j_, :], rhs=cst["FA"][0:64, :], start=True, stop=True), r=[dkey, 'hconst'], w=[psk])
                        twiddle(ps, psk, B16, 'B16', c2 * 2, False)
                    for c4 in range(0, 16, 4):
                        pr, prk, pi_, pik = stage2(B16, 'B16', c4)
                        Xr = pr[:, :].rearrange("p (c f) -> p c f", c=4); Xi = pi_[:, :].rearrange("p (c f) -> p c f", c=4)
                        Hr, Hi = Hs[:, c4:c4 + 4, 0, :], Hs[:, c4:c4 + 4, 1, :]
                        kb.op('dve', lambda e: e.tensor_tensor(out=hm[0][:], in0=Xr, in1=Hr, op=ALU.mult), r=[prk, hkey, 'hm0'], w=['hm0'])
                        kb.op('dve', lambda e: e.tensor_tensor(out=hm[1][:], in0=Xi, in1=Hi, op=ALU.mult), r=[pik, hkey, 'hm1'], w=['hm1'])
                        kb.op('dve', lambda e: e.tensor_tensor(out=hm[2][:], in0=Xr, in1=Hi, op=ALU.mult), r=[prk, hkey, 'hm2'], w=['hm2'])
                        kb.op('dve', lambda e: e.tensor_tensor(out=hm[3][:], in0=Xi, in1=Hr, op=ALU.mult), r=[pik, hkey, 'hm3'], w=['hm3'])
                        kb.op('pool', lambda e: e.tensor_tensor(out=Y16[:, c4:c4 + 4, 0, :], in0=hm[0][:], in1=hm[1][:], op=ALU.subtract), r=['hm0', 'hm1', 'Y16'], w=['Y16'])
                        kb.op('pool', lambda e: e.tensor_tensor(out=Y16[:, c4:c4 + 4, 1, :], in0=hm[2][:], in1=hm[3][:], op=ALU.add), r=['hm2', 'hm3', 'Y16'], w=['Y16'])
                    for c2 in range(8):
                        ps, psk = hb()
                        for j_ in range(2):
                            cc = c2 * 2 + j_
                            kb.op('pe', lambda e: e.matmul(ps[:, j_ * 256:(j_ + 1) * 256], lhsT=Y16[:, cc, 0, :], rhs=cst["CA"][:], start=True, stop=False), r=['Y16', 'hconst'], w=[psk])
                            kb.op('pe', lambda e: e.matmul(ps[:, j_ * 256:(j_ + 1) * 256], lhsT=Y16[:, cc, 1, :], rhs=cst["CB"][:], start=False, stop=True), r=['Y16', 'hconst'], w=[psk])
                        twiddle(ps, psk, G16, 'G16', c2 * 2, True)
                    for c4 in range(0, 16, 4):
                        py, pyk = hb()
                        kb.op('pe', lambda e: e.matmul(py[0:64, :], lhsT=cst["FREN"][:], rhs=G16[:, c4:c4 + 4, 0, :], start=True, stop=False), r=['hconst', 'G16'], w=[pyk])
                        kb.op('pe', lambda e: e.matmul(py[0:64, :], lhsT=cst["FIMN"][:], rhs=G16[:, c4:c4 + 4, 1, :], start=False, stop=True), r=['hconst', 'G16'], w=[pyk])
                        kb.op('dve', lambda e: e.tensor_tensor(out=Out[:, c4:c4 + 4, :], in0=py[0:64, :].rearrange("p (c f) -> p c f", c=4), in1=Xmul[:, c4:c4 + 4, :], op=ALU.mult),
                              r=[pyk, xkey, okey], w=[okey])

                for g in range(32):
                    gc0 = g * 16
                    lh = lambda r0: HYC[r0 + gc0:r0 + gc0 + 16, :].rearrange("c (n1 n2) -> n1 c n2", n2=128)
                    kb.dma('sp', lambda e: e.dma_start(out=v16[:], in_=lh(0)), r=['HYC'], w=['v16'])
                    kb.dma('sp', lambda e: e.dma_start(out=x116[:], in_=lh(512)), r=['HYC'], w=['x116'])
                    kb.dma('sp', lambda e: e.dma_start(out=x216[:], in_=lh(1024)), r=['HYC'], w=['x216'])
                    kb.dma('sp', lambda e: e.dma_start(out=H1[:], in_=HSPEC[gc0:gc0 + 16].rearrange("c k r f -> k c r f")), r=['HSPEC'], w=['H1'])
                    kb.dma('sp', lambda e: e.dma_start(out=H2s[:], in_=HSPEC[512 + gc0:512 + gc0 + 16].rearrange("c k r f -> k c r f")), r=['HSPEC'], w=['H2s'])
                    conv16(v16, 'v16', H1, 'H1', x116, 'x116', z16, 'z16')
                    conv16(z16, 'z16', H2s, 'H2s', x216, 'x216', y16, 'y16')
                    kb.dma('pool', lambda e: e.dma_start(out=YHYT[gc0:gc0 + 16, :].rearrange("c (n1 n2) -> n1 c n2", n2=128), in_=y16[:]), r=['y16'], w=['YHYT'])

        if stages >= 5:
            with stage() as st:
                z = sbt(st, "zt", [128, L])
                kb.op('pool', lambda e: e.memset(z[:], 0.0), w=['zt'])
                for q in range(4):
                    if dummy_mix:
                        kb.dma('sp', lambda e: e.dma_start(out=z[:], in_=QT[q * 128:(q + 1) * 128, :]), r=['zt', 'QT'], w=['zt'])
                    if dummy_mix:
                        kb.dma('sp', lambda e: e.dma_start(out=YHYT[q * 128:(q + 1) * 128, :], in_=z[:]), r=['zt'], w=['YHYT'])
                    if dummy_mix:
                        kb.dma('sp', lambda e: e.dma_start(out=z[:], in_=GT[q * 128:(q + 1) * 128, :]), r=['zt', 'GT'], w=['zt'])
                    if dummy_mix:
                        kb.dma('sp', lambda e: e.dma_start(out=YHGT[q * 128:(q + 1) * 128, :], in_=z[:]), r=['zt'], w=['YHGT'])
                zr = sbt(st, "zr", [128, D])
                kb.op('pool', lambda e: e.memset(zr[:], 0.0), w=['zr'])
                if dummy_mix or stages < 7:
                    for i in range(OWN // 128):
                        kb.dma('sp', lambda e: e.dma_start(out=ROUTED[i * 128:(i + 1) * 128, :], in_=zr[:]), r=['zr'], w=['ROUTED'])

        def row_bcast(stack, name, src_row_ap):
            t = sbt(stack, name, [128, D])
            kb.dma('sp', lambda e: e.dma_start(out=t[:], in_=src_row_ap.to_broadcast([128, D])), r=['MODROW'], w=[name])
            return t

        if stages >= 5:
            with stage() as st:
                oidx = sbt(st, "oidx", [128, 4], I32)
                kb.dma('sp', lambda e: e.dma_start(out=oidx[:], in_=OWNIDX), w=['oidx'])
                yg = [sbt(st, "yg%d" % i, [128, OWN]) for i in range(2)]
                n = 0
                for src, skey, r0 in ((YHYT, 'YHYT', 0), (YHGT, 'YHGT', 512)):
                    v = src.rearrange("c (j t) -> (c j) t", j=4)
                    for cc in range(4):
                        g = yg[n % 2]; gk = "yg%d" % (n % 2); n += 1
                        kb.dma('pool', lambda e: e.indirect_dma_start(out=g[:], out_offset=None, in_=v,
                                                                     in_offset=bass.IndirectOffsetOnAxis(ap=oidx[:, cc:cc + 1], axis=0),
                                                                     bounds_check=RB_OWN, oob_is_err=False), r=[skey, 'oidx'], w=[gk])
                        kb.dma('sp', lambda e: e.dma_start(out=YOWN[r0 + cc * 128:r0 + (cc + 1) * 128, :], in_=g[:]), r=[gk], w=['YOWN'])
            with stage() as st:
                Wy = sbt(st, "Wy", [128, 8, D]); Wo = sbt(st, "Wo", [128, 8, D])
                kb.dma('sp', lambda e: e.dma_start(out=Wy[:, 0:4, :], in_=WHY.rearrange("(k p) c -> p k c", p=128)), w=['Wy'])
                kb.dma('sp', lambda e: e.dma_start(out=Wy[:, 4:8, :], in_=WHG.rearrange("(k p) c -> p k c", p=128)), r=['Wy'], w=['Wy'])
                kb.dma('sp', lambda e: e.dma_start(out=Wo[:], in_=WOUT.rearrange("(k p) c -> p k c", p=128)), w=['Wo'])
                g1row = row_bcast(st, "g1row", MODROW[0:1, 2048:3072])
                yb = sbt(st, "yb", [128, 8, 512]); sgb = sbt(st, "sgb", [128, 16, 512]); mT = sbt(st, "mT", [128, 8, 512])
                t1 = [sbt(st, "t1_%d" % i, [128, 512]) for i in range(2)]
                xt = [sbt(st, "xt%d" % i, [128, D]) for i in range(2)]; pt = [sbt(st, "pt%d" % i, [128, D]) for i in range(2)]
                bi = 0
                for blk in range(OWN // 512):
                    t0 = blk * 512
                    kb.dma('sp', lambda e: e.dma_start(out=yb[:], in_=YOWN[:, t0:t0 + 512].rearrange("(k p) t -> p k t", p=128)), r=['YOWN'], w=['yb'])
                    kb.dma('sp', lambda e: e.dma_start(out=sgb[:], in_=SGT[:, t0:t0 + 512].rearrange("(k p) t -> p k t", p=128)), r=['SGT'], w=['sgb'])
                    for dm in range(8):
                        for br in range(2):
                            pb, pbk = banks[bi % 8]; bi += 1
                            for cc in range(4):
                                kb.op('pe', lambda e: e.matmul(pb[:, :], lhsT=Wy[:, br * 4 + cc, dm * 128:(dm + 1) * 128], rhs=yb[:, br * 4 + cc, :],
                                                               start=(cc == 0), stop=(cc == 3)), r=['Wy', 'yb'], w=[pbk])
                            if br == 0:
                                kb.op('dve', lambda e: e.tensor_tensor(out=t1[dm % 2][:], in0=pb[:, :], in1=sgb[:, dm, :], op=ALU.mult),
                                      r=[pbk, 'sgb'], w=['t1_%d' % (dm % 2)])
                            else:
                                kb.op('dve', lambda e: e.tensor_tensor(out=mT[:, dm, :], in0=pb[:, :], in1=sgb[:, 8 + dm, :], op=ALU.mult),
                                      r=[pbk, 'sgb', 'mT'], w=['mT'])
                                kb.op('pool', lambda e: e.tensor_tensor(out=mT[:, dm, :], in0=mT[:, dm, :], in1=t1[dm % 2][:], op=ALU.add),
                                      r=['mT', 't1_%d' % (dm % 2)], w=['mT'])
                    for tt in range(4):
                        ti = blk * 4 + tt; b2 = ti % 2
                        kb.dma('sp', lambda e: e.dma_start(out=xt[b2][:], in_=XOWN[ti * 128:(ti + 1) * 128, :]), w=['xt%d' % b2])
                        kb.dma('sp', lambda e: e.dma_start(out=pt[b2][:], in_=POSOWN[ti * 128:(ti + 1) * 128, :]), w=['pt%d' % b2])
                        kb.op('pool', lambda e: e.tensor_tensor(out=xt[b2][:], in0=xt[b2][:], in1=pt[b2][:], op=ALU.add), r=['xt%d' % b2, 'pt%d' % b2], w=['xt%d' % b2])
                        for hf in range(2):
                            pb, pbk = banks[bi % 8]; bi += 1
                            for kk in range(8):
                                kb.op('pe', lambda e: e.matmul(pb[:, :], lhsT=mT[:, kk, tt * 128:(tt + 1) * 128], rhs=Wo[:, kk, hf * 512:(hf + 1) * 512],
                                                               start=(kk == 0), stop=(kk == 7)), r=['mT', 'Wo'], w=[pbk])
                            kb.op('dve', lambda e: e.tensor_tensor(out=pt[b2][:, hf * 512:(hf + 1) * 512], in0=pb[:, :], in1=g1row[:, hf * 512:(hf + 1) * 512], op=ALU.mult),
                                  r=[pbk, 'g1row', 'pt%d' % b2], w=['pt%d' % b2])
                        kb.op('pool', lambda e: e.tensor_tensor(out=xt[b2][:], in0=xt[b2][:], in1=pt[b2][:], op=ALU.add), r=['xt%d' % b2, 'pt%d' % b2], w=['xt%d' % b2])
                        kb.dma('pool', lambda e: e.dma_start(out=X1D[ti * 128:(ti + 1) * 128, :], in_=xt[b2][:]), r=['xt%d' % b2], w=['X1D'])

        if stages >= 6:
            a2T = sbt(es, "a2T", [128, 8]); sh2T = sbt(es, "sh2T", [128, 8])
            kb.op('dve', lambda e: e.tensor_scalar(out=a2T[:], in0=modT[:, 32:40, 0], scalar1=1.0, scalar2=None, op0=ALU.add), r=['modT'], w=['a2T'])
            kb.op('dve', lambda e: e.tensor_tensor(out=a2T[:], in0=a2T[:], in1=g2T[:], op=ALU.mult), r=['a2T', 'g2T'], w=['a2T'])
            kb.op('dve', lambda e: e.tensor_copy(out=sh2T[:], in_=modT[:, 24:32, 0]), r=['modT'], w=['sh2T'])
            with stage() as s1:
                srcs = [(X1D[i * 128:(i + 1) * 128, :], None, i * 128) for i in range(OWN // 128)]
                kb.res.setdefault('a1T', [None, []])
                norm_to_xt(s1, srcs, H2T, a2T, sh2T, "n2", 'H2T')
            own_blocks = [(t, 512) for t in range(0, OWN, 512)]
            gemm_group("sg", SHG, 8, 256, H2T, 'H2T', own_blocks, [dict(c0=0, cn=256, mode='fm', func=AF.Silu, out=SGA, okey='SGA', oc0=0)])
            gemm_group("su", SHU, 8, 256, H2T, 'H2T', own_blocks, [dict(c0=0, cn=256, mode='fm', func=None, out=SUA, okey='SUA', oc0=0)])
            with stage() as st:
                ga = sbt(st, "ga", [128, 2, OWN]); ua = sbt(st, "ua", [128, 2, OWN])
                kb.dma('sp', lambda e: e.dma_start(out=ga[:], in_=SGA.rearrange("(k p) t -> p k t", p=128)), r=['SGA'], w=['ga'])
                kb.dma('sp', lambda e: e.dma_start(out=ua[:], in_=SUA.rearrange("(k p) t -> p k t", p=128)), r=['SUA'], w=['ua'])
                kb.op('dve', lambda e: e.tensor_tensor(out=ga[:], in0=ga[:], in1=ua[:], op=ALU.mult), r=['ga', 'ua'], w=['ga'])
                kb.dma('pool', lambda e: e.dma_start(out=ACTT.rearrange("(k p) t -> p k t", p=128), in_=ga[:]), r=['ga'], w=['ACTT'])
            gemm_group("sd", SHD, 2, 1024, ACTT, 'ACTT', own_blocks,
                       [dict(c0=0, cn=512, mode='tm', func=None, out=SHOUT, okey='SHOUT', oc0=0),
                        dict(c0=512, cn=512, mode='tm', func=None, out=SHOUT, okey='SHOUT', oc0=512)])
            if stages >= 7 and not dummy_mix:
                with stage() as st:
                    a2row = sbt(st, "a2row", [128, D]); g2nrow = sbt(st, "g2nrow", [128, D])
                    kb.dma('sp', lambda e: e.dma_start(out=a2row[:], in_=MODROW[0:1, 4096:5120].to_broadcast([128, D])), r=['MODROW'], w=['a2row'])
                    kb.dma('sp', lambda e: e.dma_start(out=g2nrow[:], in_=N2G.rearrange("(o n) -> o n", o=1).to_broadcast([128, D])), w=['g2nrow'])
                    kb.op('dve', lambda e: e.scalar_tensor_tensor(out=a2row[:], in0=a2row[:], scalar=1.0, in1=g2nrow[:], op0=ALU.add, op1=ALU.mult),
                          r=['a2row', 'g2nrow'], w=['a2row'])
                    sh2row = row_bcast(st, "sh2row", MODROW[0:1, 3072:4096])
                    xa = [sbt(st, "hxa%d" % i, [128, D]) for i in range(2)]; stt = [sbt(st, "hst%d" % i, [128, 4]) for i in range(2)]
                    junk = sbt(st, "hjunk", [128, D])
                    for ti in range(OWN // 128):
                        b2 = ti % 2; xk, tk = 'hxa%d' % b2, 'hst%d' % b2
                        rows = slice(ti * 128, (ti + 1) * 128)
                        kb.dma('sp', lambda e: e.dma_start(out=xa[b2][:], in_=X1D[rows, :]), r=['X1D'], w=[xk])
                        kb.op('act', lambda e: e.activation(out=junk[:], in_=xa[b2][:], func=AF.Square, accum_out=stt[b2][:, 0:1]), r=[xk], w=['hjunk', tk])
                        kb.op('dve', lambda e: e.tensor_scalar(out=stt[b2][:, 1:2], in0=stt[b2][:, 0:1], scalar1=1.0 / D, scalar2=EPS, op0=ALU.mult, op1=ALU.add), r=[tk], w=[tk])
                        kb.op('act', lambda e: e.activation(out=stt[b2][:, 2:3], in_=stt[b2][:, 1:2], func=AF.Sqrt), r=[tk], w=[tk])
                        kb.op('dve', lambda e: e.reciprocal(out=stt[b2][:, 3:4], in_=stt[b2][:, 2:3]), r=[tk], w=[tk])
                        kb.op('dve', lambda e: e.scalar_tensor_tensor(out=xa[b2][:], in0=xa[b2][:], scalar=stt[b2][:, 3:4], in1=a2row[:], op0=ALU.mult, op1=ALU.mult),
                              r=[xk, tk, 'a2row'], w=[xk])
                        kb.op('pool', lambda e: e.tensor_tensor(out=xa[b2][:], in0=xa[b2][:], in1=sh2row[:], op=ALU.add), r=[xk, 'sh2row'], w=[xk])
                        kb.dma('pool', lambda e: e.dma_start(out=H2[rows, :], in_=xa[b2][:]), r=[xk], w=['H2'])
                gemm_group("rt", RW, 8, NE, H2T, 'H2T', own_blocks, [dict(c0=0, cn=NE, mode='tm', func=AF.Sigmoid, out=SCORES, okey='SCORES', oc0=0)])
                with stage() as st:
                    NT = OWN // 128
                    onesm = sbt(st, "onesm", [128, 128]); stri = sbt(st, "stri", [128, 128]); slt = sbt(st, "slt", [128, 512])
                    blk128 = sbt(st, "blk128", [128, NBLK]); pidx = sbt(st, "pidx", [128, 1]); brow_ = sbt(st, "rbrow", [128, NE])
                    kb.dma('sp', lambda e: e.dma_start(out=onesm[:], in_=ONESd), w=['onesm'])
                    kb.dma('sp', lambda e: e.dma_start(out=stri[:], in_=STRId), w=['stri'])
                    kb.dma('sp', lambda e: e.dma_start(out=slt[:], in_=SLTd), w=['slt'])
                    kb.dma('sp', lambda e: e.dma_start(out=blk128[:], in_=BLKd), w=['blk128'])
                    kb.dma('sp', lambda e: e.dma_start(out=pidx[:], in_=PIDXd), w=['pidx'])
                    kb.dma('sp', lambda e: e.dma_start(out=brow_[:], in_=RB.rearrange("(o n) -> o n", o=1).to_broadcast([128, NE])), w=['rbrow'])
                    MSK = sbt(st, "MSK", [128, NT, NE]); SEL = sbt(st, "SEL", [128, NT, NE]); WD = sbt(st, "WDm", [128, NT, NE]); DST = sbt(st, "DST", [128, NT, NE])
                    V8 = sbt(st, "V8", [128, NT, 8]); D8F = sbt(st, "D8F", [128, NT, 8]); W8 = sbt(st, "W8", [128, NT, 8]); D8I = sbt(st, "D8I", [128, NT * 8], I32)
                    sc_ = [sbt(st, "rsc%d" % i, [128, NE]) for i in range(2)]; bs = sbt(st, "rbs", [128, NE])
                    M8 = sbt(st, "M8", [128, 8, 8]); gs = sbt(st, "rgs", [128, 8]); g8 = sbt(st, "rg8", [128, 8]); gm = sbt(st, "rgm", [128, 8]); pen = sbt(st, "rpen", [128, 8])
                    den = sbt(st, "rden", [128, 2]); base = sbt(st, "rbase", [128, NE]); tmpq = sbt(st, "rtmpq", [128, NE])
                    kb.op('pool', lambda e: e.memset(base[:], 0.0), w=['rbase'])
                    for ti in range(NT):
                        b2 = ti % 2; sk_ = 'rsc%d' % b2
                        kb.dma('sp', lambda e: e.dma_start(out=sc_[b2][:], in_=SCORES[ti * 128:(ti + 1) * 128, :]), r=['SCORES'], w=[sk_])
                        kb.op('dve', lambda e: e.tensor_tensor(out=bs[:], in0=sc_[b2][:], in1=brow_[:], op=ALU.add), r=[sk_, 'rbrow'], w=['rbs'])
                        for g in range(8):
                            kb.op('dve', lambda e: e.max(out=M8[:, g, :], in_=bs[:, 32 * g:32 * g + 32]), r=['rbs', 'M8'], w=['M8'])
                        kb.op('dve', lambda e: e.tensor_tensor(out=gs[:], in0=M8[:, :, 0], in1=M8[:, :, 1], op=ALU.add), r=['M8'], w=['rgs'])
                        kb.op('dve', lambda e: e.max(out=g8[:], in_=gs[:]), r=['rgs'], w=['rg8'])
                        kb.op('dve', lambda e: e.tensor_scalar(out=gm[:], in0=gs[:], scalar1=g8[:, 3:4], scalar2=None, op0=ALU.is_ge), r=['rgs', 'rg8'], w=['rgm'])
                        kb.op('dve', lambda e: e.tensor_scalar(out=pen[:], in0=gm[:], scalar1=-1.0, scalar2=1e30, op0=ALU.add, op1=ALU.mult), r=['rgm'], w=['rpen'])
                        for g in range(8):
                            kb.op('dve', lambda e: e.tensor_scalar(out=MSK[:, ti, 32 * g:32 * g + 32], in0=bs[:, 32 * g:32 * g + 32], scalar1=gm[:, g:g + 1], scalar2=pen[:, g:g + 1],
                                                                   op0=ALU.mult, op1=ALU.add), r=['rbs', 'rgm', 'rpen', 'MSK'], w=['MSK'])
                        kb.op('dve', lambda e: e.max(out=V8[:, ti, :], in_=MSK[:, ti, :]), r=['MSK', 'V8'], w=['V8'])
                        kb.op('dve', lambda e: e.tensor_scalar(out=SEL[:, ti, :], in0=MSK[:, ti, :], scalar1=V8[:, ti, 7:8], scalar2=None, op0=ALU.is_ge), r=['MSK', 'V8', 'SEL'], w=['SEL'])
                        kb.op('dve', lambda e: e.tensor_tensor(out=WD[:, ti, :], in0=SEL[:, ti, :], in1=sc_[b2][:], op=ALU.mult), r=['SEL', sk_, 'WDm'], w=['WDm'])
                        kb.op('dve', lambda e: e.tensor_reduce(out=den[:, 0:1], in_=WD[:, ti, :], axis=AX.X, op=ALU.add), r=['WDm', 'rden'], w=['rden'])
                        kb.op('dve', lambda e: e.reciprocal(out=den[:, 1:2], in_=den[:, 0:1]), r=['rden'], w=['rden'])
                        kb.op('dve', lambda e: e.tensor_scalar(out=WD[:, ti, :], in0=WD[:, ti, :], scalar1=den[:, 1:2], scalar2=2.5, op0=ALU.mult, op1=ALU.mult), r=['WDm', 'rden'], w=['WDm'])
                        p1, p1k = banks[(2 * ti) % 8]; p2, p2k = banks[(2 * ti + 1) % 8]
                        kb.op('pe', lambda e: e.matmul(p1[:, 0:NE], lhsT=stri[:], rhs=SEL[:, ti, :], start=True, stop=True), r=['stri', 'SEL'], w=[p1k])
                        kb.op('pe', lambda e: e.matmul(p2[:, 0:NE], lhsT=onesm[:], rhs=SEL[:, ti, :], start=True, stop=True), r=['onesm', 'SEL'], w=[p2k])
                        kb.op('dve', lambda e: e.tensor_tensor(out=DST[:, ti, :], in0=p1[:, 0:NE], in1=base[:], op=ALU.add), r=[p1k, 'rbase', 'DST'], w=['DST'])
                        kb.op('dve', lambda e: e.tensor_tensor(out=base[:], in0=p2[:, 0:NE], in1=base[:], op=ALU.add), r=[p2k, 'rbase'], w=['rbase'])
                    ci = sbt(st, "rci", [128, NE], I32); padded = sbt(st, "rpad", [128, NE]); pstart = sbt(st, "rpst", [128, NE]); pend = sbt(st, "rpend", [128, NE])
                    kb.op('dve', lambda e: e.tensor_scalar(out=tmpq[:], in0=base[:], scalar1=127.0, scalar2=None, op0=ALU.add), r=['rbase'], w=['rtmpq'])
                    kb.op('dve', lambda e: e.tensor_copy(out=ci[:], in_=tmpq[:]), r=['rtmpq'], w=['rci'])
                    kb.op('dve', lambda e: e.tensor_scalar(out=ci[:], in0=ci[:], scalar1=7, scalar2=None, op0=ALU.arith_shift_right), r=['rci'], w=['rci'])
                    kb.op('dve', lambda e: e.tensor_scalar(out=ci[:], in0=ci[:], scalar1=7, scalar2=None, op0=ALU.logical_shift_left), r=['rci'], w=['rci'])
                    kb.op('dve', lambda e: e.tensor_copy(out=padded[:], in_=ci[:]), r=['rci'], w=['rpad'])
                    padT = sbt(st, "rpadT", [128, 2, 128]); pendT = sbt(st, "rpendT", [128, 2, 128])
                    pa, pak = banks[0]
                    for hh in range(2):
                        kb.op('pe', lambda e: e.transpose(out=pa[:, hh * 128:(hh + 1) * 128], in_=padded[:, hh * 128:(hh + 1) * 128], identity=ident[:]), r=['rpad', 'ident'], w=[pak])
                    kb.op('dve', lambda e: e.tensor_copy(out=padT[:], in_=pa[:, 0:256].rearrange("p (h c) -> p h c", h=2)), r=[pak], w=['rpadT'])
                    pb_, pbk_ = banks[1]
                    for hh in range(2):
                        kb.op('pe', lambda e: e.matmul(pb_[:, 0:NE], lhsT=padT[:, hh, :], rhs=slt[:, hh * 256:(hh + 1) * 256], start=(hh == 0), stop=(hh == 1)), r=['rpadT', 'slt'], w=[pbk_])
                    kb.op('dve', lambda e: e.tensor_copy(out=pstart[:], in_=pb_[:, 0:NE]), r=[pbk_], w=['rpst'])
                    kb.op('dve', lambda e: e.tensor_tensor(out=pend[:], in0=pstart[:], in1=padded[:], op=ALU.add), r=['rpst', 'rpad'], w=['rpend'])
                    pc_, pck_ = banks[2]
                    for hh in range(2):
                        kb.op('pe', lambda e: e.transpose(out=pc_[:, hh * 128:(hh + 1) * 128], in_=pend[:, hh * 128:(hh + 1) * 128], identity=ident[:]), r=['rpend', 'ident'], w=[pck_])
                    kb.op('dve', lambda e: e.tensor_copy(out=pendT[:], in_=pc_[:, 0:256].rearrange("p (h c) -> p h c", h=2)), r=[pck_], w=['rpendT'])
                    cmpT = sbt(st, "rcmpT", [128, 2, NBLK]); bef = sbt(st, "rbef", [128, NBLK]); GI = sbt(st, "GI", [128, NBLK], I32)
                    for hh in range(2):
                        kb.op('dve', lambda e: e.tensor_scalar(out=cmpT[:, hh, :], in0=blk128[:], scalar1=pendT[:, hh, 0:1], scalar2=None, op0=ALU.is_ge), r=['blk128', 'rpendT', 'rcmpT'], w=['rcmpT'])
                    pd_, pdk_ = banks[3]
                    for hh in range(2):
                        kb.op('pe', lambda e: e.matmul(pd_[:, 0:NBLK], lhsT=onesm[:], rhs=cmpT[:, hh, :], start=(hh == 0), stop=(hh == 1)), r=['onesm', 'rcmpT'], w=[pdk_])
                    kb.op('dve', lambda e: e.tensor_scalar(out=bef[:], in0=pd_[:, 0:NBLK], scalar1=255.0, scalar2=128.0, op0=ALU.min, op1=ALU.mult), r=[pdk_], w=['rbef'])
                    kb.op('dve', lambda e: e.tensor_scalar(out=bef[:], in0=bef[:], scalar1=pidx[:, 0:1], scalar2=None, op0=ALU.add), r=['rbef', 'pidx'], w=['rbef'])
                    kb.op('dve', lambda e: e.tensor_copy(out=GI[:], in_=bef[:]), r=['rbef'], w=['GI'])
                    eqj = sbt(st, "reqj", [128, NE])
                    for ti in range(NT):
                        kb.op('dve', lambda e: e.tensor_tensor(out=DST[:, ti, :], in0=DST[:, ti, :], in1=pstart[:], op=ALU.add), r=['DST', 'rpst'], w=['DST'])
                        for k8 in range(8):
                            kb.op('dve', lambda e: e.scalar_tensor_tensor(out=eqj[:], in0=MSK[:, ti, :], scalar=V8[:, ti, k8:k8 + 1], in1=DST[:, ti, :], op0=ALU.is_equal, op1=ALU.mult),
                                  r=['MSK', 'V8', 'DST', 'reqj'], w=['reqj'])
                            kb.op('dve', lambda e: e.tensor_reduce(out=D8F[:, ti, k8:k8 + 1], in_=eqj[:], axis=AX.X, op=ALU.add), r=['reqj', 'D8F'], w=['D8F'])
                            kb.op('dve', lambda e: e.scalar_tensor_tensor(out=eqj[:], in0=MSK[:, ti, :], scalar=V8[:, ti, k8:k8 + 1], in1=WD[:, ti, :], op0=ALU.is_equal, op1=ALU.mult),
                                  r=['MSK', 'V8', 'WDm', 'reqj'], w=['reqj'])
                            kb.op('dve', lambda e: e.tensor_reduce(out=W8[:, ti, k8:k8 + 1], in_=eqj[:], axis=AX.X, op=ALU.add), r=['reqj', 'W8'], w=['W8'])
                    kb.op('dve', lambda e: e.tensor_copy(out=D8I[:], in_=D8F[:].rearrange("p t k -> p (t k)")), r=['D8F'], w=['D8I'])
                    dbg('D8F', D8F[:], [128, NT, 8]); dbg('W8', W8[:], [128, NT, 8]); dbg('rbef', bef[:], [128, NBLK])
                    ht = [sbt(st, "dht%d" % i, [128, D]) for i in range(2)]
                    for ti in range(NT):
                        b2 = ti % 2; hk = 'dht%d' % b2
                        kb.dma('sp', lambda e: e.dma_start(out=ht[b2][:], in_=H2[ti * 128:(ti + 1) * 128, :]), r=['H2'], w=[hk])
                        for k8 in range(8):
                            kb.dma('pool', lambda e: e.indirect_dma_start(out=XS, out_offset=bass.IndirectOffsetOnAxis(ap=D8I[:, ti * 8 + k8:ti * 8 + k8 + 1], axis=0), in_=ht[b2][:], in_offset=None,
                                                                         bounds_check=RB_XS, oob_is_err=False), r=[hk, 'D8I'], w=['XSw'])
                    kb.barrier()
                    wgu = [sbt(st, "wgu%d" % i, [128, 2, 8, 256]) for i in range(2)]; wdn = [sbt(st, "wdn%d" % i, [128, 2, D]) for i in range(2)]
                    xs = [sbt(st, "xs%d" % i, [128, D]) for i in range(2)]; xsT = [sbt(st, "xsT%d" % i, [128, 8, 128]) for i in range(2)]
                    actT = [sbt(st, "actT%d" % i, [128, 2, 128]) for i in range(2)]; sg_ = [sbt(st, "esg%d" % i, [128, 256]) for i in range(2)]
                    ys = [sbt(st, "ys%d" % i, [128, D]) for i in range(2)]
                    bi = 0
                    for blk in range(NBLK):
                        b2 = blk % 2
                        wk, dk, xk, xtk, ak, sgk, yk = 'wgu%d' % b2, 'wdn%d' % b2, 'xs%d' % b2, 'xsT%d' % b2, 'actT%d' % b2, 'esg%d' % b2, 'ys%d' % b2
                        kb.dma('pool', lambda e: e.indirect_dma_start(out=wgu[b2][:].rearrange("p a k f -> p (a k f)"), out_offset=None, in_=EWGU,
                                                                     in_offset=bass.IndirectOffsetOnAxis(ap=GI[:, blk:blk + 1], axis=0),
                                                                     bounds_check=RB_W, oob_is_err=False), r=['GI'], w=[wk])
                        kb.dma('pool', lambda e: e.indirect_dma_start(out=wdn[b2][:].rearrange("p k f -> p (k f)"), out_offset=None, in_=EWD,
                                                                     in_offset=bass.IndirectOffsetOnAxis(ap=GI[:, blk:blk + 1], axis=0),
                                                                     bounds_check=RB_W, oob_is_err=False), r=['GI'], w=[dk])
                        kb.dma('sp', lambda e: e.dma_start(out=xs[b2][:], in_=XS[blk * 128:(blk + 1) * 128, :]), r=['XSw'], w=[xk])
                        for hh in range(2):
                            pb, pbk = banks[bi % 8]; bi += 1
                            for kk in range(4):
                                k8 = hh * 4 + kk
                                kb.op('pe', lambda e: e.transpose(out=pb[:, kk * 128:(kk + 1) * 128], in_=xs[b2][:, k8 * 128:(k8 + 1) * 128], identity=ident[:]), r=[xk, 'ident'], w=[pbk])
                            evac(xsT[b2][:, hh * 4:hh * 4 + 4, :], pb[:, :].rearrange("p (k t) -> p k t", k=4), None, [pbk, xtk], [xtk])
                        ph, phk = banks[bi % 8]; bi += 1
                        for a_ in range(2):
                            for hc in range(2):
                                for kk in range(8):
                                    kb.op('pe', lambda e: e.matmul(ph[:, (a_ * 2 + hc) * 128:(a_ * 2 + hc + 1) * 128], lhsT=wgu[b2][:, a_, kk, hc * 128:(hc + 1) * 128], rhs=xsT[b2][:, kk, :],
                                                                   start=(kk == 0), stop=(kk == 7)), r=[wk, xtk], w=[phk])
                        kb.op('act', lambda e: e.activation(out=sg_[b2][:], in_=ph[:, 0:256], func=AF.Silu), r=[phk], w=[sgk])
                        kb.op('dve', lambda e: e.tensor_tensor(out=actT[b2][:].rearrange("p k t -> p (k t)"), in0=ph[:, 256:512], in1=sg_[b2][:], op=ALU.mult), r=[phk, sgk], w=[ak])
                        for hf in range(2):
                            py, pyk = banks[bi % 8]; bi += 1
                            for kk in range(2):
                                kb.op('pe', lambda e: e.matmul(py[:, :], lhsT=actT[b2][:, kk, :], rhs=wdn[b2][:, kk, hf * 512:(hf + 1) * 512], start=(kk == 0), stop=(kk == 1)), r=[ak, dk], w=[pyk])
                            evac(ys[b2][:, hf * 512:(hf + 1) * 512], py[:, :], None, [pyk, yk], [yk])
                        kb.dma('sp', lambda e: e.dma_start(out=YS[blk * 128:(blk + 1) * 128, :], in_=ys[b2][:]), r=[yk], w=['YSw'])
                    kb.barrier()
                    acc = [sbt(st, "cacc%d" % i, [128, D]) for i in range(2)]; gg = [sbt(st, "cg%d" % i, [128, D]) for i in range(3)]
                    gi_ = 0
                    for ti in range(NT):
                        b2 = ti % 2; ack = 'cacc%d' % b2
                        for k8 in range(8):
                            g3 = gi_ % 3; gi_ += 1; ggk = 'cg%d' % g3
                            kb.dma('pool', lambda e: e.indirect_dma_start(out=gg[g3][:], out_offset=None, in_=YS, in_offset=bass.IndirectOffsetOnAxis(ap=D8I[:, ti * 8 + k8:ti * 8 + k8 + 1], axis=0),
                                                                         bounds_check=RB_XS, oob_is_err=False), r=['YSw', 'D8I'], w=[ggk])
                            if k8 == 0:
                                kb.op('dve', lambda e: e.tensor_scalar(out=acc[b2][:], in0=gg[g3][:], scalar1=W8[:, ti, 0:1], scalar2=None, op0=ALU.mult), r=[ggk, 'W8', ack], w=[ack])
                            else:
                                kb.op('dve', lambda e: e.scalar_tensor_tensor(out=acc[b2][:], in0=gg[g3][:], scalar=W8[:, ti, k8:k8 + 1], in1=acc[b2][:], op0=ALU.mult, op1=ALU.add),
                                      r=[ggk, 'W8', ack], w=[ack])
                        kb.dma('sp', lambda e: e.dma_start(out=ROUTED[ti * 128:(ti + 1) * 128, :], in_=acc[b2][:]), r=[ack], w=['ROUTED'])

            with stage() as st:
                g2row = row_bcast(st, "g2row", MODROW[0:1, 5120:6144])
                fgrow = sbt(st, "fgrow", [128, D])
                kb.dma('sp', lambda e: e.dma_start(out=fgrow[:], in_=FING.rearrange("(o n) -> o n", o=1).to_broadcast([128, D])), w=['fgrow'])
                xa = [sbt(st, "xa%d" % i, [128, D]) for i in range(2)]; sa = [sbt(st, "sa%d" % i, [128, D]) for i in range(2)]
                ra = [sbt(st, "ra%d" % i, [128, D]) for i in range(2)]; stt = [sbt(st, "stt%d" % i, [128, 4]) for i in range(2)]
                junk = sbt(st, "fjunk", [128, D])
                for ti in range(OWN // 128):
                    b2 = ti % 2; xk, sk, rk, tk = 'xa%d' % b2, 'sa%d' % b2, 'ra%d' % b2, 'stt%d' % b2
                    rows = slice(ti * 128, (ti + 1) * 128)
                    kb.dma('sp', lambda e: e.dma_start(out=xa[b2][:], in_=X1D[rows, :]), r=['X1D'], w=[xk])
                    kb.dma('sp', lambda e: e.dma_start(out=sa[b2][:], in_=SHOUT[rows, :]), r=['SHOUT'], w=[sk])
                    kb.dma('sp', lambda e: e.dma_start(out=ra[b2][:], in_=ROUTED[rows, :]), r=['ROUTED'], w=[rk])
                    kb.op('pool', lambda e: e.tensor_tensor(out=sa[b2][:], in0=sa[b2][:], in1=ra[b2][:], op=ALU.add), r=[sk, rk], w=[sk])
                    kb.op('dve', lambda e: e.tensor_tensor(out=sa[b2][:], in0=sa[b2][:], in1=g2row[:], op=ALU.mult), r=[sk, 'g2row'], w=[sk])
                    kb.op('pool', lambda e: e.tensor_tensor(out=xa[b2][:], in0=xa[b2][:], in1=sa[b2][:], op=ALU.add), r=[xk, sk], w=[xk])
                    kb.op('act', lambda e: e.activation(out=junk[:], in_=xa[b2][:], func=AF.Square, accum_out=stt[b2][:, 0:1]), r=[xk], w=['fjunk', tk])
                    kb.op('dve', lambda e: e.tensor_scalar(out=stt[b2][:, 1:2], in0=stt[b2][:, 0:1], scalar1=1.0 / D, scalar2=EPS, op0=ALU.mult, op1=ALU.add), r=[tk], w=[tk])
                    kb.op('act', lambda e: e.activation(out=stt[b2][:, 2:3], in_=stt[b2][:, 1:2], func=AF.Sqrt), r=[tk], w=[tk])
                    kb.op('dve', lambda e: e.reciprocal(out=stt[b2][:, 3:4], in_=stt[b2][:, 2:3]), r=[tk], w=[tk])
                    kb.op('dve', lambda e: e.scalar_tensor_tensor(out=xa[b2][:], in0=xa[b2][:], scalar=stt[b2][:, 3:4], in1=fgrow[:], op0=ALU.mult, op1=ALU.mult),
                          r=[xk, tk, 'fgrow'], w=[xk])
                    kb.dma('pool', lambda e: e.dma_start(out=OUT[rows, :], in_=xa[b2][:]), r=[xk], w=['OUT'])

        kb.finish('sp')
        kb.finish('pool')
        pg.ninstr = kb.ninstr
    return pg


_PROG = None


def make_in_maps(pg, inputs):
    hc = host_consts()
    sq = lambda a: np.ascontiguousarray(a[0])
    in_maps = []
    shared = {}
    if 'EWGU' in pg.ins:
        wg = np.asarray(inputs['exp_w_gate'])[0].reshape(NE, 8, 128, 256)
        wu = np.asarray(inputs['exp_w_up'])[0].reshape(NE, 8, 128, 256)
        ew = np.empty((NE, 128, 2, 8, 256), np.float32)
        ew[:, :, 0] = wg.transpose(0, 2, 1, 3); ew[:, :, 1] = wu.transpose(0, 2, 1, 3)
        shared['EWGU'] = ew.reshape(NE * 128, 4096)
        shared['EWD'] = np.ascontiguousarray(np.asarray(inputs['exp_w_down'])[0].reshape(NE, 2, 128, D).transpose(0, 2, 1, 3)).reshape(NE * 128, 2048)
    for c in range(8):
        b, j = c // 4, c % 4
        own = slice(j * OWN, (j + 1) * OWN)
        idx = ((np.arange(4)[None, :] * 128 + np.arange(128)[:, None]) * 4 + j).astype(np.int32)
        full = {
            'x': inputs['x'][b], 'ctx': inputs['ctx'][b], 'xown': inputs['x'][b, own], 'posown': hc['POS'][own],
            'c': inputs['c'][b], 'c_ctx': inputs['c_ctx'], 'final_g': inputs['final_g'], 'OWNIDX': idx,
            'hg_lb_logits': np.asarray(inputs['hg_lb_logits']).reshape(2, 1024),
        }
        full.update(shared)
        for k in pg.ins:
            if k not in full and k not in hc:
                full[k] = sq(inputs[k])
        full.update(hc)
        in_maps.append({k: np.ascontiguousarray(np.asarray(full[k])) for k in pg.ins})
    return in_maps


def kernel(**inputs):
    global _PROG
    if _PROG is None:
        _PROG = build()
    pg = _PROG
    in_maps = make_in_maps(pg, inputs)
    res = run_bass_kernel_spmd(pg.nc, in_maps, core_ids=list(range(8)))
    out = np.zeros((2, L, D), np.float32)
    for c in range(8):
        b, j = c // 4, c % 4
        out[b, j * OWN:(j + 1) * OWN] = res.results[c]['out']
    return out
```

```python
import math
import numpy as np
from contextlib import ExitStack, contextmanager
import concourse.bass as bass
import concourse.mybir as mybir
from concourse.bass_utils import run_bass_kernel_spmd

F32 = mybir.dt.float32
I32 = mybir.dt.int32
U32 = mybir.dt.uint32
ALU = mybir.AluOpType
AF = mybir.ActivationFunctionType
AX = mybir.AxisListType

N_DMA_SEMS = 24
D = 1024
L = 8192
NCTX = 256
LT = L + NCTX
OWN = 2048
NE = 256
NBLK = 383
EPS = 1e-6
TWO_PI = 2.0 * math.pi


class KB:
    def __init__(self, nc, es):
        self.nc = nc
        self.engs = {'pe': nc.tensor, 'act': nc.scalar, 'dve': nc.vector, 'pool': nc.gpsimd, 'sp': nc.sync}
        self.sems = {}
        self.cnt = {}
        for e in self.engs:
            self.sems[e] = es.enter_context(nc.semaphore("s_" + e))
            self.cnt[e] = 0
        for i in range(N_DMA_SEMS):
            self.sems['d%d' % i] = es.enter_context(nc.semaphore("s_d%d" % i))
            self.cnt['d%d' % i] = 0
        self.dnext = 0
        self.waited = {e: {} for e in self.engs}
        self.res = {}
        self.ninstr = 0

    def _need(self, eng, toks):
        best = {}
        for t in toks:
            if t is None:
                continue
            sk, v = t
            if sk == eng and eng == 'pe':
                continue
            if best.get(sk, 0) < v:
                best[sk] = v
        for sk, v in best.items():
            if self.waited[eng].get(sk, 0) >= v:
                continue
            self.engs[eng].wait_ge(self.sems[sk], v)
            self.waited[eng][sk] = v

    def _deps(self, r, w):
        toks = []
        for k in r:
            st = self.res.get(k)
            if st is not None:
                toks.append(st[0])
        for k in w:
            st = self.res.get(k)
            if st is not None:
                toks.append(st[0])
                toks.extend(st[1])
        return toks

    def _commit(self, tok, r, w):
        for k in r:
            st = self.res.setdefault(k, [None, []])
            st[1].append(tok)
            if len(st[1]) > 32:
                best = {}
                for sk, v in st[1]:
                    if best.get(sk, 0) < v:
                        best[sk] = v
                st[1] = list(best.items())
        for k in w:
            self.res[k] = [tok, []]

    def op(self, eng, fn, r=(), w=()):
        self._need(eng, self._deps(r, w))
        ins = fn(self.engs[eng])
        self.cnt[eng] += 1
        ins.then_inc(self.sems[eng], 1)
        self._commit((eng, self.cnt[eng]), r, w)
        self.ninstr += 1

    def dma(self, q, fn, r=(), w=()):
        i = self.dnext
        self.dnext = (self.dnext + 1) % N_DMA_SEMS
        sk = 'd%d' % i
        toks = self._deps(r, w)
        if self.cnt[sk] > 0:
            toks.append((sk, self.cnt[sk]))
        self._need(q, toks)
        ins = fn(self.engs[q])
        self.cnt[sk] += 16
        ins.then_inc(self.sems[sk], 16)
        self._commit((sk, self.cnt[sk]), r, w)
        self.ninstr += 1

    def barrier(self):
        toks = [(sk, v) for sk, v in self.cnt.items() if v > 0]
        for e in self.engs:
            self._need(e, toks)

    def finish(self, eng):
        toks = []
        for st in self.res.values():
            toks.append(st[0])
            toks.extend(st[1])
        self._need(eng, toks)


_CONST = None


def host_consts():
    global _CONST
    if _CONST is not None:
        return _CONST
    c = {}
    quarter = D // 4
    omega = (1.0 / (np.float32(10000.0) ** (np.arange(quarter, dtype=np.float32) / np.float32(quarter)))).astype(np.float32)
    rows, cols = L // 64, 64
    ang_r = (np.arange(rows, dtype=np.float32)[:, None] * omega).astype(np.float32)
    ang_c = (np.arange(cols, dtype=np.float32)[:, None] * omega).astype(np.float32)
    emb_r = np.concatenate([np.sin(ang_r), np.cos(ang_r)], -1)
    emb_c = np.concatenate([np.sin(ang_c), np.cos(ang_c)], -1)
    emb = np.concatenate([np.broadcast_to(emb_r[:, None], (rows, cols, D // 2)),
                          np.broadcast_to(emb_c[None], (rows, cols, D // 2))], -1)
    c['POS'] = np.ascontiguousarray(emb.reshape(L, D).astype(np.float32))
    c['IDENT'] = np.eye(128, dtype=np.float32)
    si = np.arange(128)[:, None]; ti = np.arange(128)[None, :]
    same = (si // 64) == (ti // 64)
    c['TRIF'] = (same & (si <= ti)).astype(np.float32)
    c['TRIB'] = (same & (si >= ti)).astype(np.float32)
    c['ONES'] = np.ones((128, 128), np.float32)
    c['STRI'] = (si < ti).astype(np.float32)
    e1 = np.arange(128)[:, None]; e2 = np.arange(256)[None, :]
    c['SLT'] = np.concatenate([(e1 < e2), (e1 + 128 < e2)], 1).astype(np.float32)
    c['BLK128'] = np.broadcast_to((np.arange(NBLK, dtype=np.float32) * 128.0)[None, :], (128, NBLK)).copy()
    c['PIDX'] = np.arange(128, dtype=np.float32).reshape(128, 1).copy()
    NN = 16384
    a = np.arange(128, dtype=np.float64)
    ang = 2.0 * np.pi * np.outer(a, a) / 128.0
    Fre = np.cos(ang); Fim = -np.sin(ang)
    f32 = lambda v: np.ascontiguousarray(v.astype(np.float32))
    c['FA'] = f32(np.concatenate([Fre, Fim], 1)); c['FAH'] = f32(np.concatenate([Fre, Fim], 1)[64:128])
    c['FRE'] = f32(Fre); c['FIM'] = f32(Fim); c['NFIM'] = f32(-Fim)
    c['CA'] = f32(np.concatenate([Fre, -Fim], 1)); c['CB'] = f32(np.concatenate([Fim, Fre], 1))
    c['FREN'] = f32(Fre[:, :64] / NN); c['FIMN'] = f32(Fim[:, :64] / NN)
    angT = 2.0 * np.pi * np.outer(a, a) / NN
    c['TRE2'] = f32(np.concatenate([np.cos(angT), np.cos(angT)], 1)); c['TIM2'] = f32(np.concatenate([-np.sin(angT), -np.sin(angT)], 1))
    bands = np.linspace(1e-4, 15.0, 16, dtype=np.float32)
    def zfeat(t):
        t = t.astype(np.float32)
        tn = (t / np.float32(L - 1)).astype(np.float32)
        an = (np.float32(2 * math.pi / L) * t[:, None] * bands).astype(np.float32)
        return np.concatenate([tn[:, None], np.cos(an), -np.sin(an)], -1).astype(np.float32), tn
    zf, tnf = zfeat(np.arange(L)); zb, tnb = zfeat(L - np.arange(L))
    c['ZT'] = np.ascontiguousarray(np.concatenate([zf, zb], 1).T)
    lo_ = math.log(1e-2) / 1.5; hi_ = math.log(1e-2) / 0.3
    deltas = np.abs(np.linspace(lo_, hi_, 512, dtype=np.float32))
    decf = np.exp(-tnf[:, None] * deltas).astype(np.float32)
    decb = np.exp(-tnb[:, None] * deltas).astype(np.float32); decb[0] = 0.0
    c['DEC'] = np.ascontiguousarray(np.stack([decf.reshape(64, 128, 512).transpose(0, 2, 1), decb.reshape(64, 128, 512).transpose(0, 2, 1)]))
    _CONST = c
    return c


class Prog:
    def __init__(self, debug=None):
        self.debug = debug or ()
        self.nc = bass.Bass("TRN2", target_bir_lowering=False)
        self.ins = {}
        self.outs = {}

    def inp(self, name, shape, dt=F32):
        t = self.nc.dram_tensor(name, list(shape), dt, kind="ExternalInput").ap()
        self.ins[name] = t
        return t

    def scratch(self, name, shape, dt=F32):
        if name in self.debug:
            t = self.nc.dram_tensor(name, list(shape), dt, kind="ExternalOutput").ap()
            self.outs[name] = t
        else:
            t = self.nc.dram_tensor(name, list(shape), dt, kind="Internal").ap()
        return t


def build(stages=99, debug=None, dummy_mix=False):
    pg = Prog(debug)
    nc = pg.nc
    X = pg.inp("x", [L, D]); CTX = pg.inp("ctx", [NCTX, D]); XOWN = pg.inp("xown", [OWN, D]); POSOWN = pg.inp("posown", [OWN, D])
    CV = pg.inp("c", [D]); CCTX = pg.inp("c_ctx", [D])
    N1G = pg.inp("norm1_g", [D]); N2G = pg.inp("norm2_g", [D])
    ADAW = pg.inp("ada_w", [D, 6 * D]); ADAB = pg.inp("ada_b", [6 * D])
    WIN = pg.inp("w_in", [D, 6144])
    POS = pg.inp("POS", [L, D]); IDENT = pg.inp("IDENT", [128, 128])
    WHY = pg.inp("w_hy_out", [512, D]); WHG = pg.inp("w_hg_out", [512, D]); WOUT = pg.inp("w_out", [D, D])
    SHG = pg.inp("sh_w_gate", [D, 256]); SHU = pg.inp("sh_w_up", [D, 256]); SHD = pg.inp("sh_w_down", [256, D])
    FING = pg.inp("final_g", [D]); OWNIDX = pg.inp("OWNIDX", [128, 4], I32)
    CONVW = pg.inp("hy_conv_w", [3, 1536]); CONVB = pg.inp("hy_conv_b", [1536])
    TRIFd = pg.inp("TRIF", [128, 128]); TRIBd = pg.inp("TRIB", [128, 128]); ONESd = pg.inp("ONES", [128, 128])
    LBL = pg.inp("hg_lb_logits", [2, 1024]); HGNG = pg.inp("hg_norm_g", [128])
    STRId = pg.inp("STRI", [128, 128]); SLTd = pg.inp("SLT", [128, 512]); BLKd = pg.inp("BLK128", [128, NBLK]); PIDXd = pg.inp("PIDX", [128, 1])
    RW = pg.inp("router_w", [D, NE]); RB = pg.inp("router_bias", [NE])
    EWGU = pg.inp("EWGU", [NE * 128, 4096]); EWD = pg.inp("EWD", [NE * 128, 2048])
    FAd = pg.inp("FA", [128, 256]); FAHd = pg.inp("FAH", [64, 256]); FREd = pg.inp("FRE", [128, 128]); FIMd = pg.inp("FIM", [128, 128]); NFIMd = pg.inp("NFIM", [128, 128])
    CAd = pg.inp("CA", [128, 256]); CBd = pg.inp("CB", [128, 256]); FRENd = pg.inp("FREN", [128, 64]); FIMNd = pg.inp("FIMN", [128, 64])
    TRE2d = pg.inp("TRE2", [128, 256]); TIM2d = pg.inp("TIM2", [128, 256]); ZTd = pg.inp("ZT", [66, L]); DECd = pg.inp("DEC", [2, 64, 512, 128])
    FW1 = pg.inp("hy_f_w1", [33, 64]); FB1 = pg.inp("hy_f_b1", [64]); FW2 = pg.inp("hy_f_w2", [64, 64]); FB2 = pg.inp("hy_f_b2", [64])
    FW3 = pg.inp("hy_f_w3", [64, 64]); FB3 = pg.inp("hy_f_b3", [64]); FW4 = pg.inp("hy_f_w4", [64, 2048]); FFQ = pg.inp("hy_f_freq", [64])
    HSKIP = pg.inp("hy_skip", [1024])
    HSPEC = pg.scratch("HSPEC", [1024, 128, 2, 128])
    OUT = pg.nc.dram_tensor("out", [OWN, D], F32, kind="ExternalOutput").ap()
    pg.outs["out"] = OUT
    XNT = pg.scratch("XNT", [D, LT]); XNTOWN = pg.scratch("XNTOWN", [D, OWN])
    MODROW = pg.scratch("MODROW", [2, 6 * D])
    HYRAW = pg.scratch("HYRAW", [1536, L])
    QT = pg.scratch("QT", [512, L]); GT = pg.scratch("GT", [512, L])
    IFF = pg.scratch("IFF", [LT, 1536])
    SGT = pg.scratch("SGT", [2048, OWN])
    HYC = pg.scratch("HYC", [1536, L])
    YHYT = pg.scratch("YHYT", [512, L]); YHGT = pg.scratch("YHGT", [512, L])
    YOWN = pg.scratch("YOWN", [1024, OWN])
    X1D = pg.scratch("X1D", [OWN, D]); H2T = pg.scratch("H2T", [D, OWN])
    SGA = pg.scratch("SGA", [256, OWN]); SUA = pg.scratch("SUA", [256, OWN]); ACTT = pg.scratch("ACTT", [256, OWN])
    SHOUT = pg.scratch("SHOUT", [OWN, D]); ROUTED = pg.scratch("ROUTED", [OWN, D])
    H2 = pg.scratch("H2", [OWN, D]); SCORES = pg.scratch("SCORES", [OWN, NE])
    XS = pg.scratch("XS", [NBLK * 128, D]); YS = pg.scratch("YS", [NBLK * 128, D])

    with ExitStack() as es:
        kb = KB(nc, es)
        sbt = lambda stack, name, shape, dt=F32: stack.enter_context(nc.sbuf_tensor(name, list(shape), dt))
        PA = es.enter_context(nc.psum_tensor("PA", [128, 2048], F32))
        PB = es.enter_context(nc.psum_tensor("PB", [128, 2048], F32))
        banks = [(PA[:, 512 * i:512 * (i + 1)], "PA%d" % i) for i in range(4)] + \
                [(PB[:, 512 * i:512 * (i + 1)], "PB%d" % i) for i in range(4)]
        RB_OWN = nc.gpsimd.alloc_register("bc_own"); nc.gpsimd.reg_mov(RB_OWN, 2047)
        RB_XS = nc.gpsimd.alloc_register("bc_xs"); nc.gpsimd.reg_mov(RB_XS, NBLK * 128 - 1)
        RB_W = nc.gpsimd.alloc_register("bc_w"); nc.gpsimd.reg_mov(RB_W, NE * 128 - 1)
        ident = sbt(es, "ident", [128, 128])
        kb.dma('sp', lambda e: e.dma_start(out=ident[:], in_=IDENT), w=['ident'])
        modT = sbt(es, "modT", [128, 48, 2])
        a1T = sbt(es, "a1T", [128, 8, 2]); sh1T = sbt(es, "sh1T", [128, 8, 2]); g2T = sbt(es, "g2T", [128, 8])
        @contextmanager
        def stage():
            with ExitStack() as st_:
                yield st_
                kb.barrier()

        def dbg(name, ap, shape):
            if name in pg.debug:
                t = nc.dram_tensor("D_" + name, list(shape), F32, kind="ExternalOutput").ap()
                pg.outs["D_" + name] = t
                kb.dma('pool', lambda e: e.dma_start(out=t, in_=ap), r=[name], w=['DBG' + name])

        with stage() as s0:
            sT = sbt(s0, "sT", [128, 8, 2]); vraw = sbt(s0, "vraw", [80, 128]); VT = sbt(s0, "VT", [128, 80])
            kb.dma('sp', lambda e: e.dma_start(out=vraw[0:8, :], in_=CV.rearrange("(q p) -> q p", p=128)), w=['vraw'])
            kb.dma('sp', lambda e: e.dma_start(out=vraw[8:16, :], in_=CCTX.rearrange("(q p) -> q p", p=128)), r=['vraw'], w=['vraw'])
            kb.dma('sp', lambda e: e.dma_start(out=vraw[16:24, :], in_=N1G.rearrange("(q p) -> q p", p=128)), r=['vraw'], w=['vraw'])
            kb.dma('sp', lambda e: e.dma_start(out=vraw[24:32, :], in_=N2G.rearrange("(q p) -> q p", p=128)), r=['vraw'], w=['vraw'])
            kb.dma('sp', lambda e: e.dma_start(out=vraw[32:80, :], in_=ADAB.rearrange("(q p) -> q p", p=128)), r=['vraw'], w=['vraw'])
            ps, psk = banks[0]
            kb.op('pe', lambda e: e.transpose(out=ps[:, 128:208], in_=vraw[:], identity=ident[0:80, 0:80]), r=['vraw', 'ident'], w=[psk])
            kb.op('dve', lambda e: e.tensor_copy(out=VT[:], in_=ps[:, 128:208]), r=[psk], w=['VT'])
            abT = VT[:, 32:80]; g1T = VT[:, 16:24]
            for r_ in range(2):
                kb.op('act', lambda e: e.activation(out=sT[:, :, r_], in_=VT[:, 8 * r_:8 * r_ + 8], func=AF.Silu), r=['VT', 'sT'], w=['sT'])
            kb.op('dve', lambda e: e.tensor_copy(out=g2T[:], in_=VT[:, 24:32]), r=['VT'], w=['g2T'])
            wbufs = [sbt(s0, "adaw%d" % i, [128, 8, 768]) for i in range(2)]
            mrow = sbt(s0, "mrow", [2, 6144]); brow = sbt(s0, "brow", [2, 6144])
            for r_ in range(2):
                kb.dma('sp', lambda e: e.dma_start(out=brow[r_:r_ + 1, :], in_=ADAB.rearrange("(o n) -> o n", o=1)), r=['brow'], w=['brow'])
            for cb in range(8):
                wb = wbufs[cb % 2]; wk = "adaw%d" % (cb % 2)
                kb.dma('sp', lambda e: e.dma_start(out=wb[:], in_=ADAW[:, cb * 768:(cb + 1) * 768].rearrange("(k p) c -> p k c", p=128)), w=[wk])
                for hh in range(2):
                    pr, prk = banks[1 + (2 * cb + hh) % 3]
                    for kk in range(8):
                        kb.op('pe', lambda e: e.matmul(pr[0:2, 0:384], lhsT=sT[:, kk, :], rhs=wb[:, kk, hh * 384:(hh + 1) * 384],
                                                       start=(kk == 0), stop=(kk == 7)), r=[wk, 'sT'], w=[prk])
                    c0 = cb * 768 + hh * 384
                    kb.op('dve', lambda e: e.tensor_tensor(out=mrow[:, c0:c0 + 384], in0=pr[0:2, 0:384], in1=brow[:, c0:c0 + 384], op=ALU.add),
                          r=[prk, 'brow'], w=['mrow'])
            kb.dma('pool', lambda e: e.dma_start(out=MODROW, in_=mrow[:]), r=['mrow'], w=['MODROW'])
            mq = sbt(s0, "mq", [96, 128])
            kb.dma('sp', lambda e: e.dma_start(out=mq[:], in_=MODROW.rearrange("r (q p) -> (r q) p", p=128)), r=['MODROW'], w=['mq'])
            kb.op('pe', lambda e: e.transpose(out=ps[:, 256:352], in_=mq[:], identity=ident[0:96, 0:96]), r=['mq', 'ident'], w=[psk])
            for r_ in range(2):
                kb.op('dve', lambda e: e.tensor_copy(out=modT[:, :, r_], in_=ps[:, 256 + 48 * r_:256 + 48 * r_ + 48]), r=[psk, 'modT'], w=['modT'])
            kb.op('dve', lambda e: e.tensor_scalar(out=a1T[:], in0=modT[:, 8:16, :], scalar1=1.0, scalar2=None, op0=ALU.add),
                  r=['modT'], w=['a1T'])
            for r_ in range(2):
                kb.op('dve', lambda e: e.tensor_tensor(out=a1T[:, :, r_], in0=a1T[:, :, r_], in1=g1T, op=ALU.mult),
                      r=['a1T', 'VT'], w=['a1T'])
            kb.op('dve', lambda e: e.tensor_copy(out=sh1T[:], in_=modT[:, 0:8, :]), r=['modT'], w=['sh1T'])

        dbg('modT', modT[:], [128, 48, 2]); dbg('a1T', a1T[:], [128, 8, 2])
        def norm_to_xt(stack, srcs, XT, a_ap, b_ap, tag, xtk):
            xin = [sbt(stack, "%s_xin%d" % (tag, i), [128, 1024]) for i in range(2)]
            pin = [sbt(stack, "%s_pin%d" % (tag, i), [128, 1024]) for i in range(2)]
            junk = sbt(stack, "%s_junk" % tag, [128, 1024])
            st = [sbt(stack, "%s_st%d" % (tag, i), [128, 4]) for i in range(2)]
            xo = [sbt(stack, "%s_xo%d" % (tag, i), [128, 8, 128]) for i in range(2)]
            for i, (xap, pap, c0) in enumerate(srcs):
                b = i % 2
                xk, pk, sk, ok = "%s_xin%d" % (tag, b), "%s_pin%d" % (tag, b), "%s_st%d" % (tag, b), "%s_xo%d" % (tag, b)
                kb.dma('sp', lambda e: e.dma_start(out=xin[b][:], in_=xap), w=[xk])
                if pap is not None:
                    kb.dma('sp', lambda e: e.dma_start(out=pin[b][:], in_=pap), w=[pk])
                    kb.op('pool', lambda e: e.tensor_tensor(out=xin[b][:], in0=xin[b][:], in1=pin[b][:], op=ALU.add), r=[xk, pk], w=[xk])
                kb.op('act', lambda e: e.activation(out=junk[:], in_=xin[b][:], func=AF.Square, accum_out=st[b][:, 0:1]),
                      r=[xk], w=[tag + '_junk', sk])
                kb.op('dve', lambda e: e.tensor_scalar(out=st[b][:, 1:2], in0=st[b][:, 0:1], scalar1=1.0 / D, scalar2=EPS, op0=ALU.mult, op1=ALU.add),
                      r=[sk], w=[sk])
                kb.op('act', lambda e: e.activation(out=st[b][:, 2:3], in_=st[b][:, 1:2], func=AF.Sqrt), r=[sk], w=[sk])
                kb.op('dve', lambda e: e.reciprocal(out=st[b][:, 3:4], in_=st[b][:, 2:3]), r=[sk], w=[sk])
                kb.op('dve', lambda e: e.tensor_scalar(out=xin[b][:], in0=xin[b][:], scalar1=st[b][:, 3:4], scalar2=None, op0=ALU.mult),
                      r=[xk, sk], w=[xk])
                for h in range(2):
                    pb, pbk = banks[(2 * i + h) % 4]
                    for kk in range(4):
                        k8 = h * 4 + kk
                        kb.op('pe', lambda e: e.transpose(out=pb[:, kk * 128:(kk + 1) * 128], in_=xin[b][:, k8 * 128:(k8 + 1) * 128], identity=ident[:]),
                              r=[xk, 'ident'], w=[pbk])
                    for kk in range(4):
                        k8 = h * 4 + kk
                        kb.op('act', lambda e: e.activation(out=xo[b][:, k8, :], in_=pb[:, kk * 128:(kk + 1) * 128], func=AF.Identity,
                                                            bias=b_ap[:, k8:k8 + 1], scale=a_ap[:, k8:k8 + 1]),
                              r=[pbk, 'a1T', 'sh1T', 'a2T'], w=[ok])
                kb.dma('pool', lambda e: e.dma_start(out=XT[:, c0:c0 + 128].rearrange("(k p) t -> p k t", p=128), in_=xo[b][:]), r=[ok], w=[xtk])

        if stages >= 1:
            with stage() as s1:
                srcs = [(X[i * 128:(i + 1) * 128, :], POS[i * 128:(i + 1) * 128, :], i * 128) for i in range(L // 128)]
                norm_to_xt(s1, srcs, XNT, a1T[:, :, 0], sh1T[:, :, 0], "n1", 'XNT')
            with stage() as s1:
                srcs = [(CTX[i * 128:(i + 1) * 128, :], None, L + i * 128) for i in range(NCTX // 128)]
                norm_to_xt(s1, srcs, XNT, a1T[:, :, 1], sh1T[:, :, 1], "n1c", 'XNT')
            with stage() as s1:
                srcs = [(XOWN[i * 128:(i + 1) * 128, :], POSOWN[i * 128:(i + 1) * 128, :], i * 128) for i in range(OWN // 128)]
                norm_to_xt(s1, srcs, XNTOWN, a1T[:, :, 0], sh1T[:, :, 0], "n1o", 'XNTOWN')

        cp_toggle = [0]

        def evac(out_ap, in_ap, func, r, w):
            if func is None:
                cp_toggle[0] ^= 1
                if cp_toggle[0]:
                    kb.op('dve', lambda e: e.tensor_copy(out=out_ap, in_=in_ap), r=r, w=w)
                else:
                    kb.op('act', lambda e: e.copy(out=out_ap, in_=in_ap), r=r, w=w)
            else:
                kb.op('act', lambda e: e.activation(out=out_ap, in_=in_ap, func=func), r=r, w=w)

        def gemm_group(tag, Wsrc, kch, ncols, XT, xtkey, tblocks, jobs):
            with stage() as st:
                Wsb = sbt(st, tag + "_W", [128, kch, ncols])
                kb.dma('sp', lambda e: e.dma_start(out=Wsb[:], in_=Wsrc.rearrange("(k p) c -> p k c", p=128)), w=[tag + '_W'])
                Xb = [sbt(st, "%s_X%d" % (tag, i), [128, kch, 512]) for i in range(2)]
                stg = [sbt(st, "%s_s%d" % (tag, i), [128, 512]) for i in range(4)]
                si = 0
                bi = 0
                for ti, (t0, tn) in enumerate(tblocks):
                    xb = Xb[ti % 2]; xk = "%s_X%d" % (tag, ti % 2)
                    kb.dma('sp', lambda e: e.dma_start(out=xb[:, :, 0:tn], in_=XT[:, t0:t0 + tn].rearrange("(k p) t -> p k t", p=128)),
                           r=[xtkey], w=[xk])
                    for jb in jobs:
                        if jb.get('tsel') is not None and not jb['tsel'](t0):
                            continue
                        c0, cn, tofs = jb['c0'], jb['cn'], jb.get('tofs', 0)
                        if jb['mode'] == 'fm':
                            for m in range(cn // 128):
                                pb, pbk = banks[bi % 8]; bi += 1
                                for kk in range(kch):
                                    kb.op('pe', lambda e: e.matmul(pb[:, 0:tn], lhsT=Wsb[:, kk, c0 + m * 128:c0 + (m + 1) * 128], rhs=xb[:, kk, 0:tn],
                                                                   start=(kk == 0), stop=(kk == kch - 1)), r=[tag + '_W', xk], w=[pbk])
                                sg = stg[si % 4]; sgk = "%s_s%d" % (tag, si % 4); si += 1
                                evac(sg[:, 0:tn], pb[:, 0:tn], jb['func'], [pbk], [sgk])
                                orow = jb.get('oc0', 0) + m * 128
                                kb.dma('pool', lambda e: e.dma_start(out=jb['out'][orow:orow + 128, t0 - tofs:t0 - tofs + tn], in_=sg[:, 0:tn]),
                                       r=[sgk], w=[jb['okey']])
                        else:
                            for tt in range(tn // 128):
                                pb, pbk = banks[bi % 8]; bi += 1
                                for kk in range(kch):
                                    kb.op('pe', lambda e: e.matmul(pb[:, 0:cn], lhsT=xb[:, kk, tt * 128:(tt + 1) * 128], rhs=Wsb[:, kk, c0:c0 + cn],
                                                                   start=(kk == 0), stop=(kk == kch - 1)), r=[tag + '_W', xk], w=[pbk])
                                sg = stg[si % 4]; sgk = "%s_s%d" % (tag, si % 4); si += 1
                                evac(sg[:, 0:cn], pb[:, 0:cn], jb['func'], [pbk], [sgk])
                                tr = t0 - tofs + tt * 128
                                oc0 = jb.get('oc0', 0)
                                kb.dma('pool', lambda e: e.dma_start(out=jb['out'][tr:tr + 128, oc0:oc0 + cn], in_=sg[:, 0:cn]),
                                       r=[sgk], w=[jb['okey']])

        if stages >= 2:
            lat_blocks = [(t, 512) for t in range(0, L, 512)]
            all_blocks = lat_blocks + [(L, 256)]
            gemm_group("g1", WIN[:, 0:1024], 8, 1024, XNT, 'XNT', lat_blocks,
                       [dict(c0=0, cn=1024, mode='fm', func=None, out=HYRAW, okey='HYRAW', oc0=0)])
            gemm_group("g2", WIN[:, 1024:2048], 8, 1024, XNT, 'XNT', lat_blocks,
                       [dict(c0=0, cn=512, mode='fm', func=None, out=HYRAW, okey='HYRAW', oc0=1024),
                        dict(c0=512, cn=512, mode='fm', func=AF.Silu, out=QT, okey='QT', oc0=0)])
        if stages >= 3:
            gemm_group("g3", WIN[:, 2048:3072], 8, 1024, XNT, 'XNT', all_blocks,
                       [dict(c0=0, cn=512, mode='tm', func=None, out=IFF, okey='IFF', oc0=0),
                        dict(c0=512, cn=512, mode='tm', func=None, out=IFF, okey='IFF', oc0=512)])
            gemm_group("g4", WIN[:, 3072:4096], 8, 1024, XNT, 'XNT', all_blocks,
                       [dict(c0=0, cn=512, mode='tm', func=None, out=IFF, okey='IFF', oc0=1024),
                        dict(c0=512, cn=512, mode='fm', func=AF.Silu, out=GT, okey='GT', oc0=0, tsel=lambda t0: t0 < L)])

        if stages >= 3:
            own_blocks = [(t, 512) for t in range(0, OWN, 512)]
            for gi in range(2):
                gemm_group("g%d" % (5 + gi), WIN[:, 4096 + 1024 * gi:5120 + 1024 * gi], 8, 1024, XNTOWN, 'XNTOWN', own_blocks,
                           [dict(c0=0, cn=1024, mode='fm', func=AF.Sigmoid, out=SGT, okey='SGT', oc0=1024 * gi)])

        if stages >= 4:
            with stage() as st:
                cwT = sbt(st, "cwT", [128, 12, 4]); craw = sbt(st, "craw", [48, 128])
                for j3 in range(3):
                    kb.dma('sp', lambda e: e.dma_start(out=craw[12 * j3:12 * j3 + 12, :], in_=CONVW[j3].rearrange("(q p) -> q p", p=128)), r=['craw'], w=['craw'])
                kb.dma('sp', lambda e: e.dma_start(out=craw[36:48, :], in_=CONVB.rearrange("(q p) -> q p", p=128)), r=['craw'], w=['craw'])
                pb, pbk = banks[0]
                kb.op('pe', lambda e: e.transpose(out=pb[:, 0:48], in_=craw[:], identity=ident[0:48, 0:48]), r=['craw', 'ident'], w=[pbk])
                kb.op('dve', lambda e: e.tensor_copy(out=cwT[:], in_=pb[:, 0:48].rearrange("p (j q) -> p q j", j=4)), r=[pbk], w=['cwT'])
                uin = [sbt(st, "uin%d" % i, [128, L + 2]) for i in range(2)]
                uo = [sbt(st, "uo%d" % i, [128, L]) for i in range(2)]
                for i in range(2):
                    kb.op('pool', lambda e: e.memset(uin[i][:, 0:1], 0.0), w=['uin%d' % i])
                    kb.op('pool', lambda e: e.memset(uin[i][:, L + 1:L + 2], 0.0), r=['uin%d' % i], w=['uin%d' % i])
                for q in range(12):
                    b2 = q % 2; uk = 'uin%d' % b2; ok = 'uo%d' % b2
                    kb.dma('sp', lambda e: e.dma_start(out=uin[b2][:, 1:L + 1], in_=HYRAW[q * 128:(q + 1) * 128, :]), r=['HYRAW', uk], w=[uk])
                    kb.op('dve', lambda e: e.tensor_scalar(out=uo[b2][:], in0=uin[b2][:, 0:L], scalar1=cwT[:, q, 0:1], scalar2=cwT[:, q, 3:4],
                                                           op0=ALU.mult, op1=ALU.add), r=[uk, 'cwT'], w=[ok])
                    kb.op('dve', lambda e: e.scalar_tensor_tensor(out=uo[b2][:], in0=uin[b2][:, 1:L + 1], scalar=cwT[:, q, 1:2], in1=uo[b2][:],
                                                                  op0=ALU.mult, op1=ALU.add), r=[uk, 'cwT', ok], w=[ok])
                    kb.op('dve', lambda e: e.scalar_tensor_tensor(out=uo[b2][:], in0=uin[b2][:, 2:L + 2], scalar=cwT[:, q, 2:3], in1=uo[b2][:],
                                                                  op0=ALU.mult, op1=ALU.add), r=[uk, 'cwT', ok], w=[ok])
                    kb.dma('pool', lambda e: e.dma_start(out=HYC[q * 128:(q + 1) * 128, :], in_=uo[b2][:]), r=[ok], w=['HYC'])

        if stages >= 5 and not dummy_mix:
          with stage() as st:
            tri = {0: sbt(st, "trif", [128, 128]), 1: sbt(st, "trib", [128, 128])}
            ones = sbt(st, "ones", [128, 128])
            kb.dma('sp', lambda e: e.dma_start(out=tri[0][:], in_=TRIFd), w=['tri'])
            kb.dma('sp', lambda e: e.dma_start(out=tri[1][:], in_=TRIBd), r=['tri'], w=['tri'])
            kb.dma('sp', lambda e: e.dma_start(out=ones[:], in_=ONESd), w=['ones'])
            l0 = sbt(st, "l0", [128, 1024]); l1 = sbt(st, "l1", [128, 1024]); oml = sbt(st, "oml", [128, 1024])
            kb.dma('sp', lambda e: e.dma_start(out=l0[:], in_=LBL[0:1, :].to_broadcast([128, 1024])), w=['l0'])
            kb.dma('sp', lambda e: e.dma_start(out=l1[:], in_=LBL[1:2, :].to_broadcast([128, 1024])), w=['l1'])
            kb.op('dve', lambda e: e.tensor_tensor(out=l0[:], in0=l0[:], in1=l1[:], op=ALU.subtract), r=['l0', 'l1'], w=['l0'])
            kb.op('act', lambda e: e.activation(out=l0[:], in_=l0[:], func=AF.Sigmoid), r=['l0'], w=['l0'])
            kb.op('dve', lambda e: e.tensor_scalar(out=oml[:], in0=l0[:], scalar1=-1.0, scalar2=1.0, op0=ALU.mult, op1=ALU.add), r=['l0'], w=['oml'])
            ngT = sbt(st, "ngT", [128, 1])
            with nc.allow_non_contiguous_dma(reason="128-element vector to partitions"):
                kb.dma('sp', lambda e: e.dma_start(out=ngT[:], in_=HGNG.rearrange("(p o) -> p o", o=1)), w=['ngT'])
            osum = sbt(st, "osum", [128, L])
            LB8 = sbt(st, "LB8", [128, 8, 128]); OML8 = sbt(st, "OML8", [128, 8, 128])
            vseg = [sbt(st, "vseg%d" % i, [128, 8, 128]) for i in range(2)]
            fseg = [sbt(st, "fseg%d" % i, [128, 8, 128]) for i in range(2)]
            lseg = [sbt(st, "lseg%d" % i, [128, 8, 128]) for i in range(2)]
            kseg = [sbt(st, "kseg%d" % i, [128, 8, 128]) for i in range(2)]
            qseg = [sbt(st, "qseg%d" % i, [128, 1024]) for i in range(2)]
            R3 = 3
            ekb = [sbt(st, "ekb%d" % i, [128, 128]) for i in range(R3)]
            kd = [sbt(st, "kd%d" % i, [128, 128]) for i in range(R3)]
            ebT = [sbt(st, "ebT%d" % i, [128, 128]) for i in range(R3)]
            qdT = [sbt(st, "qdT%d" % i, [128, 128]) for i in range(R3)]
            kdT = [sbt(st, "kdT%d" % i, [128, 128]) for i in range(R3)]
            attm = [sbt(st, "attm%d" % i, [128, 128]) for i in range(R3)]
            Sr = [sbt(st, "S%d" % i, [128, 128]) for i in range(4)]
            Se = [sbt(st, "Se%d" % i, [128, 128]) for i in range(2)]
            pbi = [0]

            def nb():
                b_ = banks[pbi[0] % 8]; pbi[0] += 1
                return b_

            for h in range(4):
                for dr in range(2):
                    T = tri[dr]
                    fcol = 512 + 512 * dr + h * 128
                    for n in range(8):
                        kb.op('pool', lambda e: e.tensor_copy(out=LB8[:, n, :], in_=l0[:, dr * 512 + h * 128:dr * 512 + (h + 1) * 128]), r=['l0', 'LB8'], w=['LB8'])
                        kb.op('pool', lambda e: e.tensor_copy(out=OML8[:, n, :], in_=oml[:, dr * 512 + h * 128:dr * 512 + (h + 1) * 128]), r=['oml', 'OML8'], w=['OML8'])
                    si_ = 0
                    kb.op('pool', lambda e: e.memset(Sr[0][:], 0.0), w=['S0'])
                    segs = [(L, 2, False)] + [(sg * 1024, 8, True) for sg in (range(8) if dr == 0 else range(7, -1, -1))]
                    tcount = 0
                    for sgi, (r0, nt, lat) in enumerate(segs):
                        b2 = sgi % 2
                        vk, fk, lk, kk_, qk = 'vseg%d' % b2, 'fseg%d' % b2, 'lseg%d' % b2, 'kseg%d' % b2, 'qseg%d' % b2
                        kb.dma('sp', lambda e: e.dma_start(out=vseg[b2][:, 0:nt, :], in_=IFF[r0:r0 + nt * 128, h * 128:(h + 1) * 128].rearrange("(n p) c -> p n c", p=128)),
                               r=['IFF'], w=[vk])
                        kb.dma('sp', lambda e: e.dma_start(out=fseg[b2][:, 0:nt, :], in_=IFF[r0:r0 + nt * 128, fcol:fcol + 128].rearrange("(n p) c -> p n c", p=128)),
                               r=['IFF'], w=[fk])
                        if lat:
                            kb.dma('sp', lambda e: e.dma_start(out=qseg[b2][:], in_=QT[h * 128:(h + 1) * 128, r0:r0 + 1024]), r=['QT'], w=[qk])
                        kb.op('act', lambda e: e.activation(out=fseg[b2][:, 0:nt, :], in_=fseg[b2][:, 0:nt, :], func=AF.Sigmoid), r=[fk], w=[fk])
                        kb.op('dve', lambda e: e.tensor_tensor(out=fseg[b2][:, 0:nt, :], in0=fseg[b2][:, 0:nt, :], in1=OML8[:, 0:nt, :], op=ALU.mult), r=[fk, 'OML8'], w=[fk])
                        kb.op('dve', lambda e: e.tensor_tensor(out=fseg[b2][:, 0:nt, :], in0=fseg[b2][:, 0:nt, :], in1=LB8[:, 0:nt, :], op=ALU.add), r=[fk, 'LB8'], w=[fk])
                        kb.op('act', lambda e: e.activation(out=lseg[b2][:, 0:nt, :], in_=fseg[b2][:, 0:nt, :], func=AF.Ln), r=[fk], w=[lk])
                        kb.op('dve', lambda e: e.tensor_scalar(out=kseg[b2][:, 0:nt, :], in0=fseg[b2][:, 0:nt, :], scalar1=-1.0, scalar2=1.0, op0=ALU.mult, op1=ALU.add),
                              r=[fk], w=[kk_])
                        order = range(nt) if dr == 0 else range(nt - 1, -1, -1)
                        for n in order:
                            r3 = tcount % R3; tcount += 1
                            ek, kdk, ebk, qdk, ktk, amk = 'ekb%d' % r3, 'kd%d' % r3, 'ebT%d' % r3, 'qdT%d' % r3, 'kdT%d' % r3, 'attm%d' % r3
                            p1, p1k = nb()
                            kb.op('pe', lambda e: e.matmul(p1[:, 0:128], lhsT=T[:], rhs=lseg[b2][:, n, :], start=True, stop=True), r=['tri', lk], w=[p1k])
                            kb.op('act', lambda e: e.activation(out=ekb[r3][:], in_=p1[:, 0:128], func=AF.Exp, scale=-1.0), r=[p1k], w=[ek])
                            kb.op('dve', lambda e: e.tensor_tensor(out=kd[r3][:], in0=kseg[b2][:, n, :], in1=ekb[r3][:], op=ALU.mult), r=[kk_, ek], w=[kdk])
                            p2, p2k = nb()
                            kb.op('pe', lambda e: e.matmul(p2[:, 0:128], lhsT=lseg[b2][:, n, :], rhs=T[:], start=True, stop=True), r=['tri', lk], w=[p2k])
                            kb.op('act', lambda e: e.activation(out=ebT[r3][:], in_=p2[:, 0:128], func=AF.Exp), r=[p2k], w=[ebk])
                            if lat:
                                kb.op('dve', lambda e: e.tensor_tensor(out=qdT[r3][:], in0=qseg[b2][:, n * 128:(n + 1) * 128], in1=ebT[r3][:], op=ALU.mult), r=[qk, ebk], w=[qdk])
                                p3, p3k = nb()
                                kb.op('pe', lambda e: e.transpose(out=p3[:, 0:128], in_=kd[r3][:], identity=ident[:]), r=[kdk, 'ident'], w=[p3k])
                                kb.op('act', lambda e: e.copy(out=kdT[r3][:], in_=p3[:, 0:128]), r=[p3k], w=[ktk])
                                p4, p4k = nb()
                                kb.op('pe', lambda e: e.matmul(p4[:, 0:128], lhsT=kdT[r3][:], rhs=qdT[r3][:], start=True, stop=True), r=[ktk, qdk], w=[p4k])
                                kb.op('dve', lambda e: e.tensor_tensor(out=attm[r3][:], in0=p4[:, 0:128], in1=T[:], op=ALU.mult), r=[p4k, 'tri'], w=[amk])
                                po, pok = nb()
                                kb.op('pe', lambda e: e.matmul(po[:, 0:128], lhsT=vseg[b2][:, n, :], rhs=attm[r3][:], start=True, stop=False), r=[vk, amk], w=[pok])
                            halves = [(0, 64, 63), (64, 128, 127)] if dr == 0 else [(64, 128, 64), (0, 64, 0)]
                            for (c0, c1, ce) in halves:
                                Sc = Sr[si_ % 4]; Sck = 'S%d' % (si_ % 4)
                                Sn = Sr[(si_ + 1) % 4]; Snk = 'S%d' % ((si_ + 1) % 4)
                                sew = Se[si_ % 2]; sek = 'Se%d' % (si_ % 2)
                                si_ += 1
                                if lat:
                                    kb.op('pe', lambda e: e.matmul(po[:, c0:c1], lhsT=Sc[:], rhs=qdT[r3][:, c0:c1], start=False, stop=True), r=[Sck, qdk], w=[pok])
                                pd, pdk = nb()
                                kb.op('pe', lambda e: e.matmul(pd[:, 0:128], lhsT=kd[r3][c0:c1, :], rhs=vseg[b2][c0:c1, n, :], start=True, stop=True), r=[kdk, vk], w=[pdk])
                                kb.op('act', lambda e: e.activation(out=sew[:], in_=Sc[:], func=AF.Copy, scale=ebT[r3][:, ce:ce + 1]), r=[Sck, ebk], w=[sek])
                                kb.op('dve', lambda e: e.scalar_tensor_tensor(out=Sn[:], in0=pd[:, 0:128], scalar=ebT[r3][:, ce:ce + 1], in1=sew[:], op0=ALU.mult, op1=ALU.add),
                                      r=[pdk, ebk, sek], w=[Snk])
                            if lat:
                                cols = slice(r0 + n * 128, r0 + (n + 1) * 128)
                                if dr == 0:
                                    kb.op('act', lambda e: e.copy(out=osum[:, cols], in_=po[:, 0:128]), r=[pok], w=['osum%d' % (r0 // 1024)])
                                else:
                                    kb.op('dve', lambda e: e.tensor_tensor(out=osum[:, cols], in0=po[:, 0:128], in1=osum[:, cols], op=ALU.add),
                                          r=[pok, 'osum%d' % (r0 // 1024)], w=['osum%d' % (r0 // 1024)])
                    if si_ % 4 != 0:
                        pass
                    kb.res.pop('unused', None)
                sq = [sbt(st, "hsq%d_%d" % (h, i), [128, 512]) for i in range(2)] if h == 0 else sq
                gt_ = [sbt(st, "hgt%d_%d" % (h, i), [128, 512]) for i in range(2)] if h == 0 else gt_
                rs = [sbt(st, "hrs%d_%d" % (h, i), [128, 512]) for i in range(2)] if h == 0 else rs
                for blk in range(L // 512):
                    b2 = blk % 2; cols = slice(blk * 512, (blk + 1) * 512)
                    sqk, gk, rk = 'hsq%d' % b2, 'hgt%d' % b2, 'hrs%d' % b2
                    ok_ = 'osum%d' % (blk // 2)
                    kb.dma('sp', lambda e: e.dma_start(out=gt_[b2][:], in_=GT[h * 128:(h + 1) * 128, cols]), r=['GT'], w=[gk])
                    kb.op('act', lambda e: e.activation(out=sq[b2][:], in_=osum[:, cols], func=AF.Square), r=[ok_], w=[sqk])
                    pb, pbk = nb()
                    kb.op('pe', lambda e: e.matmul(pb[:, :], lhsT=ones[:], rhs=sq[b2][:], start=True, stop=True), r=['ones', sqk], w=[pbk])
                    kb.op('dve', lambda e: e.tensor_scalar(out=rs[b2][:], in0=pb[:, :], scalar1=1.0 / 128, scalar2=EPS, op0=ALU.mult, op1=ALU.add), r=[pbk], w=[rk])
                    kb.op('act', lambda e: e.activation(out=rs[b2][:], in_=rs[b2][:], func=AF.Sqrt), r=[rk], w=[rk])
                    kb.op('dve', lambda e: e.reciprocal(out=rs[b2][:], in_=rs[b2][:]), r=[rk], w=[rk])
                    kb.op('dve', lambda e: e.scalar_tensor_tensor(out=sq[b2][:], in0=osum[:, cols], scalar=ngT[:, 0:1], in1=rs[b2][:], op0=ALU.mult, op1=ALU.mult),
                          r=[ok_, 'ngT', rk, sqk], w=[sqk])
                    kb.op('pool', lambda e: e.tensor_tensor(out=sq[b2][:], in0=sq[b2][:], in1=gt_[b2][:], op=ALU.mult), r=[sqk, gk], w=[sqk])
                    kb.dma('pool', lambda e: e.dma_start(out=YHGT[h * 128:(h + 1) * 128, cols], in_=sq[b2][:]), r=[sqk], w=['YHGT'])

        if stages >= 5 and not dummy_mix:
          with stage() as st:
            cst = {}
            for nm, src, shp in (("FA", FAd, [128, 256]), ("FAH", FAHd, [64, 256]), ("FRE", FREd, [128, 128]), ("FIM", FIMd, [128, 128]), ("NFIM", NFIMd, [128, 128]),
                                 ("CA", CAd, [128, 256]), ("CB", CBd, [128, 256]), ("FREN", FRENd, [128, 64]), ("FIMN", FIMNd, [128, 64]),
                                 ("TRE2", TRE2d, [128, 256]), ("TIM2", TIM2d, [128, 256]), ("ONESH", ONESd, [128, 128])):
                cst[nm] = sbt(st, "c_" + nm, shp)
                kb.dma('sp', lambda e: e.dma_start(out=cst[nm][:], in_=src), w=['hconst'])
            skb = sbt(st, "skb", [128, 1024])
            kb.dma('sp', lambda e: e.dma_start(out=skb[:], in_=HSKIP.rearrange("(o n) -> o n", o=1).to_broadcast([128, 1024])), w=['skb'])
            tw = [sbt(st, "tw%d" % i, [128, 256]) for i in range(4)]
            hbi = [0]

            def hb():
                b_ = banks[hbi[0] % 8]; hbi[0] += 1
                return b_

            def twiddle(ps, psk, Bt, bkey, c0, conj):
                A = ps[:, :].rearrange("p (c r f) -> p c r f", c=2, r=2)
                Are, Aim = A[:, :, 0, :], A[:, :, 1, :]
                T2r = cst["TRE2"][:].rearrange("p (c f) -> p c f", c=2); T2i = cst["TIM2"][:].rearrange("p (c f) -> p c f", c=2)
                t = [x[:].rearrange("p (c f) -> p c f", c=2) for x in tw]
                kb.op('dve', lambda e: e.tensor_tensor(out=t[0], in0=Are, in1=T2r, op=ALU.mult), r=[psk, 'hconst', 'tw0'], w=['tw0'])
                kb.op('dve', lambda e: e.tensor_tensor(out=t[1], in0=Aim, in1=T2i, op=ALU.mult), r=[psk, 'hconst', 'tw1'], w=['tw1'])
                kb.op('dve', lambda e: e.tensor_tensor(out=t[2], in0=Are, in1=T2i, op=ALU.mult), r=[psk, 'hconst', 'tw2'], w=['tw2'])
                kb.op('dve', lambda e: e.tensor_tensor(out=t[3], in0=Aim, in1=T2r, op=ALU.mult), r=[psk, 'hconst', 'tw3'], w=['tw3'])
                if not conj:
                    kb.op('pool', lambda e: e.tensor_tensor(out=Bt[:, c0:c0 + 2, 0, :], in0=t[0], in1=t[1], op=ALU.subtract), r=['tw0', 'tw1', bkey], w=[bkey])
                    kb.op('pool', lambda e: e.tensor_tensor(out=Bt[:, c0:c0 + 2, 1, :], in0=t[2], in1=t[3], op=ALU.add), r=['tw2', 'tw3', bkey], w=[bkey])
                else:
                    kb.op('pool', lambda e: e.tensor_tensor(out=Bt[:, c0:c0 + 2, 0, :], in0=t[0], in1=t[1], op=ALU.add), r=['tw0', 'tw1', bkey], w=[bkey])
                    kb.op('pool', lambda e: e.tensor_tensor(out=Bt[:, c0:c0 + 2, 1, :], in0=t[3], in1=t[2], op=ALU.subtract), r=['tw2', 'tw3', bkey], w=[bkey])

            def stage2(Bt, bkey, c4):
                pr, prk = hb(); pi_, pik = hb()
                Bre, Bim = Bt[:, c4:c4 + 4, 0, :], Bt[:, c4:c4 + 4, 1, :]
                kb.op('pe', lambda e: e.matmul(pr[:, :], lhsT=cst["FRE"][:], rhs=Bre, start=True, stop=False), r=['hconst', bkey], w=[prk])
                kb.op('pe', lambda e: e.matmul(pr[:, :], lhsT=cst["NFIM"][:], rhs=Bim, start=False, stop=True), r=['hconst', bkey], w=[prk])
                kb.op('pe', lambda e: e.matmul(pi_[:, :], lhsT=cst["FIM"][:], rhs=Bre, start=True, stop=False), r=['hconst', bkey], w=[pik])
                kb.op('pe', lambda e: e.matmul(pi_[:, :], lhsT=cst["FRE"][:], rhs=Bim, start=False, stop=True), r=['hconst', bkey], w=[pik])
                return pr, prk, pi_, pik

            B16 = sbt(st, "B16", [128, 16, 2, 128])
            with stage() as sf:
                a3 = sbt(sf, "a3", [128, L])
                w4sb = sbt(sf, "w4sb", [128, 2048])
                kb.dma('sp', lambda e: e.dma_start(out=w4sb[0:64, :], in_=FW4), w=['w4sb'])
                kb.dma('sp', lambda e: e.dma_start(out=w4sb[64:128, :], in_=FW4), r=['w4sb'], w=['w4sb'])
                with stage() as sm:
                    zts = sbt(sm, "zts", [66, L]); bufB = sbt(sm, "bufB", [128, L])
                    kb.dma('sp', lambda e: e.dma_start(out=zts[:], in_=ZTd), w=['zts'])
                    wl = [sbt(sm, "w1bd", [66, 128]), sbt(sm, "w2bd", [128, 128]), sbt(sm, "w3bd", [128, 128])]
                    for i_, (wt, src, kin) in enumerate(zip(wl, (FW1, FW2, FW3), (33, 64, 64))):
                        kb.op('pool', lambda e: e.memset(wt[:], 0.0), w=['wbd%d' % i_])
                        kb.dma('sp', lambda e: e.dma_start(out=wt[0:kin, 0:64], in_=src), r=['wbd%d' % i_], w=['wbd%d' % i_])
                        kb.dma('sp', lambda e: e.dma_start(out=wt[kin:2 * kin, 64:128], in_=src), r=['wbd%d' % i_], w=['wbd%d' % i_])
                    fqb = sbt(sm, "fqb", [128, 4])
                    with nc.allow_non_contiguous_dma(reason="64-element vectors onto partitions"):
                        for j_, src in enumerate((FFQ, FB1, FB2, FB3)):
                            for hh in range(2):
                                kb.dma('sp', lambda e: e.dma_start(out=fqb[64 * hh:64 * hh + 64, j_:j_ + 1], in_=src.rearrange("(p o) -> p o", o=1)), r=['fqb'], w=['fqb'])
                    kb.op('dve', lambda e: e.tensor_scalar(out=fqb[:, 0:1], in0=fqb[:, 0:1], scalar1=1.0 / TWO_PI, scalar2=None, op0=ALU.mult), r=['fqb'], w=['fqb'])
                    kb.op('dve', lambda e: e.tensor_scalar(out=fqb[:, 1:4], in0=fqb[:, 1:4], scalar1=fqb[:, 0:1], scalar2=None, op0=ALU.mult), r=['fqb'], w=['fqb'])
                    uu = sbt(sm, "uu", [128, 2048]); ui = sbt(sm, "ui", [128, 2048], I32); uf = sbt(sm, "uf", [128, 2048])
                    srcs = [(zts, 66, 'zts'), (bufB, 128, 'bufB'), (a3, 128, 'a3')]
                    dsts = [(bufB, 'bufB'), (a3, 'a3'), (bufB, 'bufB')]
                    for l_ in range(3):
                        src_t, kdim, srck = srcs[l_]; dst_t, dstk = dsts[l_]
                        for ch in range(4):
                            Pq, pkeys = (PA, ["PA0", "PA1", "PA2", "PA3"]) if ch % 2 == 0 else (PB, ["PB0", "PB1", "PB2", "PB3"])
                            for q in range(4):
                                cs = slice(ch * 2048 + q * 512, ch * 2048 + (q + 1) * 512)
                                kb.op('pe', lambda e: e.matmul(Pq[:, q * 512:(q + 1) * 512], lhsT=wl[l_][0:kdim, :], rhs=src_t[0:kdim, cs], start=True, stop=True),
                                      r=['wbd%d' % l_, srck], w=[pkeys[q]])
                            kb.op('act', lambda e: e.activation(out=uu[:], in_=Pq[:, :], func=AF.Identity, bias=fqb[:, 1 + l_:2 + l_], scale=fqb[:, 0:1]), r=pkeys + ['fqb'], w=['uu'])
                            kb.op('dve', lambda e: e.tensor_copy(out=ui[:], in_=uu[:]), r=['uu'], w=['ui'])
                            kb.op('dve', lambda e: e.tensor_copy(out=uf[:], in_=ui[:]), r=['ui'], w=['uf'])
                            kb.op('pool', lambda e: e.tensor_tensor(out=uu[:], in0=uu[:], in1=uf[:], op=ALU.subtract), r=['uu', 'uf'], w=['uu'])
                            kb.op('dve', lambda e: e.scalar_tensor_tensor(out=uf[:], in0=uu[:], scalar=0.5, in1=uu[:], op0=ALU.is_gt, op1=ALU.subtract), r=['uu', 'uf'], w=['uf'])
                            kb.op('dve', lambda e: e.scalar_tensor_tensor(out=uu[:], in0=uf[:], scalar=0.5, in1=uf[:], op0=ALU.is_gt, op1=ALU.subtract), r=['uu', 'uf'], w=['uu'])
                            kb.op('act', lambda e: e.activation(out=dst_t[:, ch * 2048:(ch + 1) * 2048], in_=uu[:], func=AF.Sin, scale=6.283185), r=['uu', dstk], w=[dstk])
                    kb.op('pool', lambda e: e.tensor_copy(out=a3[:], in_=bufB[:]), r=['bufB', 'a3'], w=['a3'])
                Kt = [sbt(sf, "Kt%d" % i, [64, 64, 128]) for i in range(2)]
                dec = sbt(sf, "dec", [64, 64, 128]); rab = sbt(sf, "rab", [64, 2, 64]); rn = sbt(sf, "rn", [128, 64]); Hst = sbt(sf, "Hst", [128, 16, 2, 128])
                for o in range(2):
                    for cg in range(8):
                        for hh in range(2):
                            col0 = o * 1024 + hh * 512 + cg * 64
                            for nbk in range(16):
                                ps, psk = hb()
                                for j_ in range(8):
                                    n2 = nbk * 8 + j_
                                    kb.op('pe', lambda e: e.matmul(ps[0:64, j_ * 64:(j_ + 1) * 64], lhsT=a3[64 * hh:64 * hh + 64, n2:L:128], rhs=w4sb[64 * hh:64 * hh + 64, col0:col0 + 64],
                                                                   start=True, stop=True), r=['a3', 'w4sb'], w=[psk])
                                evac(Kt[hh][:, :, nbk * 8:(nbk + 1) * 8], ps[0:64, :].rearrange("p (n c) -> p c n", c=64), None, [psk, 'Kt%d' % hh], ['Kt%d' % hh])
                            kb.dma('sp', lambda e: e.dma_start(out=dec[:], in_=DECd[hh, :, cg * 64:(cg + 1) * 64, :]), r=['dec'], w=['dec'])
                            kb.op('pool', lambda e: e.tensor_tensor(out=Kt[hh][:], in0=Kt[hh][:], in1=dec[:], op=ALU.mult), r=['Kt%d' % hh, 'dec'], w=['Kt%d' % hh])
                            kb.op('dve', lambda e: e.tensor_reduce(out=rab[:, hh, :], in_=Kt[hh][:], axis=AX.X, op=ALU.add, apply_absolute_value=True), r=['Kt%d' % hh, 'rab'], w=['rab'])
                        kb.op('dve', lambda e: e.tensor_tensor(out=rab[:, 0, :], in0=rab[:, 0, :], in1=rab[:, 1, :], op=ALU.add), r=['rab'], w=['rab'])
                        ps, psk = hb()
                        kb.op('pe', lambda e: e.matmul(ps[:, 0:64], lhsT=cst["ONESH"][0:64, :], rhs=rab[:, 0, :], start=True, stop=True), r=['hconst', 'rab'], w=[psk])
                        kb.op('dve', lambda e: e.reciprocal(out=rn[:], in_=ps[:, 0:64]), r=[psk], w=['rn'])
                        for sb4 in range(4):
                            for c2 in range(8):
                                ps, psk = hb()
                                for j_ in range(2):
                                    cc = sb4 * 16 + c2 * 2 + j_
                                    kb.op('pe', lambda e: e.matmul(ps[:, j_ * 256:(j_ + 1) * 256], lhsT=Kt[0][:, cc, :], rhs=cst["FA"][0:64, :], start=True, stop=False), r=['Kt0', 'hconst'], w=[psk])
                                    kb.op('pe', lambda e: e.matmul(ps[:, j_ * 256:(j_ + 1) * 256], lhsT=Kt[1][:, cc, :], rhs=cst["FAH"][:], start=False, stop=True), r=['Kt1', 'hconst'], w=[psk])
                                twiddle(ps, psk, B16, 'B16', c2 * 2, False)
                            for c4 in range(0, 16, 4):
                                pr, prk, pi_, pik = stage2(B16, 'B16', c4)
                                for j_ in range(4):
                                    cc = sb4 * 16 + c4 + j_; gc = cg * 64 + cc
                                    kb.op('dve', lambda e: e.tensor_scalar(out=Hst[:, c4 + j_, 0, :], in0=pr[:, j_ * 128:(j_ + 1) * 128], scalar1=rn[:, cc:cc + 1], scalar2=skb[:, o * 512 + gc:o * 512 + gc + 1],
                                                                           op0=ALU.mult, op1=ALU.add), r=[prk, 'rn', 'skb', 'Hst'], w=['Hst'])
                                    kb.op('act', lambda e: e.activation(out=Hst[:, c4 + j_, 1, :], in_=pi_[:, j_ * 128:(j_ + 1) * 128], func=AF.Copy, scale=rn[:, cc:cc + 1]), r=[pik, 'rn', 'Hst'], w=['Hst'])
                            g0 = o * 512 + cg * 64 + sb4 * 16
                            kb.dma('sp', lambda e: e.dma_start(out=HSPEC[g0:g0 + 16].rearrange("c k r f -> k c r f"), in_=Hst[:]), r=['Hst'], w=['HSPEC'])
            with stage() as sc:
                v16 = sbt(sc, "v16", [64, 16, 128]); x116 = sbt(sc, "x116", [64, 16, 128]); x216 = sbt(sc, "x216", [64, 16, 128]); z16 = sbt(sc, "z16", [64, 16, 128])
                y16 = sbt(sc, "y16", [64, 16, 128])
                H1 = sbt(sc, "H1", [128, 16, 2, 128]); H2s = sbt(sc, "H2s", [128, 16, 2, 128]); Y16 = sbt(sc, "Y16", [128, 16, 2, 128]); G16 = sbt(sc, "G16", [128, 16, 2, 128])
                hm = [sbt(sc, "hm%d" % i, [128, 4, 128]) for i in range(4)]

                def conv16(Din, dkey, Hs, hkey, Xmul, xkey, Out, okey):
                    for c2 in range(8):
                        ps, psk = hb()
                        for j_ in range(2):
                            kb.op('pe', lambda e: e.matmul(ps[:, j_ * 256:(j_ + 1) * 256], lhsT=Din[:, c2 * 2 + j_, :], rhs=cst["FA"][0:64, :], start=True, stop=True), r=[dkey, 'hconst'], w=[psk])
                        twiddle(ps, psk, B16, 'B16', c2 * 2, False)
                    for c4 in range(0, 16, 4):
                        pr, prk, pi_, pik = stage2(B16, 'B16', c4)
                        Xr = pr[:, :].rearrange("p (c f) -> p c f", c=4); Xi = pi_[:, :].rearrange("p (c f) -> p c f", c=4)
                        Hr, Hi = Hs[:, c4:c4 + 4, 0, :], Hs[:, c4:c4 + 4, 1, :]
                        kb.op('dve', lambda e: e.tensor_tensor(out=hm[0][:], in0=Xr, in1=Hr, op=ALU.mult), r=[prk, hkey, 'hm0'], w=['hm0'])
                        kb.op('dve', lambda e: e.tensor_tensor(out=hm[1][:], in0=Xi, in1=Hi, op=ALU.mult), r=[pik, hkey, 'hm1'], w=['hm1'])
                        kb.op('dve', lambda e: e.tensor_tensor(out=hm[2][:], in0=Xr, in1=Hi, op=ALU.mult), r=[prk, hkey, 'hm2'], w=['hm2'])
                        kb.op('dve', lambda e: e.tensor_tensor(out=hm[3][:], in0=Xi, in1=Hr, op=ALU.mult), r=[pik, hkey, 'hm3'], w=['hm3'])
                        kb.op('pool', lambda e: e.tensor_tensor(out=Y16[:, c4:c4 + 4, 0, :], in0=hm[0][:], in1=hm[1][:], op=ALU.subtract), r=['hm0', 'hm1', 'Y16'], w=['Y16'])
                        kb.op('pool', lambda e: e.tensor_tensor(out=Y16[:, c4:c4 + 4, 1, :], in0=hm[2][:], in1=hm[3][:], op=ALU.add), r=['hm2', 'hm3', 'Y16'], w=['Y16'])
                    for c2 in range(8):
                        ps, psk = hb()
                        for j_ in range(2):
                            cc = c2 * 2 + j_
                            kb.op('pe', lambda e: e.matmul(ps[:, j_ * 256:(j_ + 1) * 256], lhsT=Y16[:, cc, 0, :], rhs=cst["CA"][:], start=True, stop=False), r=['Y16', 'hconst'], w=[psk])
                            kb.op('pe', lambda e: e.matmul(ps[:, j_ * 256:(j_ + 1) * 256], lhsT=Y16[:, cc, 1, :], rhs=cst["CB"][:], start=False, stop=True), r=['Y16', 'hconst'], w=[psk])
                        twiddle(ps, psk, G16, 'G16', c2 * 2, True)
                    for c4 in range(0, 16, 4):
                        py, pyk = hb()
                        kb.op('pe', lambda e: e.matmul(py[0:64, :], lhsT=cst["FREN"][:], rhs=G16[:, c4:c4 + 4, 0, :], start=True, stop=False), r=['hconst', 'G16'], w=[pyk])
                        kb.op('pe', lambda e: e.matmul(py[0:64, :], lhsT=cst["FIMN"][:], rhs=G16[:, c4:c4 + 4, 1, :], start=False, stop=True), r=['hconst', 'G16'], w=[pyk])
                        kb.op('dve', lambda e: e.tensor_tensor(out=Out[:, c4:c4 + 4, :], in0=py[0:64, :].rearrange("p (c f) -> p c f", c=4), in1=Xmul[:, c4:c4 + 4, :], op=ALU.mult),
                              r=[pyk, xkey, okey], w=[okey])

                for g in range(32):
                    gc0 = g * 16
                    lh = lambda r0: HYC[r0 + gc0:r0 + gc0 + 16, :].rearrange("c (n1 n2) -> n1 c n2", n2=128)
                    kb.dma('sp', lambda e: e.dma_start(out=v16[:], in_=lh(0)), r=['HYC'], w=['v16'])
                    kb.dma('sp', lambda e: e.dma_start(out=x116[:], in_=lh(512)), r=['HYC'], w=['x116'])
                    kb.dma('sp', lambda e: e.dma_start(out=x216[:], in_=lh(1024)), r=['HYC'], w=['x216'])
                    kb.dma('sp', lambda e: e.dma_start(out=H1[:], in_=HSPEC[gc0:gc0 + 16].rearrange("c k r f -> k c r f")), r=['HSPEC'], w=['H1'])
                    kb.dma('sp', lambda e: e.dma_start(out=H2s[:], in_=HSPEC[512 + gc0:512 + gc0 + 16].rearrange("c k r f -> k c r f")), r=['HSPEC'], w=['H2s'])
                    conv16(v16, 'v16', H1, 'H1', x116, 'x116', z16, 'z16')
                    conv16(z16, 'z16', H2s, 'H2s', x216, 'x216', y16, 'y16')
                    kb.dma('pool', lambda e: e.dma_start(out=YHYT[gc0:gc0 + 16, :].rearrange("c (n1 n2) -> n1 c n2", n2=128), in_=y16[:]), r=['y16'], w=['YHYT'])

        if stages >= 5:
            with stage() as st:
                z = sbt(st, "zt", [128, L])
                kb.op('pool', lambda e: e.memset(z[:], 0.0), w=['zt'])
                for q in range(4):
                    if dummy_mix:
                        kb.dma('sp', lambda e: e.dma_start(out=z[:], in_=QT[q * 128:(q + 1) * 128, :]), r=['zt', 'QT'], w=['zt'])
                    if dummy_mix:
                        kb.dma('sp', lambda e: e.dma_start(out=YHYT[q * 128:(q + 1) * 128, :], in_=z[:]), r=['zt'], w=['YHYT'])
                    if dummy_mix:
                        kb.dma('sp', lambda e: e.dma_start(out=z[:], in_=GT[q * 128:(q + 1) * 128, :]), r=['zt', 'GT'], w=['zt'])
                    if dummy_mix:
                        kb.dma('sp', lambda e: e.dma_start(out=YHGT[q * 128:(q + 1) * 128, :], in_=z[:]), r=['zt'], w=['YHGT'])
                zr = sbt(st, "zr", [128, D])
                kb.op('pool', lambda e: e.memset(zr[:], 0.0), w=['zr'])
                if dummy_mix or stages < 7:
                    for i in range(OWN // 128):
                        kb.dma('sp', lambda e: e.dma_start(out=ROUTED[i * 128:(i + 1) * 128, :], in_=zr[:]), r=['zr'], w=['ROUTED'])

        def row_bcast(stack, name, src_row_ap):
            t = sbt(stack, name, [128, D])
            kb.dma('sp', lambda e: e.dma_start(out=t[:], in_=src_row_ap.to_broadcast([128, D])), r=['MODROW'], w=[name])
            return t

        if stages >= 5:
            with stage() as st:
                oidx = sbt(st, "oidx", [128, 4], I32)
                kb.dma('sp', lambda e: e.dma_start(out=oidx[:], in_=OWNIDX), w=['oidx'])
                yg = [sbt(st, "yg%d" % i, [128, OWN]) for i in range(2)]
                n = 0
                for src, skey, r0 in ((YHYT, 'YHYT', 0), (YHGT, 'YHGT', 512)):
                    v = src.rearrange("c (j t) -> (c j) t", j=4)
                    for cc in range(4):
                        g = yg[n % 2]; gk = "yg%d" % (n % 2); n += 1
                        kb.dma('pool', lambda e: e.indirect_dma_start(out=g[:], out_offset=None, in_=v,
                                                                     in_offset=bass.IndirectOffsetOnAxis(ap=oidx[:, cc:cc + 1], axis=0),
                                                                     bounds_check=RB_OWN, oob_is_err=False), r=[skey, 'oidx'], w=[gk])
                        kb.dma('sp', lambda e: e.dma_start(out=YOWN[r0 + cc * 128:r0 + (cc + 1) * 128, :], in_=g[:]), r=[gk], w=['YOWN'])
            with stage() as st:
                Wy = sbt(st, "Wy", [128, 8, D]); Wo = sbt(st, "Wo", [128, 8, D])
                kb.dma('sp', lambda e: e.dma_start(out=Wy[:, 0:4, :], in_=WHY.rearrange("(k p) c -> p k c", p=128)), w=['Wy'])
                kb.dma('sp', lambda e: e.dma_start(out=Wy[:, 4:8, :], in_=WHG.rearrange("(k p) c -> p k c", p=128)), r=['Wy'], w=['Wy'])
                kb.dma('sp', lambda e: e.dma_start(out=Wo[:], in_=WOUT.rearrange("(k p) c -> p k c", p=128)), w=['Wo'])
                g1row = row_bcast(st, "g1row", MODROW[0:1, 2048:3072])
                yb = sbt(st, "yb", [128, 8, 512]); sgb = sbt(st, "sgb", [128, 16, 512]); mT = sbt(st, "mT", [128, 8, 512])
                t1 = [sbt(st, "t1_%d" % i, [128, 512]) for i in range(2)]
                xt = [sbt(st, "xt%d" % i, [128, D]) for i in range(2)]; pt = [sbt(st, "pt%d" % i, [128, D]) for i in range(2)]
                bi = 0
                for blk in range(OWN // 512):
                    t0 = blk * 512
                    kb.dma('sp', lambda e: e.dma_start(out=yb[:], in_=YOWN[:, t0:t0 + 512].rearrange("(k p) t -> p k t", p=128)), r=['YOWN'], w=['yb'])
                    kb.dma('sp', lambda e: e.dma_start(out=sgb[:], in_=SGT[:, t0:t0 + 512].rearrange("(k p) t -> p k t", p=128)), r=['SGT'], w=['sgb'])
                    for dm in range(8):
                        for br in range(2):
                            pb, pbk = banks[bi % 8]; bi += 1
                            for cc in range(4):
                                kb.op('pe', lambda e: e.matmul(pb[:, :], lhsT=Wy[:, br * 4 + cc, dm * 128:(dm + 1) * 128], rhs=yb[:, br * 4 + cc, :],
                                                               start=(cc == 0), stop=(cc == 3)), r=['Wy', 'yb'], w=[pbk])
                            if br == 0:
                                kb.op('dve', lambda e: e.tensor_tensor(out=t1[dm % 2][:], in0=pb[:, :], in1=sgb[:, dm, :], op=ALU.mult),
                                      r=[pbk, 'sgb'], w=['t1_%d' % (dm % 2)])
                            else:
                                kb.op('dve', lambda e: e.tensor_tensor(out=mT[:, dm, :], in0=pb[:, :], in1=sgb[:, 8 + dm, :], op=ALU.mult),
                                      r=[pbk, 'sgb', 'mT'], w=['mT'])
                                kb.op('pool', lambda e: e.tensor_tensor(out=mT[:, dm, :], in0=mT[:, dm, :], in1=t1[dm % 2][:], op=ALU.add),
                                      r=['mT', 't1_%d' % (dm % 2)], w=['mT'])
                    for tt in range(4):
                        ti = blk * 4 + tt; b2 = ti % 2
                        kb.dma('sp', lambda e: e.dma_start(out=xt[b2][:], in_=XOWN[ti * 128:(ti + 1) * 128, :]), w=['xt%d' % b2])
                        kb.dma('sp', lambda e: e.dma_start(out=pt[b2][:], in_=POSOWN[ti * 128:(ti + 1) * 128, :]), w=['pt%d' % b2])
                        kb.op('pool', lambda e: e.tensor_tensor(out=xt[b2][:], in0=xt[b2][:], in1=pt[b2][:], op=ALU.add), r=['xt%d' % b2, 'pt%d' % b2], w=['xt%d' % b2])
                        for hf in range(2):
                            pb, pbk = banks[bi % 8]; bi += 1
                            for kk in range(8):
                                kb.op('pe', lambda e: e.matmul(pb[:, :], lhsT=mT[:, kk, tt * 128:(tt + 1) * 128], rhs=Wo[:, kk, hf * 512:(hf + 1) * 512],
                                                               start=(kk == 0), stop=(kk == 7)), r=['mT', 'Wo'], w=[pbk])
                            kb.op('dve', lambda e: e.tensor_tensor(out=pt[b2][:, hf * 512:(hf + 1) * 512], in0=pb[:, :], in1=g1row[:, hf * 512:(hf + 1) * 512], op=ALU.mult),
                                  r=[pbk, 'g1row', 'pt%d' % b2], w=['pt%d' % b2])
                        kb.op('pool', lambda e: e.tensor_tensor(out=xt[b2][:], in0=xt[b2][:], in1=pt[b2][:], op=ALU.add), r=['xt%d' % b2, 'pt%d' % b2], w=['xt%d' % b2])
                        kb.dma('pool', lambda e: e.dma_start(out=X1D[ti * 128:(ti + 1) * 128, :], in_=xt[b2][:]), r=['xt%d' % b2], w=['X1D'])

        if stages >= 6:
            a2T = sbt(es, "a2T", [128, 8]); sh2T = sbt(es, "sh2T", [128, 8])
            kb.op('dve', lambda e: e.tensor_scalar(out=a2T[:], in0=modT[:, 32:40, 0], scalar1=1.0, scalar2=None, op0=ALU.add), r=['modT'], w=['a2T'])
            kb.op('dve', lambda e: e.tensor_tensor(out=a2T[:], in0=a2T[:], in1=g2T[:], op=ALU.mult), r=['a2T', 'g2T'], w=['a2T'])
            kb.op('dve', lambda e: e.tensor_copy(out=sh2T[:], in_=modT[:, 24:32, 0]), r=['modT'], w=['sh2T'])
            with stage() as s1:
                srcs = [(X1D[i * 128:(i + 1) * 128, :], None, i * 128) for i in range(OWN // 128)]
                kb.res.setdefault('a1T', [None, []])
                norm_to_xt(s1, srcs, H2T, a2T, sh2T, "n2", 'H2T')
            own_blocks = [(t, 512) for t in range(0, OWN, 512)]
            gemm_group("sg", SHG, 8, 256, H2T, 'H2T', own_blocks, [dict(c0=0, cn=256, mode='fm', func=AF.Silu, out=SGA, okey='SGA', oc0=0)])
            gemm_group("su", SHU, 8, 256, H2T, 'H2T', own_blocks, [dict(c0=0, cn=256, mode='fm', func=None, out=SUA, okey='SUA', oc0=0)])
            with stage() as st:
                ga = sbt(st, "ga", [128, 2, OWN]); ua = sbt(st, "ua", [128, 2, OWN])
                kb.dma('sp', lambda e: e.dma_start(out=ga[:], in_=SGA.rearrange("(k p) t -> p k t", p=128)), r=['SGA'], w=['ga'])
                kb.dma('sp', lambda e: e.dma_start(out=ua[:], in_=SUA.rearrange("(k p) t -> p k t", p=128)), r=['SUA'], w=['ua'])
                kb.op('dve', lambda e: e.tensor_tensor(out=ga[:], in0=ga[:], in1=ua[:], op=ALU.mult), r=['ga', 'ua'], w=['ga'])
                kb.dma('pool', lambda e: e.dma_start(out=ACTT.rearrange("(k p) t -> p k t", p=128), in_=ga[:]), r=['ga'], w=['ACTT'])
            gemm_group("sd", SHD, 2, 1024, ACTT, 'ACTT', own_blocks,
                       [dict(c0=0, cn=512, mode='tm', func=None, out=SHOUT, okey='SHOUT', oc0=0),
                        dict(c0=512, cn=512, mode='tm', func=None, out=SHOUT, okey='SHOUT', oc0=512)])
            if stages >= 7 and not dummy_mix:
                with stage() as st:
                    a2row = sbt(st, "a2row", [128, D]); g2nrow = sbt(st, "g2nrow", [128, D])
                    kb.dma('sp', lambda e: e.dma_start(out=a2row[:], in_=MODROW[0:1, 4096:5120].to_broadcast([128, D])), r=['MODROW'], w=['a2row'])
                    kb.dma('sp', lambda e: e.dma_start(out=g2nrow[:], in_=N2G.rearrange("(o n) -> o n", o=1).to_broadcast([128, D])), w=['g2nrow'])
                    kb.op('dve', lambda e: e.scalar_tensor_tensor(out=a2row[:], in0=a2row[:], scalar=1.0, in1=g2nrow[:], op0=ALU.add, op1=ALU.mult),
                          r=['a2row', 'g2nrow'], w=['a2row'])
                    sh2row = row_bcast(st, "sh2row", MODROW[0:1, 3072:4096])
                    xa = [sbt(st, "hxa%d" % i, [128, D]) for i in range(2)]; stt = [sbt(st, "hst%d" % i, [128, 4]) for i in range(2)]
                    junk = sbt(st, "hjunk", [128, D])
                    for ti in range(OWN // 128):
                        b2 = ti % 2; xk, tk = 'hxa%d' % b2, 'hst%d' % b2
                        rows = slice(ti * 128, (ti + 1) * 128)
                        kb.dma('sp', lambda e: e.dma_start(out=xa[b2][:], in_=X1D[rows, :]), r=['X1D'], w=[xk])
                        kb.op('act', lambda e: e.activation(out=junk[:], in_=xa[b2][:], func=AF.Square, accum_out=stt[b2][:, 0:1]), r=[xk], w=['hjunk', tk])
                        kb.op('dve', lambda e: e.tensor_scalar(out=stt[b2][:, 1:2], in0=stt[b2][:, 0:1], scalar1=1.0 / D, scalar2=EPS, op0=ALU.mult, op1=ALU.add), r=[tk], w=[tk])
                        kb.op('act', lambda e: e.activation(out=stt[b2][:, 2:3], in_=stt[b2][:, 1:2], func=AF.Sqrt), r=[tk], w=[tk])
                        kb.op('dve', lambda e: e.reciprocal(out=stt[b2][:, 3:4], in_=stt[b2][:, 2:3]), r=[tk], w=[tk])
                        kb.op('dve', lambda e: e.scalar_tensor_tensor(out=xa[b2][:], in0=xa[b2][:], scalar=stt[b2][:, 3:4], in1=a2row[:], op0=ALU.mult, op1=ALU.mult),
                              r=[xk, tk, 'a2row'], w=[xk])
                        kb.op('pool', lambda e: e.tensor_tensor(out=xa[b2][:], in0=xa[b2][:], in1=sh2row[:], op=ALU.add), r=[xk, 'sh2row'], w=[xk])
                        kb.dma('pool', lambda e: e.dma_start(out=H2[rows, :], in_=xa[b2][:]), r=[xk], w=['H2'])
                gemm_group("rt", RW, 8, NE, H2T, 'H2T', own_blocks, [dict(c0=0, cn=NE, mode='tm', func=AF.Sigmoid, out=SCORES, okey='SCORES', oc0=0)])
                with stage() as st:
                    NT = OWN // 128
                    onesm = sbt(st, "onesm", [128, 128]); stri = sbt(st, "stri", [128, 128]); slt = sbt(st, "slt", [128, 512])
                    blk128 = sbt(st, "blk128", [128, NBLK]); pidx = sbt(st, "pidx", [128, 1]); brow_ = sbt(st, "rbrow", [128, NE])
                    kb.dma('sp', lambda e: e.dma_start(out=onesm[:], in_=ONESd), w=['onesm'])
                    kb.dma('sp', lambda e: e.dma_start(out=stri[:], in_=STRId), w=['stri'])
                    kb.dma('sp', lambda e: e.dma_start(out=slt[:], in_=SLTd), w=['slt'])
                    kb.dma('sp', lambda e: e.dma_start(out=blk128[:], in_=BLKd), w=['blk128'])
                    kb.dma('sp', lambda e: e.dma_start(out=pidx[:], in_=PIDXd), w=['pidx'])
                    kb.dma('sp', lambda e: e.dma_start(out=brow_[:], in_=RB.rearrange("(o n) -> o n", o=1).to_broadcast([128, NE])), w=['rbrow'])
                    MSK = sbt(st, "MSK", [128, NT, NE]); SEL = sbt(st, "SEL", [128, NT, NE]); WD = sbt(st, "WDm", [128, NT, NE]); DST = sbt(st, "DST", [128, NT, NE])
                    V8 = sbt(st, "V8", [128, NT, 8]); D8F = sbt(st, "D8F", [128, NT, 8]); W8 = sbt(st, "W8", [128, NT, 8]); D8I = sbt(st, "D8I", [128, NT * 8], I32)
                    sc_ = [sbt(st, "rsc%d" % i, [128, NE]) for i in range(2)]; bs = sbt(st, "rbs", [128, NE])
                    M8 = sbt(st, "M8", [128, 8, 8]); gs = sbt(st, "rgs", [128, 8]); g8 = sbt(st, "rg8", [128, 8]); gm = sbt(st, "rgm", [128, 8]); pen = sbt(st, "rpen", [128, 8])
                    den = sbt(st, "rden", [128, 2]); base = sbt(st, "rbase", [128, NE]); tmpq = sbt(st, "rtmpq", [128, NE])
                    kb.op('pool', lambda e: e.memset(base[:], 0.0), w=['rbase'])
                    for ti in range(NT):
                        b2 = ti % 2; sk_ = 'rsc%d' % b2
                        kb.dma('sp', lambda e: e.dma_start(out=sc_[b2][:], in_=SCORES[ti * 128:(ti + 1) * 128, :]), r=['SCORES'], w=[sk_])
                        kb.op('dve', lambda e: e.tensor_tensor(out=bs[:], in0=sc_[b2][:], in1=brow_[:], op=ALU.add), r=[sk_, 'rbrow'], w=['rbs'])
                        for g in range(8):
                            kb.op('dve', lambda e: e.max(out=M8[:, g, :], in_=bs[:, 32 * g:32 * g + 32]), r=['rbs', 'M8'], w=['M8'])
                        kb.op('dve', lambda e: e.tensor_tensor(out=gs[:], in0=M8[:, :, 0], in1=M8[:, :, 1], op=ALU.add), r=['M8'], w=['rgs'])
                        kb.op('dve', lambda e: e.max(out=g8[:], in_=gs[:]), r=['rgs'], w=['rg8'])
                        kb.op('dve', lambda e: e.tensor_scalar(out=gm[:], in0=gs[:], scalar1=g8[:, 3:4], scalar2=None, op0=ALU.is_ge), r=['rgs', 'rg8'], w=['rgm'])
                        kb.op('dve', lambda e: e.tensor_scalar(out=pen[:], in0=gm[:], scalar1=-1.0, scalar2=1e30, op0=ALU.add, op1=ALU.mult), r=['rgm'], w=['rpen'])
                        for g in range(8):
                            kb.op('dve', lambda e: e.tensor_scalar(out=MSK[:, ti, 32 * g:32 * g + 32], in0=bs[:, 32 * g:32 * g + 32], scalar1=gm[:, g:g + 1], scalar2=pen[:, g:g + 1],
                                                                   op0=ALU.mult, op1=ALU.add), r=['rbs', 'rgm', 'rpen', 'MSK'], w=['MSK'])
                        kb.op('dve', lambda e: e.max(out=V8[:, ti, :], in_=MSK[:, ti, :]), r=['MSK', 'V8'], w=['V8'])
                        kb.op('dve', lambda e: e.tensor_scalar(out=SEL[:, ti, :], in0=MSK[:, ti, :], scalar1=V8[:, ti, 7:8], scalar2=None, op0=ALU.is_ge), r=['MSK', 'V8', 'SEL'], w=['SEL'])
                        kb.op('dve', lambda e: e.tensor_tensor(out=WD[:, ti, :], in0=SEL[:, ti, :], in1=sc_[b2][:], op=ALU.mult), r=['SEL', sk_, 'WDm'], w=['WDm'])
                        kb.op('dve', lambda e: e.tensor_reduce(out=den[:, 0:1], in_=WD[:, ti, :], axis=AX.X, op=ALU.add), r=['WDm', 'rden'], w=['rden'])
                        kb.op('dve', lambda e: e.reciprocal(out=den[:, 1:2], in_=den[:, 0:1]), r=['rden'], w=['rden'])
                        kb.op('dve', lambda e: e.tensor_scalar(out=WD[:, ti, :], in0=WD[:, ti, :], scalar1=den[:, 1:2], scalar2=2.5, op0=ALU.mult, op1=ALU.mult), r=['WDm', 'rden'], w=['WDm'])
                        p1, p1k = banks[(2 * ti) % 8]; p2, p2k = banks[(2 * ti + 1) % 8]
                        kb.op('pe', lambda e: e.matmul(p1[:, 0:NE], lhsT=stri[:], rhs=SEL[:, ti, :], start=True, stop=True), r=['stri', 'SEL'], w=[p1k])
                        kb.op('pe', lambda e: e.matmul(p2[:, 0:NE], lhsT=onesm[:], rhs=SEL[:, ti, :], start=True, stop=True), r=['onesm', 'SEL'], w=[p2k])
                        kb.op('dve', lambda e: e.tensor_tensor(out=DST[:, ti, :], in0=p1[:, 0:NE], in1=base[:], op=ALU.add), r=[p1k, 'rbase', 'DST'], w=['DST'])
                        kb.op('dve', lambda e: e.tensor_tensor(out=base[:], in0=p2[:, 0:NE], in1=base[:], op=ALU.add), r=[p2k, 'rbase'], w=['rbase'])
                    ci = sbt(st, "rci", [128, NE], I32); padded = sbt(st, "rpad", [128, NE]); pstart = sbt(st, "rpst", [128, NE]); pend = sbt(st, "rpend", [128, NE])
                    kb.op('dve', lambda e: e.tensor_scalar(out=tmpq[:], in0=base[:], scalar1=127.0, scalar2=None, op0=ALU.add), r=['rbase'], w=['rtmpq'])
                    kb.op('dve', lambda e: e.tensor_copy(out=ci[:], in_=tmpq[:]), r=['rtmpq'], w=['rci'])
                    kb.op('dve', lambda e: e.tensor_scalar(out=ci[:], in0=ci[:], scalar1=7, scalar2=None, op0=ALU.arith_shift_right), r=['rci'], w=['rci'])
                    kb.op('dve', lambda e: e.tensor_scalar(out=ci[:], in0=ci[:], scalar1=7, scalar2=None, op0=ALU.logical_shift_left), r=['rci'], w=['rci'])
                    kb.op('dve', lambda e: e.tensor_copy(out=padded[:], in_=ci[:]), r=['rci'], w=['rpad'])
                    padT = sbt(st, "rpadT", [128, 2, 128]); pendT = sbt(st, "rpendT", [128, 2, 128])
                    pa, pak = banks[0]
                    for hh in range(2):
                        kb.op('pe', lambda e: e.transpose(out=pa[:, hh * 128:(hh + 1) * 128], in_=padded[:, hh * 128:(hh + 1) * 128], identity=ident[:]), r=['rpad', 'ident'], w=[pak])
                    kb.op('dve', lambda e: e.tensor_copy(out=padT[:], in_=pa[:, 0:256].rearrange("p (h c) -> p h c", h=2)), r=[pak], w=['rpadT'])
                    pb_, pbk_ = banks[1]
                    for hh in range(2):
                        kb.op('pe', lambda e: e.matmul(pb_[:, 0:NE], lhsT=padT[:, hh, :], rhs=slt[:, hh * 256:(hh + 1) * 256], start=(hh == 0), stop=(hh == 1)), r=['rpadT', 'slt'], w=[pbk_])
                    kb.op('dve', lambda e: e.tensor_copy(out=pstart[:], in_=pb_[:, 0:NE]), r=[pbk_], w=['rpst'])
                    kb.op('dve', lambda e: e.tensor_tensor(out=pend[:], in0=pstart[:], in1=padded[:], op=ALU.add), r=['rpst', 'rpad'], w=['rpend'])
                    pc_, pck_ = banks[2]
                    for hh in range(2):
                        kb.op('pe', lambda e: e.transpose(out=pc_[:, hh * 128:(hh + 1) * 128], in_=pend[:, hh * 128:(hh + 1) * 128], identity=ident[:]), r=['rpend', 'ident'], w=[pck_])
                    kb.op('dve', lambda e: e.tensor_copy(out=pendT[:], in_=pc_[:, 0:256].rearrange("p (h c) -> p h c", h=2)), r=[pck_], w=['rpendT'])
                    cmpT = sbt(st, "rcmpT", [128, 2, NBLK]); bef = sbt(st, "rbef", [128, NBLK]); GI = sbt(st, "GI", [128, NBLK], I32)
                    for hh in range(2):
                        kb.op('dve', lambda e: e.tensor_scalar(out=cmpT[:, hh, :], in0=blk128[:], scalar1=pendT[:, hh, 0:1], scalar2=None, op0=ALU.is_ge), r=['blk128', 'rpendT', 'rcmpT'], w=['rcmpT'])
                    pd_, pdk_ = banks[3]
                    for hh in range(2):
                        kb.op('pe', lambda e: e.matmul(pd_[:, 0:NBLK], lhsT=onesm[:], rhs=cmpT[:, hh, :], start=(hh == 0), stop=(hh == 1)), r=['onesm', 'rcmpT'], w=[pdk_])
                    kb.op('dve', lambda e: e.tensor_scalar(out=bef[:], in0=pd_[:, 0:NBLK], scalar1=255.0, scalar2=128.0, op0=ALU.min, op1=ALU.mult), r=[pdk_], w=['rbef'])
                    kb.op('dve', lambda e: e.tensor_scalar(out=bef[:], in0=bef[:], scalar1=pidx[:, 0:1], scalar2=None, op0=ALU.add), r=['rbef', 'pidx'], w=['rbef'])
                    kb.op('dve', lambda e: e.tensor_copy(out=GI[:], in_=bef[:]), r=['rbef'], w=['GI'])
                    eqj = sbt(st, "reqj", [128, NE])
                    for ti in range(NT):
                        kb.op('dve', lambda e: e.tensor_tensor(out=DST[:, ti, :], in0=DST[:, ti, :], in1=pstart[:], op=ALU.add), r=['DST', 'rpst'], w=['DST'])
                        for k8 in range(8):
                            kb.op('dve', lambda e: e.scalar_tensor_tensor(out=eqj[:], in0=MSK[:, ti, :], scalar=V8[:, ti, k8:k8 + 1], in1=DST[:, ti, :], op0=ALU.is_equal, op1=ALU.mult),
                                  r=['MSK', 'V8', 'DST', 'reqj'], w=['reqj'])
                            kb.op('dve', lambda e: e.tensor_reduce(out=D8F[:, ti, k8:k8 + 1], in_=eqj[:], axis=AX.X, op=ALU.add), r=['reqj', 'D8F'], w=['D8F'])
                            kb.op('dve', lambda e: e.scalar_tensor_tensor(out=eqj[:], in0=MSK[:, ti, :], scalar=V8[:, ti, k8:k8 + 1], in1=WD[:, ti, :], op0=ALU.is_equal, op1=ALU.mult),
                                  r=['MSK', 'V8', 'WDm', 'reqj'], w=['reqj'])
                            kb.op('dve', lambda e: e.tensor_reduce(out=W8[:, ti, k8:k8 + 1], in_=eqj[:], axis=AX.X, op=ALU.add), r=['reqj', 'W8'], w=['W8'])
                    kb.op('dve', lambda e: e.tensor_copy(out=D8I[:], in_=D8F[:].rearrange("p t k -> p (t k)")), r=['D8F'], w=['D8I'])
                    dbg('D8F', D8F[:], [128, NT, 8]); dbg('W8', W8[:], [128, NT, 8]); dbg('rbef', bef[:], [128, NBLK])
                    ht = [sbt(st, "dht%d" % i, [128, D]) for i in range(2)]
                    for ti in range(NT):
                        b2 = ti % 2; hk = 'dht%d' % b2
                        kb.dma('sp', lambda e: e.dma_start(out=ht[b2][:], in_=H2[ti * 128:(ti + 1) * 128, :]), r=['H2'], w=[hk])
                        for k8 in range(8):
                            kb.dma('pool', lambda e: e.indirect_dma_start(out=XS, out_offset=bass.IndirectOffsetOnAxis(ap=D8I[:, ti * 8 + k8:ti * 8 + k8 + 1], axis=0), in_=ht[b2][:], in_offset=None,
                                                                         bounds_check=RB_XS, oob_is_err=False), r=[hk, 'D8I'], w=['XSw'])
                    kb.barrier()
                    wgu = [sbt(st, "wgu%d" % i, [128, 2, 8, 256]) for i in range(2)]; wdn = [sbt(st, "wdn%d" % i, [128, 2, D]) for i in range(2)]
                    xs = [sbt(st, "xs%d" % i, [128, D]) for i in range(2)]; xsT = [sbt(st, "xsT%d" % i, [128, 8, 128]) for i in range(2)]
                    actT = [sbt(st, "actT%d" % i, [128, 2, 128]) for i in range(2)]; sg_ = [sbt(st, "esg%d" % i, [128, 256]) for i in range(2)]
                    ys = [sbt(st, "ys%d" % i, [128, D]) for i in range(2)]
                    bi = 0
                    for blk in range(NBLK):
                        b2 = blk % 2
                        wk, dk, xk, xtk, ak, sgk, yk = 'wgu%d' % b2, 'wdn%d' % b2, 'xs%d' % b2, 'xsT%d' % b2, 'actT%d' % b2, 'esg%d' % b2, 'ys%d' % b2
                        kb.dma('pool', lambda e: e.indirect_dma_start(out=wgu[b2][:].rearrange("p a k f -> p (a k f)"), out_offset=None, in_=EWGU,
                                                                     in_offset=bass.IndirectOffsetOnAxis(ap=GI[:, blk:blk + 1], axis=0),
                                                                     bounds_check=RB_W, oob_is_err=False), r=['GI'], w=[wk])
                        kb.dma('pool', lambda e: e.indirect_dma_start(out=wdn[b2][:].rearrange("p k f -> p (k f)"), out_offset=None, in_=EWD,
                                                                     in_offset=bass.IndirectOffsetOnAxis(ap=GI[:, blk:blk + 1], axis=0),
                                                                     bounds_check=RB_W, oob_is_err=False), r=['GI'], w=[dk])
                        kb.dma('sp', lambda e: e.dma_start(out=xs[b2][:], in_=XS[blk * 128:(blk + 1) * 128, :]), r=['XSw'], w=[xk])
                        for hh in range(2):
                            pb, pbk = banks[bi % 8]; bi += 1
                            for kk in range(4):
                                k8 = hh * 4 + kk
                                kb.op('pe', lambda e: e.transpose(out=pb[:, kk * 128:(kk + 1) * 128], in_=xs[b2][:, k8 * 128:(k8 + 1) * 128], identity=ident[:]), r=[xk, 'ident'], w=[pbk])
                            evac(xsT[b2][:, hh * 4:hh * 4 + 4, :], pb[:, :].rearrange("p (k t) -> p k t", k=4), None, [pbk, xtk], [xtk])
                        ph, phk = banks[bi % 8]; bi += 1
                        for kk in range(8):
                            kb.op('pe', lambda e: e.matmul(ph[:, :].rearrange("p (a f) -> p a f", a=2), lhsT=xsT[b2][:, kk, :], rhs=wgu[b2][:, :, kk, :],
                                                           start=(kk == 0), stop=(kk == 7)), r=[wk, xtk], w=[phk])
                        kb.op('act', lambda e: e.activation(out=sg_[b2][:], in_=ph[:, 0:256], func=AF.Silu), r=[phk], w=[sgk])
                        kb.op('dve', lambda e: e.tensor_tensor(out=sg_[b2][:], in0=ph[:, 256:512], in1=sg_[b2][:], op=ALU.mult), r=[phk, sgk], w=[sgk])
                        pt_, ptk = banks[bi % 8]; bi += 1
                        for kk in range(2):
                            kb.op('pe', lambda e: e.transpose(out=pt_[:, kk * 128:(kk + 1) * 128], in_=sg_[b2][:, kk * 128:(kk + 1) * 128], identity=ident[:]), r=[sgk, 'ident'], w=[ptk])
                        evac(actT[b2][:].rearrange("p k t -> p (k t)"), pt_[:, 0:256], None, [ptk, ak], [ak])
                        for hf in range(2):
                            py, pyk = banks[bi % 8]; bi += 1
                            for kk in range(2):
                                kb.op('pe', lambda e: e.matmul(py[:, :], lhsT=actT[b2][:, kk, :], rhs=wdn[b2][:, kk, hf * 512:(hf + 1) * 512], start=(kk == 0), stop=(kk == 1)), r=[ak, dk], w=[pyk])
                            evac(ys[b2][:, hf * 512:(hf + 1) * 512], py[:, :], None, [pyk, yk], [yk])
                        kb.dma('sp', lambda e: e.dma_start(out=YS[blk * 128:(blk + 1) * 128, :], in_=ys[b2][:]), r=[yk], w=['YSw'])
                    kb.barrier()
                    acc = [sbt(st, "cacc%d" % i, [128, D]) for i in range(2)]; gg = [sbt(st, "cg%d" % i, [128, D]) for i in range(3)]
                    gi_ = 0
                    for ti in range(NT):
                        b2 = ti % 2; ack = 'cacc%d' % b2
                        for k8 in range(8):
                            g3 = gi_ % 3; gi_ += 1; ggk = 'cg%d' % g3
                            kb.dma('pool', lambda e: e.indirect_dma_start(out=gg[g3][:], out_offset=None, in_=YS, in_offset=bass.IndirectOffsetOnAxis(ap=D8I[:, ti * 8 + k8:ti * 8 + k8 + 1], axis=0),
                                                                         bounds_check=RB_XS, oob_is_err=False), r=['YSw', 'D8I'], w=[ggk])
                            if k8 == 0:
                                kb.op('dve', lambda e: e.tensor_scalar(out=acc[b2][:], in0=gg[g3][:], scalar1=W8[:, ti, 0:1], scalar2=None, op0=ALU.mult), r=[ggk, 'W8', ack], w=[ack])
                            else:
                                kb.op('dve', lambda e: e.scalar_tensor_tensor(out=acc[b2][:], in0=gg[g3][:], scalar=W8[:, ti, k8:k8 + 1], in1=acc[b2][:], op0=ALU.mult, op1=ALU.add),
                                      r=[ggk, 'W8', ack], w=[ack])
                        kb.dma('sp', lambda e: e.dma_start(out=ROUTED[ti * 128:(ti + 1) * 128, :], in_=acc[b2][:]), r=[ack], w=['ROUTED'])

            with stage() as st:
                g2row = row_bcast(st, "g2row", MODROW[0:1, 5120:6144])
                fgrow = sbt(st, "fgrow", [128, D])
                kb.dma('sp', lambda e: e.dma_start(out=fgrow[:], in_=FING.rearrange("(o n) -> o n", o=1).to_broadcast([128, D])), w=['fgrow'])
                xa = [sbt(st, "xa%d" % i, [128, D]) for i in range(2)]; sa = [sbt(st, "sa%d" % i, [128, D]) for i in range(2)]
                ra = [sbt(st, "ra%d" % i, [128, D]) for i in range(2)]; stt = [sbt(st, "stt%d" % i, [128, 4]) for i in range(2)]
                junk = sbt(st, "fjunk", [128, D])
                for ti in range(OWN // 128):
                    b2 = ti % 2; xk, sk, rk, tk = 'xa%d' % b2, 'sa%d' % b2, 'ra%d' % b2, 'stt%d' % b2
                    rows = slice(ti * 128, (ti + 1) * 128)
                    kb.dma('sp', lambda e: e.dma_start(out=xa[b2][:], in_=X1D[rows, :]), r=['X1D'], w=[xk])
                    kb.dma('sp', lambda e: e.dma_start(out=sa[b2][:], in_=SHOUT[rows, :]), r=['SHOUT'], w=[sk])
                    kb.dma('sp', lambda e: e.dma_start(out=ra[b2][:], in_=ROUTED[rows, :]), r=['ROUTED'], w=[rk])
                    kb.op('pool', lambda e: e.tensor_tensor(out=sa[b2][:], in0=sa[b2][:], in1=ra[b2][:], op=ALU.add), r=[sk, rk], w=[sk])
                    kb.op('dve', lambda e: e.tensor_tensor(out=sa[b2][:], in0=sa[b2][:], in1=g2row[:], op=ALU.mult), r=[sk, 'g2row'], w=[sk])
                    kb.op('pool', lambda e: e.tensor_tensor(out=xa[b2][:], in0=xa[b2][:], in1=sa[b2][:], op=ALU.add), r=[xk, sk], w=[xk])
                    kb.op('act', lambda e: e.activation(out=junk[:], in_=xa[b2][:], func=AF.Square, accum_out=stt[b2][:, 0:1]), r=[xk], w=['fjunk', tk])
                    kb.op('dve', lambda e: e.tensor_scalar(out=stt[b2][:, 1:2], in0=stt[b2][:, 0:1], scalar1=1.0 / D, scalar2=EPS, op0=ALU.mult, op1=ALU.add), r=[tk], w=[tk])
                    kb.op('act', lambda e: e.activation(out=stt[b2][:, 2:3], in_=stt[b2][:, 1:2], func=AF.Sqrt), r=[tk], w=[tk])
                    kb.op('dve', lambda e: e.reciprocal(out=stt[b2][:, 3:4], in_=stt[b2][:, 2:3]), r=[tk], w=[tk])
                    kb.op('dve', lambda e: e.scalar_tensor_tensor(out=xa[b2][:], in0=xa[b2][:], scalar=stt[b2][:, 3:4], in1=fgrow[:], op0=ALU.mult, op1=ALU.mult),
                          r=[xk, tk, 'fgrow'], w=[xk])
                    kb.dma('pool', lambda e: e.dma_start(out=OUT[rows, :], in_=xa[b2][:]), r=[xk], w=['OUT'])

        kb.finish('sp')
        kb.finish('pool')
        pg.ninstr = kb.ninstr
    return pg


_PROG = None


def make_in_maps(pg, inputs):
    hc = host_consts()
    sq = lambda a: np.ascontiguousarray(a[0])
    in_maps = []
    shared = {}
    if 'EWGU' in pg.ins:
        wg = np.asarray(inputs['exp_w_gate'])[0].reshape(NE, 8, 128, 256)
        wu = np.asarray(inputs['exp_w_up'])[0].reshape(NE, 8, 128, 256)
        ew = np.empty((NE, 128, 2, 8, 256), np.float32)
        ew[:, :, 0] = wg.transpose(0, 2, 1, 3); ew[:, :, 1] = wu.transpose(0, 2, 1, 3)
        shared['EWGU'] = ew.reshape(NE * 128, 4096)
        shared['EWD'] = np.ascontiguousarray(np.asarray(inputs['exp_w_down'])[0].reshape(NE, 2, 128, D).transpose(0, 2, 1, 3)).reshape(NE * 128, 2048)
    for c in range(8):
        b, j = c // 4, c % 4
        own = slice(j * OWN, (j + 1) * OWN)
        idx = ((np.arange(4)[None, :] * 128 + np.arange(128)[:, None]) * 4 + j).astype(np.int32)
        full = {
            'x': inputs['x'][b], 'ctx': inputs['ctx'][b], 'xown': inputs['x'][b, own], 'posown': hc['POS'][own],
            'c': inputs['c'][b], 'c_ctx': inputs['c_ctx'], 'final_g': inputs['final_g'], 'OWNIDX': idx,
            'hg_lb_logits': np.asarray(inputs['hg_lb_logits']).reshape(2, 1024),
        }
        full.update(shared)
        for k in pg.ins:
            if k not in full and k not in hc:
                full[k] = sq(inputs[k])
        full.update(hc)
        in_maps.append({k: np.ascontiguousarray(np.asarray(full[k])) for k in pg.ins})
    return in_maps


def kernel(**inputs):
    global _PROG
    if _PROG is None:
        _PROG = build()
    pg = _PROG
    in_maps = make_in_maps(pg, inputs)
    res = run_bass_kernel_spmd(pg.nc, in_maps, core_ids=list(range(8)))
    out = np.zeros((2, L, D), np.float32)
    for c in range(8):
        b, j = c // 4, c % 4
        out[b, j * OWN:(j + 1) * OWN] = res.results[c]['out']
    return out
```

```python
import math
import numpy as np
from contextlib import ExitStack, contextmanager
import concourse.bass as bass
import concourse.mybir as mybir
from concourse.bass_utils import run_bass_kernel_spmd

F32 = mybir.dt.float32
I32 = mybir.dt.int32
U32 = mybir.dt.uint32
ALU = mybir.AluOpType
AF = mybir.ActivationFunctionType
AX = mybir.AxisListType

N_DMA_SEMS = 24
D = 1024
L = 8192
NCTX = 256
LT = L + NCTX
OWN = 2048
NE = 256
NBLK = 383
EPS = 1e-6
TWO_PI = 2.0 * math.pi


class KB:
    def __init__(self, nc, es):
        self.nc = nc
        self.engs = {'pe': nc.tensor, 'act': nc.scalar, 'dve': nc.vector, 'pool': nc.gpsimd, 'sp': nc.sync}
        self.sems = {}
        self.cnt = {}
        for e in self.engs:
            self.sems[e] = es.enter_context(nc.semaphore("s_" + e))
            self.cnt[e] = 0
        for i in range(N_DMA_SEMS):
            self.sems['d%d' % i] = es.enter_context(nc.semaphore("s_d%d" % i))
            self.cnt['d%d' % i] = 0
        self.dnext = 0
        self.waited = {e: {} for e in self.engs}
        self.res = {}
        self.ninstr = 0

    def _need(self, eng, toks):
        best = {}
        for t in toks:
            if t is None:
                continue
            sk, v = t
            if sk == eng and eng == 'pe':
                continue
            if best.get(sk, 0) < v:
                best[sk] = v
        for sk, v in best.items():
            if self.waited[eng].get(sk, 0) >= v:
                continue
            self.engs[eng].wait_ge(self.sems[sk], v)
            self.waited[eng][sk] = v

    def _deps(self, r, w):
        toks = []
        for k in r:
            st = self.res.get(k)
            if st is not None:
                toks.append(st[0])
        for k in w:
            st = self.res.get(k)
            if st is not None:
                toks.append(st[0])
                toks.extend(st[1])
        return toks

    def _commit(self, tok, r, w):
        for k in r:
            st = self.res.setdefault(k, [None, []])
            st[1].append(tok)
            if len(st[1]) > 32:
                best = {}
                for sk, v in st[1]:
                    if best.get(sk, 0) < v:
                        best[sk] = v
                st[1] = list(best.items())
        for k in w:
            self.res[k] = [tok, []]

    def op(self, eng, fn, r=(), w=()):
        self._need(eng, self._deps(r, w))
        ins = fn(self.engs[eng])
        self.cnt[eng] += 1
        ins.then_inc(self.sems[eng], 1)
        self._commit((eng, self.cnt[eng]), r, w)
        self.ninstr += 1

    def dma(self, q, fn, r=(), w=()):
        i = self.dnext
        self.dnext = (self.dnext + 1) % N_DMA_SEMS
        sk = 'd%d' % i
        toks = self._deps(r, w)
        if self.cnt[sk] > 0:
            toks.append((sk, self.cnt[sk]))
        self._need(q, toks)
        ins = fn(self.engs[q])
        self.cnt[sk] += 16
        ins.then_inc(self.sems[sk], 16)
        self._commit((sk, self.cnt[sk]), r, w)
        self.ninstr += 1

    def barrier(self):
        toks = [(sk, v) for sk, v in self.cnt.items() if v > 0]
        for e in self.engs:
            self._need(e, toks)

    def finish(self, eng):
        toks = []
        for st in self.res.values():
            toks.append(st[0])
            toks.extend(st[1])
        self._need(eng, toks)


_CONST = None


def host_consts():
    global _CONST
    if _CONST is not None:
        return _CONST
    c = {}
    quarter = D // 4
    omega = (1.0 / (np.float32(10000.0) ** (np.arange(quarter, dtype=np.float32) / np.float32(quarter)))).astype(np.float32)
    rows, cols = L // 64, 64
    ang_r = (np.arange(rows, dtype=np.float32)[:, None] * omega).astype(np.float32)
    ang_c = (np.arange(cols, dtype=np.float32)[:, None] * omega).astype(np.float32)
    emb_r = np.concatenate([np.sin(ang_r), np.cos(ang_r)], -1)
    emb_c = np.concatenate([np.sin(ang_c), np.cos(ang_c)], -1)
    emb = np.concatenate([np.broadcast_to(emb_r[:, None], (rows, cols, D // 2)),
                          np.broadcast_to(emb_c[None], (rows, cols, D // 2))], -1)
    c['POS'] = np.ascontiguousarray(emb.reshape(L, D).astype(np.float32))
    c['IDENT'] = np.eye(128, dtype=np.float32)
    si = np.arange(128)[:, None]; ti = np.arange(128)[None, :]
    same = (si // 64) == (ti // 64)
    c['TRIF'] = (same & (si <= ti)).astype(np.float32)
    c['TRIB'] = (same & (si >= ti)).astype(np.float32)
    c['ONES'] = np.ones((128, 128), np.float32)
    c['STRI'] = (si < ti).astype(np.float32)
    e1 = np.arange(128)[:, None]; e2 = np.arange(256)[None, :]
    c['SLT'] = np.concatenate([(e1 < e2), (e1 + 128 < e2)], 1).astype(np.float32)
    c['BLK128'] = np.broadcast_to((np.arange(NBLK, dtype=np.float32) * 128.0)[None, :], (128, NBLK)).copy()
    c['PIDX'] = np.arange(128, dtype=np.float32).reshape(128, 1).copy()
    NN = 16384
    a = np.arange(128, dtype=np.float64)
    ang = 2.0 * np.pi * np.outer(a, a) / 128.0
    Fre = np.cos(ang); Fim = -np.sin(ang)
    f32 = lambda v: np.ascontiguousarray(v.astype(np.float32))
    c['FA'] = f32(np.concatenate([Fre, Fim], 1)); c['FAH'] = f32(np.concatenate([Fre, Fim], 1)[64:128])
    c['FRE'] = f32(Fre); c['FIM'] = f32(Fim); c['NFIM'] = f32(-Fim)
    c['CA'] = f32(np.concatenate([Fre, -Fim], 1)); c['CB'] = f32(np.concatenate([Fim, Fre], 1))
    c['FREN'] = f32(Fre[:, :64] / NN); c['FIMN'] = f32(Fim[:, :64] / NN)
    angT = 2.0 * np.pi * np.outer(a, a) / NN
    c['TRE2'] = f32(np.concatenate([np.cos(angT), np.cos(angT)], 1)); c['TIM2'] = f32(np.concatenate([-np.sin(angT), -np.sin(angT)], 1))
    bands = np.linspace(1e-4, 15.0, 16, dtype=np.float32)
    def zfeat(t):
        t = t.astype(np.float32)
        tn = (t / np.float32(L - 1)).astype(np.float32)
        an = (np.float32(2 * math.pi / L) * t[:, None] * bands).astype(np.float32)
        return np.concatenate([tn[:, None], np.cos(an), -np.sin(an)], -1).astype(np.float32), tn
    zf, tnf = zfeat(np.arange(L)); zb, tnb = zfeat(L - np.arange(L))
    c['ZT'] = np.ascontiguousarray(np.concatenate([zf, zb], 1).T)
    lo_ = math.log(1e-2) / 1.5; hi_ = math.log(1e-2) / 0.3
    deltas = np.abs(np.linspace(lo_, hi_, 512, dtype=np.float32))
    decf = np.exp(-tnf[:, None] * deltas).astype(np.float32)
    decb = np.exp(-tnb[:, None] * deltas).astype(np.float32); decb[0] = 0.0
    c['DEC'] = np.ascontiguousarray(np.stack([decf.reshape(64, 128, 512).transpose(0, 2, 1), decb.reshape(64, 128, 512).transpose(0, 2, 1)]))
    _CONST = c
    return c


class Prog:
    def __init__(self, debug=None):
        self.debug = debug or ()
        self.nc = bass.Bass("TRN2", target_bir_lowering=False)
        self.ins = {}
        self.outs = {}

    def inp(self, name, shape, dt=F32):
        t = self.nc.dram_tensor(name, list(shape), dt, kind="ExternalInput").ap()
        self.ins[name] = t
        return t

    def scratch(self, name, shape, dt=F32):
        if name in self.debug:
            t = self.nc.dram_tensor(name, list(shape), dt, kind="ExternalOutput").ap()
            self.outs[name] = t
        else:
            t = self.nc.dram_tensor(name, list(shape), dt, kind="Internal").ap()
        return t


def build(stages=99, debug=None, dummy_mix=False):
    pg = Prog(debug)
    nc = pg.nc
    X = pg.inp("x", [L, D]); CTX = pg.inp("ctx", [NCTX, D]); XOWN = pg.inp("xown", [OWN, D]); POSOWN = pg.inp("posown", [OWN, D])
    CV = pg.inp("c", [D]); CCTX = pg.inp("c_ctx", [D])
    N1G = pg.inp("norm1_g", [D]); N2G = pg.inp("norm2_g", [D])
    ADAW = pg.inp("ada_w", [D, 6 * D]); ADAB = pg.inp("ada_b", [6 * D])
    WIN = pg.inp("w_in", [D, 6144])
    POS = pg.inp("POS", [L, D]); IDENT = pg.inp("IDENT", [128, 128])
    WHY = pg.inp("w_hy_out", [512, D]); WHG = pg.inp("w_hg_out", [512, D]); WOUT = pg.inp("w_out", [D, D])
    SHG = pg.inp("sh_w_gate", [D, 256]); SHU = pg.inp("sh_w_up", [D, 256]); SHD = pg.inp("sh_w_down", [256, D])
    FING = pg.inp("final_g", [D]); OWNIDX = pg.inp("OWNIDX", [128, 4], I32)
    CONVW = pg.inp("hy_conv_w", [3, 1536]); CONVB = pg.inp("hy_conv_b", [1536])
    TRIFd = pg.inp("TRIF", [128, 128]); TRIBd = pg.inp("TRIB", [128, 128]); ONESd = pg.inp("ONES", [128, 128])
    LBL = pg.inp("hg_lb_logits", [2, 1024]); HGNG = pg.inp("hg_norm_g", [128])
    STRId = pg.inp("STRI", [128, 128]); SLTd = pg.inp("SLT", [128, 512]); BLKd = pg.inp("BLK128", [128, NBLK]); PIDXd = pg.inp("PIDX", [128, 1])
    RW = pg.inp("router_w", [D, NE]); RB = pg.inp("router_bias", [NE])
    EWGU = pg.inp("EWGU", [NE * 128, 4096]); EWD = pg.inp("EWD", [NE * 128, 2048])
    FAd = pg.inp("FA", [128, 256]); FAHd = pg.inp("FAH", [64, 256]); FREd = pg.inp("FRE", [128, 128]); FIMd = pg.inp("FIM", [128, 128]); NFIMd = pg.inp("NFIM", [128, 128])
    CAd = pg.inp("CA", [128, 256]); CBd = pg.inp("CB", [128, 256]); FRENd = pg.inp("FREN", [128, 64]); FIMNd = pg.inp("FIMN", [128, 64])
    TRE2d = pg.inp("TRE2", [128, 256]); TIM2d = pg.inp("TIM2", [128, 256]); ZTd = pg.inp("ZT", [66, L]); DECd = pg.inp("DEC", [2, 64, 512, 128])
    FW1 = pg.inp("hy_f_w1", [33, 64]); FB1 = pg.inp("hy_f_b1", [64]); FW2 = pg.inp("hy_f_w2", [64, 64]); FB2 = pg.inp("hy_f_b2", [64])
    FW3 = pg.inp("hy_f_w3", [64, 64]); FB3 = pg.inp("hy_f_b3", [64]); FW4 = pg.inp("hy_f_w4", [64, 2048]); FFQ = pg.inp("hy_f_freq", [64])
    HSKIP = pg.inp("hy_skip", [1024])
    HSPEC = pg.scratch("HSPEC", [1024, 128, 2, 128])
    OUT = pg.nc.dram_tensor("out", [OWN, D], F32, kind="ExternalOutput").ap()
    pg.outs["out"] = OUT
    XNT = pg.scratch("XNT", [D, LT]); XNTOWN = pg.scratch("XNTOWN", [D, OWN])
    MODROW = pg.scratch("MODROW", [2, 6 * D])
    HYRAW = pg.scratch("HYRAW", [1536, L])
    QT = pg.scratch("QT", [512, L]); GT = pg.scratch("GT", [512, L])
    IFF = pg.scratch("IFF", [LT, 1536])
    SGT = pg.scratch("SGT", [2048, OWN])
    HYC = pg.scratch("HYC", [1536, L])
    YHYT = pg.scratch("YHYT", [512, L]); YHGT = pg.scratch("YHGT", [512, L])
    YOWN = pg.scratch("YOWN", [1024, OWN])
    X1D = pg.scratch("X1D", [OWN, D]); H2T = pg.scratch("H2T", [D, OWN])
    SGA = pg.scratch("SGA", [256, OWN]); SUA = pg.scratch("SUA", [256, OWN]); ACTT = pg.scratch("ACTT", [256, OWN])
    SHOUT = pg.scratch("SHOUT", [OWN, D]); ROUTED = pg.scratch("ROUTED", [OWN, D])
    H2 = pg.scratch("H2", [OWN, D]); SCORES = pg.scratch("SCORES", [OWN, NE])
    XS = pg.scratch("XS", [NBLK * 128, D]); YS = pg.scratch("YS", [NBLK * 128, D])

    with ExitStack() as es:
        kb = KB(nc, es)
        sbt = lambda stack, name, shape, dt=F32: stack.enter_context(nc.sbuf_tensor(name, list(shape), dt))
        PA = es.enter_context(nc.psum_tensor("PA", [128, 2048], F32))
        PB = es.enter_context(nc.psum_tensor("PB", [128, 2048], F32))
        banks = [(PA[:, 512 * i:512 * (i + 1)], "PA%d" % i) for i in range(4)] + \
                [(PB[:, 512 * i:512 * (i + 1)], "PB%d" % i) for i in range(4)]
        RB_OWN = nc.gpsimd.alloc_register("bc_own"); nc.gpsimd.reg_mov(RB_OWN, 2047)
        RB_XS = nc.gpsimd.alloc_register("bc_xs"); nc.gpsimd.reg_mov(RB_XS, NBLK * 128 - 1)
        RB_W = nc.gpsimd.alloc_register("bc_w"); nc.gpsimd.reg_mov(RB_W, NE * 128 - 1)
        ident = sbt(es, "ident", [128, 128])
        kb.dma('sp', lambda e: e.dma_start(out=ident[:], in_=IDENT), w=['ident'])
        modT = sbt(es, "modT", [128, 48, 2])
        a1T = sbt(es, "a1T", [128, 8, 2]); sh1T = sbt(es, "sh1T", [128, 8, 2]); g2T = sbt(es, "g2T", [128, 8])
        @contextmanager
        def stage():
            with ExitStack() as st_:
                yield st_
                kb.barrier()

        def dbg(name, ap, shape):
            if name in pg.debug:
                t = nc.dram_tensor("D_" + name, list(shape), F32, kind="ExternalOutput").ap()
                pg.outs["D_" + name] = t
                kb.dma('pool', lambda e: e.dma_start(out=t, in_=ap), r=[name], w=['DBG' + name])

        with stage() as s0:
            sT = sbt(s0, "sT", [128, 8, 2]); vraw = sbt(s0, "vraw", [80, 128]); VT = sbt(s0, "VT", [128, 80])
            kb.dma('sp', lambda e: e.dma_start(out=vraw[0:8, :], in_=CV.rearrange("(q p) -> q p", p=128)), w=['vraw'])
            kb.dma('sp', lambda e: e.dma_start(out=vraw[8:16, :], in_=CCTX.rearrange("(q p) -> q p", p=128)), r=['vraw'], w=['vraw'])
            kb.dma('sp', lambda e: e.dma_start(out=vraw[16:24, :], in_=N1G.rearrange("(q p) -> q p", p=128)), r=['vraw'], w=['vraw'])
            kb.dma('sp', lambda e: e.dma_start(out=vraw[24:32, :], in_=N2G.rearrange("(q p) -> q p", p=128)), r=['vraw'], w=['vraw'])
            kb.dma('sp', lambda e: e.dma_start(out=vraw[32:80, :], in_=ADAB.rearrange("(q p) -> q p", p=128)), r=['vraw'], w=['vraw'])
            ps, psk = banks[0]
            kb.op('pe', lambda e: e.transpose(out=ps[:, 128:208], in_=vraw[:], identity=ident[0:80, 0:80]), r=['vraw', 'ident'], w=[psk])
            kb.op('dve', lambda e: e.tensor_copy(out=VT[:], in_=ps[:, 128:208]), r=[psk], w=['VT'])
            abT = VT[:, 32:80]; g1T = VT[:, 16:24]
            for r_ in range(2):
                kb.op('act', lambda e: e.activation(out=sT[:, :, r_], in_=VT[:, 8 * r_:8 * r_ + 8], func=AF.Silu), r=['VT', 'sT'], w=['sT'])
            kb.op('dve', lambda e: e.tensor_copy(out=g2T[:], in_=VT[:, 24:32]), r=['VT'], w=['g2T'])
            wbufs = [sbt(s0, "adaw%d" % i, [128, 8, 768]) for i in range(2)]
            mrow = sbt(s0, "mrow", [2, 6144]); brow = sbt(s0, "brow", [2, 6144])
            for r_ in range(2):
                kb.dma('sp', lambda e: e.dma_start(out=brow[r_:r_ + 1, :], in_=ADAB.rearrange("(o n) -> o n", o=1)), r=['brow'], w=['brow'])
            for cb in range(8):
                wb = wbufs[cb % 2]; wk = "adaw%d" % (cb % 2)
                kb.dma('sp', lambda e: e.dma_start(out=wb[:], in_=ADAW[:, cb * 768:(cb + 1) * 768].rearrange("(k p) c -> p k c", p=128)), w=[wk])
                for hh in range(2):
                    pr, prk = banks[1 + (2 * cb + hh) % 3]
                    for kk in range(8):
                        kb.op('pe', lambda e: e.matmul(pr[0:2, 0:384], lhsT=sT[:, kk, :], rhs=wb[:, kk, hh * 384:(hh + 1) * 384],
                                                       start=(kk == 0), stop=(kk == 7)), r=[wk, 'sT'], w=[prk])
                    c0 = cb * 768 + hh * 384
                    kb.op('dve', lambda e: e.tensor_tensor(out=mrow[:, c0:c0 + 384], in0=pr[0:2, 0:384], in1=brow[:, c0:c0 + 384], op=ALU.add),
                          r=[prk, 'brow'], w=['mrow'])
            kb.dma('pool', lambda e: e.dma_start(out=MODROW, in_=mrow[:]), r=['mrow'], w=['MODROW'])
            mq = sbt(s0, "mq", [96, 128])
            kb.dma('sp', lambda e: e.dma_start(out=mq[:], in_=MODROW.rearrange("r (q p) -> (r q) p", p=128)), r=['MODROW'], w=['mq'])
            kb.op('pe', lambda e: e.transpose(out=ps[:, 256:352], in_=mq[:], identity=ident[0:96, 0:96]), r=['mq', 'ident'], w=[psk])
            for r_ in range(2):
                kb.op('dve', lambda e: e.tensor_copy(out=modT[:, :, r_], in_=ps[:, 256 + 48 * r_:256 + 48 * r_ + 48]), r=[psk, 'modT'], w=['modT'])
            kb.op('dve', lambda e: e.tensor_scalar(out=a1T[:], in0=modT[:, 8:16, :], scalar1=1.0, scalar2=None, op0=ALU.add),
                  r=['modT'], w=['a1T'])
            for r_ in range(2):
                kb.op('dve', lambda e: e.tensor_tensor(out=a1T[:, :, r_], in0=a1T[:, :, r_], in1=g1T, op=ALU.mult),
                      r=['a1T', 'VT'], w=['a1T'])
            kb.op('dve', lambda e: e.tensor_copy(out=sh1T[:], in_=modT[:, 0:8, :]), r=['modT'], w=['sh1T'])

        dbg('modT', modT[:], [128, 48, 2]); dbg('a1T', a1T[:], [128, 8, 2])
        def norm_to_xt(stack, srcs, XT, a_ap, b_ap, tag, xtk):
            xin = [sbt(stack, "%s_xin%d" % (tag, i), [128, 1024]) for i in range(2)]
            pin = [sbt(stack, "%s_pin%d" % (tag, i), [128, 1024]) for i in range(2)]
            junk = sbt(stack, "%s_junk" % tag, [128, 1024])
            st = [sbt(stack, "%s_st%d" % (tag, i), [128, 4]) for i in range(2)]
            xo = [sbt(stack, "%s_xo%d" % (tag, i), [128, 8, 128]) for i in range(2)]
            for i, (xap, pap, c0) in enumerate(srcs):
                b = i % 2
                xk, pk, sk, ok = "%s_xin%d" % (tag, b), "%s_pin%d" % (tag, b), "%s_st%d" % (tag, b), "%s_xo%d" % (tag, b)
                kb.dma('sp', lambda e: e.dma_start(out=xin[b][:], in_=xap), w=[xk])
                if pap is not None:
                    kb.dma('sp', lambda e: e.dma_start(out=pin[b][:], in_=pap), w=[pk])
                    kb.op('pool', lambda e: e.tensor_tensor(out=xin[b][:], in0=xin[b][:], in1=pin[b][:], op=ALU.add), r=[xk, pk], w=[xk])
                kb.op('act', lambda e: e.activation(out=junk[:], in_=xin[b][:], func=AF.Square, accum_out=st[b][:, 0:1]),
                      r=[xk], w=[tag + '_junk', sk])
                kb.op('dve', lambda e: e.tensor_scalar(out=st[b][:, 1:2], in0=st[b][:, 0:1], scalar1=1.0 / D, scalar2=EPS, op0=ALU.mult, op1=ALU.add),
                      r=[sk], w=[sk])
                kb.op('act', lambda e: e.activation(out=st[b][:, 2:3], in_=st[b][:, 1:2], func=AF.Sqrt), r=[sk], w=[sk])
                kb.op('dve', lambda e: e.reciprocal(out=st[b][:, 3:4], in_=st[b][:, 2:3]), r=[sk], w=[sk])
                kb.op('dve', lambda e: e.tensor_scalar(out=xin[b][:], in0=xin[b][:], scalar1=st[b][:, 3:4], scalar2=None, op0=ALU.mult),
                      r=[xk, sk], w=[xk])
                for h in range(2):
                    pb, pbk = banks[(2 * i + h) % 4]
                    for kk in range(4):
                        k8 = h * 4 + kk
                        kb.op('pe', lambda e: e.transpose(out=pb[:, kk * 128:(kk + 1) * 128], in_=xin[b][:, k8 * 128:(k8 + 1) * 128], identity=ident[:]),
                              r=[xk, 'ident'], w=[pbk])
                    for kk in range(4):
                        k8 = h * 4 + kk
                        kb.op('act', lambda e: e.activation(out=xo[b][:, k8, :], in_=pb[:, kk * 128:(kk + 1) * 128], func=AF.Identity,
                                                            bias=b_ap[:, k8:k8 + 1], scale=a_ap[:, k8:k8 + 1]),
                              r=[pbk, 'a1T', 'sh1T', 'a2T'], w=[ok])
                kb.dma('pool', lambda e: e.dma_start(out=XT[:, c0:c0 + 128].rearrange("(k p) t -> p k t", p=128), in_=xo[b][:]), r=[ok], w=[xtk])

        if stages >= 1:
            with stage() as s1:
                srcs = [(X[i * 128:(i + 1) * 128, :], POS[i * 128:(i + 1) * 128, :], i * 128) for i in range(L // 128)]
                norm_to_xt(s1, srcs, XNT, a1T[:, :, 0], sh1T[:, :, 0], "n1", 'XNT')
            with stage() as s1:
                srcs = [(CTX[i * 128:(i + 1) * 128, :], None, L + i * 128) for i in range(NCTX // 128)]
                norm_to_xt(s1, srcs, XNT, a1T[:, :, 1], sh1T[:, :, 1], "n1c", 'XNT')
            with stage() as s1:
                srcs = [(XOWN[i * 128:(i + 1) * 128, :], POSOWN[i * 128:(i + 1) * 128, :], i * 128) for i in range(OWN // 128)]
                norm_to_xt(s1, srcs, XNTOWN, a1T[:, :, 0], sh1T[:, :, 0], "n1o", 'XNTOWN')

        cp_toggle = [0]

        def evac(out_ap, in_ap, func, r, w):
            if func is None:
                cp_toggle[0] ^= 1
                if cp_toggle[0]:
                    kb.op('dve', lambda e: e.tensor_copy(out=out_ap, in_=in_ap), r=r, w=w)
                else:
                    kb.op('act', lambda e: e.copy(out=out_ap, in_=in_ap), r=r, w=w)
            else:
                kb.op('act', lambda e: e.activation(out=out_ap, in_=in_ap, func=func), r=r, w=w)

        def gemm_group(tag, Wsrc, kch, ncols, XT, xtkey, tblocks, jobs):
            with stage() as st:
                Wsb = sbt(st, tag + "_W", [128, kch, ncols])
                kb.dma('sp', lambda e: e.dma_start(out=Wsb[:], in_=Wsrc.rearrange("(k p) c -> p k c", p=128)), w=[tag + '_W'])
                Xb = [sbt(st, "%s_X%d" % (tag, i), [128, kch, 512]) for i in range(2)]
                stg = [sbt(st, "%s_s%d" % (tag, i), [128, 512]) for i in range(4)]
                si = 0
                bi = 0
                for ti, (t0, tn) in enumerate(tblocks):
                    xb = Xb[ti % 2]; xk = "%s_X%d" % (tag, ti % 2)
                    kb.dma('sp', lambda e: e.dma_start(out=xb[:, :, 0:tn], in_=XT[:, t0:t0 + tn].rearrange("(k p) t -> p k t", p=128)),
                           r=[xtkey], w=[xk])
                    for jb in jobs:
                        if jb.get('tsel') is not None and not jb['tsel'](t0):
                            continue
                        c0, cn, tofs = jb['c0'], jb['cn'], jb.get('tofs', 0)
                        if jb['mode'] == 'fm':
                            for m in range(cn // 128):
                                pb, pbk = banks[bi % 8]; bi += 1
                                for kk in range(kch):
                                    kb.op('pe', lambda e: e.matmul(pb[:, 0:tn], lhsT=Wsb[:, kk, c0 + m * 128:c0 + (m + 1) * 128], rhs=xb[:, kk, 0:tn],
                                                                   start=(kk == 0), stop=(kk == kch - 1)), r=[tag + '_W', xk], w=[pbk])
                                sg = stg[si % 4]; sgk = "%s_s%d" % (tag, si % 4); si += 1
                                evac(sg[:, 0:tn], pb[:, 0:tn], jb['func'], [pbk], [sgk])
                                orow = jb.get('oc0', 0) + m * 128
                                kb.dma('pool', lambda e: e.dma_start(out=jb['out'][orow:orow + 128, t0 - tofs:t0 - tofs + tn], in_=sg[:, 0:tn]),
                                       r=[sgk], w=[jb['okey']])
                        else:
                            for tt in range(tn // 128):
                                pb, pbk = banks[bi % 8]; bi += 1
                                for kk in range(kch):
                                    kb.op('pe', lambda e: e.matmul(pb[:, 0:cn], lhsT=xb[:, kk, tt * 128:(tt + 1) * 128], rhs=Wsb[:, kk, c0:c0 + cn],
                                                                   start=(kk == 0), stop=(kk == kch - 1)), r=[tag + '_W', xk], w=[pbk])
                                sg = stg[si % 4]; sgk = "%s_s%d" % (tag, si % 4); si += 1
                                evac(sg[:, 0:cn], pb[:, 0:cn], jb['func'], [pbk], [sgk])
                                tr = t0 - tofs + tt * 128
                                oc0 = jb.get('oc0', 0)
                                kb.dma('pool', lambda e: e.dma_start(out=jb['out'][tr:tr + 128, oc0:oc0 + cn], in_=sg[:, 0:cn]),
                                       r=[sgk], w=[jb['okey']])

        if stages >= 2:
            lat_blocks = [(t, 512) for t in range(0, L, 512)]
            all_blocks = lat_blocks + [(L, 256)]
            gemm_group("g1", WIN[:, 0:1024], 8, 1024, XNT, 'XNT', lat_blocks,
                       [dict(c0=0, cn=1024, mode='fm', func=None, out=HYRAW, okey='HYRAW', oc0=0)])
            gemm_group("g2", WIN[:, 1024:2048], 8, 1024, XNT, 'XNT', lat_blocks,
                       [dict(c0=0, cn=512, mode='fm', func=None, out=HYRAW, okey='HYRAW', oc0=1024),
                        dict(c0=512, cn=512, mode='fm', func=AF.Silu, out=QT, okey='QT', oc0=0)])
        if stages >= 3:
            gemm_group("g3", WIN[:, 2048:3072], 8, 1024, XNT, 'XNT', all_blocks,
                       [dict(c0=0, cn=512, mode='tm', func=None, out=IFF, okey='IFF', oc0=0),
                        dict(c0=512, cn=512, mode='tm', func=None, out=IFF, okey='IFF', oc0=512)])
            gemm_group("g4", WIN[:, 3072:4096], 8, 1024, XNT, 'XNT', all_blocks,
                       [dict(c0=0, cn=512, mode='tm', func=None, out=IFF, okey='IFF', oc0=1024),
                        dict(c0=512, cn=512, mode='fm', func=AF.Silu, out=GT, okey='GT', oc0=0, tsel=lambda t0: t0 < L)])

        if stages >= 3:
            own_blocks = [(t, 512) for t in range(0, OWN, 512)]
            for gi in range(2):
                gemm_group("g%d" % (5 + gi), WIN[:, 4096 + 1024 * gi:5120 + 1024 * gi], 8, 1024, XNTOWN, 'XNTOWN', own_blocks,
                           [dict(c0=0, cn=1024, mode='fm', func=AF.Sigmoid, out=SGT, okey='SGT', oc0=1024 * gi)])

        if stages >= 4:
            with stage() as st:
                cwT = sbt(st, "cwT", [128, 12, 4]); craw = sbt(st, "craw", [48, 128])
                for j3 in range(3):
                    kb.dma('sp', lambda e: e.dma_start(out=craw[12 * j3:12 * j3 + 12, :], in_=CONVW[j3].rearrange("(q p) -> q p", p=128)), r=['craw'], w=['craw'])
                kb.dma('sp', lambda e: e.dma_start(out=craw[36:48, :], in_=CONVB.rearrange("(q p) -> q p", p=128)), r=['craw'], w=['craw'])
                pb, pbk = banks[0]
                kb.op('pe', lambda e: e.transpose(out=pb[:, 0:48], in_=craw[:], identity=ident[0:48, 0:48]), r=['craw', 'ident'], w=[pbk])
                kb.op('dve', lambda e: e.tensor_copy(out=cwT[:], in_=pb[:, 0:48].rearrange("p (j q) -> p q j", j=4)), r=[pbk], w=['cwT'])
                uin = [sbt(st, "uin%d" % i, [128, L + 2]) for i in range(2)]
                uo = [sbt(st, "uo%d" % i, [128, L]) for i in range(2)]
                for i in range(2):
                    kb.op('pool', lambda e: e.memset(uin[i][:, 0:1], 0.0), w=['uin%d' % i])
                    kb.op('pool', lambda e: e.memset(uin[i][:, L + 1:L + 2], 0.0), r=['uin%d' % i], w=['uin%d' % i])
                for q in range(12):
                    b2 = q % 2; uk = 'uin%d' % b2; ok = 'uo%d' % b2
                    kb.dma('sp', lambda e: e.dma_start(out=uin[b2][:, 1:L + 1], in_=HYRAW[q * 128:(q + 1) * 128, :]), r=['HYRAW', uk], w=[uk])
                    kb.op('dve', lambda e: e.tensor_scalar(out=uo[b2][:], in0=uin[b2][:, 0:L], scalar1=cwT[:, q, 0:1], scalar2=cwT[:, q, 3:4],
                                                           op0=ALU.mult, op1=ALU.add), r=[uk, 'cwT'], w=[ok])
                    kb.op('dve', lambda e: e.scalar_tensor_tensor(out=uo[b2][:], in0=uin[b2][:, 1:L + 1], scalar=cwT[:, q, 1:2], in1=uo[b2][:],
                                                                  op0=ALU.mult, op1=ALU.add), r=[uk, 'cwT', ok], w=[ok])
                    kb.op('dve', lambda e: e.scalar_tensor_tensor(out=uo[b2][:], in0=uin[b2][:, 2:L + 2], scalar=cwT[:, q, 2:3], in1=uo[b2][:],
                                                                  op0=ALU.mult, op1=ALU.add), r=[uk, 'cwT', ok], w=[ok])
                    kb.dma('pool', lambda e: e.dma_start(out=HYC[q * 128:(q + 1) * 128, :], in_=uo[b2][:]), r=[ok], w=['HYC'])

        if stages >= 5 and not dummy_mix:
          with stage() as st:
            tri = {0: sbt(st, "trif", [128, 128]), 1: sbt(st, "trib", [128, 128])}
            ones = sbt(st, "ones", [128, 128])
            kb.dma('sp', lambda e: e.dma_start(out=tri[0][:], in_=TRIFd), w=['tri'])
            kb.dma('sp', lambda e: e.dma_start(out=tri[1][:], in_=TRIBd), r=['tri'], w=['tri'])
            kb.dma('sp', lambda e: e.dma_start(out=ones[:], in_=ONESd), w=['ones'])
            l0 = sbt(st, "l0", [128, 1024]); l1 = sbt(st, "l1", [128, 1024]); oml = sbt(st, "oml", [128, 1024])
            kb.dma('sp', lambda e: e.dma_start(out=l0[:], in_=LBL[0:1, :].to_broadcast([128, 1024])), w=['l0'])
            kb.dma('sp', lambda e: e.dma_start(out=l1[:], in_=LBL[1:2, :].to_broadcast([128, 1024])), w=['l1'])
            kb.op('dve', lambda e: e.tensor_tensor(out=l0[:], in0=l0[:], in1=l1[:], op=ALU.subtract), r=['l0', 'l1'], w=['l0'])
            kb.op('act', lambda e: e.activation(out=l0[:], in_=l0[:], func=AF.Sigmoid), r=['l0'], w=['l0'])
            kb.op('dve', lambda e: e.tensor_scalar(out=oml[:], in0=l0[:], scalar1=-1.0, scalar2=1.0, op0=ALU.mult, op1=ALU.add), r=['l0'], w=['oml'])
            ngT = sbt(st, "ngT", [128, 1])
            with nc.allow_non_contiguous_dma(reason="128-element vector to partitions"):
                kb.dma('sp', lambda e: e.dma_start(out=ngT[:], in_=HGNG.rearrange("(p o) -> p o", o=1)), w=['ngT'])
            osum = sbt(st, "osum", [128, L])
            LB8 = sbt(st, "LB8", [128, 8, 128]); OML8 = sbt(st, "OML8", [128, 8, 128])
            vseg = [sbt(st, "vseg%d" % i, [128, 8, 128]) for i in range(2)]
            fseg = [sbt(st, "fseg%d" % i, [128, 8, 128]) for i in range(2)]
            lseg = [sbt(st, "lseg%d" % i, [128, 8, 128]) for i in range(2)]
            kseg = [sbt(st, "kseg%d" % i, [128, 8, 128]) for i in range(2)]
            qseg = [sbt(st, "qseg%d" % i, [128, 1024]) for i in range(2)]
            R3 = 3
            ekb = [sbt(st, "ekb%d" % i, [128, 128]) for i in range(R3)]
            kd = [sbt(st, "kd%d" % i, [128, 128]) for i in range(R3)]
            ebT = [sbt(st, "ebT%d" % i, [128, 128]) for i in range(R3)]
            qdT = [sbt(st, "qdT%d" % i, [128, 128]) for i in range(R3)]
            kdT = [sbt(st, "kdT%d" % i, [128, 128]) for i in range(R3)]
            attm = [sbt(st, "attm%d" % i, [128, 128]) for i in range(R3)]
            Sr = [sbt(st, "S%d" % i, [128, 128]) for i in range(4)]
            Se = [sbt(st, "Se%d" % i, [128, 128]) for i in range(2)]
            pbi = [0]

            def nb():
                b_ = banks[pbi[0] % 8]; pbi[0] += 1
                return b_

            for h in range(4):
                for dr in range(2):
                    T = tri[dr]
                    fcol = 512 + 512 * dr + h * 128
                    for n in range(8):
                        kb.op('pool', lambda e: e.tensor_copy(out=LB8[:, n, :], in_=l0[:, dr * 512 + h * 128:dr * 512 + (h + 1) * 128]), r=['l0', 'LB8'], w=['LB8'])
                        kb.op('pool', lambda e: e.tensor_copy(out=OML8[:, n, :], in_=oml[:, dr * 512 + h * 128:dr * 512 + (h + 1) * 128]), r=['oml', 'OML8'], w=['OML8'])
                    si_ = 0
                    kb.op('pool', lambda e: e.memset(Sr[0][:], 0.0), w=['S0'])
                    segs = [(L, 2, False)] + [(sg * 1024, 8, True) for sg in (range(8) if dr == 0 else range(7, -1, -1))]
                    tcount = 0
                    for sgi, (r0, nt, lat) in enumerate(segs):
                        b2 = sgi % 2
                        vk, fk, lk, kk_, qk = 'vseg%d' % b2, 'fseg%d' % b2, 'lseg%d' % b2, 'kseg%d' % b2, 'qseg%d' % b2
                        kb.dma('sp', lambda e: e.dma_start(out=vseg[b2][:, 0:nt, :], in_=IFF[r0:r0 + nt * 128, h * 128:(h + 1) * 128].rearrange("(n p) c -> p n c", p=128)),
                               r=['IFF'], w=[vk])
                        kb.dma('sp', lambda e: e.dma_start(out=fseg[b2][:, 0:nt, :], in_=IFF[r0:r0 + nt * 128, fcol:fcol + 128].rearrange("(n p) c -> p n c", p=128)),
                               r=['IFF'], w=[fk])
                        if lat:
                            kb.dma('sp', lambda e: e.dma_start(out=qseg[b2][:], in_=QT[h * 128:(h + 1) * 128, r0:r0 + 1024]), r=['QT'], w=[qk])
                        kb.op('act', lambda e: e.activation(out=fseg[b2][:, 0:nt, :], in_=fseg[b2][:, 0:nt, :], func=AF.Sigmoid), r=[fk], w=[fk])
                        kb.op('dve', lambda e: e.tensor_tensor(out=fseg[b2][:, 0:nt, :], in0=fseg[b2][:, 0:nt, :], in1=OML8[:, 0:nt, :], op=ALU.mult), r=[fk, 'OML8'], w=[fk])
                        kb.op('dve', lambda e: e.tensor_tensor(out=fseg[b2][:, 0:nt, :], in0=fseg[b2][:, 0:nt, :], in1=LB8[:, 0:nt, :], op=ALU.add), r=[fk, 'LB8'], w=[fk])
                        kb.op('act', lambda e: e.activation(out=lseg[b2][:, 0:nt, :], in_=fseg[b2][:, 0:nt, :], func=AF.Ln), r=[fk], w=[lk])
                        kb.op('dve', lambda e: e.tensor_scalar(out=kseg[b2][:, 0:nt, :], in0=fseg[b2][:, 0:nt, :], scalar1=-1.0, scalar2=1.0, op0=ALU.mult, op1=ALU.add),
                              r=[fk], w=[kk_])
                        order = range(nt) if dr == 0 else range(nt - 1, -1, -1)
                        for n in order:
                            r3 = tcount % R3; tcount += 1
                            ek, kdk, ebk, qdk, ktk, amk = 'ekb%d' % r3, 'kd%d' % r3, 'ebT%d' % r3, 'qdT%d' % r3, 'kdT%d' % r3, 'attm%d' % r3
                            p1, p1k = nb()
                            kb.op('pe', lambda e: e.matmul(p1[:, 0:128], lhsT=T[:], rhs=lseg[b2][:, n, :], start=True, stop=True), r=['tri', lk], w=[p1k])
                            kb.op('act', lambda e: e.activation(out=ekb[r3][:], in_=p1[:, 0:128], func=AF.Exp, scale=-1.0), r=[p1k], w=[ek])
                            kb.op('dve', lambda e: e.tensor_tensor(out=kd[r3][:], in0=kseg[b2][:, n, :], in1=ekb[r3][:], op=ALU.mult), r=[kk_, ek], w=[kdk])
                            p2, p2k = nb()
                            kb.op('pe', lambda e: e.matmul(p2[:, 0:128], lhsT=lseg[b2][:, n, :], rhs=T[:], start=True, stop=True), r=['tri', lk], w=[p2k])
                            kb.op('act', lambda e: e.activation(out=ebT[r3][:], in_=p2[:, 0:128], func=AF.Exp), r=[p2k], w=[ebk])
                            if lat:
                                kb.op('dve', lambda e: e.tensor_tensor(out=qdT[r3][:], in0=qseg[b2][:, n * 128:(n + 1) * 128], in1=ebT[r3][:], op=ALU.mult), r=[qk, ebk], w=[qdk])
                                p3, p3k = nb()
                                kb.op('pe', lambda e: e.transpose(out=p3[:, 0:128], in_=kd[r3][:], identity=ident[:]), r=[kdk, 'ident'], w=[p3k])
                                kb.op('act', lambda e: e.copy(out=kdT[r3][:], in_=p3[:, 0:128]), r=[p3k], w=[ktk])
                                p4, p4k = nb()
                                kb.op('pe', lambda e: e.matmul(p4[:, 0:128], lhsT=kdT[r3][:], rhs=qdT[r3][:], start=True, stop=True), r=[ktk, qdk], w=[p4k])
                                kb.op('dve', lambda e: e.tensor_tensor(out=attm[r3][:], in0=p4[:, 0:128], in1=T[:], op=ALU.mult), r=[p4k, 'tri'], w=[amk])
                                po, pok = nb()
                                kb.op('pe', lambda e: e.matmul(po[:, 0:128], lhsT=vseg[b2][:, n, :], rhs=attm[r3][:], start=True, stop=False), r=[vk, amk], w=[pok])
                            halves = [(0, 64, 63), (64, 128, 127)] if dr == 0 else [(64, 128, 64), (0, 64, 0)]
                            for (c0, c1, ce) in halves:
                                Sc = Sr[si_ % 4]; Sck = 'S%d' % (si_ % 4)
                                Sn = Sr[(si_ + 1) % 4]; Snk = 'S%d' % ((si_ + 1) % 4)
                                sew = Se[si_ % 2]; sek = 'Se%d' % (si_ % 2)
                                si_ += 1
                                if lat:
                                    kb.op('pe', lambda e: e.matmul(po[:, c0:c1], lhsT=Sc[:], rhs=qdT[r3][:, c0:c1], start=False, stop=True), r=[Sck, qdk], w=[pok])
                                pd, pdk = nb()
                                kb.op('pe', lambda e: e.matmul(pd[:, 0:128], lhsT=kd[r3][c0:c1, :], rhs=vseg[b2][c0:c1, n, :], start=True, stop=True), r=[kdk, vk], w=[pdk])
                                kb.op('act', lambda e: e.activation(out=sew[:], in_=Sc[:], func=AF.Copy, scale=ebT[r3][:, ce:ce + 1]), r=[Sck, ebk], w=[sek])
                                kb.op('dve', lambda e: e.scalar_tensor_tensor(out=Sn[:], in0=pd[:, 0:128], scalar=ebT[r3][:, ce:ce + 1], in1=sew[:], op0=ALU.mult, op1=ALU.add),
                                      r=[pdk, ebk, sek], w=[Snk])
                            if lat:
                                cols = slice(r0 + n * 128, r0 + (n + 1) * 128)
                                if dr == 0:
                                    kb.op('act', lambda e: e.copy(out=osum[:, cols], in_=po[:, 0:128]), r=[pok], w=['osum%d' % (r0 // 1024)])
                                else:
                                    kb.op('dve', lambda e: e.tensor_tensor(out=osum[:, cols], in0=po[:, 0:128], in1=osum[:, cols], op=ALU.add),
                                          r=[pok, 'osum%d' % (r0 // 1024)], w=['osum%d' % (r0 // 1024)])
                    if si_ % 4 != 0:
                        pass
                    kb.res.pop('unused', None)
                sq = [sbt(st, "hsq%d_%d" % (h, i), [128, 512]) for i in range(2)] if h == 0 else sq
                gt_ = [sbt(st, "hgt%d_%d" % (h, i), [128, 512]) for i in range(2)] if h == 0 else gt_
                rs = [sbt(st, "hrs%d_%d" % (h, i), [128, 512]) for i in range(2)] if h == 0 else rs
                for blk in range(L // 512):
                    b2 = blk % 2; cols = slice(blk * 512, (blk + 1) * 512)
                    sqk, gk, rk = 'hsq%d' % b2, 'hgt%d' % b2, 'hrs%d' % b2
                    ok_ = 'osum%d' % (blk // 2)
                    kb.dma('sp', lambda e: e.dma_start(out=gt_[b2][:], in_=GT[h * 128:(h + 1) * 128, cols]), r=['GT'], w=[gk])
                    kb.op('act', lambda e: e.activation(out=sq[b2][:], in_=osum[:, cols], func=AF.Square), r=[ok_], w=[sqk])
                    pb, pbk = nb()
                    kb.op('pe', lambda e: e.matmul(pb[:, :], lhsT=ones[:], rhs=sq[b2][:], start=True, stop=True), r=['ones', sqk], w=[pbk])
                    kb.op('dve', lambda e: e.tensor_scalar(out=rs[b2][:], in0=pb[:, :], scalar1=1.0 / 128, scalar2=EPS, op0=ALU.mult, op1=ALU.add), r=[pbk], w=[rk])
                    kb.op('act', lambda e: e.activation(out=rs[b2][:], in_=rs[b2][:], func=AF.Sqrt), r=[rk], w=[rk])
                    kb.op('dve', lambda e: e.reciprocal(out=rs[b2][:], in_=rs[b2][:]), r=[rk], w=[rk])
                    kb.op('dve', lambda e: e.scalar_tensor_tensor(out=sq[b2][:], in0=osum[:, cols], scalar=ngT[:, 0:1], in1=rs[b2][:], op0=ALU.mult, op1=ALU.mult),
                          r=[ok_, 'ngT', rk, sqk], w=[sqk])
                    kb.op('pool', lambda e: e.tensor_tensor(out=sq[b2][:], in0=sq[b2][:], in1=gt_[b2][:], op=ALU.mult), r=[sqk, gk], w=[sqk])
                    kb.dma('pool', lambda e: e.dma_start(out=YHGT[h * 128:(h + 1) * 128, cols], in_=sq[b2][:]), r=[sqk], w=['YHGT'])

        if stages >= 5 and not dummy_mix:
          with stage() as st:
            cst = {}
            for nm, src, shp in (("FA", FAd, [128, 256]), ("FAH", FAHd, [64, 256]), ("FRE", FREd, [128, 128]), ("FIM", FIMd, [128, 128]), ("NFIM", NFIMd, [128, 128]),
                                 ("CA", CAd, [128, 256]), ("CB", CBd, [128, 256]), ("FREN", FRENd, [128, 64]), ("FIMN", FIMNd, [128, 64]),
                                 ("TRE2", TRE2d, [128, 256]), ("TIM2", TIM2d, [128, 256]), ("ONESH", ONESd, [128, 128])):
                cst[nm] = sbt(st, "c_" + nm, shp)
                kb.dma('sp', lambda e: e.dma_start(out=cst[nm][:], in_=src), w=['hconst'])
            skb = sbt(st, "skb", [128, 1024])
            kb.dma('sp', lambda e: e.dma_start(out=skb[:], in_=HSKIP.rearrange("(o n) -> o n", o=1).to_broadcast([128, 1024])), w=['skb'])
            tw = [sbt(st, "tw%d" % i, [128, 256]) for i in range(4)]
            hbi = [0]

            def hb():
                b_ = banks[hbi[0] % 8]; hbi[0] += 1
                return b_

            def twiddle(ps, psk, Bt, bkey, c0, conj):
                A = ps[:, :].rearrange("p (c r f) -> p c r f", c=2, r=2)
                Are, Aim = A[:, :, 0, :], A[:, :, 1, :]
                T2r = cst["TRE2"][:].rearrange("p (c f) -> p c f", c=2); T2i = cst["TIM2"][:].rearrange("p (c f) -> p c f", c=2)
                t = [x[:].rearrange("p (c f) -> p c f", c=2) for x in tw]
                kb.op('dve', lambda e: e.tensor_tensor(out=t[0], in0=Are, in1=T2r, op=ALU.mult), r=[psk, 'hconst', 'tw0'], w=['tw0'])
                kb.op('dve', lambda e: e.tensor_tensor(out=t[1], in0=Aim, in1=T2i, op=ALU.mult), r=[psk, 'hconst', 'tw1'], w=['tw1'])
                kb.op('dve', lambda e: e.tensor_tensor(out=t[2], in0=Are, in1=T2i, op=ALU.mult), r=[psk, 'hconst', 'tw2'], w=['tw2'])
                kb.op('dve', lambda e: e.tensor_tensor(out=t[3], in0=Aim, in1=T2r, op=ALU.mult), r=[psk, 'hconst', 'tw3'], w=['tw3'])
                if not conj:
                    kb.op('pool', lambda e: e.tensor_tensor(out=Bt[:, c0:c0 + 2, 0, :], in0=t[0], in1=t[1], op=ALU.subtract), r=['tw0', 'tw1', bkey], w=[bkey])
                    kb.op('pool', lambda e: e.tensor_tensor(out=Bt[:, c0:c0 + 2, 1, :], in0=t[2], in1=t[3], op=ALU.add), r=['tw2', 'tw3', bkey], w=[bkey])
                else:
                    kb.op('pool', lambda e: e.tensor_tensor(out=Bt[:, c0:c0 + 2, 0, :], in0=t[0], in1=t[1], op=ALU.add), r=['tw0', 'tw1', bkey], w=[bkey])
                    kb.op('pool', lambda e: e.tensor_tensor(out=Bt[:, c0:c0 + 2, 1, :], in0=t[3], in1=t[2], op=ALU.subtract), r=['tw2', 'tw3', bkey], w=[bkey])

            def stage2(Bt, bkey, c4):
                pr, prk = hb(); pi_, pik = hb()
                Bre, Bim = Bt[:, c4:c4 + 4, 0, :], Bt[:, c4:c4 + 4, 1, :]
                kb.op('pe', lambda e: e.matmul(pr[:, :], lhsT=cst["FRE"][:], rhs=Bre, start=True, stop=False), r=['hconst', bkey], w=[prk])
                kb.op('pe', lambda e: e.matmul(pr[:, :], lhsT=cst["NFIM"][:], rhs=Bim, start=False, stop=True), r=['hconst', bkey], w=[prk])
                kb.op('pe', lambda e: e.matmul(pi_[:, :], lhsT=cst["FIM"][:], rhs=Bre, start=True, stop=False), r=['hconst', bkey], w=[pik])
                kb.op('pe', lambda e: e.matmul(pi_[:, :], lhsT=cst["FRE"][:], rhs=Bim, start=False, stop=True), r=['hconst', bkey], w=[pik])
                return pr, prk, pi_, pik

            B16 = sbt(st, "B16", [128, 16, 2, 128])
            with stage() as sf:
                a3 = sbt(sf, "a3", [128, L])
                w4sb = sbt(sf, "w4sb", [128, 2048])
                kb.dma('sp', lambda e: e.dma_start(out=w4sb[0:64, :], in_=FW4), w=['w4sb'])
                kb.dma('sp', lambda e: e.dma_start(out=w4sb[64:128, :], in_=FW4), r=['w4sb'], w=['w4sb'])
                with stage() as sm:
                    zts = sbt(sm, "zts", [66, L]); bufB = sbt(sm, "bufB", [128, L])
                    kb.dma('sp', lambda e: e.dma_start(out=zts[:], in_=ZTd), w=['zts'])
                    wl = [sbt(sm, "w1bd", [66, 128]), sbt(sm, "w2bd", [128, 128]), sbt(sm, "w3bd", [128, 128])]
                    for i_, (wt, src, kin) in enumerate(zip(wl, (FW1, FW2, FW3), (33, 64, 64))):
                        kb.op('pool', lambda e: e.memset(wt[:], 0.0), w=['wbd%d' % i_])
                        kb.dma('sp', lambda e: e.dma_start(out=wt[0:kin, 0:64], in_=src), r=['wbd%d' % i_], w=['wbd%d' % i_])
                        kb.dma('sp', lambda e: e.dma_start(out=wt[kin:2 * kin, 64:128], in_=src), r=['wbd%d' % i_], w=['wbd%d' % i_])
                    fqb = sbt(sm, "fqb", [128, 4])
                    with nc.allow_non_contiguous_dma(reason="64-element vectors onto partitions"):
                        for j_, src in enumerate((FFQ, FB1, FB2, FB3)):
                            for hh in range(2):
                                kb.dma('sp', lambda e: e.dma_start(out=fqb[64 * hh:64 * hh + 64, j_:j_ + 1], in_=src.rearrange("(p o) -> p o", o=1)), r=['fqb'], w=['fqb'])
                    kb.op('dve', lambda e: e.tensor_scalar(out=fqb[:, 0:1], in0=fqb[:, 0:1], scalar1=1.0 / TWO_PI, scalar2=None, op0=ALU.mult), r=['fqb'], w=['fqb'])
                    kb.op('dve', lambda e: e.tensor_scalar(out=fqb[:, 1:4], in0=fqb[:, 1:4], scalar1=fqb[:, 0:1], scalar2=None, op0=ALU.mult), r=['fqb'], w=['fqb'])
                    uu = sbt(sm, "uu", [128, 2048]); ui = sbt(sm, "ui", [128, 2048], I32); uf = sbt(sm, "uf", [128, 2048])
                    srcs = [(zts, 66, 'zts'), (bufB, 128, 'bufB'), (a3, 128, 'a3')]
                    dsts = [(bufB, 'bufB'), (a3, 'a3'), (bufB, 'bufB')]
                    for l_ in range(3):
                        src_t, kdim, srck = srcs[l_]; dst_t, dstk = dsts[l_]
                        for ch in range(4):
                            Pq, pkeys = (PA, ["PA0", "PA1", "PA2", "PA3"]) if ch % 2 == 0 else (PB, ["PB0", "PB1", "PB2", "PB3"])
                            for q in range(4):
                                cs = slice(ch * 2048 + q * 512, ch * 2048 + (q + 1) * 512)
                                kb.op('pe', lambda e: e.matmul(Pq[:, q * 512:(q + 1) * 512], lhsT=wl[l_][0:kdim, :], rhs=src_t[0:kdim, cs], start=True, stop=True),
                                      r=['wbd%d' % l_, srck], w=[pkeys[q]])
                            kb.op('act', lambda e: e.activation(out=uu[:], in_=Pq[:, :], func=AF.Identity, bias=fqb[:, 1 + l_:2 + l_], scale=fqb[:, 0:1]), r=pkeys + ['fqb'], w=['uu'])
                            kb.op('dve', lambda e: e.tensor_copy(out=ui[:], in_=uu[:]), r=['uu'], w=['ui'])
                            kb.op('dve', lambda e: e.tensor_copy(out=uf[:], in_=ui[:]), r=['ui'], w=['uf'])
                            kb.op('pool', lambda e: e.tensor_tensor(out=uu[:], in0=uu[:], in1=uf[:], op=ALU.subtract), r=['uu', 'uf'], w=['uu'])
                            kb.op('dve', lambda e: e.scalar_tensor_tensor(out=uf[:], in0=uu[:], scalar=0.5, in1=uu[:], op0=ALU.is_gt, op1=ALU.subtract), r=['uu', 'uf'], w=['uf'])
                            kb.op('dve', lambda e: e.scalar_tensor_tensor(out=uu[:], in0=uf[:], scalar=0.5, in1=uf[:], op0=ALU.is_gt, op1=ALU.subtract), r=['uu', 'uf'], w=['uu'])
                            kb.op('act', lambda e: e.activation(out=dst_t[:, ch * 2048:(ch + 1) * 2048], in_=uu[:], func=AF.Sin, scale=6.283185), r=['uu', dstk], w=[dstk])
                    kb.op('pool', lambda e: e.tensor_copy(out=a3[:], in_=bufB[:]), r=['bufB', 'a3'], w=['a3'])
                Kt = [sbt(sf, "Kt%d" % i, [64, 64, 128]) for i in range(2)]
                dec = sbt(sf, "dec", [64, 64, 128]); rab = sbt(sf, "rab", [64, 2, 64]); rn = sbt(sf, "rn", [128, 64]); Hst = sbt(sf, "Hst", [128, 16, 2, 128])
                for o in range(2):
                    for cg in range(8):
                        for hh in range(2):
                            col0 = o * 1024 + hh * 512 + cg * 64
                            for nbk in range(16):
                                ps, psk = hb()
                                for j_ in range(8):
                                    n2 = nbk * 8 + j_
                                    kb.op('pe', lambda e: e.matmul(ps[0:64, j_ * 64:(j_ + 1) * 64], lhsT=a3[64 * hh:64 * hh + 64, n2:L:128], rhs=w4sb[64 * hh:64 * hh + 64, col0:col0 + 64],
                                                                   start=True, stop=True), r=['a3', 'w4sb'], w=[psk])
                                evac(Kt[hh][:, :, nbk * 8:(nbk + 1) * 8], ps[0:64, :].rearrange("p (n c) -> p c n", c=64), None, [psk, 'Kt%d' % hh], ['Kt%d' % hh])
                            kb.dma('sp', lambda e: e.dma_start(out=dec[:], in_=DECd[hh, :, cg * 64:(cg + 1) * 64, :]), r=['dec'], w=['dec'])
                            kb.op('pool', lambda e: e.tensor_tensor(out=Kt[hh][:], in0=Kt[hh][:], in1=dec[:], op=ALU.mult), r=['Kt%d' % hh, 'dec'], w=['Kt%d' % hh])
                            kb.op('dve', lambda e: e.tensor_reduce(out=rab[:, hh, :], in_=Kt[hh][:], axis=AX.X, op=ALU.add, apply_absolute_value=True), r=['Kt%d' % hh, 'rab'], w=['rab'])
                        kb.op('dve', lambda e: e.tensor_tensor(out=rab[:, 0, :], in0=rab[:, 0, :], in1=rab[:, 1, :], op=ALU.add), r=['rab'], w=['rab'])
                        ps, psk = hb()
                        kb.op('pe', lambda e: e.matmul(ps[:, 0:64], lhsT=cst["ONESH"][0:64, :], rhs=rab[:, 0, :], start=True, stop=True), r=['hconst', 'rab'], w=[psk])
                        kb.op('dve', lambda e: e.reciprocal(out=rn[:], in_=ps[:, 0:64]), r=[psk], w=['rn'])
                        for sb4 in range(4):
                            for c2 in range(8):
                                ps, psk = hb()
                                for j_ in range(2):
                                    cc = sb4 * 16 + c2 * 2 + j_
                                    kb.op('pe', lambda e: e.matmul(ps[:, j_ * 256:(j_ + 1) * 256], lhsT=Kt[0][:, cc, :], rhs=cst["FA"][0:64, :], start=True, stop=False), r=['Kt0', 'hconst'], w=[psk])
                                    kb.op('pe', lambda e: e.matmul(ps[:, j_ * 256:(j_ + 1) * 256], lhsT=Kt[1][:, cc, :], rhs=cst["FAH"][:], start=False, stop=True), r=['Kt1', 'hconst'], w=[psk])
                                twiddle(ps, psk, B16, 'B16', c2 * 2, False)
                            for c4 in range(0, 16, 4):
                                pr, prk, pi_, pik = stage2(B16, 'B16', c4)
                                for j_ in range(4):
                                    cc = sb4 * 16 + c4 + j_; gc = cg * 64 + cc
                                    kb.op('dve', lambda e: e.tensor_scalar(out=Hst[:, c4 + j_, 0, :], in0=pr[:, j_ * 128:(j_ + 1) * 128], scalar1=rn[:, cc:cc + 1], scalar2=skb[:, o * 512 + gc:o * 512 + gc + 1],
                                                                           op0=ALU.mult, op1=ALU.add), r=[prk, 'rn', 'skb', 'Hst'], w=['Hst'])
                                    kb.op('act', lambda e: e.activation(out=Hst[:, c4 + j_, 1, :], in_=pi_[:, j_ * 128:(j_ + 1) * 128], func=AF.Copy, scale=rn[:, cc:cc + 1]), r=[pik, 'rn', 'Hst'], w=['Hst'])
                            g0 = o * 512 + cg * 64 + sb4 * 16
                            kb.dma('sp', lambda e: e.dma_start(out=HSPEC[g0:g0 + 16].rearrange("c k r f -> k c r f"), in_=Hst[:]), r=['Hst'], w=['HSPEC'])
            with stage() as sc:
                v16 = sbt(sc, "v16", [64, 16, 128]); x116 = sbt(sc, "x116", [64, 16, 128]); x216 = sbt(sc, "x216", [64, 16, 128]); z16 = sbt(sc, "z16", [64, 16, 128])
                y16 = sbt(sc, "y16", [64, 16, 128])
                H1 = sbt(sc, "H1", [128, 16, 2, 128]); H2s = sbt(sc, "H2s", [128, 16, 2, 128]); Y16 = sbt(sc, "Y16", [128, 16, 2, 128]); G16 = sbt(sc, "G16", [128, 16, 2, 128])
                hm = [sbt(sc, "hm%d" % i, [128, 4, 128]) for i in range(4)]

                def conv16(Din, dkey, Hs, hkey, Xmul, xkey, Out, okey):
                    for c2 in range(8):
                        ps, psk = hb()
                        for j_ in range(2):
                            kb.op('pe', lambda e: e.matmul(ps[:, j_ * 256:(j_ + 1) * 256], lhsT=Din[:, c2 * 2 + j_, :], rhs=cst["FA"][0:64, :], start=True, stop=True), r=[dkey, 'hconst'], w=[psk])
                        twiddle(ps, psk, B16, 'B16', c2 * 2, False)
                    for c4 in range(0, 16, 4):
                        pr, prk, pi_, pik = stage2(B16, 'B16', c4)
                        Xr = pr[:, :].rearrange("p (c f) -> p c f", c=4); Xi = pi_[:, :].rearrange("p (c f) -> p c f", c=4)
                        Hr, Hi = Hs[:, c4:c4 + 4, 0, :], Hs[:, c4:c4 + 4, 1, :]
                        kb.op('dve', lambda e: e.tensor_tensor(out=hm[0][:], in0=Xr, in1=Hr, op=ALU.mult), r=[prk, hkey, 'hm0'], w=['hm0'])
                        kb.op('dve', lambda e: e.tensor_tensor(out=hm[1][:], in0=Xi, in1=Hi, op=ALU.mult), r=[pik, hkey, 'hm1'], w=['hm1'])
                        kb.op('dve', lambda e: e.tensor_tensor(out=hm[2][:], in0=Xr, in1=Hi, op=ALU.mult), r=[prk, hkey, 'hm2'], w=['hm2'])
                        kb.op('dve', lambda e: e.tensor_tensor(out=hm[3][:], in0=Xi, in1=Hr, op=ALU.mult), r=[pik, hkey, 'hm3'], w=['hm3'])
                        kb.op('pool', lambda e: e.tensor_tensor(out=Y16[:, c4:c4 + 4, 0, :], in0=hm[0][:], in1=hm[1][:], op=ALU.subtract), r=['hm0', 'hm1', 'Y16'], w=['Y16'])
                        kb.op('pool', lambda e: e.tensor_tensor(out=Y16[:, c4:c4 + 4, 1, :], in0=hm[2][:], in1=hm[3][:], op=ALU.add), r=['hm2', 'hm3', 'Y16'], w=['Y16'])
                    for c2 in range(8):
                        ps, psk = hb()
                        for j_ in range(2):
                            cc = c2 * 2 + j_
                            kb.op('pe', lambda e: e.matmul(ps[:, j_ * 256:(j_ + 1) * 256], lhsT=Y16[:, cc, 0, :], rhs=cst["CA"][:], start=True, stop=False), r=['Y16', 'hconst'], w=[psk])
                            kb.op('pe', lambda e: e.matmul(ps[:, j_ * 256:(j_ + 1) * 256], lhsT=Y16[:, cc, 1, :], rhs=cst["CB"][:], start=False, stop=True), r=['Y16', 'hconst'], w=[psk])
                        twiddle(ps, psk, G16, 'G16', c2 * 2, True)
                    for c4 in range(0, 16, 4):
                        py, pyk = hb()
                        kb.op('pe', lambda e: e.matmul(py[0:64, :], lhsT=cst["FREN"][:], rhs=G16[:, c4:c4 + 4, 0, :], start=True, stop=False), r=['hconst', 'G16'], w=[pyk])
                        kb.op('pe', lambda e: e.matmul(py[0:64, :], lhsT=cst["FIMN"][:], rhs=G16[:, c4:c4 + 4, 1, :], start=False, stop=True), r=['hconst', 'G16'], w=[pyk])
                        kb.op('dve', lambda e: e.tensor_tensor(out=Out[:, c4:c4 + 4, :], in0=py[0:64, :].rearrange("p (c f) -> p c f", c=4), in1=Xmul[:, c4:c4 + 4, :], op=ALU.mult),
                              r=[pyk, xkey, okey], w=[okey])

                for g in range(32):
                    gc0 = g * 16
                    lh = lambda r0: HYC[r0 + gc0:r0 + gc0 + 16, :].rearrange("c (n1 n2) -> n1 c n2", n2=128)
                    kb.dma('sp', lambda e: e.dma_start(out=v16[:], in_=lh(0)), r=['HYC'], w=['v16'])
                    kb.dma('sp', lambda e: e.dma_start(out=x116[:], in_=lh(512)), r=['HYC'], w=['x116'])
                    kb.dma('sp', lambda e: e.dma_start(out=x216[:], in_=lh(1024)), r=['HYC'], w=['x216'])
                    kb.dma('sp', lambda e: e.dma_start(out=H1[:], in_=HSPEC[gc0:gc0 + 16].rearrange("c k r f -> k c r f")), r=['HSPEC'], w=['H1'])
                    kb.dma('sp', lambda e: e.dma_start(out=H2s[:], in_=HSPEC[512 + gc0:512 + gc0 + 16].rearrange("c k r f -> k c r f")), r=['HSPEC'], w=['H2s'])
                    conv16(v16, 'v16', H1, 'H1', x116, 'x116', z16, 'z16')
                    conv16(z16, 'z16', H2s, 'H2s', x216, 'x216', y16, 'y16')
                    kb.dma('pool', lambda e: e.dma_start(out=YHYT[gc0:gc0 + 16, :].rearrange("c (n1 n2) -> n1 c n2", n2=128), in_=y16[:]), r=['y16'], w=['YHYT'])

        if stages >= 5:
            with stage() as st:
                z = sbt(st, "zt", [128, L])
                kb.op('pool', lambda e: e.memset(z[:], 0.0), w=['zt'])
                for q in range(4):
                    if dummy_mix:
                        kb.dma('sp', lambda e: e.dma_start(out=z[:], in_=QT[q * 128:(q + 1) * 128, :]), r=['zt', 'QT'], w=['zt'])
                    if dummy_mix:
                        kb.dma('sp', lambda e: e.dma_start(out=YHYT[q * 128:(q + 1) * 128, :], in_=z[:]), r=['zt'], w=['YHYT'])
                    if dummy_mix:
                        kb.dma('sp', lambda e: e.dma_start(out=z[:], in_=GT[q * 128:(q + 1) * 128, :]), r=['zt', 'GT'], w=['zt'])
                    if dummy_mix:
                        kb.dma('sp', lambda e: e.dma_start(out=YHGT[q * 128:(q + 1) * 128, :], in_=z[:]), r=['zt'], w=['YHGT'])
                zr = sbt(st, "zr", [128, D])
                kb.op('pool', lambda e: e.memset(zr[:], 0.0), w=['zr'])
                if dummy_mix or stages < 7:
                    for i in range(OWN // 128):
                        kb.dma('sp', lambda e: e.dma_start(out=ROUTED[i * 128:(i + 1) * 128, :], in_=zr[:]), r=['zr'], w=['ROUTED'])

        def row_bcast(stack, name, src_row_ap):
            t = sbt(stack, name, [128, D])
            kb.dma('sp', lambda e: e.dma_start(out=t[:], in_=src_row_ap.to_broadcast([128, D])), r=['MODROW'], w=[name])
            return t

        if stages >= 5:
            with stage() as st:
                oidx = sbt(st, "oidx", [128, 4], I32)
                kb.dma('sp', lambda e: e.dma_start(out=oidx[:], in_=OWNIDX), w=['oidx'])
                yg = [sbt(st, "yg%d" % i, [128, OWN]) for i in range(2)]
                n = 0
                for src, skey, r0 in ((YHYT, 'YHYT', 0), (YHGT, 'YHGT', 512)):
                    v = src.rearrange("c (j t) -> (c j) t", j=4)
                    for cc in range(4):
                        g = yg[n % 2]; gk = "yg%d" % (n % 2); n += 1
                        kb.dma('pool', lambda e: e.indirect_dma_start(out=g[:], out_offset=None, in_=v,
                                                                     in_offset=bass.IndirectOffsetOnAxis(ap=oidx[:, cc:cc + 1], axis=0),
                                                                     bounds_check=RB_OWN, oob_is_err=False), r=[skey, 'oidx'], w=[gk])
                        kb.dma('sp', lambda e: e.dma_start(out=YOWN[r0 + cc * 128:r0 + (cc + 1) * 128, :], in_=g[:]), r=[gk], w=['YOWN'])
            with stage() as st:
                Wy = sbt(st, "Wy", [128, 8, D]); Wo = sbt(st, "Wo", [128, 8, D])
                kb.dma('sp', lambda e: e.dma_start(out=Wy[:, 0:4, :], in_=WHY.rearrange("(k p) c -> p k c", p=128)), w=['Wy'])
                kb.dma('sp', lambda e: e.dma_start(out=Wy[:, 4:8, :], in_=WHG.rearrange("(k p) c -> p k c", p=128)), r=['Wy'], w=['Wy'])
                kb.dma('sp', lambda e: e.dma_start(out=Wo[:], in_=WOUT.rearrange("(k p) c -> p k c", p=128)), w=['Wo'])
                g1row = row_bcast(st, "g1row", MODROW[0:1, 2048:3072])
                yb = sbt(st, "yb", [128, 8, 512]); sgb = sbt(st, "sgb", [128, 16, 512]); mT = sbt(st, "mT", [128, 8, 512])
                t1 = [sbt(st, "t1_%d" % i, [128, 512]) for i in range(2)]
                xt = [sbt(st, "xt%d" % i, [128, D]) for i in range(2)]; pt = [sbt(st, "pt%d" % i, [128, D]) for i in range(2)]
                bi = 0
                for blk in range(OWN // 512):
                    t0 = blk * 512
                    kb.dma('sp', lambda e: e.dma_start(out=yb[:], in_=YOWN[:, t0:t0 + 512].rearrange("(k p) t -> p k t", p=128)), r=['YOWN'], w=['yb'])
                    kb.dma('sp', lambda e: e.dma_start(out=sgb[:], in_=SGT[:, t0:t0 + 512].rearrange("(k p) t -> p k t", p=128)), r=['SGT'], w=['sgb'])
                    for dm in range(8):
                        for br in range(2):
                            pb, pbk = banks[bi % 8]; bi += 1
                            for cc in range(4):
                                kb.op('pe', lambda e: e.matmul(pb[:, :], lhsT=Wy[:, br * 4 + cc, dm * 128:(dm + 1) * 128], rhs=yb[:, br * 4 + cc, :],
                                                               start=(cc == 0), stop=(cc == 3)), r=['Wy', 'yb'], w=[pbk])
                            if br == 0:
                                kb.op('dve', lambda e: e.tensor_tensor(out=t1[dm % 2][:], in0=pb[:, :], in1=sgb[:, dm, :], op=ALU.mult),
                                      r=[pbk, 'sgb'], w=['t1_%d' % (dm % 2)])
                            else:
                                kb.op('dve', lambda e: e.tensor_tensor(out=mT[:, dm, :], in0=pb[:, :], in1=sgb[:, 8 + dm, :], op=ALU.mult),
                                      r=[pbk, 'sgb', 'mT'], w=['mT'])
                                kb.op('pool', lambda e: e.tensor_tensor(out=mT[:, dm, :], in0=mT[:, dm, :], in1=t1[dm % 2][:], op=ALU.add),
                                      r=['mT', 't1_%d' % (dm % 2)], w=['mT'])
                    for tt in range(4):
                        ti = blk * 4 + tt; b2 = ti % 2
                        kb.dma('sp', lambda e: e.dma_start(out=xt[b2][:], in_=XOWN[ti * 128:(ti + 1) * 128, :]), w=['xt%d' % b2])
                        kb.dma('sp', lambda e: e.dma_start(out=pt[b2][:], in_=POSOWN[ti * 128:(ti + 1) * 128, :]), w=['pt%d' % b2])
                        kb.op('pool', lambda e: e.tensor_tensor(out=xt[b2][:], in0=xt[b2][:], in1=pt[b2][:], op=ALU.add), r=['xt%d' % b2, 'pt%d' % b2], w=['xt%d' % b2])
                        for hf in range(2):
                            pb, pbk = banks[bi % 8]; bi += 1
                            for kk in range(8):
                                kb.op('pe', lambda e: e.matmul(pb[:, :], lhsT=mT[:, kk, tt * 128:(tt + 1) * 128], rhs=Wo[:, kk, hf * 512:(hf + 1) * 512],
                                                               start=(kk == 0), stop=(kk == 7)), r=['mT', 'Wo'], w=[pbk])
                            kb.op('dve', lambda e: e.tensor_tensor(out=pt[b2][:, hf * 512:(hf + 1) * 512], in0=pb[:, :], in1=g1row[:, hf * 512:(hf + 1) * 512], op=ALU.mult),
                                  r=[pbk, 'g1row', 'pt%d' % b2], w=['pt%d' % b2])
                        kb.op('pool', lambda e: e.tensor_tensor(out=xt[b2][:], in0=xt[b2][:], in1=pt[b2][:], op=ALU.add), r=['xt%d' % b2, 'pt%d' % b2], w=['xt%d' % b2])
                        kb.dma('pool', lambda e: e.dma_start(out=X1D[ti * 128:(ti + 1) * 128, :], in_=xt[b2][:]), r=['xt%d' % b2], w=['X1D'])

        if stages >= 6:
            a2T = sbt(es, "a2T", [128, 8]); sh2T = sbt(es, "sh2T", [128, 8])
            kb.op('dve', lambda e: e.tensor_scalar(out=a2T[:], in0=modT[:, 32:40, 0], scalar1=1.0, scalar2=None, op0=ALU.add), r=['modT'], w=['a2T'])
            kb.op('dve', lambda e: e.tensor_tensor(out=a2T[:], in0=a2T[:], in1=g2T[:], op=ALU.mult), r=['a2T', 'g2T'], w=['a2T'])
            kb.op('dve', lambda e: e.tensor_copy(out=sh2T[:], in_=modT[:, 24:32, 0]), r=['modT'], w=['sh2T'])
            with stage() as s1:
                srcs = [(X1D[i * 128:(i + 1) * 128, :], None, i * 128) for i in range(OWN // 128)]
                kb.res.setdefault('a1T', [None, []])
                norm_to_xt(s1, srcs, H2T, a2T, sh2T, "n2", 'H2T')
            own_blocks = [(t, 512) for t in range(0, OWN, 512)]
            gemm_group("sg", SHG, 8, 256, H2T, 'H2T', own_blocks, [dict(c0=0, cn=256, mode='fm', func=AF.Silu, out=SGA, okey='SGA', oc0=0)])
            gemm_group("su", SHU, 8, 256, H2T, 'H2T', own_blocks, [dict(c0=0, cn=256, mode='fm', func=None, out=SUA, okey='SUA', oc0=0)])
            with stage() as st:
                ga = sbt(st, "ga", [128, 2, OWN]); ua = sbt(st, "ua", [128, 2, OWN])
                kb.dma('sp', lambda e: e.dma_start(out=ga[:], in_=SGA.rearrange("(k p) t -> p k t", p=128)), r=['SGA'], w=['ga'])
                kb.dma('sp', lambda e: e.dma_start(out=ua[:], in_=SUA.rearrange("(k p) t -> p k t", p=128)), r=['SUA'], w=['ua'])
                kb.op('dve', lambda e: e.tensor_tensor(out=ga[:], in0=ga[:], in1=ua[:], op=ALU.mult), r=['ga', 'ua'], w=['ga'])
                kb.dma('pool', lambda e: e.dma_start(out=ACTT.rearrange("(k p) t -> p k t", p=128), in_=ga[:]), r=['ga'], w=['ACTT'])
            gemm_group("sd", SHD, 2, 1024, ACTT, 'ACTT', own_blocks,
                       [dict(c0=0, cn=512, mode='tm', func=None, out=SHOUT, okey='SHOUT', oc0=0),
                        dict(c0=512, cn=512, mode='tm', func=None, out=SHOUT, okey='SHOUT', oc0=512)])
            if stages >= 7 and not dummy_mix:
                with stage() as st:
                    a2row = sbt(st, "a2row", [128, D]); g2nrow = sbt(st, "g2nrow", [128, D])
                    kb.dma('sp', lambda e: e.dma_start(out=a2row[:], in_=MODROW[0:1, 4096:5120].to_broadcast([128, D])), r=['MODROW'], w=['a2row'])
                    kb.dma('sp', lambda e: e.dma_start(out=g2nrow[:], in_=N2G.rearrange("(o n) -> o n", o=1).to_broadcast([128, D])), w=['g2nrow'])
                    kb.op('dve', lambda e: e.scalar_tensor_tensor(out=a2row[:], in0=a2row[:], scalar=1.0, in1=g2nrow[:], op0=ALU.add, op1=ALU.mult),
                          r=['a2row', 'g2nrow'], w=['a2row'])
                    sh2row = row_bcast(st, "sh2row", MODROW[0:1, 3072:4096])
                    xa = [sbt(st, "hxa%d" % i, [128, D]) for i in range(2)]; stt = [sbt(st, "hst%d" % i, [128, 4]) for i in range(2)]
                    junk = sbt(st, "hjunk", [128, D])
                    for ti in range(OWN // 128):
                        b2 = ti % 2; xk, tk = 'hxa%d' % b2, 'hst%d' % b2
                        rows = slice(ti * 128, (ti + 1) * 128)
                        kb.dma('sp', lambda e: e.dma_start(out=xa[b2][:], in_=X1D[rows, :]), r=['X1D'], w=[xk])
                        kb.op('act', lambda e: e.activation(out=junk[:], in_=xa[b2][:], func=AF.Square, accum_out=stt[b2][:, 0:1]), r=[xk], w=['hjunk', tk])
                        kb.op('dve', lambda e: e.tensor_scalar(out=stt[b2][:, 1:2], in0=stt[b2][:, 0:1], scalar1=1.0 / D, scalar2=EPS, op0=ALU.mult, op1=ALU.add), r=[tk], w=[tk])
                        kb.op('act', lambda e: e.activation(out=stt[b2][:, 2:3], in_=stt[b2][:, 1:2], func=AF.Sqrt), r=[tk], w=[tk])
                        kb.op('dve', lambda e: e.reciprocal(out=stt[b2][:, 3:4], in_=stt[b2][:, 2:3]), r=[tk], w=[tk])
                        kb.op('dve', lambda e: e.scalar_tensor_tensor(out=xa[b2][:], in0=xa[b2][:], scalar=stt[b2][:, 3:4], in1=a2row[:], op0=ALU.mult, op1=ALU.mult),
                              r=[xk, tk, 'a2row'], w=[xk])
                        kb.op('pool', lambda e: e.tensor_tensor(out=xa[b2][:], in0=xa[b2][:], in1=sh2row[:], op=ALU.add), r=[xk, 'sh2row'], w=[xk])
                        kb.dma('pool', lambda e: e.dma_start(out=H2[rows, :], in_=xa[b2][:]), r=[xk], w=['H2'])
                gemm_group("rt", RW, 8, NE, H2T, 'H2T', own_blocks, [dict(c0=0, cn=NE, mode='tm', func=AF.Sigmoid, out=SCORES, okey='SCORES', oc0=0)])
                with stage() as st:
                    NT = OWN // 128
                    onesm = sbt(st, "onesm", [128, 128]); stri = sbt(st, "stri", [128, 128]); slt = sbt(st, "slt", [128, 512])
                    blk128 = sbt(st, "blk128", [128, NBLK]); pidx = sbt(st, "pidx", [128, 1]); brow_ = sbt(st, "rbrow", [128, NE])
                    kb.dma('sp', lambda e: e.dma_start(out=onesm[:], in_=ONESd), w=['onesm'])
                    kb.dma('sp', lambda e: e.dma_start(out=stri[:], in_=STRId), w=['stri'])
                    kb.dma('sp', lambda e: e.dma_start(out=slt[:], in_=SLTd), w=['slt'])
                    kb.dma('sp', lambda e: e.dma_start(out=blk128[:], in_=BLKd), w=['blk128'])
                    kb.dma('sp', lambda e: e.dma_start(out=pidx[:], in_=PIDXd), w=['pidx'])
                    kb.dma('sp', lambda e: e.dma_start(out=brow_[:], in_=RB.rearrange("(o n) -> o n", o=1).to_broadcast([128, NE])), w=['rbrow'])
                    D8F = sbt(st, "D8F", [128, NT, 8]); W8 = sbt(st, "W8", [128, NT, 8]); D8I = sbt(st, "D8I", [128, NT * 8], I32); GI = sbt(st, "GI", [128, NBLK], I32)
                    with stage() as sr:
                        MSK = sbt(sr, "MSK", [128, NT, NE]); SEL = sbt(sr, "SEL", [128, NT, NE]); WD = sbt(sr, "WDm", [128, NT, NE]); DST = sbt(sr, "DST", [128, NT, NE])
                        V8 = sbt(sr, "V8", [128, NT, 8])
                        sc_ = [sbt(sr, "rsc%d" % i, [128, NE]) for i in range(2)]; bs = sbt(sr, "rbs", [128, NE])
                        M8 = sbt(sr, "M8", [128, 8, 8]); gs = sbt(sr, "rgs", [128, 8]); g8 = sbt(sr, "rg8", [128, 8]); gm = sbt(sr, "rgm", [128, 8]); pen = sbt(sr, "rpen", [128, 8])
                        den = sbt(sr, "rden", [128, 2]); base = sbt(sr, "rbase", [128, NE]); tmpq = sbt(sr, "rtmpq", [128, NE])
                        kb.op('pool', lambda e: e.memset(base[:], 0.0), w=['rbase'])
                        for ti in range(NT):
                            b2 = ti % 2; sk_ = 'rsc%d' % b2
                            kb.dma('sp', lambda e: e.dma_start(out=sc_[b2][:], in_=SCORES[ti * 128:(ti + 1) * 128, :]), r=['SCORES'], w=[sk_])
                            kb.op('dve', lambda e: e.tensor_tensor(out=bs[:], in0=sc_[b2][:], in1=brow_[:], op=ALU.add), r=[sk_, 'rbrow'], w=['rbs'])
                            for g in range(8):
                                kb.op('dve', lambda e: e.max(out=M8[:, g, :], in_=bs[:, 32 * g:32 * g + 32]), r=['rbs', 'M8'], w=['M8'])
                            kb.op('dve', lambda e: e.tensor_tensor(out=gs[:], in0=M8[:, :, 0], in1=M8[:, :, 1], op=ALU.add), r=['M8'], w=['rgs'])
                            kb.op('dve', lambda e: e.max(out=g8[:], in_=gs[:]), r=['rgs'], w=['rg8'])
                            kb.op('dve', lambda e: e.tensor_scalar(out=gm[:], in0=gs[:], scalar1=g8[:, 3:4], scalar2=None, op0=ALU.is_ge), r=['rgs', 'rg8'], w=['rgm'])
                            kb.op('dve', lambda e: e.tensor_scalar(out=pen[:], in0=gm[:], scalar1=-1.0, scalar2=1e30, op0=ALU.add, op1=ALU.mult), r=['rgm'], w=['rpen'])
                            for g in range(8):
                                kb.op('dve', lambda e: e.tensor_scalar(out=MSK[:, ti, 32 * g:32 * g + 32], in0=bs[:, 32 * g:32 * g + 32], scalar1=gm[:, g:g + 1], scalar2=pen[:, g:g + 1],
                                                                       op0=ALU.mult, op1=ALU.add), r=['rbs', 'rgm', 'rpen', 'MSK'], w=['MSK'])
                            kb.op('dve', lambda e: e.max(out=V8[:, ti, :], in_=MSK[:, ti, :]), r=['MSK', 'V8'], w=['V8'])
                            kb.op('dve', lambda e: e.tensor_scalar(out=SEL[:, ti, :], in0=MSK[:, ti, :], scalar1=V8[:, ti, 7:8], scalar2=None, op0=ALU.is_ge), r=['MSK', 'V8', 'SEL'], w=['SEL'])
                            kb.op('dve', lambda e: e.tensor_tensor(out=WD[:, ti, :], in0=SEL[:, ti, :], in1=sc_[b2][:], op=ALU.mult), r=['SEL', sk_, 'WDm'], w=['WDm'])
                            kb.op('dve', lambda e: e.tensor_reduce(out=den[:, 0:1], in_=WD[:, ti, :], axis=AX.X, op=ALU.add), r=['WDm', 'rden'], w=['rden'])
                            kb.op('dve', lambda e: e.reciprocal(out=den[:, 1:2], in_=den[:, 0:1]), r=['rden'], w=['rden'])
                            kb.op('dve', lambda e: e.tensor_scalar(out=WD[:, ti, :], in0=WD[:, ti, :], scalar1=den[:, 1:2], scalar2=2.5, op0=ALU.mult, op1=ALU.mult), r=['WDm', 'rden'], w=['WDm'])
                            p1, p1k = banks[(2 * ti) % 8]; p2, p2k = banks[(2 * ti + 1) % 8]
                            kb.op('pe', lambda e: e.matmul(p1[:, 0:NE], lhsT=stri[:], rhs=SEL[:, ti, :], start=True, stop=True), r=['stri', 'SEL'], w=[p1k])
                            kb.op('pe', lambda e: e.matmul(p2[:, 0:NE], lhsT=onesm[:], rhs=SEL[:, ti, :], start=True, stop=True), r=['onesm', 'SEL'], w=[p2k])
                            kb.op('dve', lambda e: e.tensor_tensor(out=DST[:, ti, :], in0=p1[:, 0:NE], in1=base[:], op=ALU.add), r=[p1k, 'rbase', 'DST'], w=['DST'])
                            kb.op('dve', lambda e: e.tensor_tensor(out=base[:], in0=p2[:, 0:NE], in1=base[:], op=ALU.add), r=[p2k, 'rbase'], w=['rbase'])
                        ci = sbt(sr, "rci", [128, NE], I32); padded = sbt(sr, "rpad", [128, NE]); pstart = sbt(sr, "rpst", [128, NE]); pend = sbt(sr, "rpend", [128, NE])
                        kb.op('dve', lambda e: e.tensor_scalar(out=tmpq[:], in0=base[:], scalar1=127.0, scalar2=None, op0=ALU.add), r=['rbase'], w=['rtmpq'])
                        kb.op('dve', lambda e: e.tensor_copy(out=ci[:], in_=tmpq[:]), r=['rtmpq'], w=['rci'])
                        kb.op('dve', lambda e: e.tensor_scalar(out=ci[:], in0=ci[:], scalar1=7, scalar2=None, op0=ALU.arith_shift_right), r=['rci'], w=['rci'])
                        kb.op('dve', lambda e: e.tensor_scalar(out=ci[:], in0=ci[:], scalar1=7, scalar2=None, op0=ALU.logical_shift_left), r=['rci'], w=['rci'])
                        kb.op('dve', lambda e: e.tensor_copy(out=padded[:], in_=ci[:]), r=['rci'], w=['rpad'])
                        padT = sbt(sr, "rpadT", [128, 2, 128]); pendT = sbt(sr, "rpendT", [128, 2, 128])
                        pa, pak = banks[0]
                        for hh in range(2):
                            kb.op('pe', lambda e: e.transpose(out=pa[:, hh * 128:(hh + 1) * 128], in_=padded[:, hh * 128:(hh + 1) * 128], identity=ident[:]), r=['rpad', 'ident'], w=[pak])
                        kb.op('dve', lambda e: e.tensor_copy(out=padT[:], in_=pa[:, 0:256].rearrange("p (h c) -> p h c", h=2)), r=[pak], w=['rpadT'])
                        pb_, pbk_ = banks[1]
                        for hh in range(2):
                            kb.op('pe', lambda e: e.matmul(pb_[:, 0:NE], lhsT=padT[:, hh, :], rhs=slt[:, hh * 256:(hh + 1) * 256], start=(hh == 0), stop=(hh == 1)), r=['rpadT', 'slt'], w=[pbk_])
                        kb.op('dve', lambda e: e.tensor_copy(out=pstart[:], in_=pb_[:, 0:NE]), r=[pbk_], w=['rpst'])
                        kb.op('dve', lambda e: e.tensor_tensor(out=pend[:], in0=pstart[:], in1=padded[:], op=ALU.add), r=['rpst', 'rpad'], w=['rpend'])
                        pc_, pck_ = banks[2]
                        for hh in range(2):
                            kb.op('pe', lambda e: e.transpose(out=pc_[:, hh * 128:(hh + 1) * 128], in_=pend[:, hh * 128:(hh + 1) * 128], identity=ident[:]), r=['rpend', 'ident'], w=[pck_])
                        kb.op('dve', lambda e: e.tensor_copy(out=pendT[:], in_=pc_[:, 0:256].rearrange("p (h c) -> p h c", h=2)), r=[pck_], w=['rpendT'])
                        cmpT = sbt(sr, "rcmpT", [128, 2, NBLK]); bef = sbt(sr, "rbef", [128, NBLK])
                        for hh in range(2):
                            kb.op('dve', lambda e: e.tensor_scalar(out=cmpT[:, hh, :], in0=blk128[:], scalar1=pendT[:, hh, 0:1], scalar2=None, op0=ALU.is_ge), r=['blk128', 'rpendT', 'rcmpT'], w=['rcmpT'])
                        pd_, pdk_ = banks[3]
                        for hh in range(2):
                            kb.op('pe', lambda e: e.matmul(pd_[:, 0:NBLK], lhsT=onesm[:], rhs=cmpT[:, hh, :], start=(hh == 0), stop=(hh == 1)), r=['onesm', 'rcmpT'], w=[pdk_])
                        kb.op('dve', lambda e: e.tensor_scalar(out=bef[:], in0=pd_[:, 0:NBLK], scalar1=255.0, scalar2=128.0, op0=ALU.min, op1=ALU.mult), r=[pdk_], w=['rbef'])
                        kb.op('dve', lambda e: e.tensor_scalar(out=bef[:], in0=bef[:], scalar1=pidx[:, 0:1], scalar2=None, op0=ALU.add), r=['rbef', 'pidx'], w=['rbef'])
                        kb.op('dve', lambda e: e.tensor_copy(out=GI[:], in_=bef[:]), r=['rbef'], w=['GI'])
                        eqj = sbt(sr, "reqj", [128, NE])
                        for ti in range(NT):
                            kb.op('dve', lambda e: e.tensor_tensor(out=DST[:, ti, :], in0=DST[:, ti, :], in1=pstart[:], op=ALU.add), r=['DST', 'rpst'], w=['DST'])
                            for k8 in range(8):
                                kb.op('dve', lambda e: e.scalar_tensor_tensor(out=eqj[:], in0=MSK[:, ti, :], scalar=V8[:, ti, k8:k8 + 1], in1=DST[:, ti, :], op0=ALU.is_equal, op1=ALU.mult),
                                      r=['MSK', 'V8', 'DST', 'reqj'], w=['reqj'])
                                kb.op('dve', lambda e: e.tensor_reduce(out=D8F[:, ti, k8:k8 + 1], in_=eqj[:], axis=AX.X, op=ALU.add), r=['reqj', 'D8F'], w=['D8F'])
                                kb.op('dve', lambda e: e.scalar_tensor_tensor(out=eqj[:], in0=MSK[:, ti, :], scalar=V8[:, ti, k8:k8 + 1], in1=WD[:, ti, :], op0=ALU.is_equal, op1=ALU.mult),
                                      r=['MSK', 'V8', 'WDm', 'reqj'], w=['reqj'])
                                kb.op('dve', lambda e: e.tensor_reduce(out=W8[:, ti, k8:k8 + 1], in_=eqj[:], axis=AX.X, op=ALU.add), r=['reqj', 'W8'], w=['W8'])
                        kb.op('dve', lambda e: e.tensor_copy(out=D8I[:], in_=D8F[:].rearrange("p t k -> p (t k)")), r=['D8F'], w=['D8I'])
                    dbg('D8F', D8F[:], [128, NT, 8]); dbg('W8', W8[:], [128, NT, 8])
                    ht = [sbt(st, "dht%d" % i, [128, D]) for i in range(2)]
                    for ti in range(NT):
                        b2 = ti % 2; hk = 'dht%d' % b2
                        kb.dma('sp', lambda e: e.dma_start(out=ht[b2][:], in_=H2[ti * 128:(ti + 1) * 128, :]), r=['H2'], w=[hk])
                        for k8 in range(8):
                            kb.dma('pool', lambda e: e.indirect_dma_start(out=XS, out_offset=bass.IndirectOffsetOnAxis(ap=D8I[:, ti * 8 + k8:ti * 8 + k8 + 1], axis=0), in_=ht[b2][:], in_offset=None,
                                                                         bounds_check=RB_XS, oob_is_err=False), r=[hk, 'D8I'], w=['XSw'])
                    kb.barrier()
                    NW = 3
                    wgu = [sbt(st, "wgu%d" % i, [128, 2, 8, 256]) for i in range(NW)]; wdn = [sbt(st, "wdn%d" % i, [128, 2, D]) for i in range(4)]
                    xs = [sbt(st, "xs%d" % i, [128, D]) for i in range(2)]; xsT = [sbt(st, "xsT%d" % i, [128, 8, 128]) for i in range(3)]
                    actT = [sbt(st, "actT%d" % i, [128, 2, 128]) for i in range(3)]; sg_ = [sbt(st, "esg%d" % i, [128, 256]) for i in range(3)]
                    ys = [sbt(st, "ys%d" % i, [128, D]) for i in range(2)]
                    bctr = [0]

                    def bk():
                        b_ = banks[bctr[0] % 8]; bctr[0] += 1
                        return b_

                    def phA(blk):
                        wk, dk, xk, xtk = 'wgu%d' % (blk % NW), 'wdn%d' % (blk % 4), 'xs%d' % (blk % 2), 'xsT%d' % (blk % 3)
                        kb.dma('pool', lambda e: e.indirect_dma_start(out=wgu[blk % NW][:].rearrange("p a k f -> p (a k f)"), out_offset=None, in_=EWGU,
                                                                     in_offset=bass.IndirectOffsetOnAxis(ap=GI[:, blk:blk + 1], axis=0),
                                                                     bounds_check=RB_W, oob_is_err=False), r=['GI'], w=[wk])
                        kb.dma('pool', lambda e: e.indirect_dma_start(out=wdn[blk % 4][:].rearrange("p k f -> p (k f)"), out_offset=None, in_=EWD,
                                                                     in_offset=bass.IndirectOffsetOnAxis(ap=GI[:, blk:blk + 1], axis=0),
                                                                     bounds_check=RB_W, oob_is_err=False), r=['GI'], w=[dk])
                        kb.dma('sp', lambda e: e.dma_start(out=xs[blk % 2][:], in_=XS[blk * 128:(blk + 1) * 128, :]), r=['XSw'], w=[xk])
                        for hh in range(2):
                            pb, pbk = bk()
                            for kk in range(4):
                                k8 = hh * 4 + kk
                                kb.op('pe', lambda e: e.transpose(out=pb[:, kk * 128:(kk + 1) * 128], in_=xs[blk % 2][:, k8 * 128:(k8 + 1) * 128], identity=ident[:]), r=[xk, 'ident'], w=[pbk])
                            evac(xsT[blk % 3][:, hh * 4:hh * 4 + 4, :], pb[:, :].rearrange("p (k t) -> p k t", k=4), None, [pbk, xtk], [xtk])

                    def phB(blk):
                        wk, xtk, sgk = 'wgu%d' % (blk % NW), 'xsT%d' % (blk % 3), 'esg%d' % (blk % 3)
                        ph, phk = bk()
                        for kk in range(8):
                            kb.op('pe', lambda e: e.matmul(ph[:, :].rearrange("p (a f) -> p a f", a=2), lhsT=xsT[blk % 3][:, kk, :], rhs=wgu[blk % NW][:, :, kk, :],
                                                           start=(kk == 0), stop=(kk == 7)), r=[wk, xtk], w=[phk])
                        kb.op('act', lambda e: e.activation(out=sg_[blk % 3][:], in_=ph[:, 0:256], func=AF.Silu), r=[phk], w=[sgk])
                        kb.op('dve', lambda e: e.tensor_tensor(out=sg_[blk % 3][:], in0=ph[:, 256:512], in1=sg_[blk % 3][:], op=ALU.mult), r=[phk, sgk], w=[sgk])

                    def phC(blk):
                        sgk, ak = 'esg%d' % (blk % 3), 'actT%d' % (blk % 3)
                        pt_, ptk = bk()
                        for kk in range(2):
                            kb.op('pe', lambda e: e.transpose(out=pt_[:, kk * 128:(kk + 1) * 128], in_=sg_[blk % 3][:, kk * 128:(kk + 1) * 128], identity=ident[:]), r=[sgk, 'ident'], w=[ptk])
                        evac(actT[blk % 3][:].rearrange("p k t -> p (k t)"), pt_[:, 0:256], None, [ptk, ak], [ak])

                    def phD(blk):
                        dk, ak, yk = 'wdn%d' % (blk % 4), 'actT%d' % (blk % 3), 'ys%d' % (blk % 2)
                        for hf in range(2):
                            py, pyk = bk()
                            for kk in range(2):
                                kb.op('pe', lambda e: e.matmul(py[:, :], lhsT=actT[blk % 3][:, kk, :], rhs=wdn[blk % 4][:, kk, hf * 512:(hf + 1) * 512], start=(kk == 0), stop=(kk == 1)), r=[ak, dk], w=[pyk])
                            evac(ys[blk % 2][:, hf * 512:(hf + 1) * 512], py[:, :], None, [pyk, yk], [yk])
                        kb.dma('sp', lambda e: e.dma_start(out=YS[blk * 128:(blk + 1) * 128, :], in_=ys[blk % 2][:]), r=[yk], w=['YSw'])

                    for s_ in range(NBLK + 3):
                        if s_ < NBLK:
                            phA(s_)
                        if 0 <= s_ - 1 < NBLK:
                            phB(s_ - 1)
                        if 0 <= s_ - 2 < NBLK:
                            phC(s_ - 2)
                        if 0 <= s_ - 3 < NBLK:
                            phD(s_ - 3)
                    kb.barrier()
                    acc = [sbt(st, "cacc%d" % i, [128, D]) for i in range(2)]; gg = [sbt(st, "cg%d" % i, [128, D]) for i in range(3)]
                    gi_ = 0
                    for ti in range(NT):
                        b2 = ti % 2; ack = 'cacc%d' % b2
                        for k8 in range(8):
                            g3 = gi_ % 3; gi_ += 1; ggk = 'cg%d' % g3
                            kb.dma('pool', lambda e: e.indirect_dma_start(out=gg[g3][:], out_offset=None, in_=YS, in_offset=bass.IndirectOffsetOnAxis(ap=D8I[:, ti * 8 + k8:ti * 8 + k8 + 1], axis=0),
                                                                         bounds_check=RB_XS, oob_is_err=False), r=['YSw', 'D8I'], w=[ggk])
                            if k8 == 0:
                                kb.op('dve', lambda e: e.tensor_scalar(out=acc[b2][:], in0=gg[g3][:], scalar1=W8[:, ti, 0:1], scalar2=None, op0=ALU.mult), r=[ggk, 'W8', ack], w=[ack])
                            else:
                                kb.op('dve', lambda e: e.scalar_tensor_tensor(out=acc[b2][:], in0=gg[g3][:], scalar=W8[:, ti, k8:k8 + 1], in1=acc[b2][:], op0=ALU.mult, op1=ALU.add),
                                      r=[ggk, 'W8', ack], w=[ack])
                        kb.dma('sp', lambda e: e.dma_start(out=ROUTED[ti * 128:(ti + 1) * 128, :], in_=acc[b2][:]), r=[ack], w=['ROUTED'])

            with stage() as st:
                g2row = row_bcast(st, "g2row", MODROW[0:1, 5120:6144])
                fgrow = sbt(st, "fgrow", [128, D])
                kb.dma('sp', lambda e: e.dma_start(out=fgrow[:], in_=FING.rearrange("(o n) -> o n", o=1).to_broadcast([128, D])), w=['fgrow'])
                xa = [sbt(st, "xa%d" % i, [128, D]) for i in range(2)]; sa = [sbt(st, "sa%d" % i, [128, D]) for i in range(2)]
                ra = [sbt(st, "ra%d" % i, [128, D]) for i in range(2)]; stt = [sbt(st, "stt%d" % i, [128, 4]) for i in range(2)]
                junk = sbt(st, "fjunk", [128, D])
                for ti in range(OWN // 128):
                    b2 = ti % 2; xk, sk, rk, tk = 'xa%d' % b2, 'sa%d' % b2, 'ra%d' % b2, 'stt%d' % b2
                    rows = slice(ti * 128, (ti + 1) * 128)
                    kb.dma('sp', lambda e: e.dma_start(out=xa[b2][:], in_=X1D[rows, :]), r=['X1D'], w=[xk])
                    kb.dma('sp', lambda e: e.dma_start(out=sa[b2][:], in_=SHOUT[rows, :]), r=['SHOUT'], w=[sk])
                    kb.dma('sp', lambda e: e.dma_start(out=ra[b2][:], in_=ROUTED[rows, :]), r=['ROUTED'], w=[rk])
                    kb.op('pool', lambda e: e.tensor_tensor(out=sa[b2][:], in0=sa[b2][:], in1=ra[b2][:], op=ALU.add), r=[sk, rk], w=[sk])
                    kb.op('dve', lambda e: e.tensor_tensor(out=sa[b2][:], in0=sa[b2][:], in1=g2row[:], op=ALU.mult), r=[sk, 'g2row'], w=[sk])
                    kb.op('pool', lambda e: e.tensor_tensor(out=xa[b2][:], in0=xa[b2][:], in1=sa[b2][:], op=ALU.add), r=[xk, sk], w=[xk])
                    kb.op('act', lambda e: e.activation(out=junk[:], in_=xa[b2][:], func=AF.Square, accum_out=stt[b2][:, 0:1]), r=[xk], w=['fjunk', tk])
                    kb.op('dve', lambda e: e.tensor_scalar(out=stt[b2][:, 1:2], in0=stt[b2][:, 0:1], scalar1=1.0 / D, scalar2=EPS, op0=ALU.mult, op1=ALU.add), r=[tk], w=[tk])
                    kb.op('act', lambda e: e.activation(out=stt[b2][:, 2:3], in_=stt[b2][:, 1:2], func=AF.Sqrt), r=[tk], w=[tk])
                    kb.op('dve', lambda e: e.reciprocal(out=stt[b2][:, 3:4], in_=stt[b2][:, 2:3]), r=[tk], w=[tk])
                    kb.op('dve', lambda e: e.scalar_tensor_tensor(out=xa[b2][:], in0=xa[b2][:], scalar=stt[b2][:, 3:4], in1=fgrow[:], op0=ALU.mult, op1=ALU.mult),
                          r=[xk, tk, 'fgrow'], w=[xk])
                    kb.dma('pool', lambda e: e.dma_start(out=OUT[rows, :], in_=xa[b2][:]), r=[xk], w=['OUT'])

        kb.finish('sp')
        kb.finish('pool')
        pg.ninstr = kb.ninstr
    return pg


_PROG = None


def make_in_maps(pg, inputs):
    hc = host_consts()
    sq = lambda a: np.ascontiguousarray(a[0])
    in_maps = []
    shared = {}
    if 'EWGU' in pg.ins:
        wg = np.asarray(inputs['exp_w_gate'])[0].reshape(NE, 8, 128, 256)
        wu = np.asarray(inputs['exp_w_up'])[0].reshape(NE, 8, 128, 256)
        ew = np.empty((NE, 128, 2, 8, 256), np.float32)
        ew[:, :, 0] = wg.transpose(0, 2, 1, 3); ew[:, :, 1] = wu.transpose(0, 2, 1, 3)
        shared['EWGU'] = ew.reshape(NE * 128, 4096)
        shared['EWD'] = np.ascontiguousarray(np.asarray(inputs['exp_w_down'])[0].reshape(NE, 2, 128, D).transpose(0, 2, 1, 3)).reshape(NE * 128, 2048)
    for c in range(8):
        b, j = c // 4, c % 4
        own = slice(j * OWN, (j + 1) * OWN)
        idx = ((np.arange(4)[None, :] * 128 + np.arange(128)[:, None]) * 4 + j).astype(np.int32)
        full = {
            'x': inputs['x'][b], 'ctx': inputs['ctx'][b], 'xown': inputs['x'][b, own], 'posown': hc['POS'][own],
            'c': inputs['c'][b], 'c_ctx': inputs['c_ctx'], 'final_g': inputs['final_g'], 'OWNIDX': idx,
            'hg_lb_logits': np.asarray(inputs['hg_lb_logits']).reshape(2, 1024),
        }
        full.update(shared)
        for k in pg.ins:
            if k not in full and k not in hc:
                full[k] = sq(inputs[k])
        full.update(hc)
        in_maps.append({k: np.ascontiguousarray(np.asarray(full[k])) for k in pg.ins})
    return in_maps


def kernel(**inputs):
    global _PROG
    if _PROG is None:
        _PROG = build()
    pg = _PROG
    in_maps = make_in_maps(pg, inputs)
    res = run_bass_kernel_spmd(pg.nc, in_maps, core_ids=list(range(8)))
    out = np.zeros((2, L, D), np.float32)
    for c in range(8):
        b, j = c // 4, c % 4
        out[b, j * OWN:(j + 1) * OWN] = res.results[c]['out']
    return out
```

```python
import math
import numpy as np
from contextlib import ExitStack, contextmanager
import concourse.bass as bass
import concourse.mybir as mybir
from concourse.bass_utils import run_bass_kernel_spmd

F32 = mybir.dt.float32
I32 = mybir.dt.int32
U32 = mybir.dt.uint32
ALU = mybir.AluOpType
AF = mybir.ActivationFunctionType
AX = mybir.AxisListType

N_DMA_SEMS = 24
D = 1024
L = 8192
NCTX = 256
LT = L + NCTX
OWN = 2048
NE = 256
NBLK = 383
EPS = 1e-6
TWO_PI = 2.0 * math.pi


class KB:
    def __init__(self, nc, es):
        self.nc = nc
        self.engs = {'pe': nc.tensor, 'act': nc.scalar, 'dve': nc.vector, 'pool': nc.gpsimd, 'sp': nc.sync}
        self.sems = {}
        self.cnt = {}
        for e in self.engs:
            self.sems[e] = es.enter_context(nc.semaphore("s_" + e))
            self.cnt[e] = 0
        for i in range(N_DMA_SEMS):
            self.sems['d%d' % i] = es.enter_context(nc.semaphore("s_d%d" % i))
            self.cnt['d%d' % i] = 0
        self.dnext = 0
        self.waited = {e: {} for e in self.engs}
        self.res = {}
        self.ninstr = 0

    def _need(self, eng, toks):
        best = {}
        for t in toks:
            if t is None:
                continue
            sk, v = t
            if sk == eng and eng == 'pe':
                continue
            if best.get(sk, 0) < v:
                best[sk] = v
        for sk, v in best.items():
            if self.waited[eng].get(sk, 0) >= v:
                continue
            self.engs[eng].wait_ge(self.sems[sk], v)
            self.waited[eng][sk] = v

    def _deps(self, r, w):
        toks = []
        for k in r:
            st = self.res.get(k)
            if st is not None:
                toks.append(st[0])
        for k in w:
            st = self.res.get(k)
            if st is not None:
                toks.append(st[0])
                toks.extend(st[1])
        return toks

    def _commit(self, tok, r, w):
        for k in r:
            st = self.res.setdefault(k, [None, []])
            st[1].append(tok)
            if len(st[1]) > 32:
                best = {}
                for sk, v in st[1]:
                    if best.get(sk, 0) < v:
                        best[sk] = v
                st[1] = list(best.items())
        for k in w:
            self.res[k] = [tok, []]

    def op(self, eng, fn, r=(), w=()):
        self._need(eng, self._deps(r, w))
        ins = fn(self.engs[eng])
        self.cnt[eng] += 1
        ins.then_inc(self.sems[eng], 1)
        self._commit((eng, self.cnt[eng]), r, w)
        self.ninstr += 1

    def dma(self, q, fn, r=(), w=()):
        i = self.dnext
        self.dnext = (self.dnext + 1) % N_DMA_SEMS
        sk = 'd%d' % i
        toks = self._deps(r, w)
        if self.cnt[sk] > 0:
            toks.append((sk, self.cnt[sk]))
        self._need(q, toks)
        ins = fn(self.engs[q])
        self.cnt[sk] += 16
        ins.then_inc(self.sems[sk], 16)
        self._commit((sk, self.cnt[sk]), r, w)
        self.ninstr += 1

    def barrier(self):
        toks = [(sk, v) for sk, v in self.cnt.items() if v > 0]
        for e in self.engs:
            self._need(e, toks)

    def finish(self, eng):
        toks = []
        for st in self.res.values():
            toks.append(st[0])
            toks.extend(st[1])
        self._need(eng, toks)


_CONST = None


def host_consts():
    global _CONST
    if _CONST is not None:
        return _CONST
    c = {}
    quarter = D // 4
    omega = (1.0 / (np.float32(10000.0) ** (np.arange(quarter, dtype=np.float32) / np.float32(quarter)))).astype(np.float32)
    rows, cols = L // 64, 64
    ang_r = (np.arange(rows, dtype=np.float32)[:, None] * omega).astype(np.float32)
    ang_c = (np.arange(cols, dtype=np.float32)[:, None] * omega).astype(np.float32)
    emb_r = np.concatenate([np.sin(ang_r), np.cos(ang_r)], -1)
    emb_c = np.concatenate([np.sin(ang_c), np.cos(ang_c)], -1)
    emb = np.concatenate([np.broadcast_to(emb_r[:, None], (rows, cols, D // 2)),
                          np.broadcast_to(emb_c[None], (rows, cols, D // 2))], -1)
    c['POS'] = np.ascontiguousarray(emb.reshape(L, D).astype(np.float32))
    c['IDENT'] = np.eye(128, dtype=np.float32)
    si = np.arange(128)[:, None]; ti = np.arange(128)[None, :]
    same = (si // 64) == (ti // 64)
    c['TRIF'] = (same & (si <= ti)).astype(np.float32)
    c['TRIB'] = (same & (si >= ti)).astype(np.float32)
    c['ONES'] = np.ones((128, 128), np.float32)
    c['STRI'] = (si < ti).astype(np.float32)
    e1 = np.arange(128)[:, None]; e2 = np.arange(256)[None, :]
    c['SLT'] = np.concatenate([(e1 < e2), (e1 + 128 < e2)], 1).astype(np.float32)
    c['BLK128'] = np.broadcast_to((np.arange(NBLK, dtype=np.float32) * 128.0)[None, :], (128, NBLK)).copy()
    c['PIDX'] = np.arange(128, dtype=np.float32).reshape(128, 1).copy()
    NN = 16384
    a = np.arange(128, dtype=np.float64)
    ang = 2.0 * np.pi * np.outer(a, a) / 128.0
    Fre = np.cos(ang); Fim = -np.sin(ang)
    f32 = lambda v: np.ascontiguousarray(v.astype(np.float32))
    c['FA'] = f32(np.concatenate([Fre, Fim], 1)); c['FAH'] = f32(np.concatenate([Fre, Fim], 1)[64:128])
    c['FRE'] = f32(Fre); c['FIM'] = f32(Fim); c['NFIM'] = f32(-Fim)
    c['CA'] = f32(np.concatenate([Fre, -Fim], 1)); c['CB'] = f32(np.concatenate([Fim, Fre], 1))
    c['FREN'] = f32(Fre[:, :64] / NN); c['FIMN'] = f32(Fim[:, :64] / NN)
    angT = 2.0 * np.pi * np.outer(a, a) / NN
    c['TRE2'] = f32(np.concatenate([np.cos(angT), np.cos(angT)], 1)); c['TIM2'] = f32(np.concatenate([-np.sin(angT), -np.sin(angT)], 1))
    bands = np.linspace(1e-4, 15.0, 16, dtype=np.float32)
    def zfeat(t):
        t = t.astype(np.float32)
        tn = (t / np.float32(L - 1)).astype(np.float32)
        an = (np.float32(2 * math.pi / L) * t[:, None] * bands).astype(np.float32)
        return np.concatenate([tn[:, None], np.cos(an), -np.sin(an)], -1).astype(np.float32), tn
    zf, tnf = zfeat(np.arange(L)); zb, tnb = zfeat(L - np.arange(L))
    c['ZT'] = np.ascontiguousarray(np.concatenate([zf, zb], 1).T)
    lo_ = math.log(1e-2) / 1.5; hi_ = math.log(1e-2) / 0.3
    deltas = np.abs(np.linspace(lo_, hi_, 512, dtype=np.float32))
    decf = np.exp(-tnf[:, None] * deltas).astype(np.float32)
    decb = np.exp(-tnb[:, None] * deltas).astype(np.float32); decb[0] = 0.0
    c['DEC'] = np.ascontiguousarray(np.stack([decf.reshape(64, 128, 512).transpose(0, 2, 1), decb.reshape(64, 128, 512).transpose(0, 2, 1)]))
    _CONST = c
    return c


class Prog:
    def __init__(self, debug=None):
        self.debug = debug or ()
        self.nc = bass.Bass("TRN2", target_bir_lowering=False)
        self.ins = {}
        self.outs = {}

    def inp(self, name, shape, dt=F32):
        t = self.nc.dram_tensor(name, list(shape), dt, kind="ExternalInput").ap()
        self.ins[name] = t
        return t

    def scratch(self, name, shape, dt=F32):
        if name in self.debug:
            t = self.nc.dram_tensor(name, list(shape), dt, kind="ExternalOutput").ap()
            self.outs[name] = t
        else:
            t = self.nc.dram_tensor(name, list(shape), dt, kind="Internal").ap()
        return t


def build(stages=99, debug=None, dummy_mix=False):
    pg = Prog(debug)
    nc = pg.nc
    X = pg.inp("x", [L, D]); CTX = pg.inp("ctx", [NCTX, D]); XOWN = pg.inp("xown", [OWN, D]); POSOWN = pg.inp("posown", [OWN, D])
    CV = pg.inp("c", [D]); CCTX = pg.inp("c_ctx", [D])
    N1G = pg.inp("norm1_g", [D]); N2G = pg.inp("norm2_g", [D])
    ADAW = pg.inp("ada_w", [D, 6 * D]); ADAB = pg.inp("ada_b", [6 * D])
    WIN = pg.inp("w_in", [D, 6144])
    POS = pg.inp("POS", [L, D]); IDENT = pg.inp("IDENT", [128, 128])
    WHY = pg.inp("w_hy_out", [512, D]); WHG = pg.inp("w_hg_out", [512, D]); WOUT = pg.inp("w_out", [D, D])
    SHG = pg.inp("sh_w_gate", [D, 256]); SHU = pg.inp("sh_w_up", [D, 256]); SHD = pg.inp("sh_w_down", [256, D])
    FING = pg.inp("final_g", [D]); OWNIDX = pg.inp("OWNIDX", [128, 4], I32)
    CONVW = pg.inp("hy_conv_w", [3, 1536]); CONVB = pg.inp("hy_conv_b", [1536])
    TRIFd = pg.inp("TRIF", [128, 128]); TRIBd = pg.inp("TRIB", [128, 128]); ONESd = pg.inp("ONES", [128, 128])
    LBL = pg.inp("hg_lb_logits", [2, 1024]); HGNG = pg.inp("hg_norm_g", [128])
    STRId = pg.inp("STRI", [128, 128]); SLTd = pg.inp("SLT", [128, 512]); BLKd = pg.inp("BLK128", [128, NBLK]); PIDXd = pg.inp("PIDX", [128, 1])
    RW = pg.inp("router_w", [D, NE]); RB = pg.inp("router_bias", [NE])
    EWGU = pg.inp("EWGU", [NE * 128, 4096]); EWD = pg.inp("EWD", [NE * 128, 2048])
    FAd = pg.inp("FA", [128, 256]); FAHd = pg.inp("FAH", [64, 256]); FREd = pg.inp("FRE", [128, 128]); FIMd = pg.inp("FIM", [128, 128]); NFIMd = pg.inp("NFIM", [128, 128])
    CAd = pg.inp("CA", [128, 256]); CBd = pg.inp("CB", [128, 256]); FRENd = pg.inp("FREN", [128, 64]); FIMNd = pg.inp("FIMN", [128, 64])
    TRE2d = pg.inp("TRE2", [128, 256]); TIM2d = pg.inp("TIM2", [128, 256]); ZTd = pg.inp("ZT", [66, L]); DECd = pg.inp("DEC", [2, 64, 512, 128])
    FW1 = pg.inp("hy_f_w1", [33, 64]); FB1 = pg.inp("hy_f_b1", [64]); FW2 = pg.inp("hy_f_w2", [64, 64]); FB2 = pg.inp("hy_f_b2", [64])
    FW3 = pg.inp("hy_f_w3", [64, 64]); FB3 = pg.inp("hy_f_b3", [64]); FW4 = pg.inp("hy_f_w4", [64, 2048]); FFQ = pg.inp("hy_f_freq", [64])
    HSKIP = pg.inp("hy_skip", [1024])
    HSPEC = pg.scratch("HSPEC", [1024, 128, 2, 128])
    OUT = pg.nc.dram_tensor("out", [OWN, D], F32, kind="ExternalOutput").ap()
    pg.outs["out"] = OUT
    XNT = pg.scratch("XNT", [D, LT]); XNTOWN = pg.scratch("XNTOWN", [D, OWN])
    MODROW = pg.scratch("MODROW", [2, 6 * D])
    HYRAW = pg.scratch("HYRAW", [1536, L])
    QT = pg.scratch("QT", [512, L]); GT = pg.scratch("GT", [512, L])
    IFF = pg.scratch("IFF", [LT, 1536])
    SGT = pg.scratch("SGT", [2048, OWN])
    HYC = pg.scratch("HYC", [1536, L])
    YHYT = pg.scratch("YHYT", [512, L]); YHGT = pg.scratch("YHGT", [512, L])
    YOWN = pg.scratch("YOWN", [1024, OWN])
    X1D = pg.scratch("X1D", [OWN, D]); H2T = pg.scratch("H2T", [D, OWN])
    SGA = pg.scratch("SGA", [256, OWN]); SUA = pg.scratch("SUA", [256, OWN]); ACTT = pg.scratch("ACTT", [256, OWN])
    SHOUT = pg.scratch("SHOUT", [OWN, D]); ROUTED = pg.scratch("ROUTED", [OWN, D])
    H2 = pg.scratch("H2", [OWN, D]); SCORES = pg.scratch("SCORES", [OWN, NE])
    XS = pg.scratch("XS", [NBLK * 128, D]); YS = pg.scratch("YS", [NBLK * 128, D])

    with ExitStack() as es:
        kb = KB(nc, es)
        sbt = lambda stack, name, shape, dt=F32: stack.enter_context(nc.sbuf_tensor(name, list(shape), dt))
        PA = es.enter_context(nc.psum_tensor("PA", [128, 2048], F32))
        PB = es.enter_context(nc.psum_tensor("PB", [128, 2048], F32))
        banks = [(PA[:, 512 * i:512 * (i + 1)], "PA%d" % i) for i in range(4)] + \
                [(PB[:, 512 * i:512 * (i + 1)], "PB%d" % i) for i in range(4)]
        RB_OWN = nc.gpsimd.alloc_register("bc_own"); nc.gpsimd.reg_mov(RB_OWN, 2047)
        RB_XS = nc.gpsimd.alloc_register("bc_xs"); nc.gpsimd.reg_mov(RB_XS, NBLK * 128 - 1)
        RB_W = nc.gpsimd.alloc_register("bc_w"); nc.gpsimd.reg_mov(RB_W, NE * 128 - 1)
        ident = sbt(es, "ident", [128, 128])
        kb.dma('sp', lambda e: e.dma_start(out=ident[:], in_=IDENT), w=['ident'])
        modT = sbt(es, "modT", [128, 48, 2])
        a1T = sbt(es, "a1T", [128, 8, 2]); sh1T = sbt(es, "sh1T", [128, 8, 2]); g2T = sbt(es, "g2T", [128, 8])
        @contextmanager
        def stage():
            with ExitStack() as st_:
                yield st_
                kb.barrier()

        def dbg(name, ap, shape):
            if name in pg.debug:
                t = nc.dram_tensor("D_" + name, list(shape), F32, kind="ExternalOutput").ap()
                pg.outs["D_" + name] = t
                kb.dma('pool', lambda e: e.dma_start(out=t, in_=ap), r=[name], w=['DBG' + name])

        with stage() as s0:
            sT = sbt(s0, "sT", [128, 8, 2]); vraw = sbt(s0, "vraw", [80, 128]); VT = sbt(s0, "VT", [128, 80])
            kb.dma('sp', lambda e: e.dma_start(out=vraw[0:8, :], in_=CV.rearrange("(q p) -> q p", p=128)), w=['vraw'])
            kb.dma('sp', lambda e: e.dma_start(out=vraw[8:16, :], in_=CCTX.rearrange("(q p) -> q p", p=128)), r=['vraw'], w=['vraw'])
            kb.dma('sp', lambda e: e.dma_start(out=vraw[16:24, :], in_=N1G.rearrange("(q p) -> q p", p=128)), r=['vraw'], w=['vraw'])
            kb.dma('sp', lambda e: e.dma_start(out=vraw[24:32, :], in_=N2G.rearrange("(q p) -> q p", p=128)), r=['vraw'], w=['vraw'])
            kb.dma('sp', lambda e: e.dma_start(out=vraw[32:80, :], in_=ADAB.rearrange("(q p) -> q p", p=128)), r=['vraw'], w=['vraw'])
            ps, psk = banks[0]
            kb.op('pe', lambda e: e.transpose(out=ps[:, 128:208], in_=vraw[:], identity=ident[0:80, 0:80]), r=['vraw', 'ident'], w=[psk])
            kb.op('dve', lambda e: e.tensor_copy(out=VT[:], in_=ps[:, 128:208]), r=[psk], w=['VT'])
            abT = VT[:, 32:80]; g1T = VT[:, 16:24]
            for r_ in range(2):
                kb.op('act', lambda e: e.activation(out=sT[:, :, r_], in_=VT[:, 8 * r_:8 * r_ + 8], func=AF.Silu), r=['VT', 'sT'], w=['sT'])
            kb.op('dve', lambda e: e.tensor_copy(out=g2T[:], in_=VT[:, 24:32]), r=['VT'], w=['g2T'])
            wbufs = [sbt(s0, "adaw%d" % i, [128, 8, 768]) for i in range(2)]
            mrow = sbt(s0, "mrow", [2, 6144]); brow = sbt(s0, "brow", [2, 6144])
            for r_ in range(2):
                kb.dma('sp', lambda e: e.dma_start(out=brow[r_:r_ + 1, :], in_=ADAB.rearrange("(o n) -> o n", o=1)), r=['brow'], w=['brow'])
            for cb in range(8):
                wb = wbufs[cb % 2]; wk = "adaw%d" % (cb % 2)
                kb.dma('sp', lambda e: e.dma_start(out=wb[:], in_=ADAW[:, cb * 768:(cb + 1) * 768].rearrange("(k p) c -> p k c", p=128)), w=[wk])
                for hh in range(2):
                    pr, prk = banks[1 + (2 * cb + hh) % 3]
                    for kk in range(8):
                        kb.op('pe', lambda e: e.matmul(pr[0:2, 0:384], lhsT=sT[:, kk, :], rhs=wb[:, kk, hh * 384:(hh + 1) * 384],
                                                       start=(kk == 0), stop=(kk == 7)), r=[wk, 'sT'], w=[prk])
                    c0 = cb * 768 + hh * 384
                    kb.op('dve', lambda e: e.tensor_tensor(out=mrow[:, c0:c0 + 384], in0=pr[0:2, 0:384], in1=brow[:, c0:c0 + 384], op=ALU.add),
                          r=[prk, 'brow'], w=['mrow'])
            kb.dma('pool', lambda e: e.dma_start(out=MODROW, in_=mrow[:]), r=['mrow'], w=['MODROW'])
            mq = sbt(s0, "mq", [96, 128])
            kb.dma('sp', lambda e: e.dma_start(out=mq[:], in_=MODROW.rearrange("r (q p) -> (r q) p", p=128)), r=['MODROW'], w=['mq'])
            kb.op('pe', lambda e: e.transpose(out=ps[:, 256:352], in_=mq[:], identity=ident[0:96, 0:96]), r=['mq', 'ident'], w=[psk])
            for r_ in range(2):
                kb.op('dve', lambda e: e.tensor_copy(out=modT[:, :, r_], in_=ps[:, 256 + 48 * r_:256 + 48 * r_ + 48]), r=[psk, 'modT'], w=['modT'])
            kb.op('dve', lambda e: e.tensor_scalar(out=a1T[:], in0=modT[:, 8:16, :], scalar1=1.0, scalar2=None, op0=ALU.add),
                  r=['modT'], w=['a1T'])
            for r_ in range(2):
                kb.op('dve', lambda e: e.tensor_tensor(out=a1T[:, :, r_], in0=a1T[:, :, r_], in1=g1T, op=ALU.mult),
                      r=['a1T', 'VT'], w=['a1T'])
            kb.op('dve', lambda e: e.tensor_copy(out=sh1T[:], in_=modT[:, 0:8, :]), r=['modT'], w=['sh1T'])

        dbg('modT', modT[:], [128, 48, 2]); dbg('a1T', a1T[:], [128, 8, 2])
        def norm_to_xt(stack, srcs, XT, a_ap, b_ap, tag, xtk):
            xin = [sbt(stack, "%s_xin%d" % (tag, i), [128, 1024]) for i in range(2)]
            pin = [sbt(stack, "%s_pin%d" % (tag, i), [128, 1024]) for i in range(2)]
            junk = sbt(stack, "%s_junk" % tag, [128, 1024])
            st = [sbt(stack, "%s_st%d" % (tag, i), [128, 4]) for i in range(2)]
            xo = [sbt(stack, "%s_xo%d" % (tag, i), [128, 8, 128]) for i in range(2)]
            for i, (xap, pap, c0) in enumerate(srcs):
                b = i % 2
                xk, pk, sk, ok = "%s_xin%d" % (tag, b), "%s_pin%d" % (tag, b), "%s_st%d" % (tag, b), "%s_xo%d" % (tag, b)
                kb.dma('sp', lambda e: e.dma_start(out=xin[b][:], in_=xap), w=[xk])
                if pap is not None:
                    kb.dma('sp', lambda e: e.dma_start(out=pin[b][:], in_=pap), w=[pk])
                    kb.op('pool', lambda e: e.tensor_tensor(out=xin[b][:], in0=xin[b][:], in1=pin[b][:], op=ALU.add), r=[xk, pk], w=[xk])
                kb.op('act', lambda e: e.activation(out=junk[:], in_=xin[b][:], func=AF.Square, accum_out=st[b][:, 0:1]),
                      r=[xk], w=[tag + '_junk', sk])
                kb.op('dve', lambda e: e.tensor_scalar(out=st[b][:, 1:2], in0=st[b][:, 0:1], scalar1=1.0 / D, scalar2=EPS, op0=ALU.mult, op1=ALU.add),
                      r=[sk], w=[sk])
                kb.op('act', lambda e: e.activation(out=st[b][:, 2:3], in_=st[b][:, 1:2], func=AF.Sqrt), r=[sk], w=[sk])
                kb.op('dve', lambda e: e.reciprocal(out=st[b][:, 3:4], in_=st[b][:, 2:3]), r=[sk], w=[sk])
                kb.op('dve', lambda e: e.tensor_scalar(out=xin[b][:], in0=xin[b][:], scalar1=st[b][:, 3:4], scalar2=None, op0=ALU.mult),
                      r=[xk, sk], w=[xk])
                for h in range(2):
                    pb, pbk = banks[(2 * i + h) % 4]
                    for kk in range(4):
                        k8 = h * 4 + kk
                        kb.op('pe', lambda e: e.transpose(out=pb[:, kk * 128:(kk + 1) * 128], in_=xin[b][:, k8 * 128:(k8 + 1) * 128], identity=ident[:]),
                              r=[xk, 'ident'], w=[pbk])
                    for kk in range(4):
                        k8 = h * 4 + kk
                        kb.op('act', lambda e: e.activation(out=xo[b][:, k8, :], in_=pb[:, kk * 128:(kk + 1) * 128], func=AF.Identity,
                                                            bias=b_ap[:, k8:k8 + 1], scale=a_ap[:, k8:k8 + 1]),
                              r=[pbk, 'a1T', 'sh1T', 'a2T'], w=[ok])
                kb.dma('pool', lambda e: e.dma_start(out=XT[:, c0:c0 + 128].rearrange("(k p) t -> p k t", p=128), in_=xo[b][:]), r=[ok], w=[xtk])

        if stages >= 1:
            with stage() as s1:
                srcs = [(X[i * 128:(i + 1) * 128, :], POS[i * 128:(i + 1) * 128, :], i * 128) for i in range(L // 128)]
                norm_to_xt(s1, srcs, XNT, a1T[:, :, 0], sh1T[:, :, 0], "n1", 'XNT')
            with stage() as s1:
                srcs = [(CTX[i * 128:(i + 1) * 128, :], None, L + i * 128) for i in range(NCTX // 128)]
                norm_to_xt(s1, srcs, XNT, a1T[:, :, 1], sh1T[:, :, 1], "n1c", 'XNT')
            with stage() as s1:
                srcs = [(XOWN[i * 128:(i + 1) * 128, :], POSOWN[i * 128:(i + 1) * 128, :], i * 128) for i in range(OWN // 128)]
                norm_to_xt(s1, srcs, XNTOWN, a1T[:, :, 0], sh1T[:, :, 0], "n1o", 'XNTOWN')

        cp_toggle = [0]

        def evac(out_ap, in_ap, func, r, w):
            if func is None:
                cp_toggle[0] ^= 1
                if cp_toggle[0]:
                    kb.op('dve', lambda e: e.tensor_copy(out=out_ap, in_=in_ap), r=r, w=w)
                else:
                    kb.op('act', lambda e: e.copy(out=out_ap, in_=in_ap), r=r, w=w)
            else:
                kb.op('act', lambda e: e.activation(out=out_ap, in_=in_ap, func=func), r=r, w=w)

        def gemm_group(tag, Wsrc, kch, ncols, XT, xtkey, tblocks, jobs):
            with stage() as st:
                Wsb = sbt(st, tag + "_W", [128, kch, ncols])
                kb.dma('sp', lambda e: e.dma_start(out=Wsb[:], in_=Wsrc.rearrange("(k p) c -> p k c", p=128)), w=[tag + '_W'])
                Xb = [sbt(st, "%s_X%d" % (tag, i), [128, kch, 512]) for i in range(2)]
                stg = [sbt(st, "%s_s%d" % (tag, i), [128, 512]) for i in range(4)]
                si = 0
                bi = 0
                for ti, (t0, tn) in enumerate(tblocks):
                    xb = Xb[ti % 2]; xk = "%s_X%d" % (tag, ti % 2)
                    kb.dma('sp', lambda e: e.dma_start(out=xb[:, :, 0:tn], in_=XT[:, t0:t0 + tn].rearrange("(k p) t -> p k t", p=128)),
                           r=[xtkey], w=[xk])
                    for jb in jobs:
                        if jb.get('tsel') is not None and not jb['tsel'](t0):
                            continue
                        c0, cn, tofs = jb['c0'], jb['cn'], jb.get('tofs', 0)
                        if jb['mode'] == 'fm':
                            for m in range(cn // 128):
                                pb, pbk = banks[bi % 8]; bi += 1
                                for kk in range(kch):
                                    kb.op('pe', lambda e: e.matmul(pb[:, 0:tn], lhsT=Wsb[:, kk, c0 + m * 128:c0 + (m + 1) * 128], rhs=xb[:, kk, 0:tn],
                                                                   start=(kk == 0), stop=(kk == kch - 1)), r=[tag + '_W', xk], w=[pbk])
                                sg = stg[si % 4]; sgk = "%s_s%d" % (tag, si % 4); si += 1
                                evac(sg[:, 0:tn], pb[:, 0:tn], jb['func'], [pbk], [sgk])
                                orow = jb.get('oc0', 0) + m * 128
                                kb.dma('pool', lambda e: e.dma_start(out=jb['out'][orow:orow + 128, t0 - tofs:t0 - tofs + tn], in_=sg[:, 0:tn]),
                                       r=[sgk], w=[jb['okey']])
                        else:
                            for tt in range(tn // 128):
                                pb, pbk = banks[bi % 8]; bi += 1
                                for kk in range(kch):
                                    kb.op('pe', lambda e: e.matmul(pb[:, 0:cn], lhsT=xb[:, kk, tt * 128:(tt + 1) * 128], rhs=Wsb[:, kk, c0:c0 + cn],
                                                                   start=(kk == 0), stop=(kk == kch - 1)), r=[tag + '_W', xk], w=[pbk])
                                sg = stg[si % 4]; sgk = "%s_s%d" % (tag, si % 4); si += 1
                                evac(sg[:, 0:cn], pb[:, 0:cn], jb['func'], [pbk], [sgk])
                                tr = t0 - tofs + tt * 128
                                oc0 = jb.get('oc0', 0)
                                kb.dma('pool', lambda e: e.dma_start(out=jb['out'][tr:tr + 128, oc0:oc0 + cn], in_=sg[:, 0:cn]),
                                       r=[sgk], w=[jb['okey']])

        if stages >= 2:
            lat_blocks = [(t, 512) for t in range(0, L, 512)]
            all_blocks = lat_blocks + [(L, 256)]
            gemm_group("g1", WIN[:, 0:1024], 8, 1024, XNT, 'XNT', lat_blocks,
                       [dict(c0=0, cn=1024, mode='fm', func=None, out=HYRAW, okey='HYRAW', oc0=0)])
            gemm_group("g2", WIN[:, 1024:2048], 8, 1024, XNT, 'XNT', lat_blocks,
                       [dict(c0=0, cn=512, mode='fm', func=None, out=HYRAW, okey='HYRAW', oc0=1024),
                        dict(c0=512, cn=512, mode='fm', func=AF.Silu, out=QT, okey='QT', oc0=0)])
        if stages >= 3:
            gemm_group("g3", WIN[:, 2048:3072], 8, 1024, XNT, 'XNT', all_blocks,
                       [dict(c0=0, cn=512, mode='tm', func=None, out=IFF, okey='IFF', oc0=0),
                        dict(c0=512, cn=512, mode='tm', func=None, out=IFF, okey='IFF', oc0=512)])
            gemm_group("g4", WIN[:, 3072:4096], 8, 1024, XNT, 'XNT', all_blocks,
                       [dict(c0=0, cn=512, mode='tm', func=None, out=IFF, okey='IFF', oc0=1024),
                        dict(c0=512, cn=512, mode='fm', func=AF.Silu, out=GT, okey='GT', oc0=0, tsel=lambda t0: t0 < L)])

        if stages >= 3:
            own_blocks = [(t, 512) for t in range(0, OWN, 512)]
            for gi in range(2):
                gemm_group("g%d" % (5 + gi), WIN[:, 4096 + 1024 * gi:5120 + 1024 * gi], 8, 1024, XNTOWN, 'XNTOWN', own_blocks,
                           [dict(c0=0, cn=1024, mode='fm', func=AF.Sigmoid, out=SGT, okey='SGT', oc0=1024 * gi)])

        if stages >= 4:
            with stage() as st:
                cwT = sbt(st, "cwT", [128, 12, 4]); craw = sbt(st, "craw", [48, 128])
                for j3 in range(3):
                    kb.dma('sp', lambda e: e.dma_start(out=craw[12 * j3:12 * j3 + 12, :], in_=CONVW[j3].rearrange("(q p) -> q p", p=128)), r=['craw'], w=['craw'])
                kb.dma('sp', lambda e: e.dma_start(out=craw[36:48, :], in_=CONVB.rearrange("(q p) -> q p", p=128)), r=['craw'], w=['craw'])
                pb, pbk = banks[0]
                kb.op('pe', lambda e: e.transpose(out=pb[:, 0:48], in_=craw[:], identity=ident[0:48, 0:48]), r=['craw', 'ident'], w=[pbk])
                kb.op('dve', lambda e: e.tensor_copy(out=cwT[:], in_=pb[:, 0:48].rearrange("p (j q) -> p q j", j=4)), r=[pbk], w=['cwT'])
                uin = [sbt(st, "uin%d" % i, [128, L + 2]) for i in range(2)]
                uo = [sbt(st, "uo%d" % i, [128, L]) for i in range(2)]
                for i in range(2):
                    kb.op('pool', lambda e: e.memset(uin[i][:, 0:1], 0.0), w=['uin%d' % i])
                    kb.op('pool', lambda e: e.memset(uin[i][:, L + 1:L + 2], 0.0), r=['uin%d' % i], w=['uin%d' % i])
                for q in range(12):
                    b2 = q % 2; uk = 'uin%d' % b2; ok = 'uo%d' % b2
                    kb.dma('sp', lambda e: e.dma_start(out=uin[b2][:, 1:L + 1], in_=HYRAW[q * 128:(q + 1) * 128, :]), r=['HYRAW', uk], w=[uk])
                    kb.op('dve', lambda e: e.tensor_scalar(out=uo[b2][:], in0=uin[b2][:, 0:L], scalar1=cwT[:, q, 0:1], scalar2=cwT[:, q, 3:4],
                                                           op0=ALU.mult, op1=ALU.add), r=[uk, 'cwT'], w=[ok])
                    kb.op('dve', lambda e: e.scalar_tensor_tensor(out=uo[b2][:], in0=uin[b2][:, 1:L + 1], scalar=cwT[:, q, 1:2], in1=uo[b2][:],
                                                                  op0=ALU.mult, op1=ALU.add), r=[uk, 'cwT', ok], w=[ok])
                    kb.op('dve', lambda e: e.scalar_tensor_tensor(out=uo[b2][:], in0=uin[b2][:, 2:L + 2], scalar=cwT[:, q, 2:3], in1=uo[b2][:],
                                                                  op0=ALU.mult, op1=ALU.add), r=[uk, 'cwT', ok], w=[ok])
                    kb.dma('pool', lambda e: e.dma_start(out=HYC[q * 128:(q + 1) * 128, :], in_=uo[b2][:]), r=[ok], w=['HYC'])

        if stages >= 5 and not dummy_mix:
          with stage() as st:
            tri = {0: sbt(st, "trif", [128, 128]), 1: sbt(st, "trib", [128, 128])}
            ones = sbt(st, "ones", [128, 128])
            kb.dma('sp', lambda e: e.dma_start(out=tri[0][:], in_=TRIFd), w=['tri'])
            kb.dma('sp', lambda e: e.dma_start(out=tri[1][:], in_=TRIBd), r=['tri'], w=['tri'])
            kb.dma('sp', lambda e: e.dma_start(out=ones[:], in_=ONESd), w=['ones'])
            l0 = sbt(st, "l0", [128, 1024]); l1 = sbt(st, "l1", [128, 1024]); oml = sbt(st, "oml", [128, 1024])
            kb.dma('sp', lambda e: e.dma_start(out=l0[:], in_=LBL[0:1, :].to_broadcast([128, 1024])), w=['l0'])
            kb.dma('sp', lambda e: e.dma_start(out=l1[:], in_=LBL[1:2, :].to_broadcast([128, 1024])), w=['l1'])
            kb.op('dve', lambda e: e.tensor_tensor(out=l0[:], in0=l0[:], in1=l1[:], op=ALU.subtract), r=['l0', 'l1'], w=['l0'])
            kb.op('act', lambda e: e.activation(out=l0[:], in_=l0[:], func=AF.Sigmoid), r=['l0'], w=['l0'])
            kb.op('dve', lambda e: e.tensor_scalar(out=oml[:], in0=l0[:], scalar1=-1.0, scalar2=1.0, op0=ALU.mult, op1=ALU.add), r=['l0'], w=['oml'])
            ngT = sbt(st, "ngT", [128, 1])
            with nc.allow_non_contiguous_dma(reason="128-element vector to partitions"):
                kb.dma('sp', lambda e: e.dma_start(out=ngT[:], in_=HGNG.rearrange("(p o) -> p o", o=1)), w=['ngT'])
            osum = sbt(st, "osum", [128, L])
            LB8 = sbt(st, "LB8", [128, 8, 128]); OML8 = sbt(st, "OML8", [128, 8, 128])
            vseg = [sbt(st, "vseg%d" % i, [128, 8, 128]) for i in range(2)]
            fseg = [sbt(st, "fseg%d" % i, [128, 8, 128]) for i in range(2)]
            lseg = [sbt(st, "lseg%d" % i, [128, 8, 128]) for i in range(2)]
            kseg = [sbt(st, "kseg%d" % i, [128, 8, 128]) for i in range(2)]
            qseg = [sbt(st, "qseg%d" % i, [128, 1024]) for i in range(2)]
            R3 = 3
            ekb = [sbt(st, "ekb%d" % i, [128, 128]) for i in range(R3)]
            kd = [sbt(st, "kd%d" % i, [128, 128]) for i in range(R3)]
            ebT = [sbt(st, "ebT%d" % i, [128, 128]) for i in range(R3)]
            qdT = [sbt(st, "qdT%d" % i, [128, 128]) for i in range(R3)]
            kdT = [sbt(st, "kdT%d" % i, [128, 128]) for i in range(R3)]
            attm = [sbt(st, "attm%d" % i, [128, 128]) for i in range(R3)]
            Sr = [sbt(st, "S%d" % i, [128, 128]) for i in range(4)]
            Se = [sbt(st, "Se%d" % i, [128, 128]) for i in range(2)]
            pbi = [0]

            def nb():
                b_ = banks[pbi[0] % 8]; pbi[0] += 1
                return b_

            for h in range(4):
                for dr in range(2):
                    T = tri[dr]
                    fcol = 512 + 512 * dr + h * 128
                    for n in range(8):
                        kb.op('pool', lambda e: e.tensor_copy(out=LB8[:, n, :], in_=l0[:, dr * 512 + h * 128:dr * 512 + (h + 1) * 128]), r=['l0', 'LB8'], w=['LB8'])
                        kb.op('pool', lambda e: e.tensor_copy(out=OML8[:, n, :], in_=oml[:, dr * 512 + h * 128:dr * 512 + (h + 1) * 128]), r=['oml', 'OML8'], w=['OML8'])
                    si_ = 0
                    kb.op('pool', lambda e: e.memset(Sr[0][:], 0.0), w=['S0'])
                    segs = [(L, 2, False)] + [(sg * 1024, 8, True) for sg in (range(8) if dr == 0 else range(7, -1, -1))]
                    tcount = 0
                    for sgi, (r0, nt, lat) in enumerate(segs):
                        b2 = sgi % 2
                        vk, fk, lk, kk_, qk = 'vseg%d' % b2, 'fseg%d' % b2, 'lseg%d' % b2, 'kseg%d' % b2, 'qseg%d' % b2
                        kb.dma('sp', lambda e: e.dma_start(out=vseg[b2][:, 0:nt, :], in_=IFF[r0:r0 + nt * 128, h * 128:(h + 1) * 128].rearrange("(n p) c -> p n c", p=128)),
                               r=['IFF'], w=[vk])
                        kb.dma('sp', lambda e: e.dma_start(out=fseg[b2][:, 0:nt, :], in_=IFF[r0:r0 + nt * 128, fcol:fcol + 128].rearrange("(n p) c -> p n c", p=128)),
                               r=['IFF'], w=[fk])
                        if lat:
                            kb.dma('sp', lambda e: e.dma_start(out=qseg[b2][:], in_=QT[h * 128:(h + 1) * 128, r0:r0 + 1024]), r=['QT'], w=[qk])
                        kb.op('act', lambda e: e.activation(out=fseg[b2][:, 0:nt, :], in_=fseg[b2][:, 0:nt, :], func=AF.Sigmoid), r=[fk], w=[fk])
                        kb.op('dve', lambda e: e.tensor_tensor(out=fseg[b2][:, 0:nt, :], in0=fseg[b2][:, 0:nt, :], in1=OML8[:, 0:nt, :], op=ALU.mult), r=[fk, 'OML8'], w=[fk])
                        kb.op('dve', lambda e: e.tensor_tensor(out=fseg[b2][:, 0:nt, :], in0=fseg[b2][:, 0:nt, :], in1=LB8[:, 0:nt, :], op=ALU.add), r=[fk, 'LB8'], w=[fk])
                        kb.op('act', lambda e: e.activation(out=lseg[b2][:, 0:nt, :], in_=fseg[b2][:, 0:nt, :], func=AF.Ln), r=[fk], w=[lk])
                        kb.op('dve', lambda e: e.tensor_scalar(out=kseg[b2][:, 0:nt, :], in0=fseg[b2][:, 0:nt, :], scalar1=-1.0, scalar2=1.0, op0=ALU.mult, op1=ALU.add),
                              r=[fk], w=[kk_])
                        order = list(range(nt)) if dr == 0 else list(range(nt - 1, -1, -1))
                        slot = {}

                        def prep(n, b2=b2, lat=lat, lk=lk, kk_=kk_, qk=qk):
                            nonlocal tcount
                            r3 = tcount % R3; tcount += 1
                            slot[n] = r3
                            ek, kdk, ebk, qdk, ktk = 'ekb%d' % r3, 'kd%d' % r3, 'ebT%d' % r3, 'qdT%d' % r3, 'kdT%d' % r3
                            p1, p1k = nb()
                            kb.op('pe', lambda e: e.matmul(p1[:, 0:128], lhsT=T[:], rhs=lseg[b2][:, n, :], start=True, stop=True), r=['tri', lk], w=[p1k])
                            p2, p2k = nb()
                            kb.op('pe', lambda e: e.matmul(p2[:, 0:128], lhsT=lseg[b2][:, n, :], rhs=T[:], start=True, stop=True), r=['tri', lk], w=[p2k])
                            kb.op('act', lambda e: e.activation(out=ekb[r3][:], in_=p1[:, 0:128], func=AF.Exp, scale=-1.0), r=[p1k], w=[ek])
                            kb.op('act', lambda e: e.activation(out=ebT[r3][:], in_=p2[:, 0:128], func=AF.Exp), r=[p2k], w=[ebk])
                            kb.op('dve', lambda e: e.tensor_tensor(out=kd[r3][:], in0=kseg[b2][:, n, :], in1=ekb[r3][:], op=ALU.mult), r=[kk_, ek], w=[kdk])
                            if lat:
                                kb.op('dve', lambda e: e.tensor_tensor(out=qdT[r3][:], in0=qseg[b2][:, n * 128:(n + 1) * 128], in1=ebT[r3][:], op=ALU.mult), r=[qk, ebk], w=[qdk])
                                p3, p3k = nb()
                                kb.op('pe', lambda e: e.transpose(out=p3[:, 0:128], in_=kd[r3][:], identity=ident[:]), r=[kdk, 'ident'], w=[p3k])
                                kb.op('act', lambda e: e.copy(out=kdT[r3][:], in_=p3[:, 0:128]), r=[p3k], w=[ktk])

                        def scan(n, b2=b2, lat=lat, vk=vk, r0=r0):
                            nonlocal si_
                            r3 = slot[n]
                            kdk, ebk, qdk, ktk, amk = 'kd%d' % r3, 'ebT%d' % r3, 'qdT%d' % r3, 'kdT%d' % r3, 'attm%d' % r3
                            halves = [(0, 64, 63), (64, 128, 127)] if dr == 0 else [(64, 128, 64), (0, 64, 0)]
                            if lat:
                                p4, p4k = nb()
                                kb.op('pe', lambda e: e.matmul(p4[:, 0:128], lhsT=kdT[r3][:], rhs=qdT[r3][:], start=True, stop=True), r=[ktk, qdk], w=[p4k])
                                kb.op('dve', lambda e: e.tensor_tensor(out=attm[r3][:], in0=p4[:, 0:128], in1=T[:], op=ALU.mult), r=[p4k, 'tri'], w=[amk])
                            pds = []
                            for (c0, c1, ce) in halves:
                                pd, pdk = nb()
                                kb.op('pe', lambda e: e.matmul(pd[:, 0:128], lhsT=kd[r3][c0:c1, :], rhs=vseg[b2][c0:c1, n, :], start=True, stop=True), r=[kdk, vk], w=[pdk])
                                pds.append((pd, pdk))
                            Ss = []
                            for hi_, (c0, c1, ce) in enumerate(halves):
                                Sc = Sr[si_ % 4]; Sck = 'S%d' % (si_ % 4)
                                Sn = Sr[(si_ + 1) % 4]; Snk = 'S%d' % ((si_ + 1) % 4)
                                sew = Se[si_ % 2]; sek = 'Se%d' % (si_ % 2)
                                si_ += 1
                                Ss.append((Sc, Sck))
                                pd, pdk = pds[hi_]
                                kb.op('act', lambda e: e.activation(out=sew[:], in_=Sc[:], func=AF.Copy, scale=ebT[r3][:, ce:ce + 1]), r=[Sck, ebk], w=[sek])
                                kb.op('dve', lambda e: e.scalar_tensor_tensor(out=Sn[:], in0=pd[:, 0:128], scalar=ebT[r3][:, ce:ce + 1], in1=sew[:], op0=ALU.mult, op1=ALU.add),
                                      r=[pdk, ebk, sek], w=[Snk])
                            if lat:
                                po, pok = nb()
                                kb.op('pe', lambda e: e.matmul(po[:, 0:128], lhsT=vseg[b2][:, n, :], rhs=attm[r3][:], start=True, stop=False), r=[vk, amk], w=[pok])
                                for hi_, (c0, c1, ce) in enumerate(halves):
                                    Sc, Sck = Ss[hi_]
                                    kb.op('pe', lambda e: e.matmul(po[:, c0:c1], lhsT=Sc[:], rhs=qdT[r3][:, c0:c1], start=False, stop=True), r=[Sck, qdk], w=[pok])
                                cols = slice(r0 + n * 128, r0 + (n + 1) * 128)
                                if dr == 0:
                                    kb.op('act', lambda e: e.copy(out=osum[:, cols], in_=po[:, 0:128]), r=[pok], w=['osum%d' % (r0 // 1024)])
                                else:
                                    kb.op('dve', lambda e: e.tensor_tensor(out=osum[:, cols], in0=po[:, 0:128], in1=osum[:, cols], op=ALU.add),
                                          r=[pok, 'osum%d' % (r0 // 1024)], w=['osum%d' % (r0 // 1024)])

                        prep(order[0])
                        for oi, n in enumerate(order):
                            if oi + 1 < len(order):
                                prep(order[oi + 1])
                            scan(n)
                    if si_ % 4 != 0:
                        pass
                    kb.res.pop('unused', None)
                sq = [sbt(st, "hsq%d_%d" % (h, i), [128, 512]) for i in range(2)] if h == 0 else sq
                gt_ = [sbt(st, "hgt%d_%d" % (h, i), [128, 512]) for i in range(2)] if h == 0 else gt_
                rs = [sbt(st, "hrs%d_%d" % (h, i), [128, 512]) for i in range(2)] if h == 0 else rs
                for blk in range(L // 512):
                    b2 = blk % 2; cols = slice(blk * 512, (blk + 1) * 512)
                    sqk, gk, rk = 'hsq%d' % b2, 'hgt%d' % b2, 'hrs%d' % b2
                    ok_ = 'osum%d' % (blk // 2)
                    kb.dma('sp', lambda e: e.dma_start(out=gt_[b2][:], in_=GT[h * 128:(h + 1) * 128, cols]), r=['GT'], w=[gk])
                    kb.op('act', lambda e: e.activation(out=sq[b2][:], in_=osum[:, cols], func=AF.Square), r=[ok_], w=[sqk])
                    pb, pbk = nb()
                    kb.op('pe', lambda e: e.matmul(pb[:, :], lhsT=ones[:], rhs=sq[b2][:], start=True, stop=True), r=['ones', sqk], w=[pbk])
                    kb.op('dve', lambda e: e.tensor_scalar(out=rs[b2][:], in0=pb[:, :], scalar1=1.0 / 128, scalar2=EPS, op0=ALU.mult, op1=ALU.add), r=[pbk], w=[rk])
                    kb.op('act', lambda e: e.activation(out=rs[b2][:], in_=rs[b2][:], func=AF.Sqrt), r=[rk], w=[rk])
                    kb.op('dve', lambda e: e.reciprocal(out=rs[b2][:], in_=rs[b2][:]), r=[rk], w=[rk])
                    kb.op('dve', lambda e: e.scalar_tensor_tensor(out=sq[b2][:], in0=osum[:, cols], scalar=ngT[:, 0:1], in1=rs[b2][:], op0=ALU.mult, op1=ALU.mult),
                          r=[ok_, 'ngT', rk, sqk], w=[sqk])
                    kb.op('pool', lambda e: e.tensor_tensor(out=sq[b2][:], in0=sq[b2][:], in1=gt_[b2][:], op=ALU.mult), r=[sqk, gk], w=[sqk])
                    kb.dma('pool', lambda e: e.dma_start(out=YHGT[h * 128:(h + 1) * 128, cols], in_=sq[b2][:]), r=[sqk], w=['YHGT'])

        if stages >= 5 and not dummy_mix:
          with stage() as st:
            cst = {}
            for nm, src, shp in (("FA", FAd, [128, 256]), ("FAH", FAHd, [64, 256]), ("FRE", FREd, [128, 128]), ("FIM", FIMd, [128, 128]), ("NFIM", NFIMd, [128, 128]),
                                 ("CA", CAd, [128, 256]), ("CB", CBd, [128, 256]), ("FREN", FRENd, [128, 64]), ("FIMN", FIMNd, [128, 64]),
                                 ("TRE2", TRE2d, [128, 256]), ("TIM2", TIM2d, [128, 256]), ("ONESH", ONESd, [128, 128])):
                cst[nm] = sbt(st, "c_" + nm, shp)
                kb.dma('sp', lambda e: e.dma_start(out=cst[nm][:], in_=src), w=['hconst'])
            skb = sbt(st, "skb", [128, 1024])
            kb.dma('sp', lambda e: e.dma_start(out=skb[:], in_=HSKIP.rearrange("(o n) -> o n", o=1).to_broadcast([128, 1024])), w=['skb'])
            tw = [sbt(st, "tw%d" % i, [128, 256]) for i in range(4)]
            hbi = [0]

            def hb():
                b_ = banks[hbi[0] % 8]; hbi[0] += 1
                return b_

            def twiddle(ps, psk, Bt, bkey, c0, conj):
                A = ps[:, :].rearrange("p (c r f) -> p c r f", c=2, r=2)
                Are, Aim = A[:, :, 0, :], A[:, :, 1, :]
                T2r = cst["TRE2"][:].rearrange("p (c f) -> p c f", c=2); T2i = cst["TIM2"][:].rearrange("p (c f) -> p c f", c=2)
                t = [x[:].rearrange("p (c f) -> p c f", c=2) for x in tw]
                kb.op('dve', lambda e: e.tensor_tensor(out=t[0], in0=Are, in1=T2r, op=ALU.mult), r=[psk, 'hconst', 'tw0'], w=['tw0'])
                kb.op('dve', lambda e: e.tensor_tensor(out=t[1], in0=Aim, in1=T2i, op=ALU.mult), r=[psk, 'hconst', 'tw1'], w=['tw1'])
                kb.op('dve', lambda e: e.tensor_tensor(out=t[2], in0=Are, in1=T2i, op=ALU.mult), r=[psk, 'hconst', 'tw2'], w=['tw2'])
                kb.op('dve', lambda e: e.tensor_tensor(out=t[3], in0=Aim, in1=T2r, op=ALU.mult), r=[psk, 'hconst', 'tw3'], w=['tw3'])
                if not conj:
                    kb.op('pool', lambda e: e.tensor_tensor(out=Bt[:, c0:c0 + 2, 0, :], in0=t[0], in1=t[1], op=ALU.subtract), r=['tw0', 'tw1', bkey], w=[bkey])
                    kb.op('pool', lambda e: e.tensor_tensor(out=Bt[:, c0:c0 + 2, 1, :], in0=t[2], in1=t[3], op=ALU.add), r=['tw2', 'tw3', bkey], w=[bkey])
                else:
                    kb.op('pool', lambda e: e.tensor_tensor(out=Bt[:, c0:c0 + 2, 0, :], in0=t[0], in1=t[1], op=ALU.add), r=['tw0', 'tw1', bkey], w=[bkey])
                    kb.op('pool', lambda e: e.tensor_tensor(out=Bt[:, c0:c0 + 2, 1, :], in0=t[3], in1=t[2], op=ALU.subtract), r=['tw2', 'tw3', bkey], w=[bkey])

            def stage2(Bt, bkey, c4):
                pr, prk = hb(); pi_, pik = hb()
                Bre, Bim = Bt[:, c4:c4 + 4, 0, :], Bt[:, c4:c4 + 4, 1, :]
                kb.op('pe', lambda e: e.matmul(pr[:, :], lhsT=cst["FRE"][:], rhs=Bre, start=True, stop=False), r=['hconst', bkey], w=[prk])
                kb.op('pe', lambda e: e.matmul(pr[:, :], lhsT=cst["NFIM"][:], rhs=Bim, start=False, stop=True), r=['hconst', bkey], w=[prk])
                kb.op('pe', lambda e: e.matmul(pi_[:, :], lhsT=cst["FIM"][:], rhs=Bre, start=True, stop=False), r=['hconst', bkey], w=[pik])
                kb.op('pe', lambda e: e.matmul(pi_[:, :], lhsT=cst["FRE"][:], rhs=Bim, start=False, stop=True), r=['hconst', bkey], w=[pik])
                return pr, prk, pi_, pik

            B16 = sbt(st, "B16", [128, 16, 2, 128])
            with stage() as sf:
                a3 = sbt(sf, "a3", [128, L])
                w4sb = sbt(sf, "w4sb", [128, 2048])
                kb.dma('sp', lambda e: e.dma_start(out=w4sb[0:64, :], in_=FW4), w=['w4sb'])
                kb.dma('sp', lambda e: e.dma_start(out=w4sb[64:128, :], in_=FW4), r=['w4sb'], w=['w4sb'])
                with stage() as sm:
                    zts = sbt(sm, "zts", [66, L]); bufB = sbt(sm, "bufB", [128, L])
                    kb.dma('sp', lambda e: e.dma_start(out=zts[:], in_=ZTd), w=['zts'])
                    wl = [sbt(sm, "w1bd", [66, 128]), sbt(sm, "w2bd", [128, 128]), sbt(sm, "w3bd", [128, 128])]
                    for i_, (wt, src, kin) in enumerate(zip(wl, (FW1, FW2, FW3), (33, 64, 64))):
                        kb.op('pool', lambda e: e.memset(wt[:], 0.0), w=['wbd%d' % i_])
                        kb.dma('sp', lambda e: e.dma_start(out=wt[0:kin, 0:64], in_=src), r=['wbd%d' % i_], w=['wbd%d' % i_])
                        kb.dma('sp', lambda e: e.dma_start(out=wt[kin:2 * kin, 64:128], in_=src), r=['wbd%d' % i_], w=['wbd%d' % i_])
                    fqb = sbt(sm, "fqb", [128, 4])
                    with nc.allow_non_contiguous_dma(reason="64-element vectors onto partitions"):
                        for j_, src in enumerate((FFQ, FB1, FB2, FB3)):
                            for hh in range(2):
                                kb.dma('sp', lambda e: e.dma_start(out=fqb[64 * hh:64 * hh + 64, j_:j_ + 1], in_=src.rearrange("(p o) -> p o", o=1)), r=['fqb'], w=['fqb'])
                    kb.op('dve', lambda e: e.tensor_scalar(out=fqb[:, 0:1], in0=fqb[:, 0:1], scalar1=1.0 / TWO_PI, scalar2=None, op0=ALU.mult), r=['fqb'], w=['fqb'])
                    kb.op('dve', lambda e: e.tensor_scalar(out=fqb[:, 1:4], in0=fqb[:, 1:4], scalar1=fqb[:, 0:1], scalar2=None, op0=ALU.mult), r=['fqb'], w=['fqb'])
                    uu = sbt(sm, "uu", [128, 2048]); ui = sbt(sm, "ui", [128, 2048], I32); uf = sbt(sm, "uf", [128, 2048])
                    srcs = [(zts, 66, 'zts'), (bufB, 128, 'bufB'), (a3, 128, 'a3')]
                    dsts = [(bufB, 'bufB'), (a3, 'a3'), (bufB, 'bufB')]
                    for l_ in range(3):
                        src_t, kdim, srck = srcs[l_]; dst_t, dstk = dsts[l_]
                        for ch in range(4):
                            Pq, pkeys = (PA, ["PA0", "PA1", "PA2", "PA3"]) if ch % 2 == 0 else (PB, ["PB0", "PB1", "PB2", "PB3"])
                            for q in range(4):
                                cs = slice(ch * 2048 + q * 512, ch * 2048 + (q + 1) * 512)
                                kb.op('pe', lambda e: e.matmul(Pq[:, q * 512:(q + 1) * 512], lhsT=wl[l_][0:kdim, :], rhs=src_t[0:kdim, cs], start=True, stop=True),
                                      r=['wbd%d' % l_, srck], w=[pkeys[q]])
                            kb.op('act', lambda e: e.activation(out=uu[:], in_=Pq[:, :], func=AF.Identity, bias=fqb[:, 1 + l_:2 + l_], scale=fqb[:, 0:1]), r=pkeys + ['fqb'], w=['uu'])
                            kb.op('dve', lambda e: e.tensor_copy(out=ui[:], in_=uu[:]), r=['uu'], w=['ui'])
                            kb.op('dve', lambda e: e.tensor_copy(out=uf[:], in_=ui[:]), r=['ui'], w=['uf'])
                            kb.op('pool', lambda e: e.tensor_tensor(out=uu[:], in0=uu[:], in1=uf[:], op=ALU.subtract), r=['uu', 'uf'], w=['uu'])
                            kb.op('dve', lambda e: e.scalar_tensor_tensor(out=uf[:], in0=uu[:], scalar=0.5, in1=uu[:], op0=ALU.is_gt, op1=ALU.subtract), r=['uu', 'uf'], w=['uf'])
                            kb.op('dve', lambda e: e.scalar_tensor_tensor(out=uu[:], in0=uf[:], scalar=0.5, in1=uf[:], op0=ALU.is_gt, op1=ALU.subtract), r=['uu', 'uf'], w=['uu'])
                            kb.op('act', lambda e: e.activation(out=dst_t[:, ch * 2048:(ch + 1) * 2048], in_=uu[:], func=AF.Sin, scale=6.283185), r=['uu', dstk], w=[dstk])
                    kb.op('pool', lambda e: e.tensor_copy(out=a3[:], in_=bufB[:]), r=['bufB', 'a3'], w=['a3'])
                Kt = [sbt(sf, "Kt%d" % i, [64, 64, 128]) for i in range(2)]
                dec = sbt(sf, "dec", [64, 64, 128]); rab = sbt(sf, "rab", [64, 2, 64]); rn = sbt(sf, "rn", [128, 64]); Hst = sbt(sf, "Hst", [128, 16, 2, 128])
                for o in range(2):
                    for cg in range(8):
                        for hh in range(2):
                            col0 = o * 1024 + hh * 512 + cg * 64
                            for nbk in range(16):
                                ps, psk = hb()
                                for j_ in range(8):
                                    n2 = nbk * 8 + j_
                                    kb.op('pe', lambda e: e.matmul(ps[0:64, j_ * 64:(j_ + 1) * 64], lhsT=a3[64 * hh:64 * hh + 64, n2:L:128], rhs=w4sb[64 * hh:64 * hh + 64, col0:col0 + 64],
                                                                   start=True, stop=True), r=['a3', 'w4sb'], w=[psk])
                                evac(Kt[hh][:, :, nbk * 8:(nbk + 1) * 8], ps[0:64, :].rearrange("p (n c) -> p c n", c=64), None, [psk, 'Kt%d' % hh], ['Kt%d' % hh])
                            kb.dma('sp', lambda e: e.dma_start(out=dec[:], in_=DECd[hh, :, cg * 64:(cg + 1) * 64, :]), r=['dec'], w=['dec'])
                            kb.op('pool', lambda e: e.tensor_tensor(out=Kt[hh][:], in0=Kt[hh][:], in1=dec[:], op=ALU.mult), r=['Kt%d' % hh, 'dec'], w=['Kt%d' % hh])
                            kb.op('dve', lambda e: e.tensor_reduce(out=rab[:, hh, :], in_=Kt[hh][:], axis=AX.X, op=ALU.add, apply_absolute_value=True), r=['Kt%d' % hh, 'rab'], w=['rab'])
                        kb.op('dve', lambda e: e.tensor_tensor(out=rab[:, 0, :], in0=rab[:, 0, :], in1=rab[:, 1, :], op=ALU.add), r=['rab'], w=['rab'])
                        ps, psk = hb()
                        kb.op('pe', lambda e: e.matmul(ps[:, 0:64], lhsT=cst["ONESH"][0:64, :], rhs=rab[:, 0, :], start=True, stop=True), r=['hconst', 'rab'], w=[psk])
                        kb.op('dve', lambda e: e.reciprocal(out=rn[:], in_=ps[:, 0:64]), r=[psk], w=['rn'])
                        for sb4 in range(4):
                            for c2 in range(8):
                                ps, psk = hb()
                                for j_ in range(2):
                                    cc = sb4 * 16 + c2 * 2 + j_
                                    kb.op('pe', lambda e: e.matmul(ps[:, j_ * 256:(j_ + 1) * 256], lhsT=Kt[0][:, cc, :], rhs=cst["FA"][0:64, :], start=True, stop=False), r=['Kt0', 'hconst'], w=[psk])
                                    kb.op('pe', lambda e: e.matmul(ps[:, j_ * 256:(j_ + 1) * 256], lhsT=Kt[1][:, cc, :], rhs=cst["FAH"][:], start=False, stop=True), r=['Kt1', 'hconst'], w=[psk])
                                twiddle(ps, psk, B16, 'B16', c2 * 2, False)
                            for c4 in range(0, 16, 4):
                                pr, prk, pi_, pik = stage2(B16, 'B16', c4)
                                for j_ in range(4):
                                    cc = sb4 * 16 + c4 + j_; gc = cg * 64 + cc
                                    kb.op('dve', lambda e: e.tensor_scalar(out=Hst[:, c4 + j_, 0, :], in0=pr[:, j_ * 128:(j_ + 1) * 128], scalar1=rn[:, cc:cc + 1], scalar2=skb[:, o * 512 + gc:o * 512 + gc + 1],
                                                                           op0=ALU.mult, op1=ALU.add), r=[prk, 'rn', 'skb', 'Hst'], w=['Hst'])
                                    kb.op('act', lambda e: e.activation(out=Hst[:, c4 + j_, 1, :], in_=pi_[:, j_ * 128:(j_ + 1) * 128], func=AF.Copy, scale=rn[:, cc:cc + 1]), r=[pik, 'rn', 'Hst'], w=['Hst'])
                            g0 = o * 512 + cg * 64 + sb4 * 16
                            kb.dma('sp', lambda e: e.dma_start(out=HSPEC[g0:g0 + 16].rearrange("c k r f -> k c r f"), in_=Hst[:]), r=['Hst'], w=['HSPEC'])
            with stage() as sc:
                v16 = sbt(sc, "v16", [64, 16, 128]); x116 = sbt(sc, "x116", [64, 16, 128]); x216 = sbt(sc, "x216", [64, 16, 128]); z16 = sbt(sc, "z16", [64, 16, 128])
                y16 = sbt(sc, "y16", [64, 16, 128])
                H1 = sbt(sc, "H1", [128, 16, 2, 128]); H2s = sbt(sc, "H2s", [128, 16, 2, 128]); Y16 = sbt(sc, "Y16", [128, 16, 2, 128]); G16 = sbt(sc, "G16", [128, 16, 2, 128])
                hm = [sbt(sc, "hm%d" % i, [128, 4, 128]) for i in range(4)]

                def conv16(Din, dkey, Hs, hkey, Xmul, xkey, Out, okey):
                    for c2 in range(8):
                        ps, psk = hb()
                        for j_ in range(2):
                            kb.op('pe', lambda e: e.matmul(ps[:, j_ * 256:(j_ + 1) * 256], lhsT=Din[:, c2 * 2 + j_, :], rhs=cst["FA"][0:64, :], start=True, stop=True), r=[dkey, 'hconst'], w=[psk])
                        twiddle(ps, psk, B16, 'B16', c2 * 2, False)
                    for c4 in range(0, 16, 4):
                        pr, prk, pi_, pik = stage2(B16, 'B16', c4)
                        Xr = pr[:, :].rearrange("p (c f) -> p c f", c=4); Xi = pi_[:, :].rearrange("p (c f) -> p c f", c=4)
                        Hr, Hi = Hs[:, c4:c4 + 4, 0, :], Hs[:, c4:c4 + 4, 1, :]
                        kb.op('dve', lambda e: e.tensor_tensor(out=hm[0][:], in0=Xr, in1=Hr, op=ALU.mult), r=[prk, hkey, 'hm0'], w=['hm0'])
                        kb.op('dve', lambda e: e.tensor_tensor(out=hm[1][:], in0=Xi, in1=Hi, op=ALU.mult), r=[pik, hkey, 'hm1'], w=['hm1'])
                        kb.op('dve', lambda e: e.tensor_tensor(out=hm[2][:], in0=Xr, in1=Hi, op=ALU.mult), r=[prk, hkey, 'hm2'], w=['hm2'])
                        kb.op('dve', lambda e: e.tensor_tensor(out=hm[3][:], in0=Xi, in1=Hr, op=ALU.mult), r=[pik, hkey, 'hm3'], w=['hm3'])
                        kb.op('pool', lambda e: e.tensor_tensor(out=Y16[:, c4:c4 + 4, 0, :], in0=hm[0][:], in1=hm[1][:], op=ALU.subtract), r=['hm0', 'hm1', 'Y16'], w=['Y16'])
                        kb.op('pool', lambda e: e.tensor_tensor(out=Y16[:, c4:c4 + 4, 1, :], in0=hm[2][:], in1=hm[3][:], op=ALU.add), r=['hm2', 'hm3', 'Y16'], w=['Y16'])
                    for c2 in range(8):
                        ps, psk = hb()
                        for j_ in range(2):
                            cc = c2 * 2 + j_
                            kb.op('pe', lambda e: e.matmul(ps[:, j_ * 256:(j_ + 1) * 256], lhsT=Y16[:, cc, 0, :], rhs=cst["CA"][:], start=True, stop=False), r=['Y16', 'hconst'], w=[psk])
                            kb.op('pe', lambda e: e.matmul(ps[:, j_ * 256:(j_ + 1) * 256], lhsT=Y16[:, cc, 1, :], rhs=cst["CB"][:], start=False, stop=True), r=['Y16', 'hconst'], w=[psk])
                        twiddle(ps, psk, G16, 'G16', c2 * 2, True)
                    for c4 in range(0, 16, 4):
                        py, pyk = hb()
                        kb.op('pe', lambda e: e.matmul(py[0:64, :], lhsT=cst["FREN"][:], rhs=G16[:, c4:c4 + 4, 0, :], start=True, stop=False), r=['hconst', 'G16'], w=[pyk])
                        kb.op('pe', lambda e: e.matmul(py[0:64, :], lhsT=cst["FIMN"][:], rhs=G16[:, c4:c4 + 4, 1, :], start=False, stop=True), r=['hconst', 'G16'], w=[pyk])
                        kb.op('dve', lambda e: e.tensor_tensor(out=Out[:, c4:c4 + 4, :], in0=py[0:64, :].rearrange("p (c f) -> p c f", c=4), in1=Xmul[:, c4:c4 + 4, :], op=ALU.mult),
                              r=[pyk, xkey, okey], w=[okey])

                for g in range(32):
                    gc0 = g * 16
                    lh = lambda r0: HYC[r0 + gc0:r0 + gc0 + 16, :].rearrange("c (n1 n2) -> n1 c n2", n2=128)
                    kb.dma('sp', lambda e: e.dma_start(out=v16[:], in_=lh(0)), r=['HYC'], w=['v16'])
                    kb.dma('sp', lambda e: e.dma_start(out=x116[:], in_=lh(512)), r=['HYC'], w=['x116'])
                    kb.dma('sp', lambda e: e.dma_start(out=x216[:], in_=lh(1024)), r=['HYC'], w=['x216'])
                    kb.dma('sp', lambda e: e.dma_start(out=H1[:], in_=HSPEC[gc0:gc0 + 16].rearrange("c k r f -> k c r f")), r=['HSPEC'], w=['H1'])
                    kb.dma('sp', lambda e: e.dma_start(out=H2s[:], in_=HSPEC[512 + gc0:512 + gc0 + 16].rearrange("c k r f -> k c r f")), r=['HSPEC'], w=['H2s'])
                    conv16(v16, 'v16', H1, 'H1', x116, 'x116', z16, 'z16')
                    conv16(z16, 'z16', H2s, 'H2s', x216, 'x216', y16, 'y16')
                    kb.dma('pool', lambda e: e.dma_start(out=YHYT[gc0:gc0 + 16, :].rearrange("c (n1 n2) -> n1 c n2", n2=128), in_=y16[:]), r=['y16'], w=['YHYT'])

        if stages >= 5:
            with stage() as st:
                z = sbt(st, "zt", [128, L])
                kb.op('pool', lambda e: e.memset(z[:], 0.0), w=['zt'])
                for q in range(4):
                    if dummy_mix:
                        kb.dma('sp', lambda e: e.dma_start(out=z[:], in_=QT[q * 128:(q + 1) * 128, :]), r=['zt', 'QT'], w=['zt'])
                    if dummy_mix:
                        kb.dma('sp', lambda e: e.dma_start(out=YHYT[q * 128:(q + 1) * 128, :], in_=z[:]), r=['zt'], w=['YHYT'])
                    if dummy_mix:
                        kb.dma('sp', lambda e: e.dma_start(out=z[:], in_=GT[q * 128:(q + 1) * 128, :]), r=['zt', 'GT'], w=['zt'])
                    if dummy_mix:
                        kb.dma('sp', lambda e: e.dma_start(out=YHGT[q * 128:(q + 1) * 128, :], in_=z[:]), r=['zt'], w=['YHGT'])
                zr = sbt(st, "zr", [128, D])
                kb.op('pool', lambda e: e.memset(zr[:], 0.0), w=['zr'])
                if dummy_mix or stages < 7:
                    for i in range(OWN // 128):
                        kb.dma('sp', lambda e: e.dma_start(out=ROUTED[i * 128:(i + 1) * 128, :], in_=zr[:]), r=['zr'], w=['ROUTED'])

        def row_bcast(stack, name, src_row_ap):
            t = sbt(stack, name, [128, D])
            kb.dma('sp', lambda e: e.dma_start(out=t[:], in_=src_row_ap.to_broadcast([128, D])), r=['MODROW'], w=[name])
            return t

        if stages >= 5:
            with stage() as st:
                oidx = sbt(st, "oidx", [128, 4], I32)
                kb.dma('sp', lambda e: e.dma_start(out=oidx[:], in_=OWNIDX), w=['oidx'])
                yg = [sbt(st, "yg%d" % i, [128, OWN]) for i in range(2)]
                n = 0
                for src, skey, r0 in ((YHYT, 'YHYT', 0), (YHGT, 'YHGT', 512)):
                    v = src.rearrange("c (j t) -> (c j) t", j=4)
                    for cc in range(4):
                        g = yg[n % 2]; gk = "yg%d" % (n % 2); n += 1
                        kb.dma('pool', lambda e: e.indirect_dma_start(out=g[:], out_offset=None, in_=v,
                                                                     in_offset=bass.IndirectOffsetOnAxis(ap=oidx[:, cc:cc + 1], axis=0),
                                                                     bounds_check=RB_OWN, oob_is_err=False), r=[skey, 'oidx'], w=[gk])
                        kb.dma('sp', lambda e: e.dma_start(out=YOWN[r0 + cc * 128:r0 + (cc + 1) * 128, :], in_=g[:]), r=[gk], w=['YOWN'])
            with stage() as st:
                Wy = sbt(st, "Wy", [128, 8, D]); Wo = sbt(st, "Wo", [128, 8, D])
                kb.dma('sp', lambda e: e.dma_start(out=Wy[:, 0:4, :], in_=WHY.rearrange("(k p) c -> p k c", p=128)), w=['Wy'])
                kb.dma('sp', lambda e: e.dma_start(out=Wy[:, 4:8, :], in_=WHG.rearrange("(k p) c -> p k c", p=128)), r=['Wy'], w=['Wy'])
                kb.dma('sp', lambda e: e.dma_start(out=Wo[:], in_=WOUT.rearrange("(k p) c -> p k c", p=128)), w=['Wo'])
                g1row = row_bcast(st, "g1row", MODROW[0:1, 2048:3072])
                yb = sbt(st, "yb", [128, 8, 512]); sgb = sbt(st, "sgb", [128, 16, 512]); mT = sbt(st, "mT", [128, 8, 512])
                t1 = [sbt(st, "t1_%d" % i, [128, 512]) for i in range(2)]
                xt = [sbt(st, "xt%d" % i, [128, D]) for i in range(2)]; pt = [sbt(st, "pt%d" % i, [128, D]) for i in range(2)]
                bi = 0
                for blk in range(OWN // 512):
                    t0 = blk * 512
                    kb.dma('sp', lambda e: e.dma_start(out=yb[:], in_=YOWN[:, t0:t0 + 512].rearrange("(k p) t -> p k t", p=128)), r=['YOWN'], w=['yb'])
                    kb.dma('sp', lambda e: e.dma_start(out=sgb[:], in_=SGT[:, t0:t0 + 512].rearrange("(k p) t -> p k t", p=128)), r=['SGT'], w=['sgb'])
                    for dm in range(8):
                        for br in range(2):
                            pb, pbk = banks[bi % 8]; bi += 1
                            for cc in range(4):
                                kb.op('pe', lambda e: e.matmul(pb[:, :], lhsT=Wy[:, br * 4 + cc, dm * 128:(dm + 1) * 128], rhs=yb[:, br * 4 + cc, :],
                                                               start=(cc == 0), stop=(cc == 3)), r=['Wy', 'yb'], w=[pbk])
                            if br == 0:
                                kb.op('dve', lambda e: e.tensor_tensor(out=t1[dm % 2][:], in0=pb[:, :], in1=sgb[:, dm, :], op=ALU.mult),
                                      r=[pbk, 'sgb'], w=['t1_%d' % (dm % 2)])
                            else:
                                kb.op('dve', lambda e: e.tensor_tensor(out=mT[:, dm, :], in0=pb[:, :], in1=sgb[:, 8 + dm, :], op=ALU.mult),
                                      r=[pbk, 'sgb', 'mT'], w=['mT'])
                                kb.op('pool', lambda e: e.tensor_tensor(out=mT[:, dm, :], in0=mT[:, dm, :], in1=t1[dm % 2][:], op=ALU.add),
                                      r=['mT', 't1_%d' % (dm % 2)], w=['mT'])
                    for tt in range(4):
                        ti = blk * 4 + tt; b2 = ti % 2
                        kb.dma('sp', lambda e: e.dma_start(out=xt[b2][:], in_=XOWN[ti * 128:(ti + 1) * 128, :]), w=['xt%d' % b2])
                        kb.dma('sp', lambda e: e.dma_start(out=pt[b2][:], in_=POSOWN[ti * 128:(ti + 1) * 128, :]), w=['pt%d' % b2])
                        kb.op('pool', lambda e: e.tensor_tensor(out=xt[b2][:], in0=xt[b2][:], in1=pt[b2][:], op=ALU.add), r=['xt%d' % b2, 'pt%d' % b2], w=['xt%d' % b2])
                        for hf in range(2):
                            pb, pbk = banks[bi % 8]; bi += 1
                            for kk in range(8):
                                kb.op('pe', lambda e: e.matmul(pb[:, :], lhsT=mT[:, kk, tt * 128:(tt + 1) * 128], rhs=Wo[:, kk, hf * 512:(hf + 1) * 512],
                                                               start=(kk == 0), stop=(kk == 7)), r=['mT', 'Wo'], w=[pbk])
                            kb.op('dve', lambda e: e.tensor_tensor(out=pt[b2][:, hf * 512:(hf + 1) * 512], in0=pb[:, :], in1=g1row[:, hf * 512:(hf + 1) * 512], op=ALU.mult),
                                  r=[pbk, 'g1row', 'pt%d' % b2], w=['pt%d' % b2])
                        kb.op('pool', lambda e: e.tensor_tensor(out=xt[b2][:], in0=xt[b2][:], in1=pt[b2][:], op=ALU.add), r=['xt%d' % b2, 'pt%d' % b2], w=['xt%d' % b2])
                        kb.dma('pool', lambda e: e.dma_start(out=X1D[ti * 128:(ti + 1) * 128, :], in_=xt[b2][:]), r=['xt%d' % b2], w=['X1D'])

        if stages >= 6:
            a2T = sbt(es, "a2T", [128, 8]); sh2T = sbt(es, "sh2T", [128, 8])
            kb.op('dve', lambda e: e.tensor_scalar(out=a2T[:], in0=modT[:, 32:40, 0], scalar1=1.0, scalar2=None, op0=ALU.add), r=['modT'], w=['a2T'])
            kb.op('dve', lambda e: e.tensor_tensor(out=a2T[:], in0=a2T[:], in1=g2T[:], op=ALU.mult), r=['a2T', 'g2T'], w=['a2T'])
            kb.op('dve', lambda e: e.tensor_copy(out=sh2T[:], in_=modT[:, 24:32, 0]), r=['modT'], w=['sh2T'])
            with stage() as s1:
                srcs = [(X1D[i * 128:(i + 1) * 128, :], None, i * 128) for i in range(OWN // 128)]
                kb.res.setdefault('a1T', [None, []])
                norm_to_xt(s1, srcs, H2T, a2T, sh2T, "n2", 'H2T')
            own_blocks = [(t, 512) for t in range(0, OWN, 512)]
            gemm_group("sg", SHG, 8, 256, H2T, 'H2T', own_blocks, [dict(c0=0, cn=256, mode='fm', func=AF.Silu, out=SGA, okey='SGA', oc0=0)])
            gemm_group("su", SHU, 8, 256, H2T, 'H2T', own_blocks, [dict(c0=0, cn=256, mode='fm', func=None, out=SUA, okey='SUA', oc0=0)])
            with stage() as st:
                ga = sbt(st, "ga", [128, 2, OWN]); ua = sbt(st, "ua", [128, 2, OWN])
                kb.dma('sp', lambda e: e.dma_start(out=ga[:], in_=SGA.rearrange("(k p) t -> p k t", p=128)), r=['SGA'], w=['ga'])
                kb.dma('sp', lambda e: e.dma_start(out=ua[:], in_=SUA.rearrange("(k p) t -> p k t", p=128)), r=['SUA'], w=['ua'])
                kb.op('dve', lambda e: e.tensor_tensor(out=ga[:], in0=ga[:], in1=ua[:], op=ALU.mult), r=['ga', 'ua'], w=['ga'])
                kb.dma('pool', lambda e: e.dma_start(out=ACTT.rearrange("(k p) t -> p k t", p=128), in_=ga[:]), r=['ga'], w=['ACTT'])
            gemm_group("sd", SHD, 2, 1024, ACTT, 'ACTT', own_blocks,
                       [dict(c0=0, cn=512, mode='tm', func=None, out=SHOUT, okey='SHOUT', oc0=0),
                        dict(c0=512, cn=512, mode='tm', func=None, out=SHOUT, okey='SHOUT', oc0=512)])
            if stages >= 7 and not dummy_mix:
                with stage() as st:
                    a2row = sbt(st, "a2row", [128, D]); g2nrow = sbt(st, "g2nrow", [128, D])
                    kb.dma('sp', lambda e: e.dma_start(out=a2row[:], in_=MODROW[0:1, 4096:5120].to_broadcast([128, D])), r=['MODROW'], w=['a2row'])
                    kb.dma('sp', lambda e: e.dma_start(out=g2nrow[:], in_=N2G.rearrange("(o n) -> o n", o=1).to_broadcast([128, D])), w=['g2nrow'])
                    kb.op('dve', lambda e: e.scalar_tensor_tensor(out=a2row[:], in0=a2row[:], scalar=1.0, in1=g2nrow[:], op0=ALU.add, op1=ALU.mult),
                          r=['a2row', 'g2nrow'], w=['a2row'])
                    sh2row = row_bcast(st, "sh2row", MODROW[0:1, 3072:4096])
                    xa = [sbt(st, "hxa%d" % i, [128, D]) for i in range(2)]; stt = [sbt(st, "hst%d" % i, [128, 4]) for i in range(2)]
                    junk = sbt(st, "hjunk", [128, D])
                    for ti in range(OWN // 128):
                        b2 = ti % 2; xk, tk = 'hxa%d' % b2, 'hst%d' % b2
                        rows = slice(ti * 128, (ti + 1) * 128)
                        kb.dma('sp', lambda e: e.dma_start(out=xa[b2][:], in_=X1D[rows, :]), r=['X1D'], w=[xk])
                        kb.op('act', lambda e: e.activation(out=junk[:], in_=xa[b2][:], func=AF.Square, accum_out=stt[b2][:, 0:1]), r=[xk], w=['hjunk', tk])
                        kb.op('dve', lambda e: e.tensor_scalar(out=stt[b2][:, 1:2], in0=stt[b2][:, 0:1], scalar1=1.0 / D, scalar2=EPS, op0=ALU.mult, op1=ALU.add), r=[tk], w=[tk])
                        kb.op('act', lambda e: e.activation(out=stt[b2][:, 2:3], in_=stt[b2][:, 1:2], func=AF.Sqrt), r=[tk], w=[tk])
                        kb.op('dve', lambda e: e.reciprocal(out=stt[b2][:, 3:4], in_=stt[b2][:, 2:3]), r=[tk], w=[tk])
                        kb.op('dve', lambda e: e.scalar_tensor_tensor(out=xa[b2][:], in0=xa[b2][:], scalar=stt[b2][:, 3:4], in1=a2row[:], op0=ALU.mult, op1=ALU.mult),
                              r=[xk, tk, 'a2row'], w=[xk])
                        kb.op('pool', lambda e: e.tensor_tensor(out=xa[b2][:], in0=xa[b2][:], in1=sh2row[:], op=ALU.add), r=[xk, 'sh2row'], w=[xk])
                        kb.dma('pool', lambda e: e.dma_start(out=H2[rows, :], in_=xa[b2][:]), r=[xk], w=['H2'])
                gemm_group("rt", RW, 8, NE, H2T, 'H2T', own_blocks, [dict(c0=0, cn=NE, mode='tm', func=AF.Sigmoid, out=SCORES, okey='SCORES', oc0=0)])
                with stage() as st:
                    NT = OWN // 128
                    onesm = sbt(st, "onesm", [128, 128]); stri = sbt(st, "stri", [128, 128]); slt = sbt(st, "slt", [128, 512])
                    blk128 = sbt(st, "blk128", [128, NBLK]); pidx = sbt(st, "pidx", [128, 1]); brow_ = sbt(st, "rbrow", [128, NE])
                    kb.dma('sp', lambda e: e.dma_start(out=onesm[:], in_=ONESd), w=['onesm'])
                    kb.dma('sp', lambda e: e.dma_start(out=stri[:], in_=STRId), w=['stri'])
                    kb.dma('sp', lambda e: e.dma_start(out=slt[:], in_=SLTd), w=['slt'])
                    kb.dma('sp', lambda e: e.dma_start(out=blk128[:], in_=BLKd), w=['blk128'])
                    kb.dma('sp', lambda e: e.dma_start(out=pidx[:], in_=PIDXd), w=['pidx'])
                    kb.dma('sp', lambda e: e.dma_start(out=brow_[:], in_=RB.rearrange("(o n) -> o n", o=1).to_broadcast([128, NE])), w=['rbrow'])
                    D8F = sbt(st, "D8F", [128, NT, 8]); W8 = sbt(st, "W8", [128, NT, 8]); D8I = sbt(st, "D8I", [128, NT * 8], I32); GI = sbt(st, "GI", [128, NBLK], I32)
                    with stage() as sr:
                        MSK = sbt(sr, "MSK", [128, NT, NE]); SEL = sbt(sr, "SEL", [128, NT, NE]); WD = sbt(sr, "WDm", [128, NT, NE]); DST = sbt(sr, "DST", [128, NT, NE])
                        V8 = sbt(sr, "V8", [128, NT, 8])
                        sc_ = [sbt(sr, "rsc%d" % i, [128, NE]) for i in range(2)]; bs = sbt(sr, "rbs", [128, NE])
                        M8 = sbt(sr, "M8", [128, 8, 8]); gs = sbt(sr, "rgs", [128, 8]); g8 = sbt(sr, "rg8", [128, 8]); gm = sbt(sr, "rgm", [128, 8]); pen = sbt(sr, "rpen", [128, 8])
                        den = sbt(sr, "rden", [128, 2]); base = sbt(sr, "rbase", [128, NE]); tmpq = sbt(sr, "rtmpq", [128, NE])
                        kb.op('pool', lambda e: e.memset(base[:], 0.0), w=['rbase'])
                        for ti in range(NT):
                            b2 = ti % 2; sk_ = 'rsc%d' % b2
                            kb.dma('sp', lambda e: e.dma_start(out=sc_[b2][:], in_=SCORES[ti * 128:(ti + 1) * 128, :]), r=['SCORES'], w=[sk_])
                            kb.op('dve', lambda e: e.tensor_tensor(out=bs[:], in0=sc_[b2][:], in1=brow_[:], op=ALU.add), r=[sk_, 'rbrow'], w=['rbs'])
                            for g in range(8):
                                kb.op('dve', lambda e: e.max(out=M8[:, g, :], in_=bs[:, 32 * g:32 * g + 32]), r=['rbs', 'M8'], w=['M8'])
                            kb.op('dve', lambda e: e.tensor_tensor(out=gs[:], in0=M8[:, :, 0], in1=M8[:, :, 1], op=ALU.add), r=['M8'], w=['rgs'])
                            kb.op('dve', lambda e: e.max(out=g8[:], in_=gs[:]), r=['rgs'], w=['rg8'])
                            kb.op('dve', lambda e: e.tensor_scalar(out=gm[:], in0=gs[:], scalar1=g8[:, 3:4], scalar2=None, op0=ALU.is_ge), r=['rgs', 'rg8'], w=['rgm'])
                            kb.op('dve', lambda e: e.tensor_scalar(out=pen[:], in0=gm[:], scalar1=-1.0, scalar2=1e30, op0=ALU.add, op1=ALU.mult), r=['rgm'], w=['rpen'])
                            for g in range(8):
                                kb.op('dve', lambda e: e.tensor_scalar(out=MSK[:, ti, 32 * g:32 * g + 32], in0=bs[:, 32 * g:32 * g + 32], scalar1=gm[:, g:g + 1], scalar2=pen[:, g:g + 1],
                                                                       op0=ALU.mult, op1=ALU.add), r=['rbs', 'rgm', 'rpen', 'MSK'], w=['MSK'])
                            kb.op('dve', lambda e: e.max(out=V8[:, ti, :], in_=MSK[:, ti, :]), r=['MSK', 'V8'], w=['V8'])
                            kb.op('dve', lambda e: e.tensor_scalar(out=SEL[:, ti, :], in0=MSK[:, ti, :], scalar1=V8[:, ti, 7:8], scalar2=None, op0=ALU.is_ge), r=['MSK', 'V8', 'SEL'], w=['SEL'])
                            kb.op('dve', lambda e: e.tensor_tensor(out=WD[:, ti, :], in0=SEL[:, ti, :], in1=sc_[b2][:], op=ALU.mult), r=['SEL', sk_, 'WDm'], w=['WDm'])
                            kb.op('dve', lambda e: e.tensor_reduce(out=den[:, 0:1], in_=WD[:, ti, :], axis=AX.X, op=ALU.add), r=['WDm', 'rden'], w=['rden'])
                            kb.op('dve', lambda e: e.reciprocal(out=den[:, 1:2], in_=den[:, 0:1]), r=['rden'], w=['rden'])
                            kb.op('dve', lambda e: e.tensor_scalar(out=WD[:, ti, :], in0=WD[:, ti, :], scalar1=den[:, 1:2], scalar2=2.5, op0=ALU.mult, op1=ALU.mult), r=['WDm', 'rden'], w=['WDm'])
                            p1, p1k = banks[(2 * ti) % 8]; p2, p2k = banks[(2 * ti + 1) % 8]
                            kb.op('pe', lambda e: e.matmul(p1[:, 0:NE], lhsT=stri[:], rhs=SEL[:, ti, :], start=True, stop=True), r=['stri', 'SEL'], w=[p1k])
                            kb.op('pe', lambda e: e.matmul(p2[:, 0:NE], lhsT=onesm[:], rhs=SEL[:, ti, :], start=True, stop=True), r=['onesm', 'SEL'], w=[p2k])
                            kb.op('dve', lambda e: e.tensor_tensor(out=DST[:, ti, :], in0=p1[:, 0:NE], in1=base[:], op=ALU.add), r=[p1k, 'rbase', 'DST'], w=['DST'])
                            kb.op('dve', lambda e: e.tensor_tensor(out=base[:], in0=p2[:, 0:NE], in1=base[:], op=ALU.add), r=[p2k, 'rbase'], w=['rbase'])
                        ci = sbt(sr, "rci", [128, NE], I32); padded = sbt(sr, "rpad", [128, NE]); pstart = sbt(sr, "rpst", [128, NE]); pend = sbt(sr, "rpend", [128, NE])
                        kb.op('dve', lambda e: e.tensor_scalar(out=tmpq[:], in0=base[:], scalar1=127.0, scalar2=None, op0=ALU.add), r=['rbase'], w=['rtmpq'])
                        kb.op('dve', lambda e: e.tensor_copy(out=ci[:], in_=tmpq[:]), r=['rtmpq'], w=['rci'])
                        kb.op('dve', lambda e: e.tensor_scalar(out=ci[:], in0=ci[:], scalar1=7, scalar2=None, op0=ALU.arith_shift_right), r=['rci'], w=['rci'])
                        kb.op('dve', lambda e: e.tensor_scalar(out=ci[:], in0=ci[:], scalar1=7, scalar2=None, op0=ALU.logical_shift_left), r=['rci'], w=['rci'])
                        kb.op('dve', lambda e: e.tensor_copy(out=padded[:], in_=ci[:]), r=['rci'], w=['rpad'])
                        padT = sbt(sr, "rpadT", [128, 2, 128]); pendT = sbt(sr, "rpendT", [128, 2, 128])
                        pa, pak = banks[0]
                        for hh in range(2):
                            kb.op('pe', lambda e: e.transpose(out=pa[:, hh * 128:(hh + 1) * 128], in_=padded[:, hh * 128:(hh + 1) * 128], identity=ident[:]), r=['rpad', 'ident'], w=[pak])
                        kb.op('dve', lambda e: e.tensor_copy(out=padT[:], in_=pa[:, 0:256].rearrange("p (h c) -> p h c", h=2)), r=[pak], w=['rpadT'])
                        pb_, pbk_ = banks[1]
                        for hh in range(2):
                            kb.op('pe', lambda e: e.matmul(pb_[:, 0:NE], lhsT=padT[:, hh, :], rhs=slt[:, hh * 256:(hh + 1) * 256], start=(hh == 0), stop=(hh == 1)), r=['rpadT', 'slt'], w=[pbk_])
                        kb.op('dve', lambda e: e.tensor_copy(out=pstart[:], in_=pb_[:, 0:NE]), r=[pbk_], w=['rpst'])
                        kb.op('dve', lambda e: e.tensor_tensor(out=pend[:], in0=pstart[:], in1=padded[:], op=ALU.add), r=['rpst', 'rpad'], w=['rpend'])
                        pc_, pck_ = banks[2]
                        for hh in range(2):
                            kb.op('pe', lambda e: e.transpose(out=pc_[:, hh * 128:(hh + 1) * 128], in_=pend[:, hh * 128:(hh + 1) * 128], identity=ident[:]), r=['rpend', 'ident'], w=[pck_])
                        kb.op('dve', lambda e: e.tensor_copy(out=pendT[:], in_=pc_[:, 0:256].rearrange("p (h c) -> p h c", h=2)), r=[pck_], w=['rpendT'])
                        cmpT = sbt(sr, "rcmpT", [128, 2, NBLK]); bef = sbt(sr, "rbef", [128, NBLK])
                        for hh in range(2):
                            kb.op('dve', lambda e: e.tensor_scalar(out=cmpT[:, hh, :], in0=blk128[:], scalar1=pendT[:, hh, 0:1], scalar2=None, op0=ALU.is_ge), r=['blk128', 'rpendT', 'rcmpT'], w=['rcmpT'])
                        pd_, pdk_ = banks[3]
                        for hh in range(2):
                            kb.op('pe', lambda e: e.matmul(pd_[:, 0:NBLK], lhsT=onesm[:], rhs=cmpT[:, hh, :], start=(hh == 0), stop=(hh == 1)), r=['onesm', 'rcmpT'], w=[pdk_])
                        kb.op('dve', lambda e: e.tensor_scalar(out=bef[:], in0=pd_[:, 0:NBLK], scalar1=255.0, scalar2=128.0, op0=ALU.min, op1=ALU.mult), r=[pdk_], w=['rbef'])
                        kb.op('dve', lambda e: e.tensor_scalar(out=bef[:], in0=bef[:], scalar1=pidx[:, 0:1], scalar2=None, op0=ALU.add), r=['rbef', 'pidx'], w=['rbef'])
                        kb.op('dve', lambda e: e.tensor_copy(out=GI[:], in_=bef[:]), r=['rbef'], w=['GI'])
                        eqj = sbt(sr, "reqj", [128, NE])
                        for ti in range(NT):
                            kb.op('dve', lambda e: e.tensor_tensor(out=DST[:, ti, :], in0=DST[:, ti, :], in1=pstart[:], op=ALU.add), r=['DST', 'rpst'], w=['DST'])
                            for k8 in range(8):
                                kb.op('dve', lambda e: e.scalar_tensor_tensor(out=eqj[:], in0=MSK[:, ti, :], scalar=V8[:, ti, k8:k8 + 1], in1=DST[:, ti, :], op0=ALU.is_equal, op1=ALU.mult),
                                      r=['MSK', 'V8', 'DST', 'reqj'], w=['reqj'])
                                kb.op('dve', lambda e: e.tensor_reduce(out=D8F[:, ti, k8:k8 + 1], in_=eqj[:], axis=AX.X, op=ALU.add), r=['reqj', 'D8F'], w=['D8F'])
                                kb.op('dve', lambda e: e.scalar_tensor_tensor(out=eqj[:], in0=MSK[:, ti, :], scalar=V8[:, ti, k8:k8 + 1], in1=WD[:, ti, :], op0=ALU.is_equal, op1=ALU.mult),
                                      r=['MSK', 'V8', 'WDm', 'reqj'], w=['reqj'])
                                kb.op('dve', lambda e: e.tensor_reduce(out=W8[:, ti, k8:k8 + 1], in_=eqj[:], axis=AX.X, op=ALU.add), r=['reqj', 'W8'], w=['W8'])
                        kb.op('dve', lambda e: e.tensor_copy(out=D8I[:], in_=D8F[:].rearrange("p t k -> p (t k)")), r=['D8F'], w=['D8I'])
                    dbg('D8F', D8F[:], [128, NT, 8]); dbg('W8', W8[:], [128, NT, 8])
                    ht = [sbt(st, "dht%d" % i, [128, D]) for i in range(2)]
                    for ti in range(NT):
                        b2 = ti % 2; hk = 'dht%d' % b2
                        kb.dma('sp', lambda e: e.dma_start(out=ht[b2][:], in_=H2[ti * 128:(ti + 1) * 128, :]), r=['H2'], w=[hk])
                        for k8 in range(8):
                            kb.dma('pool', lambda e: e.indirect_dma_start(out=XS, out_offset=bass.IndirectOffsetOnAxis(ap=D8I[:, ti * 8 + k8:ti * 8 + k8 + 1], axis=0), in_=ht[b2][:], in_offset=None,
                                                                         bounds_check=RB_XS, oob_is_err=False), r=[hk, 'D8I'], w=['XSw'])
                    kb.barrier()
                    NW = 3
                    wgu = [sbt(st, "wgu%d" % i, [128, 2, 8, 256]) for i in range(NW)]; wdn = [sbt(st, "wdn%d" % i, [128, 2, D]) for i in range(4)]
                    xs = [sbt(st, "xs%d" % i, [128, D]) for i in range(2)]; xsT = [sbt(st, "xsT%d" % i, [128, 8, 128]) for i in range(3)]
                    actT = [sbt(st, "actT%d" % i, [128, 2, 128]) for i in range(3)]; sg_ = [sbt(st, "esg%d" % i, [128, 256]) for i in range(3)]
                    ys = [sbt(st, "ys%d" % i, [128, D]) for i in range(2)]
                    bctr = [0]

                    def bk():
                        b_ = banks[bctr[0] % 8]; bctr[0] += 1
                        return b_

                    def phA(blk):
                        wk, dk, xk, xtk = 'wgu%d' % (blk % NW), 'wdn%d' % (blk % 4), 'xs%d' % (blk % 2), 'xsT%d' % (blk % 3)
                        kb.dma('pool', lambda e: e.indirect_dma_start(out=wgu[blk % NW][:].rearrange("p a k f -> p (a k f)"), out_offset=None, in_=EWGU,
                                                                     in_offset=bass.IndirectOffsetOnAxis(ap=GI[:, blk:blk + 1], axis=0),
                                                                     bounds_check=RB_W, oob_is_err=False), r=['GI'], w=[wk])
                        kb.dma('pool', lambda e: e.indirect_dma_start(out=wdn[blk % 4][:].rearrange("p k f -> p (k f)"), out_offset=None, in_=EWD,
                                                                     in_offset=bass.IndirectOffsetOnAxis(ap=GI[:, blk:blk + 1], axis=0),
                                                                     bounds_check=RB_W, oob_is_err=False), r=['GI'], w=[dk])
                        kb.dma('sp', lambda e: e.dma_start(out=xs[blk % 2][:], in_=XS[blk * 128:(blk + 1) * 128, :]), r=['XSw'], w=[xk])
                        for hh in range(2):
                            pb, pbk = bk()
                            for kk in range(4):
                                k8 = hh * 4 + kk
                                kb.op('pe', lambda e: e.transpose(out=pb[:, kk * 128:(kk + 1) * 128], in_=xs[blk % 2][:, k8 * 128:(k8 + 1) * 128], identity=ident[:]), r=[xk, 'ident'], w=[pbk])
                            evac(xsT[blk % 3][:, hh * 4:hh * 4 + 4, :], pb[:, :].rearrange("p (k t) -> p k t", k=4), None, [pbk, xtk], [xtk])

                    def phB(blk):
                        wk, xtk, sgk = 'wgu%d' % (blk % NW), 'xsT%d' % (blk % 3), 'esg%d' % (blk % 3)
                        ph, phk = bk()
                        for kk in range(8):
                            kb.op('pe', lambda e: e.matmul(ph[:, :].rearrange("p (a f) -> p a f", a=2), lhsT=xsT[blk % 3][:, kk, :], rhs=wgu[blk % NW][:, :, kk, :],
                                                           start=(kk == 0), stop=(kk == 7)), r=[wk, xtk], w=[phk])
                        kb.op('act', lambda e: e.activation(out=sg_[blk % 3][:], in_=ph[:, 0:256], func=AF.Silu), r=[phk], w=[sgk])
                        kb.op('dve', lambda e: e.tensor_tensor(out=sg_[blk % 3][:], in0=ph[:, 256:512], in1=sg_[blk % 3][:], op=ALU.mult), r=[phk, sgk], w=[sgk])

                    def phC(blk):
                        sgk, ak = 'esg%d' % (blk % 3), 'actT%d' % (blk % 3)
                        pt_, ptk = bk()
                        for kk in range(2):
                            kb.op('pe', lambda e: e.transpose(out=pt_[:, kk * 128:(kk + 1) * 128], in_=sg_[blk % 3][:, kk * 128:(kk + 1) * 128], identity=ident[:]), r=[sgk, 'ident'], w=[ptk])
                        evac(actT[blk % 3][:].rearrange("p k t -> p (k t)"), pt_[:, 0:256], None, [ptk, ak], [ak])

                    def phD(blk):
                        dk, ak, yk = 'wdn%d' % (blk % 4), 'actT%d' % (blk % 3), 'ys%d' % (blk % 2)
                        for hf in range(2):
                            py, pyk = bk()
                            for kk in range(2):
                                kb.op('pe', lambda e: e.matmul(py[:, :], lhsT=actT[blk % 3][:, kk, :], rhs=wdn[blk % 4][:, kk, hf * 512:(hf + 1) * 512], start=(kk == 0), stop=(kk == 1)), r=[ak, dk], w=[pyk])
                            evac(ys[blk % 2][:, hf * 512:(hf + 1) * 512], py[:, :], None, [pyk, yk], [yk])
                        kb.dma('sp', lambda e: e.dma_start(out=YS[blk * 128:(blk + 1) * 128, :], in_=ys[blk % 2][:]), r=[yk], w=['YSw'])

                    for s_ in range(NBLK + 3):
                        if s_ < NBLK:
                            phA(s_)
                        if 0 <= s_ - 1 < NBLK:
                            phB(s_ - 1)
                        if 0 <= s_ - 2 < NBLK:
                            phC(s_ - 2)
                        if 0 <= s_ - 3 < NBLK:
                            phD(s_ - 3)
                    kb.barrier()
                    acc = [sbt(st, "cacc%d" % i, [128, D]) for i in range(2)]; gg = [sbt(st, "cg%d" % i, [128, D]) for i in range(3)]
                    gi_ = 0
                    for ti in range(NT):
                        b2 = ti % 2; ack = 'cacc%d' % b2
                        for k8 in range(8):
                            g3 = gi_ % 3; gi_ += 1; ggk = 'cg%d' % g3
                            kb.dma('pool', lambda e: e.indirect_dma_start(out=gg[g3][:], out_offset=None, in_=YS, in_offset=bass.IndirectOffsetOnAxis(ap=D8I[:, ti * 8 + k8:ti * 8 + k8 + 1], axis=0),
                                                                         bounds_check=RB_XS, oob_is_err=False), r=['YSw', 'D8I'], w=[ggk])
                            if k8 == 0:
                                kb.op('dve', lambda e: e.tensor_scalar(out=acc[b2][:], in0=gg[g3][:], scalar1=W8[:, ti, 0:1], scalar2=None, op0=ALU.mult), r=[ggk, 'W8', ack], w=[ack])
                            else:
                                kb.op('dve', lambda e: e.scalar_tensor_tensor(out=acc[b2][:], in0=gg[g3][:], scalar=W8[:, ti, k8:k8 + 1], in1=acc[b2][:], op0=ALU.mult, op1=ALU.add),
                                      r=[ggk, 'W8', ack], w=[ack])
                        kb.dma('sp', lambda e: e.dma_start(out=ROUTED[ti * 128:(ti + 1) * 128, :], in_=acc[b2][:]), r=[ack], w=['ROUTED'])

            with stage() as st:
                g2row = row_bcast(st, "g2row", MODROW[0:1, 5120:6144])
                fgrow = sbt(st, "fgrow", [128, D])
                kb.dma('sp', lambda e: e.dma_start(out=fgrow[:], in_=FING.rearrange("(o n) -> o n", o=1).to_broadcast([128, D])), w=['fgrow'])
                xa = [sbt(st, "xa%d" % i, [128, D]) for i in range(2)]; sa = [sbt(st, "sa%d" % i, [128, D]) for i in range(2)]
                ra = [sbt(st, "ra%d" % i, [128, D]) for i in range(2)]; stt = [sbt(st, "stt%d" % i, [128, 4]) for i in range(2)]
                junk = sbt(st, "fjunk", [128, D])
                for ti in range(OWN // 128):
                    b2 = ti % 2; xk, sk, rk, tk = 'xa%d' % b2, 'sa%d' % b2, 'ra%d' % b2, 'stt%d' % b2
                    rows = slice(ti * 128, (ti + 1) * 128)
                    kb.dma('sp', lambda e: e.dma_start(out=xa[b2][:], in_=X1D[rows, :]), r=['X1D'], w=[xk])
                    kb.dma('sp', lambda e: e.dma_start(out=sa[b2][:], in_=SHOUT[rows, :]), r=['SHOUT'], w=[sk])
                    kb.dma('sp', lambda e: e.dma_start(out=ra[b2][:], in_=ROUTED[rows, :]), r=['ROUTED'], w=[rk])
                    kb.op('pool', lambda e: e.tensor_tensor(out=sa[b2][:], in0=sa[b2][:], in1=ra[b2][:], op=ALU.add), r=[sk, rk], w=[sk])
                    kb.op('dve', lambda e: e.tensor_tensor(out=sa[b2][:], in0=sa[b2][:], in1=g2row[:], op=ALU.mult), r=[sk, 'g2row'], w=[sk])
                    kb.op('pool', lambda e: e.tensor_tensor(out=xa[b2][:], in0=xa[b2][:], in1=sa[b2][:], op=ALU.add), r=[xk, sk], w=[xk])
                    kb.op('act', lambda e: e.activation(out=junk[:], in_=xa[b2][:], func=AF.Square, accum_out=stt[b2][:, 0:1]), r=[xk], w=['fjunk', tk])
                    kb.op('dve', lambda e: e.tensor_scalar(out=stt[b2][:, 1:2], in0=stt[b2][:, 0:1], scalar1=1.0 / D, scalar2=EPS, op0=ALU.mult, op1=ALU.add), r=[tk], w=[tk])
                    kb.op('act', lambda e: e.activation(out=stt[b2][:, 2:3], in_=stt[b2][:, 1:2], func=AF.Sqrt), r=[tk], w=[tk])
                    kb.op('dve', lambda e: e.reciprocal(out=stt[b2][:, 3:4], in_=stt[b2][:, 2:3]), r=[tk], w=[tk])
                    kb.op('dve', lambda e: e.scalar_tensor_tensor(out=xa[b2][:], in0=xa[b2][:], scalar=stt[b2][:, 3:4], in1=fgrow[:], op0=ALU.mult, op1=ALU.mult),
                          r=[xk, tk, 'fgrow'], w=[xk])
                    kb.dma('pool', lambda e: e.dma_start(out=OUT[rows, :], in_=xa[b2][:]), r=[xk], w=['OUT'])

        kb.finish('sp')
        kb.finish('pool')
        pg.ninstr = kb.ninstr
    return pg


_PROG = None


def make_in_maps(pg, inputs):
    hc = host_consts()
    sq = lambda a: np.ascontiguousarray(a[0])
    in_maps = []
    shared = {}
    if 'EWGU' in pg.ins:
        wg = np.asarray(inputs['exp_w_gate'])[0].reshape(NE, 8, 128, 256)
        wu = np.asarray(inputs['exp_w_up'])[0].reshape(NE, 8, 128, 256)
        ew = np.empty((NE, 128, 2, 8, 256), np.float32)
        ew[:, :, 0] = wg.transpose(0, 2, 1, 3); ew[:, :, 1] = wu.transpose(0, 2, 1, 3)
        shared['EWGU'] = ew.reshape(NE * 128, 4096)
        shared['EWD'] = np.ascontiguousarray(np.asarray(inputs['exp_w_down'])[0].reshape(NE, 2, 128, D).transpose(0, 2, 1, 3)).reshape(NE * 128, 2048)
    for c in range(8):
        b, j = c // 4, c % 4
        own = slice(j * OWN, (j + 1) * OWN)
        idx = ((np.arange(4)[None, :] * 128 + np.arange(128)[:, None]) * 4 + j).astype(np.int32)
        full = {
            'x': inputs['x'][b], 'ctx': inputs['ctx'][b], 'xown': inputs['x'][b, own], 'posown': hc['POS'][own],
            'c': inputs['c'][b], 'c_ctx': inputs['c_ctx'], 'final_g': inputs['final_g'], 'OWNIDX': idx,
            'hg_lb_logits': np.asarray(inputs['hg_lb_logits']).reshape(2, 1024),
        }
        full.update(shared)
        for k in pg.ins:
            if k not in full and k not in hc:
                full[k] = sq(inputs[k])
        full.update(hc)
        in_maps.append({k: np.ascontiguousarray(np.asarray(full[k])) for k in pg.ins})
    return in_maps


def kernel(**inputs):
    global _PROG
    if _PROG is None:
        _PROG = build()
    pg = _PROG
    in_maps = make_in_maps(pg, inputs)
    res = run_bass_kernel_spmd(pg.nc, in_maps, core_ids=list(range(8)))
    out = np.zeros((2, L, D), np.float32)
    for c in range(8):
        b, j = c // 4, c % 4
        out[b, j * OWN:(j + 1) * OWN] = res.results[c]['out']
    return out
```

```python
import math
import numpy as np
from contextlib import ExitStack, contextmanager
import concourse.bass as bass
import concourse.mybir as mybir
from concourse.bass_utils import run_bass_kernel_spmd

F32 = mybir.dt.float32
I32 = mybir.dt.int32
U32 = mybir.dt.uint32
ALU = mybir.AluOpType
AF = mybir.ActivationFunctionType
AX = mybir.AxisListType

N_DMA_SEMS = 24
D = 1024
L = 8192
NCTX = 256
LT = L + NCTX
OWN = 2048
NE = 256
NBLK = 383
EPS = 1e-6
TWO_PI = 2.0 * math.pi


class KB:
    def __init__(self, nc, es):
        self.nc = nc
        self.engs = {'pe': nc.tensor, 'act': nc.scalar, 'dve': nc.vector, 'pool': nc.gpsimd, 'sp': nc.sync}
        self.sems = {}
        self.cnt = {}
        for e in self.engs:
            self.sems[e] = es.enter_context(nc.semaphore("s_" + e))
            self.cnt[e] = 0
        for i in range(N_DMA_SEMS):
            self.sems['d%d' % i] = es.enter_context(nc.semaphore("s_d%d" % i))
            self.cnt['d%d' % i] = 0
        self.dnext = 0
        self.waited = {e: {} for e in self.engs}
        self.res = {}
        self.ninstr = 0

    def _need(self, eng, toks):
        best = {}
        for t in toks:
            if t is None:
                continue
            sk, v = t
            if sk == eng and eng == 'pe':
                continue
            if best.get(sk, 0) < v:
                best[sk] = v
        for sk, v in best.items():
            if self.waited[eng].get(sk, 0) >= v:
                continue
            self.engs[eng].wait_ge(self.sems[sk], v)
            self.waited[eng][sk] = v

    def _deps(self, r, w):
        toks = []
        for k in r:
            st = self.res.get(k)
            if st is not None:
                toks.append(st[0])
        for k in w:
            st = self.res.get(k)
            if st is not None:
                toks.append(st[0])
                toks.extend(st[1])
        return toks

    def _commit(self, tok, r, w):
        for k in r:
            st = self.res.setdefault(k, [None, []])
            st[1].append(tok)
            if len(st[1]) > 32:
                best = {}
                for sk, v in st[1]:
                    if best.get(sk, 0) < v:
                        best[sk] = v
                st[1] = list(best.items())
        for k in w:
            self.res[k] = [tok, []]

    def op(self, eng, fn, r=(), w=()):
        self._need(eng, self._deps(r, w))
        ins = fn(self.engs[eng])
        self.cnt[eng] += 1
        ins.then_inc(self.sems[eng], 1)
        self._commit((eng, self.cnt[eng]), r, w)
        self.ninstr += 1

    def dma(self, q, fn, r=(), w=()):
        i = self.dnext
        self.dnext = (self.dnext + 1) % N_DMA_SEMS
        sk = 'd%d' % i
        toks = self._deps(r, w)
        if self.cnt[sk] > 0:
            toks.append((sk, self.cnt[sk]))
        self._need(q, toks)
        ins = fn(self.engs[q])
        self.cnt[sk] += 16
        ins.then_inc(self.sems[sk], 16)
        self._commit((sk, self.cnt[sk]), r, w)
        self.ninstr += 1

    def barrier(self):
        toks = [(sk, v) for sk, v in self.cnt.items() if v > 0]
        for e in self.engs:
            self._need(e, toks)

    def finish(self, eng):
        toks = []
        for st in self.res.values():
            toks.append(st[0])
            toks.extend(st[1])
        self._need(eng, toks)


_CONST = None


def host_consts():
    global _CONST
    if _CONST is not None:
        return _CONST
    c = {}
    quarter = D // 4
    omega = (1.0 / (np.float32(10000.0) ** (np.arange(quarter, dtype=np.float32) / np.float32(quarter)))).astype(np.float32)
    rows, cols = L // 64, 64
    ang_r = (np.arange(rows, dtype=np.float32)[:, None] * omega).astype(np.float32)
    ang_c = (np.arange(cols, dtype=np.float32)[:, None] * omega).astype(np.float32)
    emb_r = np.concatenate([np.sin(ang_r), np.cos(ang_r)], -1)
    emb_c = np.concatenate([np.sin(ang_c), np.cos(ang_c)], -1)
    emb = np.concatenate([np.broadcast_to(emb_r[:, None], (rows, cols, D // 2)),
                          np.broadcast_to(emb_c[None], (rows, cols, D // 2))], -1)
    c['POS'] = np.ascontiguousarray(emb.reshape(L, D).astype(np.float32))
    c['IDENT'] = np.eye(128, dtype=np.float32)
    si = np.arange(128)[:, None]; ti = np.arange(128)[None, :]
    same = (si // 64) == (ti // 64)
    c['TRIF'] = (same & (si <= ti)).astype(np.float32)
    c['TRIB'] = (same & (si >= ti)).astype(np.float32)
    c['ONES'] = np.ones((128, 128), np.float32)
    c['STRI'] = (si < ti).astype(np.float32)
    e1 = np.arange(128)[:, None]; e2 = np.arange(256)[None, :]
    c['SLT'] = np.concatenate([(e1 < e2), (e1 + 128 < e2)], 1).astype(np.float32)
    c['BLK128'] = np.broadcast_to((np.arange(NBLK, dtype=np.float32) * 128.0)[None, :], (128, NBLK)).copy()
    c['PIDX'] = np.arange(128, dtype=np.float32).reshape(128, 1).copy()
    NN = 16384
    a = np.arange(128, dtype=np.float64)
    ang = 2.0 * np.pi * np.outer(a, a) / 128.0
    Fre = np.cos(ang); Fim = -np.sin(ang)
    f32 = lambda v: np.ascontiguousarray(v.astype(np.float32))
    c['FA'] = f32(np.concatenate([Fre, Fim], 1)); c['FAH'] = f32(np.concatenate([Fre, Fim], 1)[64:128])
    c['FRE'] = f32(Fre); c['FIM'] = f32(Fim); c['NFIM'] = f32(-Fim)
    c['CA'] = f32(np.concatenate([Fre, -Fim], 1)); c['CB'] = f32(np.concatenate([Fim, Fre], 1))
    c['FREN'] = f32(Fre[:, :64] / NN); c['FIMN'] = f32(Fim[:, :64] / NN)
    angT = 2.0 * np.pi * np.outer(a, a) / NN
    c['TRE2'] = f32(np.concatenate([np.cos(angT), np.cos(angT)], 1)); c['TIM2'] = f32(np.concatenate([-np.sin(angT), -np.sin(angT)], 1))
    bands = np.linspace(1e-4, 15.0, 16, dtype=np.float32)
    def zfeat(t):
        t = t.astype(np.float32)
        tn = (t / np.float32(L - 1)).astype(np.float32)
        an = (np.float32(2 * math.pi / L) * t[:, None] * bands).astype(np.float32)
        return np.concatenate([tn[:, None], np.cos(an), -np.sin(an)], -1).astype(np.float32), tn
    zf, tnf = zfeat(np.arange(L)); zb, tnb = zfeat(L - np.arange(L))
    c['ZT'] = np.ascontiguousarray(np.concatenate([zf, zb], 1).T)
    lo_ = math.log(1e-2) / 1.5; hi_ = math.log(1e-2) / 0.3
    deltas = np.abs(np.linspace(lo_, hi_, 512, dtype=np.float32))
    decf = np.exp(-tnf[:, None] * deltas).astype(np.float32)
    decb = np.exp(-tnb[:, None] * deltas).astype(np.float32); decb[0] = 0.0
    c['DEC'] = np.ascontiguousarray(np.stack([decf.reshape(64, 128, 512).transpose(0, 2, 1), decb.reshape(64, 128, 512).transpose(0, 2, 1)]))
    _CONST = c
    return c


class Prog:
    def __init__(self, debug=None):
        self.debug = debug or ()
        self.nc = bass.Bass("TRN2", target_bir_lowering=False)
        self.ins = {}
        self.outs = {}

    def inp(self, name, shape, dt=F32):
        t = self.nc.dram_tensor(name, list(shape), dt, kind="ExternalInput").ap()
        self.ins[name] = t
        return t

    def scratch(self, name, shape, dt=F32):
        if name in self.debug:
            t = self.nc.dram_tensor(name, list(shape), dt, kind="ExternalOutput").ap()
            self.outs[name] = t
        else:
            t = self.nc.dram_tensor(name, list(shape), dt, kind="Internal").ap()
        return t


def build(stages=99, debug=None, dummy_mix=False):
    pg = Prog(debug)
    nc = pg.nc
    X = pg.inp("x", [L, D]); CTX = pg.inp("ctx", [NCTX, D]); XOWN = pg.inp("xown", [OWN, D]); POSOWN = pg.inp("posown", [OWN, D])
    CV = pg.inp("c", [D]); CCTX = pg.inp("c_ctx", [D])
    N1G = pg.inp("norm1_g", [D]); N2G = pg.inp("norm2_g", [D])
    ADAW = pg.inp("ada_w", [D, 6 * D]); ADAB = pg.inp("ada_b", [6 * D])
    WIN = pg.inp("w_in", [D, 6144])
    POS = pg.inp("POS", [L, D]); IDENT = pg.inp("IDENT", [128, 128])
    WHY = pg.inp("w_hy_out", [512, D]); WHG = pg.inp("w_hg_out", [512, D]); WOUT = pg.inp("w_out", [D, D])
    SHG = pg.inp("sh_w_gate", [D, 256]); SHU = pg.inp("sh_w_up", [D, 256]); SHD = pg.inp("sh_w_down", [256, D])
    FING = pg.inp("final_g", [D]); OWNIDX = pg.inp("OWNIDX", [128, 4], I32)
    CONVW = pg.inp("hy_conv_w", [3, 1536]); CONVB = pg.inp("hy_conv_b", [1536])
    TRIFd = pg.inp("TRIF", [128, 128]); TRIBd = pg.inp("TRIB", [128, 128]); ONESd = pg.inp("ONES", [128, 128])
    LBL = pg.inp("hg_lb_logits", [2, 1024]); HGNG = pg.inp("hg_norm_g", [128])
    STRId = pg.inp("STRI", [128, 128]); SLTd = pg.inp("SLT", [128, 512]); BLKd = pg.inp("BLK128", [128, NBLK]); PIDXd = pg.inp("PIDX", [128, 1])
    RW = pg.inp("router_w", [D, NE]); RB = pg.inp("router_bias", [NE])
    EWGU = pg.inp("EWGU", [NE * 128, 4096]); EWD = pg.inp("EWD", [NE * 128, 2048])
    FAd = pg.inp("FA", [128, 256]); FAHd = pg.inp("FAH", [64, 256]); FREd = pg.inp("FRE", [128, 128]); FIMd = pg.inp("FIM", [128, 128]); NFIMd = pg.inp("NFIM", [128, 128])
    CAd = pg.inp("CA", [128, 256]); CBd = pg.inp("CB", [128, 256]); FRENd = pg.inp("FREN", [128, 64]); FIMNd = pg.inp("FIMN", [128, 64])
    TRE2d = pg.inp("TRE2", [128, 256]); TIM2d = pg.inp("TIM2", [128, 256]); ZTd = pg.inp("ZT", [66, L]); DECd = pg.inp("DEC", [2, 64, 512, 128])
    FW1 = pg.inp("hy_f_w1", [33, 64]); FB1 = pg.inp("hy_f_b1", [64]); FW2 = pg.inp("hy_f_w2", [64, 64]); FB2 = pg.inp("hy_f_b2", [64])
    FW3 = pg.inp("hy_f_w3", [64, 64]); FB3 = pg.inp("hy_f_b3", [64]); FW4 = pg.inp("hy_f_w4", [64, 2048]); FFQ = pg.inp("hy_f_freq", [64])
    HSKIP = pg.inp("hy_skip", [1024])
    HSPEC = pg.scratch("HSPEC", [1024, 128, 2, 128])
    OUT = pg.nc.dram_tensor("out", [OWN, D], F32, kind="ExternalOutput").ap()
    pg.outs["out"] = OUT
    XNT = pg.scratch("XNT", [D, LT]); XNTOWN = pg.scratch("XNTOWN", [D, OWN])
    MODROW = pg.scratch("MODROW", [2, 6 * D])
    HYRAW = pg.scratch("HYRAW", [1536, L])
    QT = pg.scratch("QT", [512, L]); GT = pg.scratch("GT", [512, L])
    IFF = pg.scratch("IFF", [LT, 1536])
    SGT = pg.scratch("SGT", [2048, OWN])
    HYC = pg.scratch("HYC", [1536, L])
    YHYT = pg.scratch("YHYT", [512, L]); YHGT = pg.scratch("YHGT", [512, L])
    YOWN = pg.scratch("YOWN", [1024, OWN])
    X1D = pg.scratch("X1D", [OWN, D]); H2T = pg.scratch("H2T", [D, OWN])
    SGA = pg.scratch("SGA", [256, OWN]); SUA = pg.scratch("SUA", [256, OWN]); ACTT = pg.scratch("ACTT", [256, OWN])
    SHOUT = pg.scratch("SHOUT", [OWN, D]); ROUTED = pg.scratch("ROUTED", [OWN, D])
    H2 = pg.scratch("H2", [OWN, D]); SCORES = pg.scratch("SCORES", [OWN, NE])
    XS = pg.scratch("XS", [NBLK * 128, D]); YS = pg.scratch("YS", [NBLK * 128, D])

    with ExitStack() as es:
        kb = KB(nc, es)
        sbt = lambda stack, name, shape, dt=F32: stack.enter_context(nc.sbuf_tensor(name, list(shape), dt))
        PA = es.enter_context(nc.psum_tensor("PA", [128, 2048], F32))
        PB = es.enter_context(nc.psum_tensor("PB", [128, 2048], F32))
        banks = [(PA[:, 512 * i:512 * (i + 1)], "PA%d" % i) for i in range(4)] + \
                [(PB[:, 512 * i:512 * (i + 1)], "PB%d" % i) for i in range(4)]
        RB_OWN = nc.gpsimd.alloc_register("bc_own"); nc.gpsimd.reg_mov(RB_OWN, 2047)
        RB_XS = nc.gpsimd.alloc_register("bc_xs"); nc.gpsimd.reg_mov(RB_XS, NBLK * 128 - 1)
        RB_W = nc.gpsimd.alloc_register("bc_w"); nc.gpsimd.reg_mov(RB_W, NE * 128 - 1)
        ident = sbt(es, "ident", [128, 128])
        kb.dma('sp', lambda e: e.dma_start(out=ident[:], in_=IDENT), w=['ident'])
        modT = sbt(es, "modT", [128, 48, 2])
        a1T = sbt(es, "a1T", [128, 8, 2]); sh1T = sbt(es, "sh1T", [128, 8, 2]); g2T = sbt(es, "g2T", [128, 8])
        @contextmanager
        def stage():
            with ExitStack() as st_:
                yield st_
                kb.barrier()

        def dbg(name, ap, shape):
            if name in pg.debug:
                t = nc.dram_tensor("D_" + name, list(shape), F32, kind="ExternalOutput").ap()
                pg.outs["D_" + name] = t
                kb.dma('pool', lambda e: e.dma_start(out=t, in_=ap), r=[name], w=['DBG' + name])

        with stage() as s0:
            sT = sbt(s0, "sT", [128, 8, 2]); vraw = sbt(s0, "vraw", [80, 128]); VT = sbt(s0, "VT", [128, 80])
            kb.dma('sp', lambda e: e.dma_start(out=vraw[0:8, :], in_=CV.rearrange("(q p) -> q p", p=128)), w=['vraw'])
            kb.dma('sp', lambda e: e.dma_start(out=vraw[8:16, :], in_=CCTX.rearrange("(q p) -> q p", p=128)), r=['vraw'], w=['vraw'])
            kb.dma('sp', lambda e: e.dma_start(out=vraw[16:24, :], in_=N1G.rearrange("(q p) -> q p", p=128)), r=['vraw'], w=['vraw'])
            kb.dma('sp', lambda e: e.dma_start(out=vraw[24:32, :], in_=N2G.rearrange("(q p) -> q p", p=128)), r=['vraw'], w=['vraw'])
            kb.dma('sp', lambda e: e.dma_start(out=vraw[32:80, :], in_=ADAB.rearrange("(q p) -> q p", p=128)), r=['vraw'], w=['vraw'])
            ps, psk = banks[0]
            kb.op('pe', lambda e: e.transpose(out=ps[:, 128:208], in_=vraw[:], identity=ident[0:80, 0:80]), r=['vraw', 'ident'], w=[psk])
            kb.op('dve', lambda e: e.tensor_copy(out=VT[:], in_=ps[:, 128:208]), r=[psk], w=['VT'])
            abT = VT[:, 32:80]; g1T = VT[:, 16:24]
            for r_ in range(2):
                kb.op('act', lambda e: e.activation(out=sT[:, :, r_], in_=VT[:, 8 * r_:8 * r_ + 8], func=AF.Silu), r=['VT', 'sT'], w=['sT'])
            kb.op('dve', lambda e: e.tensor_copy(out=g2T[:], in_=VT[:, 24:32]), r=['VT'], w=['g2T'])
            wbufs = [sbt(s0, "adaw%d" % i, [128, 8, 768]) for i in range(2)]
            mrow = sbt(s0, "mrow", [2, 6144]); brow = sbt(s0, "brow", [2, 6144])
            for r_ in range(2):
                kb.dma('sp', lambda e: e.dma_start(out=brow[r_:r_ + 1, :], in_=ADAB.rearrange("(o n) -> o n", o=1)), r=['brow'], w=['brow'])
            for cb in range(8):
                wb = wbufs[cb % 2]; wk = "adaw%d" % (cb % 2)
                kb.dma('sp', lambda e: e.dma_start(out=wb[:], in_=ADAW[:, cb * 768:(cb + 1) * 768].rearrange("(k p) c -> p k c", p=128)), w=[wk])
                for hh in range(2):
                    pr, prk = banks[1 + (2 * cb + hh) % 3]
                    for kk in range(8):
                        kb.op('pe', lambda e: e.matmul(pr[0:2, 0:384], lhsT=sT[:, kk, :], rhs=wb[:, kk, hh * 384:(hh + 1) * 384],
                                                       start=(kk == 0), stop=(kk == 7)), r=[wk, 'sT'], w=[prk])
                    c0 = cb * 768 + hh * 384
                    kb.op('dve', lambda e: e.tensor_tensor(out=mrow[:, c0:c0 + 384], in0=pr[0:2, 0:384], in1=brow[:, c0:c0 + 384], op=ALU.add),
                          r=[prk, 'brow'], w=['mrow'])
            kb.dma('pool', lambda e: e.dma_start(out=MODROW, in_=mrow[:]), r=['mrow'], w=['MODROW'])
            mq = sbt(s0, "mq", [96, 128])
            kb.dma('sp', lambda e: e.dma_start(out=mq[:], in_=MODROW.rearrange("r (q p) -> (r q) p", p=128)), r=['MODROW'], w=['mq'])
            kb.op('pe', lambda e: e.transpose(out=ps[:, 256:352], in_=mq[:], identity=ident[0:96, 0:96]), r=['mq', 'ident'], w=[psk])
            for r_ in range(2):
                kb.op('dve', lambda e: e.tensor_copy(out=modT[:, :, r_], in_=ps[:, 256 + 48 * r_:256 + 48 * r_ + 48]), r=[psk, 'modT'], w=['modT'])
            kb.op('dve', lambda e: e.tensor_scalar(out=a1T[:], in0=modT[:, 8:16, :], scalar1=1.0, scalar2=None, op0=ALU.add),
                  r=['modT'], w=['a1T'])
            for r_ in range(2):
                kb.op('dve', lambda e: e.tensor_tensor(out=a1T[:, :, r_], in0=a1T[:, :, r_], in1=g1T, op=ALU.mult),
                      r=['a1T', 'VT'], w=['a1T'])
            kb.op('dve', lambda e: e.tensor_copy(out=sh1T[:], in_=modT[:, 0:8, :]), r=['modT'], w=['sh1T'])

        dbg('modT', modT[:], [128, 48, 2]); dbg('a1T', a1T[:], [128, 8, 2])
        def norm_to_xt(stack, srcs, XT, a_ap, b_ap, tag, xtk):
            xin = [sbt(stack, "%s_xin%d" % (tag, i), [128, 1024]) for i in range(2)]
            pin = [sbt(stack, "%s_pin%d" % (tag, i), [128, 1024]) for i in range(2)]
            junk = sbt(stack, "%s_junk" % tag, [128, 1024])
            st = [sbt(stack, "%s_st%d" % (tag, i), [128, 4]) for i in range(2)]
            xo = [sbt(stack, "%s_xo%d" % (tag, i), [128, 8, 128]) for i in range(2)]
            for i, (xap, pap, c0) in enumerate(srcs):
                b = i % 2
                xk, pk, sk, ok = "%s_xin%d" % (tag, b), "%s_pin%d" % (tag, b), "%s_st%d" % (tag, b), "%s_xo%d" % (tag, b)
                kb.dma('sp', lambda e: e.dma_start(out=xin[b][:], in_=xap), w=[xk])
                if pap is not None:
                    kb.dma('sp', lambda e: e.dma_start(out=pin[b][:], in_=pap), w=[pk])
                    kb.op('pool', lambda e: e.tensor_tensor(out=xin[b][:], in0=xin[b][:], in1=pin[b][:], op=ALU.add), r=[xk, pk], w=[xk])
                kb.op('act', lambda e: e.activation(out=junk[:], in_=xin[b][:], func=AF.Square, accum_out=st[b][:, 0:1]),
                      r=[xk], w=[tag + '_junk', sk])
                kb.op('dve', lambda e: e.tensor_scalar(out=st[b][:, 1:2], in0=st[b][:, 0:1], scalar1=1.0 / D, scalar2=EPS, op0=ALU.mult, op1=ALU.add),
                      r=[sk], w=[sk])
                kb.op('act', lambda e: e.activation(out=st[b][:, 2:3], in_=st[b][:, 1:2], func=AF.Sqrt), r=[sk], w=[sk])
                kb.op('dve', lambda e: e.reciprocal(out=st[b][:, 3:4], in_=st[b][:, 2:3]), r=[sk], w=[sk])
                kb.op('dve', lambda e: e.tensor_scalar(out=xin[b][:], in0=xin[b][:], scalar1=st[b][:, 3:4], scalar2=None, op0=ALU.mult),
                      r=[xk, sk], w=[xk])
                for h in range(2):
                    pb, pbk = banks[(2 * i + h) % 4]
                    for kk in range(4):
                        k8 = h * 4 + kk
                        kb.op('pe', lambda e: e.transpose(out=pb[:, kk * 128:(kk + 1) * 128], in_=xin[b][:, k8 * 128:(k8 + 1) * 128], identity=ident[:]),
                              r=[xk, 'ident'], w=[pbk])
                    for kk in range(4):
                        k8 = h * 4 + kk
                        kb.op('act', lambda e: e.activation(out=xo[b][:, k8, :], in_=pb[:, kk * 128:(kk + 1) * 128], func=AF.Identity,
                                                            bias=b_ap[:, k8:k8 + 1], scale=a_ap[:, k8:k8 + 1]),
                              r=[pbk, 'a1T', 'sh1T', 'a2T'], w=[ok])
                kb.dma('pool', lambda e: e.dma_start(out=XT[:, c0:c0 + 128].rearrange("(k p) t -> p k t", p=128), in_=xo[b][:]), r=[ok], w=[xtk])

        if stages >= 1:
            with stage() as s1:
                srcs = [(X[i * 128:(i + 1) * 128, :], POS[i * 128:(i + 1) * 128, :], i * 128) for i in range(L // 128)]
                norm_to_xt(s1, srcs, XNT, a1T[:, :, 0], sh1T[:, :, 0], "n1", 'XNT')
            with stage() as s1:
                srcs = [(CTX[i * 128:(i + 1) * 128, :], None, L + i * 128) for i in range(NCTX // 128)]
                norm_to_xt(s1, srcs, XNT, a1T[:, :, 1], sh1T[:, :, 1], "n1c", 'XNT')
            with stage() as s1:
                srcs = [(XOWN[i * 128:(i + 1) * 128, :], POSOWN[i * 128:(i + 1) * 128, :], i * 128) for i in range(OWN // 128)]
                norm_to_xt(s1, srcs, XNTOWN, a1T[:, :, 0], sh1T[:, :, 0], "n1o", 'XNTOWN')

        cp_toggle = [0]

        def evac(out_ap, in_ap, func, r, w):
            if func is None:
                cp_toggle[0] ^= 1
                if cp_toggle[0]:
                    kb.op('dve', lambda e: e.tensor_copy(out=out_ap, in_=in_ap), r=r, w=w)
                else:
                    kb.op('act', lambda e: e.copy(out=out_ap, in_=in_ap), r=r, w=w)
            else:
                kb.op('act', lambda e: e.activation(out=out_ap, in_=in_ap, func=func), r=r, w=w)

        def gemm_group(tag, Wsrc, kch, ncols, XT, xtkey, tblocks, jobs):
            with stage() as st:
                Wsb = sbt(st, tag + "_W", [128, kch, ncols])
                kb.dma('sp', lambda e: e.dma_start(out=Wsb[:], in_=Wsrc.rearrange("(k p) c -> p k c", p=128)), w=[tag + '_W'])
                Xb = [sbt(st, "%s_X%d" % (tag, i), [128, kch, 512]) for i in range(2)]
                stg = [sbt(st, "%s_s%d" % (tag, i), [128, 512]) for i in range(4)]
                si = 0
                bi = 0
                for ti, (t0, tn) in enumerate(tblocks):
                    xb = Xb[ti % 2]; xk = "%s_X%d" % (tag, ti % 2)
                    kb.dma('sp', lambda e: e.dma_start(out=xb[:, :, 0:tn], in_=XT[:, t0:t0 + tn].rearrange("(k p) t -> p k t", p=128)),
                           r=[xtkey], w=[xk])
                    for jb in jobs:
                        if jb.get('tsel') is not None and not jb['tsel'](t0):
                            continue
                        c0, cn, tofs = jb['c0'], jb['cn'], jb.get('tofs', 0)
                        if jb['mode'] == 'fm':
                            for m in range(cn // 128):
                                pb, pbk = banks[bi % 8]; bi += 1
                                for kk in range(kch):
                                    kb.op('pe', lambda e: e.matmul(pb[:, 0:tn], lhsT=Wsb[:, kk, c0 + m * 128:c0 + (m + 1) * 128], rhs=xb[:, kk, 0:tn],
                                                                   start=(kk == 0), stop=(kk == kch - 1)), r=[tag + '_W', xk], w=[pbk])
                                sg = stg[si % 4]; sgk = "%s_s%d" % (tag, si % 4); si += 1
                                evac(sg[:, 0:tn], pb[:, 0:tn], jb['func'], [pbk], [sgk])
                                orow = jb.get('oc0', 0) + m * 128
                                kb.dma('pool', lambda e: e.dma_start(out=jb['out'][orow:orow + 128, t0 - tofs:t0 - tofs + tn], in_=sg[:, 0:tn]),
                                       r=[sgk], w=[jb['okey']])
                        else:
                            for tt in range(tn // 128):
                                pb, pbk = banks[bi % 8]; bi += 1
                                for kk in range(kch):
                                    kb.op('pe', lambda e: e.matmul(pb[:, 0:cn], lhsT=xb[:, kk, tt * 128:(tt + 1) * 128], rhs=Wsb[:, kk, c0:c0 + cn],
                                                                   start=(kk == 0), stop=(kk == kch - 1)), r=[tag + '_W', xk], w=[pbk])
                                sg = stg[si % 4]; sgk = "%s_s%d" % (tag, si % 4); si += 1
                                evac(sg[:, 0:cn], pb[:, 0:cn], jb['func'], [pbk], [sgk])
                                tr = t0 - tofs + tt * 128
                                oc0 = jb.get('oc0', 0)
                                kb.dma('pool', lambda e: e.dma_start(out=jb['out'][tr:tr + 128, oc0:oc0 + cn], in_=sg[:, 0:cn]),
                                       r=[sgk], w=[jb['okey']])

        if stages >= 2:
            lat_blocks = [(t, 512) for t in range(0, L, 512)]
            all_blocks = lat_blocks + [(L, 256)]
            gemm_group("g1", WIN[:, 0:1024], 8, 1024, XNT, 'XNT', lat_blocks,
                       [dict(c0=0, cn=1024, mode='fm', func=None, out=HYRAW, okey='HYRAW', oc0=0)])
            gemm_group("g2", WIN[:, 1024:2048], 8, 1024, XNT, 'XNT', lat_blocks,
                       [dict(c0=0, cn=512, mode='fm', func=None, out=HYRAW, okey='HYRAW', oc0=1024),
                        dict(c0=512, cn=512, mode='fm', func=AF.Silu, out=QT, okey='QT', oc0=0)])
        if stages >= 3:
            gemm_group("g3", WIN[:, 2048:3072], 8, 1024, XNT, 'XNT', all_blocks,
                       [dict(c0=0, cn=512, mode='tm', func=None, out=IFF, okey='IFF', oc0=0),
                        dict(c0=512, cn=512, mode='tm', func=None, out=IFF, okey='IFF', oc0=512)])
            gemm_group("g4", WIN[:, 3072:4096], 8, 1024, XNT, 'XNT', all_blocks,
                       [dict(c0=0, cn=512, mode='tm', func=None, out=IFF, okey='IFF', oc0=1024),
                        dict(c0=512, cn=512, mode='fm', func=AF.Silu, out=GT, okey='GT', oc0=0, tsel=lambda t0: t0 < L)])

        if stages >= 3:
            own_blocks = [(t, 512) for t in range(0, OWN, 512)]
            for gi in range(2):
                gemm_group("g%d" % (5 + gi), WIN[:, 4096 + 1024 * gi:5120 + 1024 * gi], 8, 1024, XNTOWN, 'XNTOWN', own_blocks,
                           [dict(c0=0, cn=1024, mode='fm', func=AF.Sigmoid, out=SGT, okey='SGT', oc0=1024 * gi)])

        if stages >= 4:
            with stage() as st:
                cwT = sbt(st, "cwT", [128, 12, 4]); craw = sbt(st, "craw", [48, 128])
                for j3 in range(3):
                    kb.dma('sp', lambda e: e.dma_start(out=craw[12 * j3:12 * j3 + 12, :], in_=CONVW[j3].rearrange("(q p) -> q p", p=128)), r=['craw'], w=['craw'])
                kb.dma('sp', lambda e: e.dma_start(out=craw[36:48, :], in_=CONVB.rearrange("(q p) -> q p", p=128)), r=['craw'], w=['craw'])
                pb, pbk = banks[0]
                kb.op('pe', lambda e: e.transpose(out=pb[:, 0:48], in_=craw[:], identity=ident[0:48, 0:48]), r=['craw', 'ident'], w=[pbk])
                kb.op('dve', lambda e: e.tensor_copy(out=cwT[:], in_=pb[:, 0:48].rearrange("p (j q) -> p q j", j=4)), r=[pbk], w=['cwT'])
                uin = [sbt(st, "uin%d" % i, [128, L + 2]) for i in range(2)]
                uo = [sbt(st, "uo%d" % i, [128, L]) for i in range(2)]
                for i in range(2):
                    kb.op('pool', lambda e: e.memset(uin[i][:, 0:1], 0.0), w=['uin%d' % i])
                    kb.op('pool', lambda e: e.memset(uin[i][:, L + 1:L + 2], 0.0), r=['uin%d' % i], w=['uin%d' % i])
                for q in range(12):
                    b2 = q % 2; uk = 'uin%d' % b2; ok = 'uo%d' % b2
                    kb.dma('sp', lambda e: e.dma_start(out=uin[b2][:, 1:L + 1], in_=HYRAW[q * 128:(q + 1) * 128, :]), r=['HYRAW', uk], w=[uk])
                    kb.op('dve', lambda e: e.tensor_scalar(out=uo[b2][:], in0=uin[b2][:, 0:L], scalar1=cwT[:, q, 0:1], scalar2=cwT[:, q, 3:4],
                                                           op0=ALU.mult, op1=ALU.add), r=[uk, 'cwT'], w=[ok])
                    kb.op('dve', lambda e: e.scalar_tensor_tensor(out=uo[b2][:], in0=uin[b2][:, 1:L + 1], scalar=cwT[:, q, 1:2], in1=uo[b2][:],
                                                                  op0=ALU.mult, op1=ALU.add), r=[uk, 'cwT', ok], w=[ok])
                    kb.op('dve', lambda e: e.scalar_tensor_tensor(out=uo[b2][:], in0=uin[b2][:, 2:L + 2], scalar=cwT[:, q, 2:3], in1=uo[b2][:],
                                                                  op0=ALU.mult, op1=ALU.add), r=[uk, 'cwT', ok], w=[ok])
                    kb.dma('pool', lambda e: e.dma_start(out=HYC[q * 128:(q + 1) * 128, :], in_=uo[b2][:]), r=[ok], w=['HYC'])

        if stages >= 5 and not dummy_mix:
          with stage() as st:
            tri = {0: sbt(st, "trif", [128, 128]), 1: sbt(st, "trib", [128, 128])}
            ones = sbt(st, "ones", [128, 128])
            kb.dma('sp', lambda e: e.dma_start(out=tri[0][:], in_=TRIFd), w=['tri'])
            kb.dma('sp', lambda e: e.dma_start(out=tri[1][:], in_=TRIBd), r=['tri'], w=['tri'])
            kb.dma('sp', lambda e: e.dma_start(out=ones[:], in_=ONESd), w=['ones'])
            l0 = sbt(st, "l0", [128, 1024]); l1 = sbt(st, "l1", [128, 1024]); oml = sbt(st, "oml", [128, 1024])
            kb.dma('sp', lambda e: e.dma_start(out=l0[:], in_=LBL[0:1, :].to_broadcast([128, 1024])), w=['l0'])
            kb.dma('sp', lambda e: e.dma_start(out=l1[:], in_=LBL[1:2, :].to_broadcast([128, 1024])), w=['l1'])
            kb.op('dve', lambda e: e.tensor_tensor(out=l0[:], in0=l0[:], in1=l1[:], op=ALU.subtract), r=['l0', 'l1'], w=['l0'])
            kb.op('act', lambda e: e.activation(out=l0[:], in_=l0[:], func=AF.Sigmoid), r=['l0'], w=['l0'])
            kb.op('dve', lambda e: e.tensor_scalar(out=oml[:], in0=l0[:], scalar1=-1.0, scalar2=1.0, op0=ALU.mult, op1=ALU.add), r=['l0'], w=['oml'])
            ngT = sbt(st, "ngT", [128, 1])
            with nc.allow_non_contiguous_dma(reason="128-element vector to partitions"):
                kb.dma('sp', lambda e: e.dma_start(out=ngT[:], in_=HGNG.rearrange("(p o) -> p o", o=1)), w=['ngT'])
            osum = sbt(st, "osum", [128, L])
            LB8 = sbt(st, "LB8", [128, 8, 128]); OML8 = sbt(st, "OML8", [128, 8, 128])
            vseg = [sbt(st, "vseg%d" % i, [128, 8, 128]) for i in range(2)]
            fseg = [sbt(st, "fseg%d" % i, [128, 8, 128]) for i in range(2)]
            lseg = [sbt(st, "lseg%d" % i, [128, 8, 128]) for i in range(2)]
            kseg = [sbt(st, "kseg%d" % i, [128, 8, 128]) for i in range(2)]
            qseg = [sbt(st, "qseg%d" % i, [128, 1024]) for i in range(2)]
            R3 = 3
            ekb = [sbt(st, "ekb%d" % i, [128, 128]) for i in range(R3)]
            kd = [sbt(st, "kd%d" % i, [128, 128]) for i in range(R3)]
            ebT = [sbt(st, "ebT%d" % i, [128, 128]) for i in range(R3)]
            qdT = [sbt(st, "qdT%d" % i, [128, 128]) for i in range(R3)]
            kdT = [sbt(st, "kdT%d" % i, [128, 128]) for i in range(R3)]
            attm = [sbt(st, "attm%d" % i, [128, 128]) for i in range(R3)]
            Sr = [sbt(st, "S%d" % i, [128, 128]) for i in range(4)]
            Se = [sbt(st, "Se%d" % i, [128, 128]) for i in range(2)]
            pbi = [0]

            def nb():
                b_ = banks[pbi[0] % 8]; pbi[0] += 1
                return b_

            for h in range(4):
                for dr in range(2):
                    T = tri[dr]
                    fcol = 512 + 512 * dr + h * 128
                    for n in range(8):
                        kb.op('pool', lambda e: e.tensor_copy(out=LB8[:, n, :], in_=l0[:, dr * 512 + h * 128:dr * 512 + (h + 1) * 128]), r=['l0', 'LB8'], w=['LB8'])
                        kb.op('pool', lambda e: e.tensor_copy(out=OML8[:, n, :], in_=oml[:, dr * 512 + h * 128:dr * 512 + (h + 1) * 128]), r=['oml', 'OML8'], w=['OML8'])
                    si_ = 0
                    kb.op('pool', lambda e: e.memset(Sr[0][:], 0.0), w=['S0'])
                    segs = [(L, 2, False)] + [(sg * 1024, 8, True) for sg in (range(8) if dr == 0 else range(7, -1, -1))]
                    tcount = 0
                    for sgi, (r0, nt, lat) in enumerate(segs):
                        b2 = sgi % 2
                        vk, fk, lk, kk_, qk = 'vseg%d' % b2, 'fseg%d' % b2, 'lseg%d' % b2, 'kseg%d' % b2, 'qseg%d' % b2
                        kb.dma('sp', lambda e: e.dma_start(out=vseg[b2][:, 0:nt, :], in_=IFF[r0:r0 + nt * 128, h * 128:(h + 1) * 128].rearrange("(n p) c -> p n c", p=128)),
                               r=['IFF'], w=[vk])
                        kb.dma('sp', lambda e: e.dma_start(out=fseg[b2][:, 0:nt, :], in_=IFF[r0:r0 + nt * 128, fcol:fcol + 128].rearrange("(n p) c -> p n c", p=128)),
                               r=['IFF'], w=[fk])
                        if lat:
                            kb.dma('sp', lambda e: e.dma_start(out=qseg[b2][:], in_=QT[h * 128:(h + 1) * 128, r0:r0 + 1024]), r=['QT'], w=[qk])
                        kb.op('act', lambda e: e.activation(out=fseg[b2][:, 0:nt, :], in_=fseg[b2][:, 0:nt, :], func=AF.Sigmoid), r=[fk], w=[fk])
                        kb.op('dve', lambda e: e.tensor_tensor(out=fseg[b2][:, 0:nt, :], in0=fseg[b2][:, 0:nt, :], in1=OML8[:, 0:nt, :], op=ALU.mult), r=[fk, 'OML8'], w=[fk])
                        kb.op('dve', lambda e: e.tensor_tensor(out=fseg[b2][:, 0:nt, :], in0=fseg[b2][:, 0:nt, :], in1=LB8[:, 0:nt, :], op=ALU.add), r=[fk, 'LB8'], w=[fk])
                        kb.op('act', lambda e: e.activation(out=lseg[b2][:, 0:nt, :], in_=fseg[b2][:, 0:nt, :], func=AF.Ln), r=[fk], w=[lk])
                        kb.op('dve', lambda e: e.tensor_scalar(out=kseg[b2][:, 0:nt, :], in0=fseg[b2][:, 0:nt, :], scalar1=-1.0, scalar2=1.0, op0=ALU.mult, op1=ALU.add),
                              r=[fk], w=[kk_])
                        order = list(range(nt)) if dr == 0 else list(range(nt - 1, -1, -1))
                        slot = {}

                        def prep(n, b2=b2, lat=lat, lk=lk, kk_=kk_, qk=qk):
                            nonlocal tcount
                            r3 = tcount % R3; tcount += 1
                            slot[n] = r3
                            ek, kdk, ebk, qdk, ktk = 'ekb%d' % r3, 'kd%d' % r3, 'ebT%d' % r3, 'qdT%d' % r3, 'kdT%d' % r3
                            p1, p1k = nb()
                            kb.op('pe', lambda e: e.matmul(p1[:, 0:128], lhsT=T[:], rhs=lseg[b2][:, n, :], start=True, stop=True), r=['tri', lk], w=[p1k])
                            p2, p2k = nb()
                            kb.op('pe', lambda e: e.matmul(p2[:, 0:128], lhsT=lseg[b2][:, n, :], rhs=T[:], start=True, stop=True), r=['tri', lk], w=[p2k])
                            kb.op('act', lambda e: e.activation(out=ekb[r3][:], in_=p1[:, 0:128], func=AF.Exp, scale=-1.0), r=[p1k], w=[ek])
                            kb.op('act', lambda e: e.activation(out=ebT[r3][:], in_=p2[:, 0:128], func=AF.Exp), r=[p2k], w=[ebk])
                            kb.op('dve', lambda e: e.tensor_tensor(out=kd[r3][:], in0=kseg[b2][:, n, :], in1=ekb[r3][:], op=ALU.mult), r=[kk_, ek], w=[kdk])
                            if lat:
                                kb.op('dve', lambda e: e.tensor_tensor(out=qdT[r3][:], in0=qseg[b2][:, n * 128:(n + 1) * 128], in1=ebT[r3][:], op=ALU.mult), r=[qk, ebk], w=[qdk])
                                p3, p3k = nb()
                                kb.op('pe', lambda e: e.transpose(out=p3[:, 0:128], in_=kd[r3][:], identity=ident[:]), r=[kdk, 'ident'], w=[p3k])
                                kb.op('act', lambda e: e.copy(out=kdT[r3][:], in_=p3[:, 0:128]), r=[p3k], w=[ktk])

                        def scan(n, b2=b2, lat=lat, vk=vk, r0=r0):
                            nonlocal si_
                            r3 = slot[n]
                            kdk, ebk, qdk, ktk, amk = 'kd%d' % r3, 'ebT%d' % r3, 'qdT%d' % r3, 'kdT%d' % r3, 'attm%d' % r3
                            halves = [(0, 64, 63), (64, 128, 127)] if dr == 0 else [(64, 128, 64), (0, 64, 0)]
                            if lat:
                                p4, p4k = nb()
                                kb.op('pe', lambda e: e.matmul(p4[:, 0:128], lhsT=kdT[r3][:], rhs=qdT[r3][:], start=True, stop=True), r=[ktk, qdk], w=[p4k])
                                kb.op('dve', lambda e: e.tensor_tensor(out=attm[r3][:], in0=p4[:, 0:128], in1=T[:], op=ALU.mult), r=[p4k, 'tri'], w=[amk])
                            pds = []
                            for (c0, c1, ce) in halves:
                                pd, pdk = nb()
                                kb.op('pe', lambda e: e.matmul(pd[:, 0:128], lhsT=kd[r3][c0:c1, :], rhs=vseg[b2][c0:c1, n, :], start=True, stop=True), r=[kdk, vk], w=[pdk])
                                pds.append((pd, pdk))
                            Ss = []
                            for hi_, (c0, c1, ce) in enumerate(halves):
                                Sc = Sr[si_ % 4]; Sck = 'S%d' % (si_ % 4)
                                Sn = Sr[(si_ + 1) % 4]; Snk = 'S%d' % ((si_ + 1) % 4)
                                sew = Se[si_ % 2]; sek = 'Se%d' % (si_ % 2)
                                si_ += 1
                                Ss.append((Sc, Sck))
                                pd, pdk = pds[hi_]
                                kb.op('act', lambda e: e.activation(out=sew[:], in_=Sc[:], func=AF.Copy, scale=ebT[r3][:, ce:ce + 1]), r=[Sck, ebk], w=[sek])
                                kb.op('dve', lambda e: e.scalar_tensor_tensor(out=Sn[:], in0=pd[:, 0:128], scalar=ebT[r3][:, ce:ce + 1], in1=sew[:], op0=ALU.mult, op1=ALU.add),
                                      r=[pdk, ebk, sek], w=[Snk])
                            if lat:
                                po, pok = nb()
                                kb.op('pe', lambda e: e.matmul(po[:, 0:128], lhsT=vseg[b2][:, n, :], rhs=attm[r3][:], start=True, stop=False), r=[vk, amk], w=[pok])
                                for hi_, (c0, c1, ce) in enumerate(halves):
                                    Sc, Sck = Ss[hi_]
                                    kb.op('pe', lambda e: e.matmul(po[:, c0:c1], lhsT=Sc[:], rhs=qdT[r3][:, c0:c1], start=False, stop=True), r=[Sck, qdk], w=[pok])
                                cols = slice(r0 + n * 128, r0 + (n + 1) * 128)
                                if dr == 0:
                                    kb.op('act', lambda e: e.copy(out=osum[:, cols], in_=po[:, 0:128]), r=[pok], w=['osum%d' % (r0 // 1024)])
                                else:
                                    kb.op('dve', lambda e: e.tensor_tensor(out=osum[:, cols], in0=po[:, 0:128], in1=osum[:, cols], op=ALU.add),
                                          r=[pok, 'osum%d' % (r0 // 1024)], w=['osum%d' % (r0 // 1024)])

                        prep(order[0])
                        for oi, n in enumerate(order):
                            if oi + 1 < len(order):
                                prep(order[oi + 1])
                            scan(n)
                    if si_ % 4 != 0:
                        pass
                    kb.res.pop('unused', None)
                sq = [sbt(st, "hsq%d_%d" % (h, i), [128, 512]) for i in range(2)] if h == 0 else sq
                gt_ = [sbt(st, "hgt%d_%d" % (h, i), [128, 512]) for i in range(2)] if h == 0 else gt_
                rs = [sbt(st, "hrs%d_%d" % (h, i), [128, 512]) for i in range(2)] if h == 0 else rs
                for blk in range(L // 512):
                    b2 = blk % 2; cols = slice(blk * 512, (blk + 1) * 512)
                    sqk, gk, rk = 'hsq%d' % b2, 'hgt%d' % b2, 'hrs%d' % b2
                    ok_ = 'osum%d' % (blk // 2)
                    kb.dma('sp', lambda e: e.dma_start(out=gt_[b2][:], in_=GT[h * 128:(h + 1) * 128, cols]), r=['GT'], w=[gk])
                    kb.op('act', lambda e: e.activation(out=sq[b2][:], in_=osum[:, cols], func=AF.Square), r=[ok_], w=[sqk])
                    pb, pbk = nb()
                    kb.op('pe', lambda e: e.matmul(pb[:, :], lhsT=ones[:], rhs=sq[b2][:], start=True, stop=True), r=['ones', sqk], w=[pbk])
                    kb.op('dve', lambda e: e.tensor_scalar(out=rs[b2][:], in0=pb[:, :], scalar1=1.0 / 128, scalar2=EPS, op0=ALU.mult, op1=ALU.add), r=[pbk], w=[rk])
                    kb.op('act', lambda e: e.activation(out=rs[b2][:], in_=rs[b2][:], func=AF.Sqrt), r=[rk], w=[rk])
                    kb.op('dve', lambda e: e.reciprocal(out=rs[b2][:], in_=rs[b2][:]), r=[rk], w=[rk])
                    kb.op('dve', lambda e: e.scalar_tensor_tensor(out=sq[b2][:], in0=osum[:, cols], scalar=ngT[:, 0:1], in1=rs[b2][:], op0=ALU.mult, op1=ALU.mult),
                          r=[ok_, 'ngT', rk, sqk], w=[sqk])
                    kb.op('pool', lambda e: e.tensor_tensor(out=sq[b2][:], in0=sq[b2][:], in1=gt_[b2][:], op=ALU.mult), r=[sqk, gk], w=[sqk])
                    kb.dma('pool', lambda e: e.dma_start(out=YHGT[h * 128:(h + 1) * 128, cols], in_=sq[b2][:]), r=[sqk], w=['YHGT'])

        if stages >= 5 and not dummy_mix:
          with stage() as st:
            cst = {}
            for nm, src, shp in (("FA", FAd, [128, 256]), ("FAH", FAHd, [64, 256]), ("FRE", FREd, [128, 128]), ("FIM", FIMd, [128, 128]), ("NFIM", NFIMd, [128, 128]),
                                 ("CA", CAd, [128, 256]), ("CB", CBd, [128, 256]), ("FREN", FRENd, [128, 64]), ("FIMN", FIMNd, [128, 64]),
                                 ("TRE2", TRE2d, [128, 256]), ("TIM2", TIM2d, [128, 256]), ("ONESH", ONESd, [128, 128])):
                cst[nm] = sbt(st, "c_" + nm, shp)
                kb.dma('sp', lambda e: e.dma_start(out=cst[nm][:], in_=src), w=['hconst'])
            skb = sbt(st, "skb", [128, 1024])
            kb.dma('sp', lambda e: e.dma_start(out=skb[:], in_=HSKIP.rearrange("(o n) -> o n", o=1).to_broadcast([128, 1024])), w=['skb'])
            tw = [sbt(st, "tw%d" % i, [128, 256]) for i in range(4)]
            hbi = [0]

            def hb():
                b_ = banks[hbi[0] % 8]; hbi[0] += 1
                return b_

            def twiddle(ps, psk, Bt, bkey, c0, conj):
                A = ps[:, :].rearrange("p (c r f) -> p c r f", c=2, r=2)
                Are, Aim = A[:, :, 0, :], A[:, :, 1, :]
                T2r = cst["TRE2"][:].rearrange("p (c f) -> p c f", c=2); T2i = cst["TIM2"][:].rearrange("p (c f) -> p c f", c=2)
                t = [x[:].rearrange("p (c f) -> p c f", c=2) for x in tw]
                kb.op('dve', lambda e: e.tensor_tensor(out=t[0], in0=Are, in1=T2r, op=ALU.mult), r=[psk, 'hconst', 'tw0'], w=['tw0'])
                kb.op('dve', lambda e: e.tensor_tensor(out=t[1], in0=Aim, in1=T2i, op=ALU.mult), r=[psk, 'hconst', 'tw1'], w=['tw1'])
                kb.op('dve', lambda e: e.tensor_tensor(out=t[2], in0=Are, in1=T2i, op=ALU.mult), r=[psk, 'hconst', 'tw2'], w=['tw2'])
                kb.op('dve', lambda e: e.tensor_tensor(out=t[3], in0=Aim, in1=T2r, op=ALU.mult), r=[psk, 'hconst', 'tw3'], w=['tw3'])
                if not conj:
                    kb.op('pool', lambda e: e.tensor_tensor(out=Bt[:, c0:c0 + 2, 0, :], in0=t[0], in1=t[1], op=ALU.subtract), r=['tw0', 'tw1', bkey], w=[bkey])
                    kb.op('pool', lambda e: e.tensor_tensor(out=Bt[:, c0:c0 + 2, 1, :], in0=t[2], in1=t[3], op=ALU.add), r=['tw2', 'tw3', bkey], w=[bkey])
                else:
                    kb.op('pool', lambda e: e.tensor_tensor(out=Bt[:, c0:c0 + 2, 0, :], in0=t[0], in1=t[1], op=ALU.add), r=['tw0', 'tw1', bkey], w=[bkey])
                    kb.op('pool', lambda e: e.tensor_tensor(out=Bt[:, c0:c0 + 2, 1, :], in0=t[3], in1=t[2], op=ALU.subtract), r=['tw2', 'tw3', bkey], w=[bkey])

            def stage2(Bt, bkey, c4):
                pr, prk = hb(); pi_, pik = hb()
                Bre, Bim = Bt[:, c4:c4 + 4, 0, :], Bt[:, c4:c4 + 4, 1, :]
                kb.op('pe', lambda e: e.matmul(pr[:, :], lhsT=cst["FRE"][:], rhs=Bre, start=True, stop=False), r=['hconst', bkey], w=[prk])
                kb.op('pe', lambda e: e.matmul(pr[:, :], lhsT=cst["NFIM"][:], rhs=Bim, start=False, stop=True), r=['hconst', bkey], w=[prk])
                kb.op('pe', lambda e: e.matmul(pi_[:, :], lhsT=cst["FIM"][:], rhs=Bre, start=True, stop=False), r=['hconst', bkey], w=[pik])
                kb.op('pe', lambda e: e.matmul(pi_[:, :], lhsT=cst["FRE"][:], rhs=Bim, start=False, stop=True), r=['hconst', bkey], w=[pik])
                return pr, prk, pi_, pik

            B16 = sbt(st, "B16", [128, 16, 2, 128])
            with stage() as sf:
                A3Z = sbt(sf, "A3Z", [128, 2 * L])
                kb.op('pool', lambda e: e.memset(A3Z[0:64, L:2 * L], 0.0), w=['A3Z'])
                kb.op('pool', lambda e: e.memset(A3Z[64:128, 0:L], 0.0), r=['A3Z'], w=['A3Z'])
                w4sb = sbt(sf, "w4sb", [128, 2048])
                kb.dma('sp', lambda e: e.dma_start(out=w4sb[0:64, :], in_=FW4), w=['w4sb'])
                kb.dma('sp', lambda e: e.dma_start(out=w4sb[64:128, :], in_=FW4), r=['w4sb'], w=['w4sb'])
                with stage() as sm:
                    wl = [sbt(sm, "w1bd", [66, 128]), sbt(sm, "w2bd", [128, 128]), sbt(sm, "w3bd", [128, 128])]
                    for i_, (wt, src, kin) in enumerate(zip(wl, (FW1, FW2, FW3), (33, 64, 64))):
                        kb.op('pool', lambda e: e.memset(wt[:], 0.0), w=['wbd%d' % i_])
                        kb.dma('sp', lambda e: e.dma_start(out=wt[0:kin, 0:64], in_=src), r=['wbd%d' % i_], w=['wbd%d' % i_])
                        kb.dma('sp', lambda e: e.dma_start(out=wt[kin:2 * kin, 64:128], in_=src), r=['wbd%d' % i_], w=['wbd%d' % i_])
                    fqb = sbt(sm, "fqb", [128, 4])
                    with nc.allow_non_contiguous_dma(reason="64-element vectors onto partitions"):
                        for j_, src in enumerate((FFQ, FB1, FB2, FB3)):
                            for hh in range(2):
                                kb.dma('sp', lambda e: e.dma_start(out=fqb[64 * hh:64 * hh + 64, j_:j_ + 1], in_=src.rearrange("(p o) -> p o", o=1)), r=['fqb'], w=['fqb'])
                    kb.op('dve', lambda e: e.tensor_scalar(out=fqb[:, 0:1], in0=fqb[:, 0:1], scalar1=1.0 / TWO_PI, scalar2=None, op0=ALU.mult), r=['fqb'], w=['fqb'])
                    kb.op('dve', lambda e: e.tensor_scalar(out=fqb[:, 1:4], in0=fqb[:, 1:4], scalar1=fqb[:, 0:1], scalar2=None, op0=ALU.mult), r=['fqb'], w=['fqb'])
                    zc = [sbt(sm, "zc%d" % i, [66, 2048]) for i in range(2)]
                    hid = [sbt(sm, "hid%d" % i, [128, 2048]) for i in range(2)]
                    uu = sbt(sm, "uu", [128, 2048]); ui = sbt(sm, "ui", [128, 2048], I32); uf = sbt(sm, "uf", [128, 2048])
                    pi_ = 0
                    for ch in range(4):
                        zk = 'zc%d' % (ch % 2)
                        kb.dma('sp', lambda e: e.dma_start(out=zc[ch % 2][:], in_=ZTd[:, ch * 2048:(ch + 1) * 2048]), w=[zk])
                        src_t, kdim, srck = zc[ch % 2], 66, zk
                        for l_ in range(3):
                            Pq, pkeys = (PA, ["PA0", "PA1", "PA2", "PA3"]) if pi_ % 2 == 0 else (PB, ["PB0", "PB1", "PB2", "PB3"])
                            pi_ += 1
                            for q in range(4):
                                kb.op('pe', lambda e: e.matmul(Pq[:, q * 512:(q + 1) * 512], lhsT=wl[l_][0:kdim, :], rhs=src_t[0:kdim, q * 512:(q + 1) * 512], start=True, stop=True),
                                      r=['wbd%d' % l_, srck], w=[pkeys[q]])
                            kb.op('act', lambda e: e.activation(out=uu[:], in_=Pq[:, :], func=AF.Identity, bias=fqb[:, 1 + l_:2 + l_], scale=fqb[:, 0:1]), r=pkeys + ['fqb'], w=['uu'])
                            kb.op('dve', lambda e: e.tensor_copy(out=ui[:], in_=uu[:]), r=['uu'], w=['ui'])
                            kb.op('dve', lambda e: e.tensor_copy(out=uf[:], in_=ui[:]), r=['ui'], w=['uf'])
                            kb.op('pool', lambda e: e.tensor_tensor(out=uu[:], in0=uu[:], in1=uf[:], op=ALU.subtract), r=['uu', 'uf'], w=['uu'])
                            kb.op('dve', lambda e: e.scalar_tensor_tensor(out=uf[:], in0=uu[:], scalar=0.5, in1=uu[:], op0=ALU.is_gt, op1=ALU.subtract), r=['uu', 'uf'], w=['uf'])
                            kb.op('dve', lambda e: e.scalar_tensor_tensor(out=uu[:], in0=uf[:], scalar=0.5, in1=uf[:], op0=ALU.is_gt, op1=ALU.subtract), r=['uu', 'uf'], w=['uu'])
                            if l_ < 2:
                                hk = 'hid%d' % l_
                                kb.op('act', lambda e: e.activation(out=hid[l_][:], in_=uu[:], func=AF.Sin, scale=6.283185), r=['uu', hk], w=[hk])
                                src_t, kdim, srck = hid[l_], 128, hk
                            else:
                                kb.op('act', lambda e: e.activation(out=A3Z[0:64, ch * 2048:(ch + 1) * 2048], in_=uu[0:64, :], func=AF.Sin, scale=6.283185), r=['uu', 'A3Z'], w=['A3Z'])
                                kb.op('act', lambda e: e.activation(out=A3Z[64:128, L + ch * 2048:L + (ch + 1) * 2048], in_=uu[64:128, :], func=AF.Sin, scale=6.283185), r=['uu', 'A3Z'], w=['A3Z'])
                Kt = sbt(sf, "Kt", [128, 64, 128])
                dec = sbt(sf, "dec", [128, 64, 128]); rab = sbt(sf, "rab", [128, 64]); rn = sbt(sf, "rn", [128, 64]); Hst = sbt(sf, "Hst", [128, 16, 2, 128])
                w4c = sbt(sf, "w4c", [128, 64])
                DEC2 = DECd.rearrange("h n c f -> (h n) c f")
                for o in range(2):
                    for cg in range(8):
                        kb.op('pool', lambda e: e.tensor_copy(out=w4c[0:64, :], in_=w4sb[0:64, o * 1024 + cg * 64:o * 1024 + cg * 64 + 64]), r=['w4sb', 'w4c'], w=['w4c'])
                        kb.op('pool', lambda e: e.tensor_copy(out=w4c[64:128, :], in_=w4sb[64:128, o * 1024 + 512 + cg * 64:o * 1024 + 512 + cg * 64 + 64]), r=['w4sb', 'w4c'], w=['w4c'])
                        kb.dma('sp', lambda e: e.dma_start(out=dec[:], in_=DEC2[:, cg * 64:(cg + 1) * 64, :]), r=['dec'], w=['dec'])
                        for nbk in range(16):
                            ps, psk = hb()
                            for j_ in range(8):
                                n2 = nbk * 8 + j_
                                kb.op('pe', lambda e: e.matmul(ps[:, j_ * 64:(j_ + 1) * 64], lhsT=A3Z[:, n2:2 * L:128], rhs=w4c[:], start=True, stop=True), r=['A3Z', 'w4c'], w=[psk])
                            evac(Kt[:, :, nbk * 8:(nbk + 1) * 8], ps[:, :].rearrange("p (n c) -> p c n", c=64), None, [psk, 'Kt'], ['Kt'])
                        kb.op('pool', lambda e: e.tensor_tensor(out=Kt[:], in0=Kt[:], in1=dec[:], op=ALU.mult), r=['Kt', 'dec'], w=['Kt'])
                        kb.op('dve', lambda e: e.tensor_reduce(out=rab[:], in_=Kt[:], axis=AX.X, op=ALU.add, apply_absolute_value=True), r=['Kt', 'rab'], w=['rab'])
                        ps, psk = hb()
                        kb.op('pe', lambda e: e.matmul(ps[:, 0:64], lhsT=cst["ONESH"][:], rhs=rab[:], start=True, stop=True), r=['hconst', 'rab'], w=[psk])
                        kb.op('dve', lambda e: e.reciprocal(out=rn[:], in_=ps[:, 0:64]), r=[psk], w=['rn'])
                        for sb4 in range(4):
                            for c2 in range(8):
                                ps, psk = hb()
                                for j_ in range(2):
                                    cc = sb4 * 16 + c2 * 2 + j_
                                    kb.op('pe', lambda e: e.matmul(ps[:, j_ * 256:(j_ + 1) * 256], lhsT=Kt[:, cc, :], rhs=cst["FA"][:], start=True, stop=True), r=['Kt', 'hconst'], w=[psk])
                                twiddle(ps, psk, B16, 'B16', c2 * 2, False)
                            for c4 in range(0, 16, 4):
                                pr, prk, pi_, pik = stage2(B16, 'B16', c4)
                                for j_ in range(4):
                                    cc = sb4 * 16 + c4 + j_; gc = cg * 64 + cc
                                    kb.op('dve', lambda e: e.tensor_scalar(out=Hst[:, c4 + j_, 0, :], in0=pr[:, j_ * 128:(j_ + 1) * 128], scalar1=rn[:, cc:cc + 1], scalar2=skb[:, o * 512 + gc:o * 512 + gc + 1],
                                                                           op0=ALU.mult, op1=ALU.add), r=[prk, 'rn', 'skb', 'Hst'], w=['Hst'])
                                    kb.op('act', lambda e: e.activation(out=Hst[:, c4 + j_, 1, :], in_=pi_[:, j_ * 128:(j_ + 1) * 128], func=AF.Copy, scale=rn[:, cc:cc + 1]), r=[pik, 'rn', 'Hst'], w=['Hst'])
                            g0 = o * 512 + cg * 64 + sb4 * 16
                            kb.dma('sp', lambda e: e.dma_start(out=HSPEC[g0:g0 + 16].rearrange("c k r f -> k c r f"), in_=Hst[:]), r=['Hst'], w=['HSPEC'])
            with stage() as sc:
                v16 = sbt(sc, "v16", [64, 16, 128]); x116 = sbt(sc, "x116", [64, 16, 128]); x216 = sbt(sc, "x216", [64, 16, 128]); z16 = sbt(sc, "z16", [64, 16, 128])
                y16 = sbt(sc, "y16", [64, 16, 128])
                H1 = sbt(sc, "H1", [128, 16, 2, 128]); H2s = sbt(sc, "H2s", [128, 16, 2, 128]); Y16 = sbt(sc, "Y16", [128, 16, 2, 128]); G16 = sbt(sc, "G16", [128, 16, 2, 128])
                hm = [sbt(sc, "hm%d" % i, [128, 4, 128]) for i in range(4)]

                def conv16(Din, dkey, Hs, hkey, Xmul, xkey, Out, okey):
                    for c2 in range(8):
                        ps, psk = hb()
                        for j_ in range(2):
                            kb.op('pe', lambda e: e.matmul(ps[:, j_ * 256:(j_ + 1) * 256], lhsT=Din[:, c2 * 2 + j_, :], rhs=cst["FA"][0:64, :], start=True, stop=True), r=[dkey, 'hconst'], w=[psk])
                        twiddle(ps, psk, B16, 'B16', c2 * 2, False)
                    for c4 in range(0, 16, 4):
                        pr, prk, pi_, pik = stage2(B16, 'B16', c4)
                        Xr = pr[:, :].rearrange("p (c f) -> p c f", c=4); Xi = pi_[:, :].rearrange("p (c f) -> p c f", c=4)
                        Hr, Hi = Hs[:, c4:c4 + 4, 0, :], Hs[:, c4:c4 + 4, 1, :]
                        kb.op('dve', lambda e: e.tensor_tensor(out=hm[0][:], in0=Xr, in1=Hr, op=ALU.mult), r=[prk, hkey, 'hm0'], w=['hm0'])
                        kb.op('dve', lambda e: e.tensor_tensor(out=hm[1][:], in0=Xi, in1=Hi, op=ALU.mult), r=[pik, hkey, 'hm1'], w=['hm1'])
                        kb.op('dve', lambda e: e.tensor_tensor(out=hm[2][:], in0=Xr, in1=Hi, op=ALU.mult), r=[prk, hkey, 'hm2'], w=['hm2'])
                        kb.op('dve', lambda e: e.tensor_tensor(out=hm[3][:], in0=Xi, in1=Hr, op=ALU.mult), r=[pik, hkey, 'hm3'], w=['hm3'])
                        kb.op('pool', lambda e: e.tensor_tensor(out=Y16[:, c4:c4 + 4, 0, :], in0=hm[0][:], in1=hm[1][:], op=ALU.subtract), r=['hm0', 'hm1', 'Y16'], w=['Y16'])
                        kb.op('pool', lambda e: e.tensor_tensor(out=Y16[:, c4:c4 + 4, 1, :], in0=hm[2][:], in1=hm[3][:], op=ALU.add), r=['hm2', 'hm3', 'Y16'], w=['Y16'])
                    for c2 in range(8):
                        ps, psk = hb()
                        for j_ in range(2):
                            cc = c2 * 2 + j_
                            kb.op('pe', lambda e: e.matmul(ps[:, j_ * 256:(j_ + 1) * 256], lhsT=Y16[:, cc, 0, :], rhs=cst["CA"][:], start=True, stop=False), r=['Y16', 'hconst'], w=[psk])
                            kb.op('pe', lambda e: e.matmul(ps[:, j_ * 256:(j_ + 1) * 256], lhsT=Y16[:, cc, 1, :], rhs=cst["CB"][:], start=False, stop=True), r=['Y16', 'hconst'], w=[psk])
                        twiddle(ps, psk, G16, 'G16', c2 * 2, True)
                    for c4 in range(0, 16, 4):
                        py, pyk = hb()
                        kb.op('pe', lambda e: e.matmul(py[0:64, :], lhsT=cst["FREN"][:], rhs=G16[:, c4:c4 + 4, 0, :], start=True, stop=False), r=['hconst', 'G16'], w=[pyk])
                        kb.op('pe', lambda e: e.matmul(py[0:64, :], lhsT=cst["FIMN"][:], rhs=G16[:, c4:c4 + 4, 1, :], start=False, stop=True), r=['hconst', 'G16'], w=[pyk])
                        kb.op('dve', lambda e: e.tensor_tensor(out=Out[:, c4:c4 + 4, :], in0=py[0:64, :].rearrange("p (c f) -> p c f", c=4), in1=Xmul[:, c4:c4 + 4, :], op=ALU.mult),
                              r=[pyk, xkey, okey], w=[okey])

                for g in range(32):
                    gc0 = g * 16
                    lh = lambda r0: HYC[r0 + gc0:r0 + gc0 + 16, :].rearrange("c (n1 n2) -> n1 c n2", n2=128)
                    kb.dma('sp', lambda e: e.dma_start(out=v16[:], in_=lh(0)), r=['HYC'], w=['v16'])
                    kb.dma('sp', lambda e: e.dma_start(out=x116[:], in_=lh(512)), r=['HYC'], w=['x116'])
                    kb.dma('sp', lambda e: e.dma_start(out=x216[:], in_=lh(1024)), r=['HYC'], w=['x216'])
                    kb.dma('sp', lambda e: e.dma_start(out=H1[:], in_=HSPEC[gc0:gc0 + 16].rearrange("c k r f -> k c r f")), r=['HSPEC'], w=['H1'])
                    kb.dma('sp', lambda e: e.dma_start(out=H2s[:], in_=HSPEC[512 + gc0:512 + gc0 + 16].rearrange("c k r f -> k c r f")), r=['HSPEC'], w=['H2s'])
                    conv16(v16, 'v16', H1, 'H1', x116, 'x116', z16, 'z16')
                    conv16(z16, 'z16', H2s, 'H2s', x216, 'x216', y16, 'y16')
                    kb.dma('pool', lambda e: e.dma_start(out=YHYT[gc0:gc0 + 16, :].rearrange("c (n1 n2) -> n1 c n2", n2=128), in_=y16[:]), r=['y16'], w=['YHYT'])

        if stages >= 5:
            with stage() as st:
                z = sbt(st, "zt", [128, L])
                kb.op('pool', lambda e: e.memset(z[:], 0.0), w=['zt'])
                for q in range(4):
                    if dummy_mix:
                        kb.dma('sp', lambda e: e.dma_start(out=z[:], in_=QT[q * 128:(q + 1) * 128, :]), r=['zt', 'QT'], w=['zt'])
                    if dummy_mix:
                        kb.dma('sp', lambda e: e.dma_start(out=YHYT[q * 128:(q + 1) * 128, :], in_=z[:]), r=['zt'], w=['YHYT'])
                    if dummy_mix:
                        kb.dma('sp', lambda e: e.dma_start(out=z[:], in_=GT[q * 128:(q + 1) * 128, :]), r=['zt', 'GT'], w=['zt'])
                    if dummy_mix:
                        kb.dma('sp', lambda e: e.dma_start(out=YHGT[q * 128:(q + 1) * 128, :], in_=z[:]), r=['zt'], w=['YHGT'])
                zr = sbt(st, "zr", [128, D])
                kb.op('pool', lambda e: e.memset(zr[:], 0.0), w=['zr'])
                if dummy_mix or stages < 7:
                    for i in range(OWN // 128):
                        kb.dma('sp', lambda e: e.dma_start(out=ROUTED[i * 128:(i + 1) * 128, :], in_=zr[:]), r=['zr'], w=['ROUTED'])

        def row_bcast(stack, name, src_row_ap):
            t = sbt(stack, name, [128, D])
            kb.dma('sp', lambda e: e.dma_start(out=t[:], in_=src_row_ap.to_broadcast([128, D])), r=['MODROW'], w=[name])
            return t

        if stages >= 5:
            with stage() as st:
                oidx = sbt(st, "oidx", [128, 4], I32)
                kb.dma('sp', lambda e: e.dma_start(out=oidx[:], in_=OWNIDX), w=['oidx'])
                yg = [sbt(st, "yg%d" % i, [128, OWN]) for i in range(2)]
                n = 0
                for src, skey, r0 in ((YHYT, 'YHYT', 0), (YHGT, 'YHGT', 512)):
                    v = src.rearrange("c (j t) -> (c j) t", j=4)
                    for cc in range(4):
                        g = yg[n % 2]; gk = "yg%d" % (n % 2); n += 1
                        kb.dma('pool', lambda e: e.indirect_dma_start(out=g[:], out_offset=None, in_=v,
                                                                     in_offset=bass.IndirectOffsetOnAxis(ap=oidx[:, cc:cc + 1], axis=0),
                                                                     bounds_check=RB_OWN, oob_is_err=False), r=[skey, 'oidx'], w=[gk])
                        kb.dma('sp', lambda e: e.dma_start(out=YOWN[r0 + cc * 128:r0 + (cc + 1) * 128, :], in_=g[:]), r=[gk], w=['YOWN'])
            with stage() as st:
                Wy = sbt(st, "Wy", [128, 8, D]); Wo = sbt(st, "Wo", [128, 8, D])
                kb.dma('sp', lambda e: e.dma_start(out=Wy[:, 0:4, :], in_=WHY.rearrange("(k p) c -> p k c", p=128)), w=['Wy'])
                kb.dma('sp', lambda e: e.dma_start(out=Wy[:, 4:8, :], in_=WHG.rearrange("(k p) c -> p k c", p=128)), r=['Wy'], w=['Wy'])
                kb.dma('sp', lambda e: e.dma_start(out=Wo[:], in_=WOUT.rearrange("(k p) c -> p k c", p=128)), w=['Wo'])
                g1row = row_bcast(st, "g1row", MODROW[0:1, 2048:3072])
                yb = sbt(st, "yb", [128, 8, 512]); sgb = sbt(st, "sgb", [128, 16, 512]); mT = sbt(st, "mT", [128, 8, 512])
                t1 = [sbt(st, "t1_%d" % i, [128, 512]) for i in range(2)]
                xt = [sbt(st, "xt%d" % i, [128, D]) for i in range(2)]; pt = [sbt(st, "pt%d" % i, [128, D]) for i in range(2)]
                bi = 0
                for blk in range(OWN // 512):
                    t0 = blk * 512
                    kb.dma('sp', lambda e: e.dma_start(out=yb[:], in_=YOWN[:, t0:t0 + 512].rearrange("(k p) t -> p k t", p=128)), r=['YOWN'], w=['yb'])
                    kb.dma('sp', lambda e: e.dma_start(out=sgb[:], in_=SGT[:, t0:t0 + 512].rearrange("(k p) t -> p k t", p=128)), r=['SGT'], w=['sgb'])
                    for dm in range(8):
                        for br in range(2):
                            pb, pbk = banks[bi % 8]; bi += 1
                            for cc in range(4):
                                kb.op('pe', lambda e: e.matmul(pb[:, :], lhsT=Wy[:, br * 4 + cc, dm * 128:(dm + 1) * 128], rhs=yb[:, br * 4 + cc, :],
                                                               start=(cc == 0), stop=(cc == 3)), r=['Wy', 'yb'], w=[pbk])
                            if br == 0:
                                kb.op('dve', lambda e: e.tensor_tensor(out=t1[dm % 2][:], in0=pb[:, :], in1=sgb[:, dm, :], op=ALU.mult),
                                      r=[pbk, 'sgb'], w=['t1_%d' % (dm % 2)])
                            else:
                                kb.op('dve', lambda e: e.tensor_tensor(out=mT[:, dm, :], in0=pb[:, :], in1=sgb[:, 8 + dm, :], op=ALU.mult),
                                      r=[pbk, 'sgb', 'mT'], w=['mT'])
                                kb.op('pool', lambda e: e.tensor_tensor(out=mT[:, dm, :], in0=mT[:, dm, :], in1=t1[dm % 2][:], op=ALU.add),
                                      r=['mT', 't1_%d' % (dm % 2)], w=['mT'])
                    for tt in range(4):
                        ti = blk * 4 + tt; b2 = ti % 2
                        kb.dma('sp', lambda e: e.dma_start(out=xt[b2][:], in_=XOWN[ti * 128:(ti + 1) * 128, :]), w=['xt%d' % b2])
                        kb.dma('sp', lambda e: e.dma_start(out=pt[b2][:], in_=POSOWN[ti * 128:(ti + 1) * 128, :]), w=['pt%d' % b2])
                        kb.op('pool', lambda e: e.tensor_tensor(out=xt[b2][:], in0=xt[b2][:], in1=pt[b2][:], op=ALU.add), r=['xt%d' % b2, 'pt%d' % b2], w=['xt%d' % b2])
                        for hf in range(2):
                            pb, pbk = banks[bi % 8]; bi += 1
                            for kk in range(8):
                                kb.op('pe', lambda e: e.matmul(pb[:, :], lhsT=mT[:, kk, tt * 128:(tt + 1) * 128], rhs=Wo[:, kk, hf * 512:(hf + 1) * 512],
                                                               start=(kk == 0), stop=(kk == 7)), r=['mT', 'Wo'], w=[pbk])
                            kb.op('dve', lambda e: e.tensor_tensor(out=pt[b2][:, hf * 512:(hf + 1) * 512], in0=pb[:, :], in1=g1row[:, hf * 512:(hf + 1) * 512], op=ALU.mult),
                                  r=[pbk, 'g1row', 'pt%d' % b2], w=['pt%d' % b2])
                        kb.op('pool', lambda e: e.tensor_tensor(out=xt[b2][:], in0=xt[b2][:], in1=pt[b2][:], op=ALU.add), r=['xt%d' % b2, 'pt%d' % b2], w=['xt%d' % b2])
                        kb.dma('pool', lambda e: e.dma_start(out=X1D[ti * 128:(ti + 1) * 128, :], in_=xt[b2][:]), r=['xt%d' % b2], w=['X1D'])

        if stages >= 6:
            a2T = sbt(es, "a2T", [128, 8]); sh2T = sbt(es, "sh2T", [128, 8])
            kb.op('dve', lambda e: e.tensor_scalar(out=a2T[:], in0=modT[:, 32:40, 0], scalar1=1.0, scalar2=None, op0=ALU.add), r=['modT'], w=['a2T'])
            kb.op('dve', lambda e: e.tensor_tensor(out=a2T[:], in0=a2T[:], in1=g2T[:], op=ALU.mult), r=['a2T', 'g2T'], w=['a2T'])
            kb.op('dve', lambda e: e.tensor_copy(out=sh2T[:], in_=modT[:, 24:32, 0]), r=['modT'], w=['sh2T'])
            with stage() as s1:
                srcs = [(X1D[i * 128:(i + 1) * 128, :], None, i * 128) for i in range(OWN // 128)]
                kb.res.setdefault('a1T', [None, []])
                norm_to_xt(s1, srcs, H2T, a2T, sh2T, "n2", 'H2T')
            own_blocks = [(t, 512) for t in range(0, OWN, 512)]
            gemm_group("sg", SHG, 8, 256, H2T, 'H2T', own_blocks, [dict(c0=0, cn=256, mode='fm', func=AF.Silu, out=SGA, okey='SGA', oc0=0)])
            gemm_group("su", SHU, 8, 256, H2T, 'H2T', own_blocks, [dict(c0=0, cn=256, mode='fm', func=None, out=SUA, okey='SUA', oc0=0)])
            with stage() as st:
                ga = sbt(st, "ga", [128, 2, OWN]); ua = sbt(st, "ua", [128, 2, OWN])
                kb.dma('sp', lambda e: e.dma_start(out=ga[:], in_=SGA.rearrange("(k p) t -> p k t", p=128)), r=['SGA'], w=['ga'])
                kb.dma('sp', lambda e: e.dma_start(out=ua[:], in_=SUA.rearrange("(k p) t -> p k t", p=128)), r=['SUA'], w=['ua'])
                kb.op('dve', lambda e: e.tensor_tensor(out=ga[:], in0=ga[:], in1=ua[:], op=ALU.mult), r=['ga', 'ua'], w=['ga'])
                kb.dma('pool', lambda e: e.dma_start(out=ACTT.rearrange("(k p) t -> p k t", p=128), in_=ga[:]), r=['ga'], w=['ACTT'])
            gemm_group("sd", SHD, 2, 1024, ACTT, 'ACTT', own_blocks,
                       [dict(c0=0, cn=512, mode='tm', func=None, out=SHOUT, okey='SHOUT', oc0=0),
                        dict(c0=512, cn=512, mode='tm', func=None, out=SHOUT, okey='SHOUT', oc0=512)])
            if stages >= 7 and not dummy_mix:
                with stage() as st:
                    a2row = sbt(st, "a2row", [128, D]); g2nrow = sbt(st, "g2nrow", [128, D])
                    kb.dma('sp', lambda e: e.dma_start(out=a2row[:], in_=MODROW[0:1, 4096:5120].to_broadcast([128, D])), r=['MODROW'], w=['a2row'])
                    kb.dma('sp', lambda e: e.dma_start(out=g2nrow[:], in_=N2G.rearrange("(o n) -> o n", o=1).to_broadcast([128, D])), w=['g2nrow'])
                    kb.op('dve', lambda e: e.scalar_tensor_tensor(out=a2row[:], in0=a2row[:], scalar=1.0, in1=g2nrow[:], op0=ALU.add, op1=ALU.mult),
                          r=['a2row', 'g2nrow'], w=['a2row'])
                    sh2row = row_bcast(st, "sh2row", MODROW[0:1, 3072:4096])
                    xa = [sbt(st, "hxa%d" % i, [128, D]) for i in range(2)]; stt = [sbt(st, "hst%d" % i, [128, 4]) for i in range(2)]
                    junk = sbt(st, "hjunk", [128, D])
                    for ti in range(OWN // 128):
                        b2 = ti % 2; xk, tk = 'hxa%d' % b2, 'hst%d' % b2
                        rows = slice(ti * 128, (ti + 1) * 128)
                        kb.dma('sp', lambda e: e.dma_start(out=xa[b2][:], in_=X1D[rows, :]), r=['X1D'], w=[xk])
                        kb.op('act', lambda e: e.activation(out=junk[:], in_=xa[b2][:], func=AF.Square, accum_out=stt[b2][:, 0:1]), r=[xk], w=['hjunk', tk])
                        kb.op('dve', lambda e: e.tensor_scalar(out=stt[b2][:, 1:2], in0=stt[b2][:, 0:1], scalar1=1.0 / D, scalar2=EPS, op0=ALU.mult, op1=ALU.add), r=[tk], w=[tk])
                        kb.op('act', lambda e: e.activation(out=stt[b2][:, 2:3], in_=stt[b2][:, 1:2], func=AF.Sqrt), r=[tk], w=[tk])
                        kb.op('dve', lambda e: e.reciprocal(out=stt[b2][:, 3:4], in_=stt[b2][:, 2:3]), r=[tk], w=[tk])
                        kb.op('dve', lambda e: e.scalar_tensor_tensor(out=xa[b2][:], in0=xa[b2][:], scalar=stt[b2][:, 3:4], in1=a2row[:], op0=ALU.mult, op1=ALU.mult),
                              r=[xk, tk, 'a2row'], w=[xk])
                        kb.op('pool', lambda e: e.tensor_tensor(out=xa[b2][:], in0=xa[b2][:], in1=sh2row[:], op=ALU.add), r=[xk, 'sh2row'], w=[xk])
                        kb.dma('pool', lambda e: e.dma_start(out=H2[rows, :], in_=xa[b2][:]), r=[xk], w=['H2'])
                gemm_group("rt", RW, 8, NE, H2T, 'H2T', own_blocks, [dict(c0=0, cn=NE, mode='tm', func=AF.Sigmoid, out=SCORES, okey='SCORES', oc0=0)])
                with stage() as st:
                    NT = OWN // 128
                    onesm = sbt(st, "onesm", [128, 128]); stri = sbt(st, "stri", [128, 128]); slt = sbt(st, "slt", [128, 512])
                    blk128 = sbt(st, "blk128", [128, NBLK]); pidx = sbt(st, "pidx", [128, 1]); brow_ = sbt(st, "rbrow", [128, NE])
                    kb.dma('sp', lambda e: e.dma_start(out=onesm[:], in_=ONESd), w=['onesm'])
                    kb.dma('sp', lambda e: e.dma_start(out=stri[:], in_=STRId), w=['stri'])
                    kb.dma('sp', lambda e: e.dma_start(out=slt[:], in_=SLTd), w=['slt'])
                    kb.dma('sp', lambda e: e.dma_start(out=blk128[:], in_=BLKd), w=['blk128'])
                    kb.dma('sp', lambda e: e.dma_start(out=pidx[:], in_=PIDXd), w=['pidx'])
                    kb.dma('sp', lambda e: e.dma_start(out=brow_[:], in_=RB.rearrange("(o n) -> o n", o=1).to_broadcast([128, NE])), w=['rbrow'])
                    D8F = sbt(st, "D8F", [128, NT, 8]); W8 = sbt(st, "W8", [128, NT, 8]); D8I = sbt(st, "D8I", [128, NT * 8], I32); GI = sbt(st, "GI", [128, NBLK], I32)
                    with stage() as sr:
                        MSK = sbt(sr, "MSK", [128, NT, NE]); SEL = sbt(sr, "SEL", [128, NT, NE]); WD = sbt(sr, "WDm", [128, NT, NE]); DST = sbt(sr, "DST", [128, NT, NE])
                        V8 = sbt(sr, "V8", [128, NT, 8])
                        sc_ = [sbt(sr, "rsc%d" % i, [128, NE]) for i in range(2)]; bs = sbt(sr, "rbs", [128, NE])
                        M8 = sbt(sr, "M8", [128, 8, 8]); gs = sbt(sr, "rgs", [128, 8]); g8 = sbt(sr, "rg8", [128, 8]); gm = sbt(sr, "rgm", [128, 8]); pen = sbt(sr, "rpen", [128, 8])
                        den = sbt(sr, "rden", [128, 2]); base = sbt(sr, "rbase", [128, NE]); tmpq = sbt(sr, "rtmpq", [128, NE])
                        kb.op('pool', lambda e: e.memset(base[:], 0.0), w=['rbase'])
                        for ti in range(NT):
                            b2 = ti % 2; sk_ = 'rsc%d' % b2
                            kb.dma('sp', lambda e: e.dma_start(out=sc_[b2][:], in_=SCORES[ti * 128:(ti + 1) * 128, :]), r=['SCORES'], w=[sk_])
                            kb.op('dve', lambda e: e.tensor_tensor(out=bs[:], in0=sc_[b2][:], in1=brow_[:], op=ALU.add), r=[sk_, 'rbrow'], w=['rbs'])
                            for g in range(8):
                                kb.op('dve', lambda e: e.max(out=M8[:, g, :], in_=bs[:, 32 * g:32 * g + 32]), r=['rbs', 'M8'], w=['M8'])
                            kb.op('dve', lambda e: e.tensor_tensor(out=gs[:], in0=M8[:, :, 0], in1=M8[:, :, 1], op=ALU.add), r=['M8'], w=['rgs'])
                            kb.op('dve', lambda e: e.max(out=g8[:], in_=gs[:]), r=['rgs'], w=['rg8'])
                            kb.op('dve', lambda e: e.tensor_scalar(out=gm[:], in0=gs[:], scalar1=g8[:, 3:4], scalar2=None, op0=ALU.is_ge), r=['rgs', 'rg8'], w=['rgm'])
                            kb.op('dve', lambda e: e.tensor_scalar(out=pen[:], in0=gm[:], scalar1=-1.0, scalar2=1e30, op0=ALU.add, op1=ALU.mult), r=['rgm'], w=['rpen'])
                            for g in range(8):
                                kb.op('dve', lambda e: e.tensor_scalar(out=MSK[:, ti, 32 * g:32 * g + 32], in0=bs[:, 32 * g:32 * g + 32], scalar1=gm[:, g:g + 1], scalar2=pen[:, g:g + 1],
                                                                       op0=ALU.mult, op1=ALU.add), r=['rbs', 'rgm', 'rpen', 'MSK'], w=['MSK'])
                            kb.op('dve', lambda e: e.max(out=V8[:, ti, :], in_=MSK[:, ti, :]), r=['MSK', 'V8'], w=['V8'])
                            kb.op('dve', lambda e: e.tensor_scalar(out=SEL[:, ti, :], in0=MSK[:, ti, :], scalar1=V8[:, ti, 7:8], scalar2=None, op0=ALU.is_ge), r=['MSK', 'V8', 'SEL'], w=['SEL'])
                            kb.op('dve', lambda e: e.tensor_tensor(out=WD[:, ti, :], in0=SEL[:, ti, :], in1=sc_[b2][:], op=ALU.mult), r=['SEL', sk_, 'WDm'], w=['WDm'])
                            kb.op('dve', lambda e: e.tensor_reduce(out=den[:, 0:1], in_=WD[:, ti, :], axis=AX.X, op=ALU.add), r=['WDm', 'rden'], w=['rden'])
                            kb.op('dve', lambda e: e.reciprocal(out=den[:, 1:2], in_=den[:, 0:1]), r=['rden'], w=['rden'])
                            kb.op('dve', lambda e: e.tensor_scalar(out=WD[:, ti, :], in0=WD[:, ti, :], scalar1=den[:, 1:2], scalar2=2.5, op0=ALU.mult, op1=ALU.mult), r=['WDm', 'rden'], w=['WDm'])
                            p1, p1k = banks[(2 * ti) % 8]; p2, p2k = banks[(2 * ti + 1) % 8]
                            kb.op('pe', lambda e: e.matmul(p1[:, 0:NE], lhsT=stri[:], rhs=SEL[:, ti, :], start=True, stop=True), r=['stri', 'SEL'], w=[p1k])
                            kb.op('pe', lambda e: e.matmul(p2[:, 0:NE], lhsT=onesm[:], rhs=SEL[:, ti, :], start=True, stop=True), r=['onesm', 'SEL'], w=[p2k])
                            kb.op('dve', lambda e: e.tensor_tensor(out=DST[:, ti, :], in0=p1[:, 0:NE], in1=base[:], op=ALU.add), r=[p1k, 'rbase', 'DST'], w=['DST'])
                            kb.op('dve', lambda e: e.tensor_tensor(out=base[:], in0=p2[:, 0:NE], in1=base[:], op=ALU.add), r=[p2k, 'rbase'], w=['rbase'])
                        ci = sbt(sr, "rci", [128, NE], I32); padded = sbt(sr, "rpad", [128, NE]); pstart = sbt(sr, "rpst", [128, NE]); pend = sbt(sr, "rpend", [128, NE])
                        kb.op('dve', lambda e: e.tensor_scalar(out=tmpq[:], in0=base[:], scalar1=127.0, scalar2=None, op0=ALU.add), r=['rbase'], w=['rtmpq'])
                        kb.op('dve', lambda e: e.tensor_copy(out=ci[:], in_=tmpq[:]), r=['rtmpq'], w=['rci'])
                        kb.op('dve', lambda e: e.tensor_scalar(out=ci[:], in0=ci[:], scalar1=7, scalar2=None, op0=ALU.arith_shift_right), r=['rci'], w=['rci'])
                        kb.op('dve', lambda e: e.tensor_scalar(out=ci[:], in0=ci[:], scalar1=7, scalar2=None, op0=ALU.logical_shift_left), r=['rci'], w=['rci'])
                        kb.op('dve', lambda e: e.tensor_copy(out=padded[:], in_=ci[:]), r=['rci'], w=['rpad'])
                        padT = sbt(sr, "rpadT", [128, 2, 128]); pendT = sbt(sr, "rpendT", [128, 2, 128])
                        pa, pak = banks[0]
                        for hh in range(2):
                            kb.op('pe', lambda e: e.transpose(out=pa[:, hh * 128:(hh + 1) * 128], in_=padded[:, hh * 128:(hh + 1) * 128], identity=ident[:]), r=['rpad', 'ident'], w=[pak])
                        kb.op('dve', lambda e: e.tensor_copy(out=padT[:], in_=pa[:, 0:256].rearrange("p (h c) -> p h c", h=2)), r=[pak], w=['rpadT'])
                        pb_, pbk_ = banks[1]
                        for hh in range(2):
                            kb.op('pe', lambda e: e.matmul(pb_[:, 0:NE], lhsT=padT[:, hh, :], rhs=slt[:, hh * 256:(hh + 1) * 256], start=(hh == 0), stop=(hh == 1)), r=['rpadT', 'slt'], w=[pbk_])
                        kb.op('dve', lambda e: e.tensor_copy(out=pstart[:], in_=pb_[:, 0:NE]), r=[pbk_], w=['rpst'])
                        kb.op('dve', lambda e: e.tensor_tensor(out=pend[:], in0=pstart[:], in1=padded[:], op=ALU.add), r=['rpst', 'rpad'], w=['rpend'])
                        pc_, pck_ = banks[2]
                        for hh in range(2):
                            kb.op('pe', lambda e: e.transpose(out=pc_[:, hh * 128:(hh + 1) * 128], in_=pend[:, hh * 128:(hh + 1) * 128], identity=ident[:]), r=['rpend', 'ident'], w=[pck_])
                        kb.op('dve', lambda e: e.tensor_copy(out=pendT[:], in_=pc_[:, 0:256].rearrange("p (h c) -> p h c", h=2)), r=[pck_], w=['rpendT'])
                        cmpT = sbt(sr, "rcmpT", [128, 2, NBLK]); bef = sbt(sr, "rbef", [128, NBLK])
                        for hh in range(2):
                            kb.op('dve', lambda e: e.tensor_scalar(out=cmpT[:, hh, :], in0=blk128[:], scalar1=pendT[:, hh, 0:1], scalar2=None, op0=ALU.is_ge), r=['blk128', 'rpendT', 'rcmpT'], w=['rcmpT'])
                        pd_, pdk_ = banks[3]
                        for hh in range(2):
                            kb.op('pe', lambda e: e.matmul(pd_[:, 0:NBLK], lhsT=onesm[:], rhs=cmpT[:, hh, :], start=(hh == 0), stop=(hh == 1)), r=['onesm', 'rcmpT'], w=[pdk_])
                        kb.op('dve', lambda e: e.tensor_scalar(out=bef[:], in0=pd_[:, 0:NBLK], scalar1=255.0, scalar2=128.0, op0=ALU.min, op1=ALU.mult), r=[pdk_], w=['rbef'])
                        kb.op('dve', lambda e: e.tensor_scalar(out=bef[:], in0=bef[:], scalar1=pidx[:, 0:1], scalar2=None, op0=ALU.add), r=['rbef', 'pidx'], w=['rbef'])
                        kb.op('dve', lambda e: e.tensor_copy(out=GI[:], in_=bef[:]), r=['rbef'], w=['GI'])
                        eqj = sbt(sr, "reqj", [128, NE])
                        for ti in range(NT):
                            kb.op('dve', lambda e: e.tensor_tensor(out=DST[:, ti, :], in0=DST[:, ti, :], in1=pstart[:], op=ALU.add), r=['DST', 'rpst'], w=['DST'])
                            for k8 in range(8):
                                kb.op('dve', lambda e: e.scalar_tensor_tensor(out=eqj[:], in0=MSK[:, ti, :], scalar=V8[:, ti, k8:k8 + 1], in1=DST[:, ti, :], op0=ALU.is_equal, op1=ALU.mult),
                                      r=['MSK', 'V8', 'DST', 'reqj'], w=['reqj'])
                                kb.op('dve', lambda e: e.tensor_reduce(out=D8F[:, ti, k8:k8 + 1], in_=eqj[:], axis=AX.X, op=ALU.add), r=['reqj', 'D8F'], w=['D8F'])
                                kb.op('dve', lambda e: e.scalar_tensor_tensor(out=eqj[:], in0=MSK[:, ti, :], scalar=V8[:, ti, k8:k8 + 1], in1=WD[:, ti, :], op0=ALU.is_equal, op1=ALU.mult),
                                      r=['MSK', 'V8', 'WDm', 'reqj'], w=['reqj'])
                                kb.op('dve', lambda e: e.tensor_reduce(out=W8[:, ti, k8:k8 + 1], in_=eqj[:], axis=AX.X, op=ALU.add), r=['reqj', 'W8'], w=['W8'])
                        kb.op('dve', lambda e: e.tensor_copy(out=D8I[:], in_=D8F[:].rearrange("p t k -> p (t k)")), r=['D8F'], w=['D8I'])
                    dbg('D8F', D8F[:], [128, NT, 8]); dbg('W8', W8[:], [128, NT, 8])
                    ht = [sbt(st, "dht%d" % i, [128, D]) for i in range(2)]
                    for ti in range(NT):
                        b2 = ti % 2; hk = 'dht%d' % b2
                        kb.dma('sp', lambda e: e.dma_start(out=ht[b2][:], in_=H2[ti * 128:(ti + 1) * 128, :]), r=['H2'], w=[hk])
                        for k8 in range(8):
                            kb.dma('pool', lambda e: e.indirect_dma_start(out=XS, out_offset=bass.IndirectOffsetOnAxis(ap=D8I[:, ti * 8 + k8:ti * 8 + k8 + 1], axis=0), in_=ht[b2][:], in_offset=None,
                                                                         bounds_check=RB_XS, oob_is_err=False), r=[hk, 'D8I'], w=['XSw'])
                    kb.barrier()
                    NW = 3
                    wgu = [sbt(st, "wgu%d" % i, [128, 2, 8, 256]) for i in range(NW)]; wdn = [sbt(st, "wdn%d" % i, [128, 2, D]) for i in range(4)]
                    xs = [sbt(st, "xs%d" % i, [128, D]) for i in range(2)]; xsT = [sbt(st, "xsT%d" % i, [128, 8, 128]) for i in range(3)]
                    actT = [sbt(st, "actT%d" % i, [128, 2, 128]) for i in range(3)]; sg_ = [sbt(st, "esg%d" % i, [128, 256]) for i in range(3)]
                    ys = [sbt(st, "ys%d" % i, [128, D]) for i in range(2)]
                    bctr = [0]

                    def bk():
                        b_ = banks[bctr[0] % 8]; bctr[0] += 1
                        return b_

                    def phA(blk):
                        wk, dk, xk, xtk = 'wgu%d' % (blk % NW), 'wdn%d' % (blk % 4), 'xs%d' % (blk % 2), 'xsT%d' % (blk % 3)
                        kb.dma('pool', lambda e: e.indirect_dma_start(out=wgu[blk % NW][:].rearrange("p a k f -> p (a k f)"), out_offset=None, in_=EWGU,
                                                                     in_offset=bass.IndirectOffsetOnAxis(ap=GI[:, blk:blk + 1], axis=0),
                                                                     bounds_check=RB_W, oob_is_err=False), r=['GI'], w=[wk])
                        kb.dma('pool', lambda e: e.indirect_dma_start(out=wdn[blk % 4][:].rearrange("p k f -> p (k f)"), out_offset=None, in_=EWD,
                                                                     in_offset=bass.IndirectOffsetOnAxis(ap=GI[:, blk:blk + 1], axis=0),
                                                                     bounds_check=RB_W, oob_is_err=False), r=['GI'], w=[dk])
                        kb.dma('sp', lambda e: e.dma_start(out=xs[blk % 2][:], in_=XS[blk * 128:(blk + 1) * 128, :]), r=['XSw'], w=[xk])
                        for hh in range(2):
                            pb, pbk = bk()
                            for kk in range(4):
                                k8 = hh * 4 + kk
                                kb.op('pe', lambda e: e.transpose(out=pb[:, kk * 128:(kk + 1) * 128], in_=xs[blk % 2][:, k8 * 128:(k8 + 1) * 128], identity=ident[:]), r=[xk, 'ident'], w=[pbk])
                            evac(xsT[blk % 3][:, hh * 4:hh * 4 + 4, :], pb[:, :].rearrange("p (k t) -> p k t", k=4), None, [pbk, xtk], [xtk])

                    def phB(blk):
                        wk, xtk, sgk = 'wgu%d' % (blk % NW), 'xsT%d' % (blk % 3), 'esg%d' % (blk % 3)
                        ph, phk = bk()
                        for kk in range(8):
                            kb.op('pe', lambda e: e.matmul(ph[:, :].rearrange("p (a f) -> p a f", a=2), lhsT=xsT[blk % 3][:, kk, :], rhs=wgu[blk % NW][:, :, kk, :],
                                                           start=(kk == 0), stop=(kk == 7)), r=[wk, xtk], w=[phk])
                        kb.op('act', lambda e: e.activation(out=sg_[blk % 3][:], in_=ph[:, 0:256], func=AF.Silu), r=[phk], w=[sgk])
                        kb.op('dve', lambda e: e.tensor_tensor(out=sg_[blk % 3][:], in0=ph[:, 256:512], in1=sg_[blk % 3][:], op=ALU.mult), r=[phk, sgk], w=[sgk])

                    def phC(blk):
                        sgk, ak = 'esg%d' % (blk % 3), 'actT%d' % (blk % 3)
                        pt_, ptk = bk()
                        for kk in range(2):
                            kb.op('pe', lambda e: e.transpose(out=pt_[:, kk * 128:(kk + 1) * 128], in_=sg_[blk % 3][:, kk * 128:(kk + 1) * 128], identity=ident[:]), r=[sgk, 'ident'], w=[ptk])
                        evac(actT[blk % 3][:].rearrange("p k t -> p (k t)"), pt_[:, 0:256], None, [ptk, ak], [ak])

                    def phD(blk):
                        dk, ak, yk = 'wdn%d' % (blk % 4), 'actT%d' % (blk % 3), 'ys%d' % (blk % 2)
                        for hf in range(2):
                            py, pyk = bk()
                            for kk in range(2):
                                kb.op('pe', lambda e: e.matmul(py[:, :], lhsT=actT[blk % 3][:, kk, :], rhs=wdn[blk % 4][:, kk, hf * 512:(hf + 1) * 512], start=(kk == 0), stop=(kk == 1)), r=[ak, dk], w=[pyk])
                            evac(ys[blk % 2][:, hf * 512:(hf + 1) * 512], py[:, :], None, [pyk, yk], [yk])
                        kb.dma('sp', lambda e: e.dma_start(out=YS[blk * 128:(blk + 1) * 128, :], in_=ys[blk % 2][:]), r=[yk], w=['YSw'])

                    for s_ in range(NBLK + 3):
                        if s_ < NBLK:
                            phA(s_)
                        if 0 <= s_ - 1 < NBLK:
                            phB(s_ - 1)
                        if 0 <= s_ - 2 < NBLK:
                            phC(s_ - 2)
                        if 0 <= s_ - 3 < NBLK:
                            phD(s_ - 3)
                    kb.barrier()
                    acc = [sbt(st, "cacc%d" % i, [128, D]) for i in range(2)]; gg = [sbt(st, "cg%d" % i, [128, D]) for i in range(3)]
                    gi_ = 0
                    for ti in range(NT):
                        b2 = ti % 2; ack = 'cacc%d' % b2
                        for k8 in range(8):
                            g3 = gi_ % 3; gi_ += 1; ggk = 'cg%d' % g3
                            kb.dma('pool', lambda e: e.indirect_dma_start(out=gg[g3][:], out_offset=None, in_=YS, in_offset=bass.IndirectOffsetOnAxis(ap=D8I[:, ti * 8 + k8:ti * 8 + k8 + 1], axis=0),
                                                                         bounds_check=RB_XS, oob_is_err=False), r=['YSw', 'D8I'], w=[ggk])
                            if k8 == 0:
                                kb.op('dve', lambda e: e.tensor_scalar(out=acc[b2][:], in0=gg[g3][:], scalar1=W8[:, ti, 0:1], scalar2=None, op0=ALU.mult), r=[ggk, 'W8', ack], w=[ack])
                            else:
                                kb.op('dve', lambda e: e.scalar_tensor_tensor(out=acc[b2][:], in0=gg[g3][:], scalar=W8[:, ti, k8:k8 + 1], in1=acc[b2][:], op0=ALU.mult, op1=ALU.add),
                                      r=[ggk, 'W8', ack], w=[ack])
                        kb.dma('sp', lambda e: e.dma_start(out=ROUTED[ti * 128:(ti + 1) * 128, :], in_=acc[b2][:]), r=[ack], w=['ROUTED'])

            with stage() as st:
                g2row = row_bcast(st, "g2row", MODROW[0:1, 5120:6144])
                fgrow = sbt(st, "fgrow", [128, D])
                kb.dma('sp', lambda e: e.dma_start(out=fgrow[:], in_=FING.rearrange("(o n) -> o n", o=1).to_broadcast([128, D])), w=['fgrow'])
                xa = [sbt(st, "xa%d" % i, [128, D]) for i in range(2)]; sa = [sbt(st, "sa%d" % i, [128, D]) for i in range(2)]
                ra = [sbt(st, "ra%d" % i, [128, D]) for i in range(2)]; stt = [sbt(st, "stt%d" % i, [128, 4]) for i in range(2)]
                junk = sbt(st, "fjunk", [128, D])
                for ti in range(OWN // 128):
                    b2 = ti % 2; xk, sk, rk, tk = 'xa%d' % b2, 'sa%d' % b2, 'ra%d' % b2, 'stt%d' % b2
                    rows = slice(ti * 128, (ti + 1) * 128)
                    kb.dma('sp', lambda e: e.dma_start(out=xa[b2][:], in_=X1D[rows, :]), r=['X1D'], w=[xk])
                    kb.dma('sp', lambda e: e.dma_start(out=sa[b2][:], in_=SHOUT[rows, :]), r=['SHOUT'], w=[sk])
                    kb.dma('sp', lambda e: e.dma_start(out=ra[b2][:], in_=ROUTED[rows, :]), r=['ROUTED'], w=[rk])
                    kb.op('pool', lambda e: e.tensor_tensor(out=sa[b2][:], in0=sa[b2][:], in1=ra[b2][:], op=ALU.add), r=[sk, rk], w=[sk])
                    kb.op('dve', lambda e: e.tensor_tensor(out=sa[b2][:], in0=sa[b2][:], in1=g2row[:], op=ALU.mult), r=[sk, 'g2row'], w=[sk])
                    kb.op('pool', lambda e: e.tensor_tensor(out=xa[b2][:], in0=xa[b2][:], in1=sa[b2][:], op=ALU.add), r=[xk, sk], w=[xk])
                    kb.op('act', lambda e: e.activation(out=junk[:], in_=xa[b2][:], func=AF.Square, accum_out=stt[b2][:, 0:1]), r=[xk], w=['fjunk', tk])
                    kb.op('dve', lambda e: e.tensor_scalar(out=stt[b2][:, 1:2], in0=stt[b2][:, 0:1], scalar1=1.0 / D, scalar2=EPS, op0=ALU.mult, op1=ALU.add), r=[tk], w=[tk])
                    kb.op('act', lambda e: e.activation(out=stt[b2][:, 2:3], in_=stt[b2][:, 1:2], func=AF.Sqrt), r=[tk], w=[tk])
                    kb.op('dve', lambda e: e.reciprocal(out=stt[b2][:, 3:4], in_=stt[b2][:, 2:3]), r=[tk], w=[tk])
                    kb.op('dve', lambda e: e.scalar_tensor_tensor(out=xa[b2][:], in0=xa[b2][:], scalar=stt[b2][:, 3:4], in1=fgrow[:], op0=ALU.mult, op1=ALU.mult),
                          r=[xk, tk, 'fgrow'], w=[xk])
                    kb.dma('pool', lambda e: e.dma_start(out=OUT[rows, :], in_=xa[b2][:]), r=[xk], w=['OUT'])

        kb.finish('sp')
        kb.finish('pool')
        pg.ninstr = kb.ninstr
    return pg


_PROG = None


def make_in_maps(pg, inputs):
    hc = host_consts()
    sq = lambda a: np.ascontiguousarray(a[0])
    in_maps = []
    shared = {}
    if 'EWGU' in pg.ins:
        wg = np.asarray(inputs['exp_w_gate'])[0].reshape(NE, 8, 128, 256)
        wu = np.asarray(inputs['exp_w_up'])[0].reshape(NE, 8, 128, 256)
        ew = np.empty((NE, 128, 2, 8, 256), np.float32)
        ew[:, :, 0] = wg.transpose(0, 2, 1, 3); ew[:, :, 1] = wu.transpose(0, 2, 1, 3)
        shared['EWGU'] = ew.reshape(NE * 128, 4096)
        shared['EWD'] = np.ascontiguousarray(np.asarray(inputs['exp_w_down'])[0].reshape(NE, 2, 128, D).transpose(0, 2, 1, 3)).reshape(NE * 128, 2048)
    for c in range(8):
        b, j = c // 4, c % 4
        own = slice(j * OWN, (j + 1) * OWN)
        idx = ((np.arange(4)[None, :] * 128 + np.arange(128)[:, None]) * 4 + j).astype(np.int32)
        full = {
            'x': inputs['x'][b], 'ctx': inputs['ctx'][b], 'xown': inputs['x'][b, own], 'posown': hc['POS'][own],
            'c': inputs['c'][b], 'c_ctx': inputs['c_ctx'], 'final_g': inputs['final_g'], 'OWNIDX': idx,
            'hg_lb_logits': np.asarray(inputs['hg_lb_logits']).reshape(2, 1024),
        }
        full.update(shared)
        for k in pg.ins:
            if k not in full and k not in hc:
                full[k] = sq(inputs[k])
        full.update(hc)
        in_maps.append({k: np.ascontiguousarray(np.asarray(full[k])) for k in pg.ins})
    return in_maps


def kernel(**inputs):
    global _PROG
    if _PROG is None:
        _PROG = build()
    pg = _PROG
    in_maps = make_in_maps(pg, inputs)
    res = run_bass_kernel_spmd(pg.nc, in_maps, core_ids=list(range(8)))
    out = np.zeros((2, L, D), np.float32)
    for c in range(8):
        b, j = c // 4, c % 4
        out[b, j * OWN:(j + 1) * OWN] = res.results[c]['out']
    return out
```

```python
import math
import numpy as np
from contextlib import ExitStack, contextmanager
import concourse.bass as bass
import concourse.mybir as mybir
from concourse.bass_utils import run_bass_kernel_spmd

F32 = mybir.dt.float32
I32 = mybir.dt.int32
U32 = mybir.dt.uint32
ALU = mybir.AluOpType
AF = mybir.ActivationFunctionType
AX = mybir.AxisListType

N_DMA_SEMS = 24
D = 1024
L = 8192
NCTX = 256
LT = L + NCTX
OWN = 2048
NE = 256
NBLK = 383
EPS = 1e-6
TWO_PI = 2.0 * math.pi


class KB:
    def __init__(self, nc, es):
        self.nc = nc
        self.engs = {'pe': nc.tensor, 'act': nc.scalar, 'dve': nc.vector, 'pool': nc.gpsimd, 'sp': nc.sync}
        self.sems = {}
        self.cnt = {}
        for e in self.engs:
            self.sems[e] = es.enter_context(nc.semaphore("s_" + e))
            self.cnt[e] = 0
        for i in range(N_DMA_SEMS):
            self.sems['d%d' % i] = es.enter_context(nc.semaphore("s_d%d" % i))
            self.cnt['d%d' % i] = 0
        self.dnext = 0
        self.waited = {e: {} for e in self.engs}
        self.res = {}
        self.ninstr = 0

    def _need(self, eng, toks):
        best = {}
        for t in toks:
            if t is None:
                continue
            sk, v = t
            if sk == eng and eng == 'pe':
                continue
            if best.get(sk, 0) < v:
                best[sk] = v
        for sk, v in best.items():
            if self.waited[eng].get(sk, 0) >= v:
                continue
            self.engs[eng].wait_ge(self.sems[sk], v)
            self.waited[eng][sk] = v

    def _deps(self, r, w):
        toks = []
        for k in r:
            st = self.res.get(k)
            if st is not None:
                toks.append(st[0])
        for k in w:
            st = self.res.get(k)
            if st is not None:
                toks.append(st[0])
                toks.extend(st[1])
        return toks

    def _commit(self, tok, r, w):
        for k in r:
            st = self.res.setdefault(k, [None, []])
            st[1].append(tok)
            if len(st[1]) > 32:
                best = {}
                for sk, v in st[1]:
                    if best.get(sk, 0) < v:
                        best[sk] = v
                st[1] = list(best.items())
        for k in w:
            self.res[k] = [tok, []]

    def op(self, eng, fn, r=(), w=()):
        self._need(eng, self._deps(r, w))
        ins = fn(self.engs[eng])
        self.cnt[eng] += 1
        ins.then_inc(self.sems[eng], 1)
        self._commit((eng, self.cnt[eng]), r, w)
        self.ninstr += 1

    def dma(self, q, fn, r=(), w=()):
        i = self.dnext
        self.dnext = (self.dnext + 1) % N_DMA_SEMS
        sk = 'd%d' % i
        toks = self._deps(r, w)
        if self.cnt[sk] > 0:
            toks.append((sk, self.cnt[sk]))
        self._need(q, toks)
        ins = fn(self.engs[q])
        self.cnt[sk] += 16
        ins.then_inc(self.sems[sk], 16)
        self._commit((sk, self.cnt[sk]), r, w)
        self.ninstr += 1

    def barrier(self):
        toks = [(sk, v) for sk, v in self.cnt.items() if v > 0]
        for e in self.engs:
            self._need(e, toks)

    def finish(self, eng):
        toks = []
        for st in self.res.values():
            toks.append(st[0])
            toks.extend(st[1])
        self._need(eng, toks)


_CONST = None


def host_consts():
    global _CONST
    if _CONST is not None:
        return _CONST
    c = {}
    quarter = D // 4
    omega = (1.0 / (np.float32(10000.0) ** (np.arange(quarter, dtype=np.float32) / np.float32(quarter)))).astype(np.float32)
    rows, cols = L // 64, 64
    ang_r = (np.arange(rows, dtype=np.float32)[:, None] * omega).astype(np.float32)
    ang_c = (np.arange(cols, dtype=np.float32)[:, None] * omega).astype(np.float32)
    emb_r = np.concatenate([np.sin(ang_r), np.cos(ang_r)], -1)
    emb_c = np.concatenate([np.sin(ang_c), np.cos(ang_c)], -1)
    emb = np.concatenate([np.broadcast_to(emb_r[:, None], (rows, cols, D // 2)),
                          np.broadcast_to(emb_c[None], (rows, cols, D // 2))], -1)
    c['POS'] = np.ascontiguousarray(emb.reshape(L, D).astype(np.float32))
    c['IDENT'] = np.eye(128, dtype=np.float32)
    si = np.arange(128)[:, None]; ti = np.arange(128)[None, :]
    same = (si // 64) == (ti // 64)
    c['TRIF'] = (same & (si <= ti)).astype(np.float32)
    c['TRIB'] = (same & (si >= ti)).astype(np.float32)
    c['ONES'] = np.ones((128, 128), np.float32)
    c['STRI'] = (si < ti).astype(np.float32)
    e1 = np.arange(128)[:, None]; e2 = np.arange(256)[None, :]
    c['SLT'] = np.concatenate([(e1 < e2), (e1 + 128 < e2)], 1).astype(np.float32)
    c['BLK128'] = np.broadcast_to((np.arange(NBLK, dtype=np.float32) * 128.0)[None, :], (128, NBLK)).copy()
    c['PIDX'] = np.arange(128, dtype=np.float32).reshape(128, 1).copy()
    NN = 16384
    a = np.arange(128, dtype=np.float64)
    ang = 2.0 * np.pi * np.outer(a, a) / 128.0
    Fre = np.cos(ang); Fim = -np.sin(ang)
    f32 = lambda v: np.ascontiguousarray(v.astype(np.float32))
    c['FA'] = f32(np.concatenate([Fre, Fim], 1)); c['FAH'] = f32(np.concatenate([Fre, Fim], 1)[64:128])
    c['FRE'] = f32(Fre); c['FIM'] = f32(Fim); c['NFIM'] = f32(-Fim)
    c['CA'] = f32(np.concatenate([Fre, -Fim], 1)); c['CB'] = f32(np.concatenate([Fim, Fre], 1))
    c['FREN'] = f32(Fre[:, :64] / NN); c['FIMN'] = f32(Fim[:, :64] / NN)
    angT = 2.0 * np.pi * np.outer(a, a) / NN
    c['TRE2'] = f32(np.concatenate([np.cos(angT), np.cos(angT)], 1)); c['TIM2'] = f32(np.concatenate([-np.sin(angT), -np.sin(angT)], 1))
    bands = np.linspace(1e-4, 15.0, 16, dtype=np.float32)
    def zfeat(t):
        t = t.astype(np.float32)
        tn = (t / np.float32(L - 1)).astype(np.float32)
        an = (np.float32(2 * math.pi / L) * t[:, None] * bands).astype(np.float32)
        return np.concatenate([tn[:, None], np.cos(an), -np.sin(an)], -1).astype(np.float32), tn
    zf, tnf = zfeat(np.arange(L)); zb, tnb = zfeat(L - np.arange(L))
    c['ZT'] = np.ascontiguousarray(np.concatenate([zf, zb], 1).T)
    lo_ = math.log(1e-2) / 1.5; hi_ = math.log(1e-2) / 0.3
    deltas = np.abs(np.linspace(lo_, hi_, 512, dtype=np.float32))
    decf = np.exp(-tnf[:, None] * deltas).astype(np.float32)
    decb = np.exp(-tnb[:, None] * deltas).astype(np.float32); decb[0] = 0.0
    c['DEC'] = np.ascontiguousarray(np.stack([decf.reshape(64, 128, 512).transpose(0, 2, 1), decb.reshape(64, 128, 512).transpose(0, 2, 1)]))
    _CONST = c
    return c


class Prog:
    def __init__(self, debug=None):
        self.debug = debug or ()
        self.nc = bass.Bass("TRN2", target_bir_lowering=False)
        self.ins = {}
        self.outs = {}

    def inp(self, name, shape, dt=F32):
        t = self.nc.dram_tensor(name, list(shape), dt, kind="ExternalInput").ap()
        self.ins[name] = t
        return t

    def scratch(self, name, shape, dt=F32):
        if name in self.debug:
            t = self.nc.dram_tensor(name, list(shape), dt, kind="ExternalOutput").ap()
            self.outs[name] = t
        else:
            t = self.nc.dram_tensor(name, list(shape), dt, kind="Internal").ap()
        return t


def build(stages=99, debug=None, dummy_mix=False):
    pg = Prog(debug)
    nc = pg.nc
    X = pg.inp("x", [L, D]); CTX = pg.inp("ctx", [NCTX, D]); XOWN = pg.inp("xown", [OWN, D]); POSOWN = pg.inp("posown", [OWN, D])
    CV = pg.inp("c", [D]); CCTX = pg.inp("c_ctx", [D])
    N1G = pg.inp("norm1_g", [D]); N2G = pg.inp("norm2_g", [D])
    ADAW = pg.inp("ada_w", [D, 6 * D]); ADAB = pg.inp("ada_b", [6 * D])
    WIN = pg.inp("w_in", [D, 6144])
    POS = pg.inp("POS", [L, D]); IDENT = pg.inp("IDENT", [128, 128])
    WHY = pg.inp("w_hy_out", [512, D]); WHG = pg.inp("w_hg_out", [512, D]); WOUT = pg.inp("w_out", [D, D])
    SHG = pg.inp("sh_w_gate", [D, 256]); SHU = pg.inp("sh_w_up", [D, 256]); SHD = pg.inp("sh_w_down", [256, D])
    FING = pg.inp("final_g", [D]); OWNIDX = pg.inp("OWNIDX", [128, 4], I32)
    CONVW = pg.inp("hy_conv_w", [3, 1536]); CONVB = pg.inp("hy_conv_b", [1536])
    TRIFd = pg.inp("TRIF", [128, 128]); TRIBd = pg.inp("TRIB", [128, 128]); ONESd = pg.inp("ONES", [128, 128])
    LBL = pg.inp("hg_lb_logits", [2, 1024]); HGNG = pg.inp("hg_norm_g", [128])
    STRId = pg.inp("STRI", [128, 128]); SLTd = pg.inp("SLT", [128, 512]); BLKd = pg.inp("BLK128", [128, NBLK]); PIDXd = pg.inp("PIDX", [128, 1])
    RW = pg.inp("router_w", [D, NE]); RB = pg.inp("router_bias", [NE])
    EWGU = pg.inp("EWGU", [NE * 128, 4096]); EWD = pg.inp("EWD", [NE * 128, 2048])
    FAd = pg.inp("FA", [128, 256]); FAHd = pg.inp("FAH", [64, 256]); FREd = pg.inp("FRE", [128, 128]); FIMd = pg.inp("FIM", [128, 128]); NFIMd = pg.inp("NFIM", [128, 128])
    CAd = pg.inp("CA", [128, 256]); CBd = pg.inp("CB", [128, 256]); FRENd = pg.inp("FREN", [128, 64]); FIMNd = pg.inp("FIMN", [128, 64])
    TRE2d = pg.inp("TRE2", [128, 256]); TIM2d = pg.inp("TIM2", [128, 256]); ZTd = pg.inp("ZT", [66, L]); DECd = pg.inp("DEC", [2, 64, 512, 128])
    FW1 = pg.inp("hy_f_w1", [33, 64]); FB1 = pg.inp("hy_f_b1", [64]); FW2 = pg.inp("hy_f_w2", [64, 64]); FB2 = pg.inp("hy_f_b2", [64])
    FW3 = pg.inp("hy_f_w3", [64, 64]); FB3 = pg.inp("hy_f_b3", [64]); FW4 = pg.inp("hy_f_w4", [64, 2048]); FFQ = pg.inp("hy_f_freq", [64])
    HSKIP = pg.inp("hy_skip", [1024])
    HSPEC = pg.scratch("HSPEC", [1024, 128, 2, 128])
    OUT = pg.nc.dram_tensor("out", [OWN, D], F32, kind="ExternalOutput").ap()
    pg.outs["out"] = OUT
    XNT = pg.scratch("XNT", [D, LT]); XNTOWN = pg.scratch("XNTOWN", [D, OWN])
    MODROW = pg.scratch("MODROW", [2, 6 * D])
    HYRAW = pg.scratch("HYRAW", [1536, L])
    QT = pg.scratch("QT", [512, L]); GT = pg.scratch("GT", [512, L])
    IFF = pg.scratch("IFF", [LT, 1536])
    SGT = pg.scratch("SGT", [2048, OWN])
    HYC = pg.scratch("HYC", [1536, L])
    YHYT = pg.scratch("YHYT", [512, L]); YHGT = pg.scratch("YHGT", [512, L])
    YOWN = pg.scratch("YOWN", [1024, OWN])
    X1D = pg.scratch("X1D", [OWN, D]); H2T = pg.scratch("H2T", [D, OWN])
    SGA = pg.scratch("SGA", [256, OWN]); SUA = pg.scratch("SUA", [256, OWN]); ACTT = pg.scratch("ACTT", [256, OWN])
    SHOUT = pg.scratch("SHOUT", [OWN, D]); ROUTED = pg.scratch("ROUTED", [OWN, D])
    H2 = pg.scratch("H2", [OWN, D]); SCORES = pg.scratch("SCORES", [OWN, NE])
    XS = pg.scratch("XS", [NBLK * 128, D]); YS = pg.scratch("YS", [NBLK * 128, D])

    with ExitStack() as es:
        kb = KB(nc, es)
        sbt = lambda stack, name, shape, dt=F32: stack.enter_context(nc.sbuf_tensor(name, list(shape), dt))
        PA = es.enter_context(nc.psum_tensor("PA", [128, 2048], F32))
        PB = es.enter_context(nc.psum_tensor("PB", [128, 2048], F32))
        banks = [(PA[:, 512 * i:512 * (i + 1)], "PA%d" % i) for i in range(4)] + \
                [(PB[:, 512 * i:512 * (i + 1)], "PB%d" % i) for i in range(4)]
        RB_OWN = nc.gpsimd.alloc_register("bc_own"); nc.gpsimd.reg_mov(RB_OWN, 2047)
        RB_XS = nc.gpsimd.alloc_register("bc_xs"); nc.gpsimd.reg_mov(RB_XS, NBLK * 128 - 1)
        RB_W = nc.gpsimd.alloc_register("bc_w"); nc.gpsimd.reg_mov(RB_W, NE * 128 - 1)
        ident = sbt(es, "ident", [128, 128])
        kb.dma('sp', lambda e: e.dma_start(out=ident[:], in_=IDENT), w=['ident'])
        modT = sbt(es, "modT", [128, 48, 2])
        a1T = sbt(es, "a1T", [128, 8, 2]); sh1T = sbt(es, "sh1T", [128, 8, 2]); g2T = sbt(es, "g2T", [128, 8])
        @contextmanager
        def stage():
            with ExitStack() as st_:
                yield st_
                kb.barrier()

        def dbg(name, ap, shape):
            if name in pg.debug:
                t = nc.dram_tensor("D_" + name, list(shape), F32, kind="ExternalOutput").ap()
                pg.outs["D_" + name] = t
                kb.dma('pool', lambda e: e.dma_start(out=t, in_=ap), r=[name], w=['DBG' + name])

        with stage() as s0:
            sT = sbt(s0, "sT", [128, 8, 2]); vraw = sbt(s0, "vraw", [80, 128]); VT = sbt(s0, "VT", [128, 80])
            kb.dma('sp', lambda e: e.dma_start(out=vraw[0:8, :], in_=CV.rearrange("(q p) -> q p", p=128)), w=['vraw'])
            kb.dma('sp', lambda e: e.dma_start(out=vraw[8:16, :], in_=CCTX.rearrange("(q p) -> q p", p=128)), r=['vraw'], w=['vraw'])
            kb.dma('sp', lambda e: e.dma_start(out=vraw[16:24, :], in_=N1G.rearrange("(q p) -> q p", p=128)), r=['vraw'], w=['vraw'])
            kb.dma('sp', lambda e: e.dma_start(out=vraw[24:32, :], in_=N2G.rearrange("(q p) -> q p", p=128)), r=['vraw'], w=['vraw'])
            kb.dma('sp', lambda e: e.dma_start(out=vraw[32:80, :], in_=ADAB.rearrange("(q p) -> q p", p=128)), r=['vraw'], w=['vraw'])
            ps, psk = banks[0]
            kb.op('pe', lambda e: e.transpose(out=ps[:, 128:208], in_=vraw[:], identity=ident[0:80, 0:80]), r=['vraw', 'ident'], w=[psk])
            kb.op('dve', lambda e: e.tensor_copy(out=VT[:], in_=ps[:, 128:208]), r=[psk], w=['VT'])
            abT = VT[:, 32:80]; g1T = VT[:, 16:24]
            for r_ in range(2):
                kb.op('act', lambda e: e.activation(out=sT[:, :, r_], in_=VT[:, 8 * r_:8 * r_ + 8], func=AF.Silu), r=['VT', 'sT'], w=['sT'])
            kb.op('dve', lambda e: e.tensor_copy(out=g2T[:], in_=VT[:, 24:32]), r=['VT'], w=['g2T'])
            wbufs = [sbt(s0, "adaw%d" % i, [128, 8, 768]) for i in range(2)]
            mrow = sbt(s0, "mrow", [2, 6144]); brow = sbt(s0, "brow", [2, 6144])
            for r_ in range(2):
                kb.dma('sp', lambda e: e.dma_start(out=brow[r_:r_ + 1, :], in_=ADAB.rearrange("(o n) -> o n", o=1)), r=['brow'], w=['brow'])
            for cb in range(8):
                wb = wbufs[cb % 2]; wk = "adaw%d" % (cb % 2)
                kb.dma('sp', lambda e: e.dma_start(out=wb[:], in_=ADAW[:, cb * 768:(cb + 1) * 768].rearrange("(k p) c -> p k c", p=128)), w=[wk])
                for hh in range(2):
                    pr, prk = banks[1 + (2 * cb + hh) % 3]
                    for kk in range(8):
                        kb.op('pe', lambda e: e.matmul(pr[0:2, 0:384], lhsT=sT[:, kk, :], rhs=wb[:, kk, hh * 384:(hh + 1) * 384],
                                                       start=(kk == 0), stop=(kk == 7)), r=[wk, 'sT'], w=[prk])
                    c0 = cb * 768 + hh * 384
                    kb.op('dve', lambda e: e.tensor_tensor(out=mrow[:, c0:c0 + 384], in0=pr[0:2, 0:384], in1=brow[:, c0:c0 + 384], op=ALU.add),
                          r=[prk, 'brow'], w=['mrow'])
            kb.dma('pool', lambda e: e.dma_start(out=MODROW, in_=mrow[:]), r=['mrow'], w=['MODROW'])
            mq = sbt(s0, "mq", [96, 128])
            kb.dma('sp', lambda e: e.dma_start(out=mq[:], in_=MODROW.rearrange("r (q p) -> (r q) p", p=128)), r=['MODROW'], w=['mq'])
            kb.op('pe', lambda e: e.transpose(out=ps[:, 256:352], in_=mq[:], identity=ident[0:96, 0:96]), r=['mq', 'ident'], w=[psk])
            for r_ in range(2):
                kb.op('dve', lambda e: e.tensor_copy(out=modT[:, :, r_], in_=ps[:, 256 + 48 * r_:256 + 48 * r_ + 48]), r=[psk, 'modT'], w=['modT'])
            kb.op('dve', lambda e: e.tensor_scalar(out=a1T[:], in0=modT[:, 8:16, :], scalar1=1.0, scalar2=None, op0=ALU.add),
                  r=['modT'], w=['a1T'])
            for r_ in range(2):
                kb.op('dve', lambda e: e.tensor_tensor(out=a1T[:, :, r_], in0=a1T[:, :, r_], in1=g1T, op=ALU.mult),
                      r=['a1T', 'VT'], w=['a1T'])
            kb.op('dve', lambda e: e.tensor_copy(out=sh1T[:], in_=modT[:, 0:8, :]), r=['modT'], w=['sh1T'])

        dbg('modT', modT[:], [128, 48, 2]); dbg('a1T', a1T[:], [128, 8, 2])
        def norm_to_xt(stack, srcs, XT, a_ap, b_ap, tag, xtk):
            xin = [sbt(stack, "%s_xin%d" % (tag, i), [128, 1024]) for i in range(2)]
            pin = [sbt(stack, "%s_pin%d" % (tag, i), [128, 1024]) for i in range(2)]
            junk = sbt(stack, "%s_junk" % tag, [128, 1024])
            st = [sbt(stack, "%s_st%d" % (tag, i), [128, 4]) for i in range(2)]
            xo = [sbt(stack, "%s_xo%d" % (tag, i), [128, 8, 128]) for i in range(2)]
            for i, (xap, pap, c0) in enumerate(srcs):
                b = i % 2
                xk, pk, sk, ok = "%s_xin%d" % (tag, b), "%s_pin%d" % (tag, b), "%s_st%d" % (tag, b), "%s_xo%d" % (tag, b)
                kb.dma('sp', lambda e: e.dma_start(out=xin[b][:], in_=xap), w=[xk])
                if pap is not None:
                    kb.dma('sp', lambda e: e.dma_start(out=pin[b][:], in_=pap), w=[pk])
                    kb.op('pool', lambda e: e.tensor_tensor(out=xin[b][:], in0=xin[b][:], in1=pin[b][:], op=ALU.add), r=[xk, pk], w=[xk])
                kb.op('act', lambda e: e.activation(out=junk[:], in_=xin[b][:], func=AF.Square, accum_out=st[b][:, 0:1]),
                      r=[xk], w=[tag + '_junk', sk])
                kb.op('dve', lambda e: e.tensor_scalar(out=st[b][:, 1:2], in0=st[b][:, 0:1], scalar1=1.0 / D, scalar2=EPS, op0=ALU.mult, op1=ALU.add),
                      r=[sk], w=[sk])
                kb.op('act', lambda e: e.activation(out=st[b][:, 2:3], in_=st[b][:, 1:2], func=AF.Sqrt), r=[sk], w=[sk])
                kb.op('dve', lambda e: e.reciprocal(out=st[b][:, 3:4], in_=st[b][:, 2:3]), r=[sk], w=[sk])
                kb.op('dve', lambda e: e.tensor_scalar(out=xin[b][:], in0=xin[b][:], scalar1=st[b][:, 3:4], scalar2=None, op0=ALU.mult),
                      r=[xk, sk], w=[xk])
                for h in range(2):
                    pb, pbk = banks[(2 * i + h) % 4]
                    for kk in range(4):
                        k8 = h * 4 + kk
                        kb.op('pe', lambda e: e.transpose(out=pb[:, kk * 128:(kk + 1) * 128], in_=xin[b][:, k8 * 128:(k8 + 1) * 128], identity=ident[:]),
                              r=[xk, 'ident'], w=[pbk])
                    for kk in range(4):
                        k8 = h * 4 + kk
                        kb.op('act', lambda e: e.activation(out=xo[b][:, k8, :], in_=pb[:, kk * 128:(kk + 1) * 128], func=AF.Identity,
                                                            bias=b_ap[:, k8:k8 + 1], scale=a_ap[:, k8:k8 + 1]),
                              r=[pbk, 'a1T', 'sh1T', 'a2T'], w=[ok])
                kb.dma('pool', lambda e: e.dma_start(out=XT[:, c0:c0 + 128].rearrange("(k p) t -> p k t", p=128), in_=xo[b][:]), r=[ok], w=[xtk])

        if stages >= 1:
            with stage() as s1:
                srcs = [(X[i * 128:(i + 1) * 128, :], POS[i * 128:(i + 1) * 128, :], i * 128) for i in range(L // 128)]
                norm_to_xt(s1, srcs, XNT, a1T[:, :, 0], sh1T[:, :, 0], "n1", 'XNT')
            with stage() as s1:
                srcs = [(CTX[i * 128:(i + 1) * 128, :], None, L + i * 128) for i in range(NCTX // 128)]
                norm_to_xt(s1, srcs, XNT, a1T[:, :, 1], sh1T[:, :, 1], "n1c", 'XNT')
            with stage() as s1:
                srcs = [(XOWN[i * 128:(i + 1) * 128, :], POSOWN[i * 128:(i + 1) * 128, :], i * 128) for i in range(OWN // 128)]
                norm_to_xt(s1, srcs, XNTOWN, a1T[:, :, 0], sh1T[:, :, 0], "n1o", 'XNTOWN')

        cp_toggle = [0]

        def evac(out_ap, in_ap, func, r, w):
            if func is None:
                cp_toggle[0] ^= 1
                if cp_toggle[0]:
                    kb.op('dve', lambda e: e.tensor_copy(out=out_ap, in_=in_ap), r=r, w=w)
                else:
                    kb.op('act', lambda e: e.copy(out=out_ap, in_=in_ap), r=r, w=w)
            else:
                kb.op('act', lambda e: e.activation(out=out_ap, in_=in_ap, func=func), r=r, w=w)

        def gemm_group(tag, Wsrc, kch, ncols, XT, xtkey, tblocks, jobs):
            with stage() as st:
                Wsb = sbt(st, tag + "_W", [128, kch, ncols])
                kb.dma('sp', lambda e: e.dma_start(out=Wsb[:], in_=Wsrc.rearrange("(k p) c -> p k c", p=128)), w=[tag + '_W'])
                Xb = [sbt(st, "%s_X%d" % (tag, i), [128, kch, 512]) for i in range(2)]
                stg = [sbt(st, "%s_s%d" % (tag, i), [128, 512]) for i in range(4)]
                si = 0
                bi = 0
                for ti, (t0, tn) in enumerate(tblocks):
                    xb = Xb[ti % 2]; xk = "%s_X%d" % (tag, ti % 2)
                    kb.dma('sp', lambda e: e.dma_start(out=xb[:, :, 0:tn], in_=XT[:, t0:t0 + tn].rearrange("(k p) t -> p k t", p=128)),
                           r=[xtkey], w=[xk])
                    for jb in jobs:
                        if jb.get('tsel') is not None and not jb['tsel'](t0):
                            continue
                        c0, cn, tofs = jb['c0'], jb['cn'], jb.get('tofs', 0)
                        if jb['mode'] == 'fm':
                            for m in range(cn // 128):
                                pb, pbk = banks[bi % 8]; bi += 1
                                for kk in range(kch):
                                    kb.op('pe', lambda e: e.matmul(pb[:, 0:tn], lhsT=Wsb[:, kk, c0 + m * 128:c0 + (m + 1) * 128], rhs=xb[:, kk, 0:tn],
                                                                   start=(kk == 0), stop=(kk == kch - 1)), r=[tag + '_W', xk], w=[pbk])
                                sg = stg[si % 4]; sgk = "%s_s%d" % (tag, si % 4); si += 1
                                evac(sg[:, 0:tn], pb[:, 0:tn], jb['func'], [pbk], [sgk])
                                orow = jb.get('oc0', 0) + m * 128
                                kb.dma('pool', lambda e: e.dma_start(out=jb['out'][orow:orow + 128, t0 - tofs:t0 - tofs + tn], in_=sg[:, 0:tn]),
                                       r=[sgk], w=[jb['okey']])
                        else:
                            for tt in range(tn // 128):
                                pb, pbk = banks[bi % 8]; bi += 1
                                for kk in range(kch):
                                    kb.op('pe', lambda e: e.matmul(pb[:, 0:cn], lhsT=xb[:, kk, tt * 128:(tt + 1) * 128], rhs=Wsb[:, kk, c0:c0 + cn],
                                                                   start=(kk == 0), stop=(kk == kch - 1)), r=[tag + '_W', xk], w=[pbk])
                                sg = stg[si % 4]; sgk = "%s_s%d" % (tag, si % 4); si += 1
                                evac(sg[:, 0:cn], pb[:, 0:cn], jb['func'], [pbk], [sgk])
                                tr = t0 - tofs + tt * 128
                                oc0 = jb.get('oc0', 0)
                                kb.dma('pool', lambda e: e.dma_start(out=jb['out'][tr:tr + 128, oc0:oc0 + cn], in_=sg[:, 0:cn]),
                                       r=[sgk], w=[jb['okey']])

        if stages >= 2:
            lat_blocks = [(t, 512) for t in range(0, L, 512)]
            all_blocks = lat_blocks + [(L, 256)]
            gemm_group("g1", WIN[:, 0:1024], 8, 1024, XNT, 'XNT', lat_blocks,
                       [dict(c0=0, cn=1024, mode='fm', func=None, out=HYRAW, okey='HYRAW', oc0=0)])
            gemm_group("g2", WIN[:, 1024:2048], 8, 1024, XNT, 'XNT', lat_blocks,
                       [dict(c0=0, cn=512, mode='fm', func=None, out=HYRAW, okey='HYRAW', oc0=1024),
                        dict(c0=512, cn=512, mode='fm', func=AF.Silu, out=QT, okey='QT', oc0=0)])
        if stages >= 3:
            gemm_group("g3", WIN[:, 2048:3072], 8, 1024, XNT, 'XNT', all_blocks,
                       [dict(c0=0, cn=512, mode='tm', func=None, out=IFF, okey='IFF', oc0=0),
                        dict(c0=512, cn=512, mode='tm', func=None, out=IFF, okey='IFF', oc0=512)])
            gemm_group("g4", WIN[:, 3072:4096], 8, 1024, XNT, 'XNT', all_blocks,
                       [dict(c0=0, cn=512, mode='tm', func=None, out=IFF, okey='IFF', oc0=1024),
                        dict(c0=512, cn=512, mode='fm', func=AF.Silu, out=GT, okey='GT', oc0=0, tsel=lambda t0: t0 < L)])

        if stages >= 3:
            own_blocks = [(t, 512) for t in range(0, OWN, 512)]
            for gi in range(2):
                gemm_group("g%d" % (5 + gi), WIN[:, 4096 + 1024 * gi:5120 + 1024 * gi], 8, 1024, XNTOWN, 'XNTOWN', own_blocks,
                           [dict(c0=0, cn=1024, mode='fm', func=AF.Sigmoid, out=SGT, okey='SGT', oc0=1024 * gi)])

        if stages >= 4:
            with stage() as st:
                cwT = sbt(st, "cwT", [128, 12, 4]); craw = sbt(st, "craw", [48, 128])
                for j3 in range(3):
                    kb.dma('sp', lambda e: e.dma_start(out=craw[12 * j3:12 * j3 + 12, :], in_=CONVW[j3].rearrange("(q p) -> q p", p=128)), r=['craw'], w=['craw'])
                kb.dma('sp', lambda e: e.dma_start(out=craw[36:48, :], in_=CONVB.rearrange("(q p) -> q p", p=128)), r=['craw'], w=['craw'])
                pb, pbk = banks[0]
                kb.op('pe', lambda e: e.transpose(out=pb[:, 0:48], in_=craw[:], identity=ident[0:48, 0:48]), r=['craw', 'ident'], w=[pbk])
                kb.op('dve', lambda e: e.tensor_copy(out=cwT[:], in_=pb[:, 0:48].rearrange("p (j q) -> p q j", j=4)), r=[pbk], w=['cwT'])
                uin = [sbt(st, "uin%d" % i, [128, L + 2]) for i in range(2)]
                uo = [sbt(st, "uo%d" % i, [128, L]) for i in range(2)]
                for i in range(2):
                    kb.op('pool', lambda e: e.memset(uin[i][:, 0:1], 0.0), w=['uin%d' % i])
                    kb.op('pool', lambda e: e.memset(uin[i][:, L + 1:L + 2], 0.0), r=['uin%d' % i], w=['uin%d' % i])
                for q in range(12):
                    b2 = q % 2; uk = 'uin%d' % b2; ok = 'uo%d' % b2
                    kb.dma('sp', lambda e: e.dma_start(out=uin[b2][:, 1:L + 1], in_=HYRAW[q * 128:(q + 1) * 128, :]), r=['HYRAW', uk], w=[uk])
                    kb.op('dve', lambda e: e.tensor_scalar(out=uo[b2][:], in0=uin[b2][:, 0:L], scalar1=cwT[:, q, 0:1], scalar2=cwT[:, q, 3:4],
                                                           op0=ALU.mult, op1=ALU.add), r=[uk, 'cwT'], w=[ok])
                    kb.op('dve', lambda e: e.scalar_tensor_tensor(out=uo[b2][:], in0=uin[b2][:, 1:L + 1], scalar=cwT[:, q, 1:2], in1=uo[b2][:],
                                                                  op0=ALU.mult, op1=ALU.add), r=[uk, 'cwT', ok], w=[ok])
                    kb.op('dve', lambda e: e.scalar_tensor_tensor(out=uo[b2][:], in0=uin[b2][:, 2:L + 2], scalar=cwT[:, q, 2:3], in1=uo[b2][:],
                                                                  op0=ALU.mult, op1=ALU.add), r=[uk, 'cwT', ok], w=[ok])
                    kb.dma('pool', lambda e: e.dma_start(out=HYC[q * 128:(q + 1) * 128, :], in_=uo[b2][:]), r=[ok], w=['HYC'])

        if stages >= 5 and not dummy_mix:
          with stage() as st:
            tri = {0: sbt(st, "trif", [128, 128]), 1: sbt(st, "trib", [128, 128])}
            ones = sbt(st, "ones", [128, 128])
            kb.dma('sp', lambda e: e.dma_start(out=tri[0][:], in_=TRIFd), w=['tri'])
            kb.dma('sp', lambda e: e.dma_start(out=tri[1][:], in_=TRIBd), r=['tri'], w=['tri'])
            kb.dma('sp', lambda e: e.dma_start(out=ones[:], in_=ONESd), w=['ones'])
            l0 = sbt(st, "l0", [128, 1024]); l1 = sbt(st, "l1", [128, 1024]); oml = sbt(st, "oml", [128, 1024])
            kb.dma('sp', lambda e: e.dma_start(out=l0[:], in_=LBL[0:1, :].to_broadcast([128, 1024])), w=['l0'])
            kb.dma('sp', lambda e: e.dma_start(out=l1[:], in_=LBL[1:2, :].to_broadcast([128, 1024])), w=['l1'])
            kb.op('dve', lambda e: e.tensor_tensor(out=l0[:], in0=l0[:], in1=l1[:], op=ALU.subtract), r=['l0', 'l1'], w=['l0'])
            kb.op('act', lambda e: e.activation(out=l0[:], in_=l0[:], func=AF.Sigmoid), r=['l0'], w=['l0'])
            kb.op('dve', lambda e: e.tensor_scalar(out=oml[:], in0=l0[:], scalar1=-1.0, scalar2=1.0, op0=ALU.mult, op1=ALU.add), r=['l0'], w=['oml'])
            ngT = sbt(st, "ngT", [128, 1])
            with nc.allow_non_contiguous_dma(reason="128-element vector to partitions"):
                kb.dma('sp', lambda e: e.dma_start(out=ngT[:], in_=HGNG.rearrange("(p o) -> p o", o=1)), w=['ngT'])
            osum = sbt(st, "osum", [128, L])
            LB8 = sbt(st, "LB8", [128, 8, 128]); OML8 = sbt(st, "OML8", [128, 8, 128])
            vseg = [sbt(st, "vseg%d" % i, [128, 8, 128]) for i in range(2)]
            fseg = [sbt(st, "fseg%d" % i, [128, 8, 128]) for i in range(2)]
            lseg = [sbt(st, "lseg%d" % i, [128, 8, 128]) for i in range(2)]
            kseg = [sbt(st, "kseg%d" % i, [128, 8, 128]) for i in range(2)]
            qseg = [sbt(st, "qseg%d" % i, [128, 1024]) for i in range(2)]
            R3 = 3
            ekb = [sbt(st, "ekb%d" % i, [128, 128]) for i in range(R3)]
            kd = [sbt(st, "kd%d" % i, [128, 128]) for i in range(R3)]
            ebT = [sbt(st, "ebT%d" % i, [128, 128]) for i in range(R3)]
            qdT = [sbt(st, "qdT%d" % i, [128, 128]) for i in range(R3)]
            kdT = [sbt(st, "kdT%d" % i, [128, 128]) for i in range(R3)]
            attm = [sbt(st, "attm%d" % i, [128, 128]) for i in range(R3)]
            Sr = [sbt(st, "S%d" % i, [128, 128]) for i in range(4)]
            Se = [sbt(st, "Se%d" % i, [128, 128]) for i in range(2)]
            pbi = [0]

            def nb():
                b_ = banks[pbi[0] % 8]; pbi[0] += 1
                return b_

            for h in range(4):
                for dr in range(2):
                    T = tri[dr]
                    fcol = 512 + 512 * dr + h * 128
                    for n in range(8):
                        kb.op('pool', lambda e: e.tensor_copy(out=LB8[:, n, :], in_=l0[:, dr * 512 + h * 128:dr * 512 + (h + 1) * 128]), r=['l0', 'LB8'], w=['LB8'])
                        kb.op('pool', lambda e: e.tensor_copy(out=OML8[:, n, :], in_=oml[:, dr * 512 + h * 128:dr * 512 + (h + 1) * 128]), r=['oml', 'OML8'], w=['OML8'])
                    si_ = 0
                    kb.op('pool', lambda e: e.memset(Sr[0][:], 0.0), w=['S0'])
                    segs = [(L, 2, False)] + [(sg * 1024, 8, True) for sg in (range(8) if dr == 0 else range(7, -1, -1))]
                    tcount = 0
                    for sgi, (r0, nt, lat) in enumerate(segs):
                        b2 = sgi % 2
                        vk, fk, lk, kk_, qk = 'vseg%d' % b2, 'fseg%d' % b2, 'lseg%d' % b2, 'kseg%d' % b2, 'qseg%d' % b2
                        kb.dma('sp', lambda e: e.dma_start(out=vseg[b2][:, 0:nt, :], in_=IFF[r0:r0 + nt * 128, h * 128:(h + 1) * 128].rearrange("(n p) c -> p n c", p=128)),
                               r=['IFF'], w=[vk])
                        kb.dma('sp', lambda e: e.dma_start(out=fseg[b2][:, 0:nt, :], in_=IFF[r0:r0 + nt * 128, fcol:fcol + 128].rearrange("(n p) c -> p n c", p=128)),
                               r=['IFF'], w=[fk])
                        if lat:
                            kb.dma('sp', lambda e: e.dma_start(out=qseg[b2][:], in_=QT[h * 128:(h + 1) * 128, r0:r0 + 1024]), r=['QT'], w=[qk])
                        kb.op('act', lambda e: e.activation(out=fseg[b2][:, 0:nt, :], in_=fseg[b2][:, 0:nt, :], func=AF.Sigmoid), r=[fk], w=[fk])
                        kb.op('dve', lambda e: e.tensor_tensor(out=fseg[b2][:, 0:nt, :], in0=fseg[b2][:, 0:nt, :], in1=OML8[:, 0:nt, :], op=ALU.mult), r=[fk, 'OML8'], w=[fk])
                        kb.op('dve', lambda e: e.tensor_tensor(out=fseg[b2][:, 0:nt, :], in0=fseg[b2][:, 0:nt, :], in1=LB8[:, 0:nt, :], op=ALU.add), r=[fk, 'LB8'], w=[fk])
                        kb.op('act', lambda e: e.activation(out=lseg[b2][:, 0:nt, :], in_=fseg[b2][:, 0:nt, :], func=AF.Ln), r=[fk], w=[lk])
                        kb.op('dve', lambda e: e.tensor_scalar(out=kseg[b2][:, 0:nt, :], in0=fseg[b2][:, 0:nt, :], scalar1=-1.0, scalar2=1.0, op0=ALU.mult, op1=ALU.add),
                              r=[fk], w=[kk_])
                        order = list(range(nt)) if dr == 0 else list(range(nt - 1, -1, -1))
                        slot = {}

                        def prep(n, b2=b2, lat=lat, lk=lk, kk_=kk_, qk=qk):
                            nonlocal tcount
                            r3 = tcount % R3; tcount += 1
                            slot[n] = r3
                            ek, kdk, ebk, qdk, ktk = 'ekb%d' % r3, 'kd%d' % r3, 'ebT%d' % r3, 'qdT%d' % r3, 'kdT%d' % r3
                            p1, p1k = nb()
                            kb.op('pe', lambda e: e.matmul(p1[:, 0:128], lhsT=T[:], rhs=lseg[b2][:, n, :], start=True, stop=True), r=['tri', lk], w=[p1k])
                            p2, p2k = nb()
                            kb.op('pe', lambda e: e.matmul(p2[:, 0:128], lhsT=lseg[b2][:, n, :], rhs=T[:], start=True, stop=True), r=['tri', lk], w=[p2k])
                            kb.op('act', lambda e: e.activation(out=ekb[r3][:], in_=p1[:, 0:128], func=AF.Exp, scale=-1.0), r=[p1k], w=[ek])
                            kb.op('act', lambda e: e.activation(out=ebT[r3][:], in_=p2[:, 0:128], func=AF.Exp), r=[p2k], w=[ebk])
                            kb.op('dve', lambda e: e.tensor_tensor(out=kd[r3][:], in0=kseg[b2][:, n, :], in1=ekb[r3][:], op=ALU.mult), r=[kk_, ek], w=[kdk])
                            if lat:
                                kb.op('dve', lambda e: e.tensor_tensor(out=qdT[r3][:], in0=qseg[b2][:, n * 128:(n + 1) * 128], in1=ebT[r3][:], op=ALU.mult), r=[qk, ebk], w=[qdk])
                                p3, p3k = nb()
                                kb.op('pe', lambda e: e.transpose(out=p3[:, 0:128], in_=kd[r3][:], identity=ident[:]), r=[kdk, 'ident'], w=[p3k])
                                kb.op('act', lambda e: e.copy(out=kdT[r3][:], in_=p3[:, 0:128]), r=[p3k], w=[ktk])

                        def scan(n, b2=b2, lat=lat, vk=vk, r0=r0):
                            nonlocal si_
                            r3 = slot[n]
                            kdk, ebk, qdk, ktk, amk = 'kd%d' % r3, 'ebT%d' % r3, 'qdT%d' % r3, 'kdT%d' % r3, 'attm%d' % r3
                            halves = [(0, 64, 63), (64, 128, 127)] if dr == 0 else [(64, 128, 64), (0, 64, 0)]
                            if lat:
                                p4, p4k = nb()
                                kb.op('pe', lambda e: e.matmul(p4[:, 0:128], lhsT=kdT[r3][:], rhs=qdT[r3][:], start=True, stop=True), r=[ktk, qdk], w=[p4k])
                                kb.op('dve', lambda e: e.tensor_tensor(out=attm[r3][:], in0=p4[:, 0:128], in1=T[:], op=ALU.mult), r=[p4k, 'tri'], w=[amk])
                            pds = []
                            for (c0, c1, ce) in halves:
                                pd, pdk = nb()
                                kb.op('pe', lambda e: e.matmul(pd[:, 0:128], lhsT=kd[r3][c0:c1, :], rhs=vseg[b2][c0:c1, n, :], start=True, stop=True), r=[kdk, vk], w=[pdk])
                                pds.append((pd, pdk))
                            Ss = []
                            for hi_, (c0, c1, ce) in enumerate(halves):
                                Sc = Sr[si_ % 4]; Sck = 'S%d' % (si_ % 4)
                                Sn = Sr[(si_ + 1) % 4]; Snk = 'S%d' % ((si_ + 1) % 4)
                                sew = Se[si_ % 2]; sek = 'Se%d' % (si_ % 2)
                                si_ += 1
                                Ss.append((Sc, Sck))
                                pd, pdk = pds[hi_]
                                kb.op('act', lambda e: e.activation(out=sew[:], in_=Sc[:], func=AF.Copy, scale=ebT[r3][:, ce:ce + 1]), r=[Sck, ebk], w=[sek])
                                kb.op('dve', lambda e: e.scalar_tensor_tensor(out=Sn[:], in0=pd[:, 0:128], scalar=ebT[r3][:, ce:ce + 1], in1=sew[:], op0=ALU.mult, op1=ALU.add),
                                      r=[pdk, ebk, sek], w=[Snk])
                            if lat:
                                po, pok = nb()
                                kb.op('pe', lambda e: e.matmul(po[:, 0:128], lhsT=vseg[b2][:, n, :], rhs=attm[r3][:], start=True, stop=False), r=[vk, amk], w=[pok])
                                for hi_, (c0, c1, ce) in enumerate(halves):
                                    Sc, Sck = Ss[hi_]
                                    kb.op('pe', lambda e: e.matmul(po[:, c0:c1], lhsT=Sc[:], rhs=qdT[r3][:, c0:c1], start=False, stop=True), r=[Sck, qdk], w=[pok])
                                cols = slice(r0 + n * 128, r0 + (n + 1) * 128)
                                if dr == 0:
                                    kb.op('act', lambda e: e.copy(out=osum[:, cols], in_=po[:, 0:128]), r=[pok], w=['osum%d' % (r0 // 1024)])
                                else:
                                    kb.op('dve', lambda e: e.tensor_tensor(out=osum[:, cols], in0=po[:, 0:128], in1=osum[:, cols], op=ALU.add),
                                          r=[pok, 'osum%d' % (r0 // 1024)], w=['osum%d' % (r0 // 1024)])

                        prep(order[0])
                        for oi, n in enumerate(order):
                            if oi + 1 < len(order):
                                prep(order[oi + 1])
                            scan(n)
                    if si_ % 4 != 0:
                        pass
                    kb.res.pop('unused', None)
                sq = [sbt(st, "hsq%d_%d" % (h, i), [128, 512]) for i in range(2)] if h == 0 else sq
                gt_ = [sbt(st, "hgt%d_%d" % (h, i), [128, 512]) for i in range(2)] if h == 0 else gt_
                rs = [sbt(st, "hrs%d_%d" % (h, i), [128, 512]) for i in range(2)] if h == 0 else rs
                for blk in range(L // 512):
                    b2 = blk % 2; cols = slice(blk * 512, (blk + 1) * 512)
                    sqk, gk, rk = 'hsq%d' % b2, 'hgt%d' % b2, 'hrs%d' % b2
                    ok_ = 'osum%d' % (blk // 2)
                    kb.dma('sp', lambda e: e.dma_start(out=gt_[b2][:], in_=GT[h * 128:(h + 1) * 128, cols]), r=['GT'], w=[gk])
                    kb.op('act', lambda e: e.activation(out=sq[b2][:], in_=osum[:, cols], func=AF.Square), r=[ok_], w=[sqk])
                    pb, pbk = nb()
                    kb.op('pe', lambda e: e.matmul(pb[:, :], lhsT=ones[:], rhs=sq[b2][:], start=True, stop=True), r=['ones', sqk], w=[pbk])
                    kb.op('dve', lambda e: e.tensor_scalar(out=rs[b2][:], in0=pb[:, :], scalar1=1.0 / 128, scalar2=EPS, op0=ALU.mult, op1=ALU.add), r=[pbk], w=[rk])
                    kb.op('act', lambda e: e.activation(out=rs[b2][:], in_=rs[b2][:], func=AF.Sqrt), r=[rk], w=[rk])
                    kb.op('dve', lambda e: e.reciprocal(out=rs[b2][:], in_=rs[b2][:]), r=[rk], w=[rk])
                    kb.op('dve', lambda e: e.scalar_tensor_tensor(out=sq[b2][:], in0=osum[:, cols], scalar=ngT[:, 0:1], in1=rs[b2][:], op0=ALU.mult, op1=ALU.mult),
                          r=[ok_, 'ngT', rk, sqk], w=[sqk])
                    kb.op('pool', lambda e: e.tensor_tensor(out=sq[b2][:], in0=sq[b2][:], in1=gt_[b2][:], op=ALU.mult), r=[sqk, gk], w=[sqk])
                    kb.dma('pool', lambda e: e.dma_start(out=YHGT[h * 128:(h + 1) * 128, cols], in_=sq[b2][:]), r=[sqk], w=['YHGT'])

        if stages >= 5 and not dummy_mix:
          with stage() as st:
            cst = {}
            for nm, src, shp in (("FA", FAd, [128, 256]), ("FAH", FAHd, [64, 256]), ("FRE", FREd, [128, 128]), ("FIM", FIMd, [128, 128]), ("NFIM", NFIMd, [128, 128]),
                                 ("CA", CAd, [128, 256]), ("CB", CBd, [128, 256]), ("FREN", FRENd, [128, 64]), ("FIMN", FIMNd, [128, 64]),
                                 ("TRE2", TRE2d, [128, 256]), ("TIM2", TIM2d, [128, 256]), ("ONESH", ONESd, [128, 128])):
                cst[nm] = sbt(st, "c_" + nm, shp)
                kb.dma('sp', lambda e: e.dma_start(out=cst[nm][:], in_=src), w=['hconst'])
            skb = sbt(st, "skb", [128, 1024])
            kb.dma('sp', lambda e: e.dma_start(out=skb[:], in_=HSKIP.rearrange("(o n) -> o n", o=1).to_broadcast([128, 1024])), w=['skb'])
            tw = [sbt(st, "tw%d" % i, [128, 256]) for i in range(8)]
            hbi = [0]

            def hb():
                b_ = banks[hbi[0] % 8]; hbi[0] += 1
                return b_

            def twiddle(ps, psk, Bt, bkey, c0, conj):
                A = ps[:, :].rearrange("p (c r f) -> p c r f", c=2, r=2)
                Are, Aim = A[:, :, 0, :], A[:, :, 1, :]
                T2r = cst["TRE2"][:].rearrange("p (c f) -> p c f", c=2); T2i = cst["TIM2"][:].rearrange("p (c f) -> p c f", c=2)
                par = (c0 // 2) % 2
                t = [tw[4 * par + i][:].rearrange("p (c f) -> p c f", c=2) for i in range(4)]
                tk = ['tw%d' % (4 * par + i) for i in range(4)]
                bkey = '%sq%d' % (bkey, c0 // 4)
                kb.op('dve', lambda e: e.tensor_tensor(out=t[0], in0=Are, in1=T2r, op=ALU.mult), r=[psk, 'hconst', tk[0]], w=[tk[0]])
                kb.op('dve', lambda e: e.tensor_tensor(out=t[1], in0=Aim, in1=T2i, op=ALU.mult), r=[psk, 'hconst', tk[1]], w=[tk[1]])
                kb.op('dve', lambda e: e.tensor_tensor(out=t[2], in0=Are, in1=T2i, op=ALU.mult), r=[psk, 'hconst', tk[2]], w=[tk[2]])
                kb.op('dve', lambda e: e.tensor_tensor(out=t[3], in0=Aim, in1=T2r, op=ALU.mult), r=[psk, 'hconst', tk[3]], w=[tk[3]])
                if not conj:
                    kb.op('pool', lambda e: e.tensor_tensor(out=Bt[:, c0:c0 + 2, 0, :], in0=t[0], in1=t[1], op=ALU.subtract), r=[tk[0], tk[1], bkey], w=[bkey])
                    kb.op('pool', lambda e: e.tensor_tensor(out=Bt[:, c0:c0 + 2, 1, :], in0=t[2], in1=t[3], op=ALU.add), r=[tk[2], tk[3], bkey], w=[bkey])
                else:
                    kb.op('pool', lambda e: e.tensor_tensor(out=Bt[:, c0:c0 + 2, 0, :], in0=t[0], in1=t[1], op=ALU.add), r=[tk[0], tk[1], bkey], w=[bkey])
                    kb.op('pool', lambda e: e.tensor_tensor(out=Bt[:, c0:c0 + 2, 1, :], in0=t[3], in1=t[2], op=ALU.subtract), r=[tk[2], tk[3], bkey], w=[bkey])

            def stage2(Bt, bkey, c4):
                pr, prk = hb(); pi_, pik = hb()
                bkey = '%sq%d' % (bkey, c4 // 4)
                Bre, Bim = Bt[:, c4:c4 + 4, 0, :], Bt[:, c4:c4 + 4, 1, :]
                kb.op('pe', lambda e: e.matmul(pr[:, :], lhsT=cst["FRE"][:], rhs=Bre, start=True, stop=False), r=['hconst', bkey], w=[prk])
                kb.op('pe', lambda e: e.matmul(pr[:, :], lhsT=cst["NFIM"][:], rhs=Bim, start=False, stop=True), r=['hconst', bkey], w=[prk])
                kb.op('pe', lambda e: e.matmul(pi_[:, :], lhsT=cst["FIM"][:], rhs=Bre, start=True, stop=False), r=['hconst', bkey], w=[pik])
                kb.op('pe', lambda e: e.matmul(pi_[:, :], lhsT=cst["FRE"][:], rhs=Bim, start=False, stop=True), r=['hconst', bkey], w=[pik])
                return pr, prk, pi_, pik

            B16 = sbt(st, "B16", [128, 16, 2, 128])
            with stage() as sf:
                A3Z = sbt(sf, "A3Z", [128, 2 * L])
                kb.op('pool', lambda e: e.memset(A3Z[0:64, L:2 * L], 0.0), w=['A3Z'])
                kb.op('pool', lambda e: e.memset(A3Z[64:128, 0:L], 0.0), r=['A3Z'], w=['A3Z'])
                w4sb = sbt(sf, "w4sb", [128, 2048])
                kb.dma('sp', lambda e: e.dma_start(out=w4sb[0:64, :], in_=FW4), w=['w4sb'])
                kb.dma('sp', lambda e: e.dma_start(out=w4sb[64:128, :], in_=FW4), r=['w4sb'], w=['w4sb'])
                with stage() as sm:
                    wl = [sbt(sm, "w1bd", [66, 128]), sbt(sm, "w2bd", [128, 128]), sbt(sm, "w3bd", [128, 128])]
                    for i_, (wt, src, kin) in enumerate(zip(wl, (FW1, FW2, FW3), (33, 64, 64))):
                        kb.op('pool', lambda e: e.memset(wt[:], 0.0), w=['wbd%d' % i_])
                        kb.dma('sp', lambda e: e.dma_start(out=wt[0:kin, 0:64], in_=src), r=['wbd%d' % i_], w=['wbd%d' % i_])
                        kb.dma('sp', lambda e: e.dma_start(out=wt[kin:2 * kin, 64:128], in_=src), r=['wbd%d' % i_], w=['wbd%d' % i_])
                    fqb = sbt(sm, "fqb", [128, 4])
                    with nc.allow_non_contiguous_dma(reason="64-element vectors onto partitions"):
                        for j_, src in enumerate((FFQ, FB1, FB2, FB3)):
                            for hh in range(2):
                                kb.dma('sp', lambda e: e.dma_start(out=fqb[64 * hh:64 * hh + 64, j_:j_ + 1], in_=src.rearrange("(p o) -> p o", o=1)), r=['fqb'], w=['fqb'])
                    kb.op('dve', lambda e: e.tensor_scalar(out=fqb[:, 0:1], in0=fqb[:, 0:1], scalar1=1.0 / TWO_PI, scalar2=None, op0=ALU.mult), r=['fqb'], w=['fqb'])
                    kb.op('dve', lambda e: e.tensor_scalar(out=fqb[:, 1:4], in0=fqb[:, 1:4], scalar1=fqb[:, 0:1], scalar2=None, op0=ALU.mult), r=['fqb'], w=['fqb'])
                    zc = [sbt(sm, "zc%d" % i, [66, 2048]) for i in range(2)]
                    hid = [sbt(sm, "hid%d" % i, [128, 2048]) for i in range(2)]
                    uu = sbt(sm, "uu", [128, 2048]); ui = sbt(sm, "ui", [128, 2048], I32); uf = sbt(sm, "uf", [128, 2048])
                    pi_ = 0
                    for ch in range(4):
                        zk = 'zc%d' % (ch % 2)
                        kb.dma('sp', lambda e: e.dma_start(out=zc[ch % 2][:], in_=ZTd[:, ch * 2048:(ch + 1) * 2048]), w=[zk])
                        src_t, kdim, srck = zc[ch % 2], 66, zk
                        for l_ in range(3):
                            Pq, pkeys = (PA, ["PA0", "PA1", "PA2", "PA3"]) if pi_ % 2 == 0 else (PB, ["PB0", "PB1", "PB2", "PB3"])
                            pi_ += 1
                            for q in range(4):
                                kb.op('pe', lambda e: e.matmul(Pq[:, q * 512:(q + 1) * 512], lhsT=wl[l_][0:kdim, :], rhs=src_t[0:kdim, q * 512:(q + 1) * 512], start=True, stop=True),
                                      r=['wbd%d' % l_, srck], w=[pkeys[q]])
                            kb.op('act', lambda e: e.activation(out=uu[:], in_=Pq[:, :], func=AF.Identity, bias=fqb[:, 1 + l_:2 + l_], scale=fqb[:, 0:1]), r=pkeys + ['fqb'], w=['uu'])
                            kb.op('dve', lambda e: e.tensor_copy(out=ui[:], in_=uu[:]), r=['uu'], w=['ui'])
                            kb.op('dve', lambda e: e.tensor_copy(out=uf[:], in_=ui[:]), r=['ui'], w=['uf'])
                            kb.op('pool', lambda e: e.tensor_tensor(out=uu[:], in0=uu[:], in1=uf[:], op=ALU.subtract), r=['uu', 'uf'], w=['uu'])
                            kb.op('dve', lambda e: e.scalar_tensor_tensor(out=uf[:], in0=uu[:], scalar=0.5, in1=uu[:], op0=ALU.is_gt, op1=ALU.subtract), r=['uu', 'uf'], w=['uf'])
                            kb.op('dve', lambda e: e.scalar_tensor_tensor(out=uu[:], in0=uf[:], scalar=0.5, in1=uf[:], op0=ALU.is_gt, op1=ALU.subtract), r=['uu', 'uf'], w=['uu'])
                            if l_ < 2:
                                hk = 'hid%d' % l_
                                kb.op('act', lambda e: e.activation(out=hid[l_][:], in_=uu[:], func=AF.Sin, scale=6.283185), r=['uu', hk], w=[hk])
                                src_t, kdim, srck = hid[l_], 128, hk
                            else:
                                kb.op('act', lambda e: e.activation(out=A3Z[0:64, ch * 2048:(ch + 1) * 2048], in_=uu[0:64, :], func=AF.Sin, scale=6.283185), r=['uu', 'A3Z'], w=['A3Z'])
                                kb.op('act', lambda e: e.activation(out=A3Z[64:128, L + ch * 2048:L + (ch + 1) * 2048], in_=uu[64:128, :], func=AF.Sin, scale=6.283185), r=['uu', 'A3Z'], w=['A3Z'])
                Kt = sbt(sf, "Kt", [128, 64, 128])
                dec = sbt(sf, "dec", [128, 64, 128]); rab = sbt(sf, "rab", [128, 64]); rn = sbt(sf, "rn", [128, 64]); Hst = sbt(sf, "Hst", [128, 16, 2, 128])
                w4c = sbt(sf, "w4c", [128, 64])
                DEC2 = DECd.rearrange("h n c f -> (h n) c f")
                for o in range(2):
                    for cg in range(8):
                        kb.op('pool', lambda e: e.tensor_copy(out=w4c[0:64, :], in_=w4sb[0:64, o * 1024 + cg * 64:o * 1024 + cg * 64 + 64]), r=['w4sb', 'w4c'], w=['w4c'])
                        kb.op('pool', lambda e: e.tensor_copy(out=w4c[64:128, :], in_=w4sb[64:128, o * 1024 + 512 + cg * 64:o * 1024 + 512 + cg * 64 + 64]), r=['w4sb', 'w4c'], w=['w4c'])
                        kb.dma('sp', lambda e: e.dma_start(out=dec[:], in_=DEC2[:, cg * 64:(cg + 1) * 64, :]), r=['dec'], w=['dec'])
                        for nbk in range(16):
                            ps, psk = hb()
                            for j_ in range(8):
                                n2 = nbk * 8 + j_
                                kb.op('pe', lambda e: e.matmul(ps[:, j_ * 64:(j_ + 1) * 64], lhsT=A3Z[:, n2:2 * L:128], rhs=w4c[:], start=True, stop=True), r=['A3Z', 'w4c'], w=[psk])
                            evac(Kt[:, :, nbk * 8:(nbk + 1) * 8], ps[:, :].rearrange("p (n c) -> p c n", c=64), None, [psk, 'Kt'], ['Kt'])
                        kb.op('pool', lambda e: e.tensor_tensor(out=Kt[:], in0=Kt[:], in1=dec[:], op=ALU.mult), r=['Kt', 'dec'], w=['Kt'])
                        kb.op('dve', lambda e: e.tensor_reduce(out=rab[:], in_=Kt[:], axis=AX.X, op=ALU.add, apply_absolute_value=True), r=['Kt', 'rab'], w=['rab'])
                        ps, psk = hb()
                        kb.op('pe', lambda e: e.matmul(ps[:, 0:64], lhsT=cst["ONESH"][:], rhs=rab[:], start=True, stop=True), r=['hconst', 'rab'], w=[psk])
                        kb.op('dve', lambda e: e.reciprocal(out=rn[:], in_=ps[:, 0:64]), r=[psk], w=['rn'])
                        for sb4 in range(4):
                            for c2 in range(8):
                                ps, psk = hb()
                                for j_ in range(2):
                                    cc = sb4 * 16 + c2 * 2 + j_
                                    kb.op('pe', lambda e: e.matmul(ps[:, j_ * 256:(j_ + 1) * 256], lhsT=Kt[:, cc, :], rhs=cst["FA"][:], start=True, stop=True), r=['Kt', 'hconst'], w=[psk])
                                twiddle(ps, psk, B16, 'B16', c2 * 2, False)
                            for c4 in range(0, 16, 4):
                                pr, prk, pi_, pik = stage2(B16, 'B16', c4)
                                for j_ in range(4):
                                    cc = sb4 * 16 + c4 + j_; gc = cg * 64 + cc
                                    kb.op('dve', lambda e: e.tensor_scalar(out=Hst[:, c4 + j_, 0, :], in0=pr[:, j_ * 128:(j_ + 1) * 128], scalar1=rn[:, cc:cc + 1], scalar2=skb[:, o * 512 + gc:o * 512 + gc + 1],
                                                                           op0=ALU.mult, op1=ALU.add), r=[prk, 'rn', 'skb', 'Hst'], w=['Hst'])
                                    kb.op('act', lambda e: e.activation(out=Hst[:, c4 + j_, 1, :], in_=pi_[:, j_ * 128:(j_ + 1) * 128], func=AF.Copy, scale=rn[:, cc:cc + 1]), r=[pik, 'rn', 'Hst'], w=['Hst'])
                            g0 = o * 512 + cg * 64 + sb4 * 16
                            kb.dma('sp', lambda e: e.dma_start(out=HSPEC[g0:g0 + 16].rearrange("c k r f -> k c r f"), in_=Hst[:]), r=['Hst'], w=['HSPEC'])
            with stage() as sc:
                v16 = sbt(sc, "v16", [64, 16, 128]); x116 = sbt(sc, "x116", [64, 16, 128]); x216 = sbt(sc, "x216", [64, 16, 128]); z16 = sbt(sc, "z16", [64, 16, 128])
                y16 = sbt(sc, "y16", [64, 16, 128])
                H1 = sbt(sc, "H1", [128, 16, 2, 128]); H2s = sbt(sc, "H2s", [128, 16, 2, 128]); Y16 = sbt(sc, "Y16", [128, 16, 2, 128]); G16 = sbt(sc, "G16", [128, 16, 2, 128])
                hm = [sbt(sc, "hm%d" % i, [128, 4, 128]) for i in range(8)]

                def conv16(Din, dkey, Hs, hkey, Xmul, xkey, Out, okey):
                    for c2 in range(8):
                        ps, psk = hb()
                        for j_ in range(2):
                            kb.op('pe', lambda e: e.matmul(ps[:, j_ * 256:(j_ + 1) * 256], lhsT=Din[:, c2 * 2 + j_, :], rhs=cst["FA"][0:64, :], start=True, stop=True), r=[dkey, 'hconst'], w=[psk])
                        twiddle(ps, psk, B16, 'B16', c2 * 2, False)
                    for c4 in range(0, 16, 4):
                        pr, prk, pi_, pik = stage2(B16, 'B16', c4)
                        Xr = pr[:, :].rearrange("p (c f) -> p c f", c=4); Xi = pi_[:, :].rearrange("p (c f) -> p c f", c=4)
                        Hr, Hi = Hs[:, c4:c4 + 4, 0, :], Hs[:, c4:c4 + 4, 1, :]
                        hp = 4 * ((c4 // 4) % 2)
                        hmk = ['hm%d' % (hp + i) for i in range(4)]
                        yk_ = 'Y16q%d' % (c4 // 4)
                        kb.op('dve', lambda e: e.tensor_tensor(out=hm[hp + 0][:], in0=Xr, in1=Hr, op=ALU.mult), r=[prk, hkey, hmk[0]], w=[hmk[0]])
                        kb.op('dve', lambda e: e.tensor_tensor(out=hm[hp + 1][:], in0=Xi, in1=Hi, op=ALU.mult), r=[pik, hkey, hmk[1]], w=[hmk[1]])
                        kb.op('dve', lambda e: e.tensor_tensor(out=hm[hp + 2][:], in0=Xr, in1=Hi, op=ALU.mult), r=[prk, hkey, hmk[2]], w=[hmk[2]])
                        kb.op('dve', lambda e: e.tensor_tensor(out=hm[hp + 3][:], in0=Xi, in1=Hr, op=ALU.mult), r=[pik, hkey, hmk[3]], w=[hmk[3]])
                        kb.op('pool', lambda e: e.tensor_tensor(out=Y16[:, c4:c4 + 4, 0, :], in0=hm[hp + 0][:], in1=hm[hp + 1][:], op=ALU.subtract), r=[hmk[0], hmk[1], yk_], w=[yk_])
                        kb.op('pool', lambda e: e.tensor_tensor(out=Y16[:, c4:c4 + 4, 1, :], in0=hm[hp + 2][:], in1=hm[hp + 3][:], op=ALU.add), r=[hmk[2], hmk[3], yk_], w=[yk_])
                    for c2 in range(8):
                        ps, psk = hb()
                        for j_ in range(2):
                            cc = c2 * 2 + j_
                            kb.op('pe', lambda e: e.matmul(ps[:, j_ * 256:(j_ + 1) * 256], lhsT=Y16[:, cc, 0, :], rhs=cst["CA"][:], start=True, stop=False), r=['Y16q%d' % (cc // 4), 'hconst'], w=[psk])
                            kb.op('pe', lambda e: e.matmul(ps[:, j_ * 256:(j_ + 1) * 256], lhsT=Y16[:, cc, 1, :], rhs=cst["CB"][:], start=False, stop=True), r=['Y16q%d' % (cc // 4), 'hconst'], w=[psk])
                        twiddle(ps, psk, G16, 'G16', c2 * 2, True)
                    for c4 in range(0, 16, 4):
                        py, pyk = hb()
                        kb.op('pe', lambda e: e.matmul(py[0:64, :], lhsT=cst["FREN"][:], rhs=G16[:, c4:c4 + 4, 0, :], start=True, stop=False), r=['hconst', 'G16q%d' % (c4 // 4)], w=[pyk])
                        kb.op('pe', lambda e: e.matmul(py[0:64, :], lhsT=cst["FIMN"][:], rhs=G16[:, c4:c4 + 4, 1, :], start=False, stop=True), r=['hconst', 'G16q%d' % (c4 // 4)], w=[pyk])
                        kb.op('dve', lambda e: e.tensor_tensor(out=Out[:, c4:c4 + 4, :], in0=py[0:64, :].rearrange("p (c f) -> p c f", c=4), in1=Xmul[:, c4:c4 + 4, :], op=ALU.mult),
                              r=[pyk, xkey, okey], w=[okey])

                for g in range(32):
                    gc0 = g * 16
                    lh = lambda r0: HYC[r0 + gc0:r0 + gc0 + 16, :].rearrange("c (n1 n2) -> n1 c n2", n2=128)
                    kb.dma('sp', lambda e: e.dma_start(out=v16[:], in_=lh(0)), r=['HYC'], w=['v16'])
                    kb.dma('sp', lambda e: e.dma_start(out=x116[:], in_=lh(512)), r=['HYC'], w=['x116'])
                    kb.dma('sp', lambda e: e.dma_start(out=x216[:], in_=lh(1024)), r=['HYC'], w=['x216'])
                    kb.dma('sp', lambda e: e.dma_start(out=H1[:], in_=HSPEC[gc0:gc0 + 16].rearrange("c k r f -> k c r f")), r=['HSPEC'], w=['H1'])
                    kb.dma('sp', lambda e: e.dma_start(out=H2s[:], in_=HSPEC[512 + gc0:512 + gc0 + 16].rearrange("c k r f -> k c r f")), r=['HSPEC'], w=['H2s'])
                    conv16(v16, 'v16', H1, 'H1', x116, 'x116', z16, 'z16')
                    conv16(z16, 'z16', H2s, 'H2s', x216, 'x216', y16, 'y16')
                    kb.dma('pool', lambda e: e.dma_start(out=YHYT[gc0:gc0 + 16, :].rearrange("c (n1 n2) -> n1 c n2", n2=128), in_=y16[:]), r=['y16'], w=['YHYT'])

        if stages >= 5:
            with stage() as st:
                z = sbt(st, "zt", [128, L])
                kb.op('pool', lambda e: e.memset(z[:], 0.0), w=['zt'])
                for q in range(4):
                    if dummy_mix:
                        kb.dma('sp', lambda e: e.dma_start(out=z[:], in_=QT[q * 128:(q + 1) * 128, :]), r=['zt', 'QT'], w=['zt'])
                    if dummy_mix:
                        kb.dma('sp', lambda e: e.dma_start(out=YHYT[q * 128:(q + 1) * 128, :], in_=z[:]), r=['zt'], w=['YHYT'])
                    if dummy_mix:
                        kb.dma('sp', lambda e: e.dma_start(out=z[:], in_=GT[q * 128:(q + 1) * 128, :]), r=['zt', 'GT'], w=['zt'])
                    if dummy_mix:
                        kb.dma('sp', lambda e: e.dma_start(out=YHGT[q * 128:(q + 1) * 128, :], in_=z[:]), r=['zt'], w=['YHGT'])
                zr = sbt(st, "zr", [128, D])
                kb.op('pool', lambda e: e.memset(zr[:], 0.0), w=['zr'])
                if dummy_mix or stages < 7:
                    for i in range(OWN // 128):
                        kb.dma('sp', lambda e: e.dma_start(out=ROUTED[i * 128:(i + 1) * 128, :], in_=zr[:]), r=['zr'], w=['ROUTED'])

        def row_bcast(stack, name, src_row_ap):
            t = sbt(stack, name, [128, D])
            kb.dma('sp', lambda e: e.dma_start(out=t[:], in_=src_row_ap.to_broadcast([128, D])), r=['MODROW'], w=[name])
            return t

        if stages >= 5:
            with stage() as st:
                oidx = sbt(st, "oidx", [128, 4], I32)
                kb.dma('sp', lambda e: e.dma_start(out=oidx[:], in_=OWNIDX), w=['oidx'])
                yg = [sbt(st, "yg%d" % i, [128, OWN]) for i in range(2)]
                n = 0
                for src, skey, r0 in ((YHYT, 'YHYT', 0), (YHGT, 'YHGT', 512)):
                    v = src.rearrange("c (j t) -> (c j) t", j=4)
                    for cc in range(4):
                        g = yg[n % 2]; gk = "yg%d" % (n % 2); n += 1
                        kb.dma('pool', lambda e: e.indirect_dma_start(out=g[:], out_offset=None, in_=v,
                                                                     in_offset=bass.IndirectOffsetOnAxis(ap=oidx[:, cc:cc + 1], axis=0),
                                                                     bounds_check=RB_OWN, oob_is_err=False), r=[skey, 'oidx'], w=[gk])
                        kb.dma('sp', lambda e: e.dma_start(out=YOWN[r0 + cc * 128:r0 + (cc + 1) * 128, :], in_=g[:]), r=[gk], w=['YOWN'])
            with stage() as st:
                Wy = sbt(st, "Wy", [128, 8, D]); Wo = sbt(st, "Wo", [128, 8, D])
                kb.dma('sp', lambda e: e.dma_start(out=Wy[:, 0:4, :], in_=WHY.rearrange("(k p) c -> p k c", p=128)), w=['Wy'])
                kb.dma('sp', lambda e: e.dma_start(out=Wy[:, 4:8, :], in_=WHG.rearrange("(k p) c -> p k c", p=128)), r=['Wy'], w=['Wy'])
                kb.dma('sp', lambda e: e.dma_start(out=Wo[:], in_=WOUT.rearrange("(k p) c -> p k c", p=128)), w=['Wo'])
                g1row = row_bcast(st, "g1row", MODROW[0:1, 2048:3072])
                yb = sbt(st, "yb", [128, 8, 512]); sgb = sbt(st, "sgb", [128, 16, 512]); mT = sbt(st, "mT", [128, 8, 512])
                t1 = [sbt(st, "t1_%d" % i, [128, 512]) for i in range(2)]
                xt = [sbt(st, "xt%d" % i, [128, D]) for i in range(2)]; pt = [sbt(st, "pt%d" % i, [128, D]) for i in range(2)]
                bi = 0
                for blk in range(OWN // 512):
                    t0 = blk * 512
                    kb.dma('sp', lambda e: e.dma_start(out=yb[:], in_=YOWN[:, t0:t0 + 512].rearrange("(k p) t -> p k t", p=128)), r=['YOWN'], w=['yb'])
                    kb.dma('sp', lambda e: e.dma_start(out=sgb[:], in_=SGT[:, t0:t0 + 512].rearrange("(k p) t -> p k t", p=128)), r=['SGT'], w=['sgb'])
                    for dm in range(8):
                        for br in range(2):
                            pb, pbk = banks[bi % 8]; bi += 1
                            for cc in range(4):
                                kb.op('pe', lambda e: e.matmul(pb[:, :], lhsT=Wy[:, br * 4 + cc, dm * 128:(dm + 1) * 128], rhs=yb[:, br * 4 + cc, :],
                                                               start=(cc == 0), stop=(cc == 3)), r=['Wy', 'yb'], w=[pbk])
                            if br == 0:
                                kb.op('dve', lambda e: e.tensor_tensor(out=t1[dm % 2][:], in0=pb[:, :], in1=sgb[:, dm, :], op=ALU.mult),
                                      r=[pbk, 'sgb'], w=['t1_%d' % (dm % 2)])
                            else:
                                kb.op('dve', lambda e: e.tensor_tensor(out=mT[:, dm, :], in0=pb[:, :], in1=sgb[:, 8 + dm, :], op=ALU.mult),
                                      r=[pbk, 'sgb', 'mT'], w=['mT'])
                                kb.op('pool', lambda e: e.tensor_tensor(out=mT[:, dm, :], in0=mT[:, dm, :], in1=t1[dm % 2][:], op=ALU.add),
                                      r=['mT', 't1_%d' % (dm % 2)], w=['mT'])
                    for tt in range(4):
                        ti = blk * 4 + tt; b2 = ti % 2
                        kb.dma('sp', lambda e: e.dma_start(out=xt[b2][:], in_=XOWN[ti * 128:(ti + 1) * 128, :]), w=['xt%d' % b2])
                        kb.dma('sp', lambda e: e.dma_start(out=pt[b2][:], in_=POSOWN[ti * 128:(ti + 1) * 128, :]), w=['pt%d' % b2])
                        kb.op('pool', lambda e: e.tensor_tensor(out=xt[b2][:], in0=xt[b2][:], in1=pt[b2][:], op=ALU.add), r=['xt%d' % b2, 'pt%d' % b2], w=['xt%d' % b2])
                        for hf in range(2):
                            pb, pbk = banks[bi % 8]; bi += 1
                            for kk in range(8):
                                kb.op('pe', lambda e: e.matmul(pb[:, :], lhsT=mT[:, kk, tt * 128:(tt + 1) * 128], rhs=Wo[:, kk, hf * 512:(hf + 1) * 512],
                                                               start=(kk == 0), stop=(kk == 7)), r=['mT', 'Wo'], w=[pbk])
                            kb.op('dve', lambda e: e.tensor_tensor(out=pt[b2][:, hf * 512:(hf + 1) * 512], in0=pb[:, :], in1=g1row[:, hf * 512:(hf + 1) * 512], op=ALU.mult),
                                  r=[pbk, 'g1row', 'pt%d' % b2], w=['pt%d' % b2])
                        kb.op('pool', lambda e: e.tensor_tensor(out=xt[b2][:], in0=xt[b2][:], in1=pt[b2][:], op=ALU.add), r=['xt%d' % b2, 'pt%d' % b2], w=['xt%d' % b2])
                        kb.dma('pool', lambda e: e.dma_start(out=X1D[ti * 128:(ti + 1) * 128, :], in_=xt[b2][:]), r=['xt%d' % b2], w=['X1D'])

        if stages >= 6:
            a2T = sbt(es, "a2T", [128, 8]); sh2T = sbt(es, "sh2T", [128, 8])
            kb.op('dve', lambda e: e.tensor_scalar(out=a2T[:], in0=modT[:, 32:40, 0], scalar1=1.0, scalar2=None, op0=ALU.add), r=['modT'], w=['a2T'])
            kb.op('dve', lambda e: e.tensor_tensor(out=a2T[:], in0=a2T[:], in1=g2T[:], op=ALU.mult), r=['a2T', 'g2T'], w=['a2T'])
            kb.op('dve', lambda e: e.tensor_copy(out=sh2T[:], in_=modT[:, 24:32, 0]), r=['modT'], w=['sh2T'])
            with stage() as s1:
                srcs = [(X1D[i * 128:(i + 1) * 128, :], None, i * 128) for i in range(OWN // 128)]
                kb.res.setdefault('a1T', [None, []])
                norm_to_xt(s1, srcs, H2T, a2T, sh2T, "n2", 'H2T')
            own_blocks = [(t, 512) for t in range(0, OWN, 512)]
            gemm_group("sg", SHG, 8, 256, H2T, 'H2T', own_blocks, [dict(c0=0, cn=256, mode='fm', func=AF.Silu, out=SGA, okey='SGA', oc0=0)])
            gemm_group("su", SHU, 8, 256, H2T, 'H2T', own_blocks, [dict(c0=0, cn=256, mode='fm', func=None, out=SUA, okey='SUA', oc0=0)])
            with stage() as st:
                ga = sbt(st, "ga", [128, 2, OWN]); ua = sbt(st, "ua", [128, 2, OWN])
                kb.dma('sp', lambda e: e.dma_start(out=ga[:], in_=SGA.rearrange("(k p) t -> p k t", p=128)), r=['SGA'], w=['ga'])
                kb.dma('sp', lambda e: e.dma_start(out=ua[:], in_=SUA.rearrange("(k p) t -> p k t", p=128)), r=['SUA'], w=['ua'])
                kb.op('dve', lambda e: e.tensor_tensor(out=ga[:], in0=ga[:], in1=ua[:], op=ALU.mult), r=['ga', 'ua'], w=['ga'])
                kb.dma('pool', lambda e: e.dma_start(out=ACTT.rearrange("(k p) t -> p k t", p=128), in_=ga[:]), r=['ga'], w=['ACTT'])
            gemm_group("sd", SHD, 2, 1024, ACTT, 'ACTT', own_blocks,
                       [dict(c0=0, cn=512, mode='tm', func=None, out=SHOUT, okey='SHOUT', oc0=0),
                        dict(c0=512, cn=512, mode='tm', func=None, out=SHOUT, okey='SHOUT', oc0=512)])
            if stages >= 7 and not dummy_mix:
                with stage() as st:
                    a2row = sbt(st, "a2row", [128, D]); g2nrow = sbt(st, "g2nrow", [128, D])
                    kb.dma('sp', lambda e: e.dma_start(out=a2row[:], in_=MODROW[0:1, 4096:5120].to_broadcast([128, D])), r=['MODROW'], w=['a2row'])
                    kb.dma('sp', lambda e: e.dma_start(out=g2nrow[:], in_=N2G.rearrange("(o n) -> o n", o=1).to_broadcast([128, D])), w=['g2nrow'])
                    kb.op('dve', lambda e: e.scalar_tensor_tensor(out=a2row[:], in0=a2row[:], scalar=1.0, in1=g2nrow[:], op0=ALU.add, op1=ALU.mult),
                          r=['a2row', 'g2nrow'], w=['a2row'])
                    sh2row = row_bcast(st, "sh2row", MODROW[0:1, 3072:4096])
                    xa = [sbt(st, "hxa%d" % i, [128, D]) for i in range(2)]; stt = [sbt(st, "hst%d" % i, [128, 4]) for i in range(2)]
                    junk = sbt(st, "hjunk", [128, D])
                    for ti in range(OWN // 128):
                        b2 = ti % 2; xk, tk = 'hxa%d' % b2, 'hst%d' % b2
                        rows = slice(ti * 128, (ti + 1) * 128)
                        kb.dma('sp', lambda e: e.dma_start(out=xa[b2][:], in_=X1D[rows, :]), r=['X1D'], w=[xk])
                        kb.op('act', lambda e: e.activation(out=junk[:], in_=xa[b2][:], func=AF.Square, accum_out=stt[b2][:, 0:1]), r=[xk], w=['hjunk', tk])
                        kb.op('dve', lambda e: e.tensor_scalar(out=stt[b2][:, 1:2], in0=stt[b2][:, 0:1], scalar1=1.0 / D, scalar2=EPS, op0=ALU.mult, op1=ALU.add), r=[tk], w=[tk])
                        kb.op('act', lambda e: e.activation(out=stt[b2][:, 2:3], in_=stt[b2][:, 1:2], func=AF.Sqrt), r=[tk], w=[tk])
                        kb.op('dve', lambda e: e.reciprocal(out=stt[b2][:, 3:4], in_=stt[b2][:, 2:3]), r=[tk], w=[tk])
                        kb.op('dve', lambda e: e.scalar_tensor_tensor(out=xa[b2][:], in0=xa[b2][:], scalar=stt[b2][:, 3:4], in1=a2row[:], op0=ALU.mult, op1=ALU.mult),
                              r=[xk, tk, 'a2row'], w=[xk])
                        kb.op('pool', lambda e: e.tensor_tensor(out=xa[b2][:], in0=xa[b2][:], in1=sh2row[:], op=ALU.add), r=[xk, 'sh2row'], w=[xk])
                        kb.dma('pool', lambda e: e.dma_start(out=H2[rows, :], in_=xa[b2][:]), r=[xk], w=['H2'])
                gemm_group("rt", RW, 8, NE, H2T, 'H2T', own_blocks, [dict(c0=0, cn=NE, mode='tm', func=AF.Sigmoid, out=SCORES, okey='SCORES', oc0=0)])
                with stage() as st:
                    NT = OWN // 128
                    onesm = sbt(st, "onesm", [128, 128]); stri = sbt(st, "stri", [128, 128]); slt = sbt(st, "slt", [128, 512])
                    blk128 = sbt(st, "blk128", [128, NBLK]); pidx = sbt(st, "pidx", [128, 1]); brow_ = sbt(st, "rbrow", [128, NE])
                    kb.dma('sp', lambda e: e.dma_start(out=onesm[:], in_=ONESd), w=['onesm'])
                    kb.dma('sp', lambda e: e.dma_start(out=stri[:], in_=STRId), w=['stri'])
                    kb.dma('sp', lambda e: e.dma_start(out=slt[:], in_=SLTd), w=['slt'])
                    kb.dma('sp', lambda e: e.dma_start(out=blk128[:], in_=BLKd), w=['blk128'])
                    kb.dma('sp', lambda e: e.dma_start(out=pidx[:], in_=PIDXd), w=['pidx'])
                    kb.dma('sp', lambda e: e.dma_start(out=brow_[:], in_=RB.rearrange("(o n) -> o n", o=1).to_broadcast([128, NE])), w=['rbrow'])
                    D8F = sbt(st, "D8F", [128, NT, 8]); W8 = sbt(st, "W8", [128, NT, 8]); D8I = sbt(st, "D8I", [128, NT * 8], I32); GI = sbt(st, "GI", [128, NBLK], I32)
                    with stage() as sr:
                        MSK = sbt(sr, "MSK", [128, NT, NE]); SEL = sbt(sr, "SEL", [128, NT, NE]); WD = sbt(sr, "WDm", [128, NT, NE]); DST = sbt(sr, "DST", [128, NT, NE])
                        V8 = sbt(sr, "V8", [128, NT, 8])
                        sc_ = [sbt(sr, "rsc%d" % i, [128, NE]) for i in range(2)]; bs = sbt(sr, "rbs", [128, NE])
                        M8 = sbt(sr, "M8", [128, 8, 8]); gs = sbt(sr, "rgs", [128, 8]); g8 = sbt(sr, "rg8", [128, 8]); gm = sbt(sr, "rgm", [128, 8]); pen = sbt(sr, "rpen", [128, 8])
                        den = sbt(sr, "rden", [128, 2]); base = sbt(sr, "rbase", [128, NE]); tmpq = sbt(sr, "rtmpq", [128, NE])
                        kb.op('pool', lambda e: e.memset(base[:], 0.0), w=['rbase'])
                        for ti in range(NT):
                            b2 = ti % 2; sk_ = 'rsc%d' % b2
                            kb.dma('sp', lambda e: e.dma_start(out=sc_[b2][:], in_=SCORES[ti * 128:(ti + 1) * 128, :]), r=['SCORES'], w=[sk_])
                            kb.op('dve', lambda e: e.tensor_tensor(out=bs[:], in0=sc_[b2][:], in1=brow_[:], op=ALU.add), r=[sk_, 'rbrow'], w=['rbs'])
                            for g in range(8):
                                kb.op('dve', lambda e: e.max(out=M8[:, g, :], in_=bs[:, 32 * g:32 * g + 32]), r=['rbs', 'M8'], w=['M8'])
                            kb.op('dve', lambda e: e.tensor_tensor(out=gs[:], in0=M8[:, :, 0], in1=M8[:, :, 1], op=ALU.add), r=['M8'], w=['rgs'])
                            kb.op('dve', lambda e: e.max(out=g8[:], in_=gs[:]), r=['rgs'], w=['rg8'])
                            kb.op('dve', lambda e: e.tensor_scalar(out=gm[:], in0=gs[:], scalar1=g8[:, 3:4], scalar2=None, op0=ALU.is_ge), r=['rgs', 'rg8'], w=['rgm'])
                            kb.op('dve', lambda e: e.tensor_scalar(out=pen[:], in0=gm[:], scalar1=-1.0, scalar2=1e30, op0=ALU.add, op1=ALU.mult), r=['rgm'], w=['rpen'])
                            for g in range(8):
                                kb.op('dve', lambda e: e.tensor_scalar(out=MSK[:, ti, 32 * g:32 * g + 32], in0=bs[:, 32 * g:32 * g + 32], scalar1=gm[:, g:g + 1], scalar2=pen[:, g:g + 1],
                                                                       op0=ALU.mult, op1=ALU.add), r=['rbs', 'rgm', 'rpen', 'MSK'], w=['MSK'])
                            kb.op('dve', lambda e: e.max(out=V8[:, ti, :], in_=MSK[:, ti, :]), r=['MSK', 'V8'], w=['V8'])
                            kb.op('dve', lambda e: e.tensor_scalar(out=SEL[:, ti, :], in0=MSK[:, ti, :], scalar1=V8[:, ti, 7:8], scalar2=None, op0=ALU.is_ge), r=['MSK', 'V8', 'SEL'], w=['SEL'])
                            kb.op('dve', lambda e: e.tensor_tensor(out=WD[:, ti, :], in0=SEL[:, ti, :], in1=sc_[b2][:], op=ALU.mult), r=['SEL', sk_, 'WDm'], w=['WDm'])
                            kb.op('dve', lambda e: e.tensor_reduce(out=den[:, 0:1], in_=WD[:, ti, :], axis=AX.X, op=ALU.add), r=['WDm', 'rden'], w=['rden'])
                            kb.op('dve', lambda e: e.reciprocal(out=den[:, 1:2], in_=den[:, 0:1]), r=['rden'], w=['rden'])
                            kb.op('dve', lambda e: e.tensor_scalar(out=WD[:, ti, :], in0=WD[:, ti, :], scalar1=den[:, 1:2], scalar2=2.5, op0=ALU.mult, op1=ALU.mult), r=['WDm', 'rden'], w=['WDm'])
                            p1, p1k = banks[(2 * ti) % 8]; p2, p2k = banks[(2 * ti + 1) % 8]
                            kb.op('pe', lambda e: e.matmul(p1[:, 0:NE], lhsT=stri[:], rhs=SEL[:, ti, :], start=True, stop=True), r=['stri', 'SEL'], w=[p1k])
                            kb.op('pe', lambda e: e.matmul(p2[:, 0:NE], lhsT=onesm[:], rhs=SEL[:, ti, :], start=True, stop=True), r=['onesm', 'SEL'], w=[p2k])
                            kb.op('dve', lambda e: e.tensor_tensor(out=DST[:, ti, :], in0=p1[:, 0:NE], in1=base[:], op=ALU.add), r=[p1k, 'rbase', 'DST'], w=['DST'])
                            kb.op('dve', lambda e: e.tensor_tensor(out=base[:], in0=p2[:, 0:NE], in1=base[:], op=ALU.add), r=[p2k, 'rbase'], w=['rbase'])
                        ci = sbt(sr, "rci", [128, NE], I32); padded = sbt(sr, "rpad", [128, NE]); pstart = sbt(sr, "rpst", [128, NE]); pend = sbt(sr, "rpend", [128, NE])
                        kb.op('dve', lambda e: e.tensor_scalar(out=tmpq[:], in0=base[:], scalar1=127.0, scalar2=None, op0=ALU.add), r=['rbase'], w=['rtmpq'])
                        kb.op('dve', lambda e: e.tensor_copy(out=ci[:], in_=tmpq[:]), r=['rtmpq'], w=['rci'])
                        kb.op('dve', lambda e: e.tensor_scalar(out=ci[:], in0=ci[:], scalar1=7, scalar2=None, op0=ALU.arith_shift_right), r=['rci'], w=['rci'])
                        kb.op('dve', lambda e: e.tensor_scalar(out=ci[:], in0=ci[:], scalar1=7, scalar2=None, op0=ALU.logical_shift_left), r=['rci'], w=['rci'])
                        kb.op('dve', lambda e: e.tensor_copy(out=padded[:], in_=ci[:]), r=['rci'], w=['rpad'])
                        padT = sbt(sr, "rpadT", [128, 2, 128]); pendT = sbt(sr, "rpendT", [128, 2, 128])
                        pa, pak = banks[0]
                        for hh in range(2):
                            kb.op('pe', lambda e: e.transpose(out=pa[:, hh * 128:(hh + 1) * 128], in_=padded[:, hh * 128:(hh + 1) * 128], identity=ident[:]), r=['rpad', 'ident'], w=[pak])
                        kb.op('dve', lambda e: e.tensor_copy(out=padT[:], in_=pa[:, 0:256].rearrange("p (h c) -> p h c", h=2)), r=[pak], w=['rpadT'])
                        pb_, pbk_ = banks[1]
                        for hh in range(2):
                            kb.op('pe', lambda e: e.matmul(pb_[:, 0:NE], lhsT=padT[:, hh, :], rhs=slt[:, hh * 256:(hh + 1) * 256], start=(hh == 0), stop=(hh == 1)), r=['rpadT', 'slt'], w=[pbk_])
                        kb.op('dve', lambda e: e.tensor_copy(out=pstart[:], in_=pb_[:, 0:NE]), r=[pbk_], w=['rpst'])
                        kb.op('dve', lambda e: e.tensor_tensor(out=pend[:], in0=pstart[:], in1=padded[:], op=ALU.add), r=['rpst', 'rpad'], w=['rpend'])
                        pc_, pck_ = banks[2]
                        for hh in range(2):
                            kb.op('pe', lambda e: e.transpose(out=pc_[:, hh * 128:(hh + 1) * 128], in_=pend[:, hh * 128:(hh + 1) * 128], identity=ident[:]), r=['rpend', 'ident'], w=[pck_])
                        kb.op('dve', lambda e: e.tensor_copy(out=pendT[:], in_=pc_[:, 0:256].rearrange("p (h c) -> p h c", h=2)), r=[pck_], w=['rpendT'])
                        cmpT = sbt(sr, "rcmpT", [128, 2, NBLK]); bef = sbt(sr, "rbef", [128, NBLK])
                        for hh in range(2):
                            kb.op('dve', lambda e: e.tensor_scalar(out=cmpT[:, hh, :], in0=blk128[:], scalar1=pendT[:, hh, 0:1], scalar2=None, op0=ALU.is_ge), r=['blk128', 'rpendT', 'rcmpT'], w=['rcmpT'])
                        pd_, pdk_ = banks[3]
                        for hh in range(2):
                            kb.op('pe', lambda e: e.matmul(pd_[:, 0:NBLK], lhsT=onesm[:], rhs=cmpT[:, hh, :], start=(hh == 0), stop=(hh == 1)), r=['onesm', 'rcmpT'], w=[pdk_])
                        kb.op('dve', lambda e: e.tensor_scalar(out=bef[:], in0=pd_[:, 0:NBLK], scalar1=255.0, scalar2=128.0, op0=ALU.min, op1=ALU.mult), r=[pdk_], w=['rbef'])
                        kb.op('dve', lambda e: e.tensor_scalar(out=bef[:], in0=bef[:], scalar1=pidx[:, 0:1], scalar2=None, op0=ALU.add), r=['rbef', 'pidx'], w=['rbef'])
                        kb.op('dve', lambda e: e.tensor_copy(out=GI[:], in_=bef[:]), r=['rbef'], w=['GI'])
                        eqj = sbt(sr, "reqj", [128, NE])
                        for ti in range(NT):
                            kb.op('dve', lambda e: e.tensor_tensor(out=DST[:, ti, :], in0=DST[:, ti, :], in1=pstart[:], op=ALU.add), r=['DST', 'rpst'], w=['DST'])
                            for k8 in range(8):
                                kb.op('dve', lambda e: e.scalar_tensor_tensor(out=eqj[:], in0=MSK[:, ti, :], scalar=V8[:, ti, k8:k8 + 1], in1=DST[:, ti, :], op0=ALU.is_equal, op1=ALU.mult),
                                      r=['MSK', 'V8', 'DST', 'reqj'], w=['reqj'])
                                kb.op('dve', lambda e: e.tensor_reduce(out=D8F[:, ti, k8:k8 + 1], in_=eqj[:], axis=AX.X, op=ALU.add), r=['reqj', 'D8F'], w=['D8F'])
                                kb.op('dve', lambda e: e.scalar_tensor_tensor(out=eqj[:], in0=MSK[:, ti, :], scalar=V8[:, ti, k8:k8 + 1], in1=WD[:, ti, :], op0=ALU.is_equal, op1=ALU.mult),
                                      r=['MSK', 'V8', 'WDm', 'reqj'], w=['reqj'])
                                kb.op('dve', lambda e: e.tensor_reduce(out=W8[:, ti, k8:k8 + 1], in_=eqj[:], axis=AX.X, op=ALU.add), r=['reqj', 'W8'], w=['W8'])
                        kb.op('dve', lambda e: e.tensor_copy(out=D8I[:], in_=D8F[:].rearrange("p t k -> p (t k)")), r=['D8F'], w=['D8I'])
                    dbg('D8F', D8F[:], [128, NT, 8]); dbg('W8', W8[:], [128, NT, 8])
                    ht = [sbt(st, "dht%d" % i, [128, D]) for i in range(2)]
                    for ti in range(NT):
                        b2 = ti % 2; hk = 'dht%d' % b2
                        kb.dma('sp', lambda e: e.dma_start(out=ht[b2][:], in_=H2[ti * 128:(ti + 1) * 128, :]), r=['H2'], w=[hk])
                        for k8 in range(8):
                            kb.dma('pool', lambda e: e.indirect_dma_start(out=XS, out_offset=bass.IndirectOffsetOnAxis(ap=D8I[:, ti * 8 + k8:ti * 8 + k8 + 1], axis=0), in_=ht[b2][:], in_offset=None,
                                                                         bounds_check=RB_XS, oob_is_err=False), r=[hk, 'D8I'], w=['XSw'])
                    kb.barrier()
                    NW = 3
                    wgu = [sbt(st, "wgu%d" % i, [128, 2, 8, 256]) for i in range(NW)]; wdn = [sbt(st, "wdn%d" % i, [128, 2, D]) for i in range(4)]
                    xs = [sbt(st, "xs%d" % i, [128, D]) for i in range(2)]; xsT = [sbt(st, "xsT%d" % i, [128, 8, 128]) for i in range(3)]
                    actT = [sbt(st, "actT%d" % i, [128, 2, 128]) for i in range(3)]; sg_ = [sbt(st, "esg%d" % i, [128, 256]) for i in range(3)]
                    ys = [sbt(st, "ys%d" % i, [128, D]) for i in range(2)]
                    bctr = [0]

                    def bk():
                        b_ = banks[bctr[0] % 8]; bctr[0] += 1
                        return b_

                    def phA(blk):
                        wk, dk, xk, xtk = 'wgu%d' % (blk % NW), 'wdn%d' % (blk % 4), 'xs%d' % (blk % 2), 'xsT%d' % (blk % 3)
                        kb.dma('pool', lambda e: e.indirect_dma_start(out=wgu[blk % NW][:].rearrange("p a k f -> p (a k f)"), out_offset=None, in_=EWGU,
                                                                     in_offset=bass.IndirectOffsetOnAxis(ap=GI[:, blk:blk + 1], axis=0),
                                                                     bounds_check=RB_W, oob_is_err=False), r=['GI'], w=[wk])
                        kb.dma('pool', lambda e: e.indirect_dma_start(out=wdn[blk % 4][:].rearrange("p k f -> p (k f)"), out_offset=None, in_=EWD,
                                                                     in_offset=bass.IndirectOffsetOnAxis(ap=GI[:, blk:blk + 1], axis=0),
                                                                     bounds_check=RB_W, oob_is_err=False), r=['GI'], w=[dk])
                        kb.dma('sp', lambda e: e.dma_start(out=xs[blk % 2][:], in_=XS[blk * 128:(blk + 1) * 128, :]), r=['XSw'], w=[xk])
                        for hh in range(2):
                            pb, pbk = bk()
                            for kk in range(4):
                                k8 = hh * 4 + kk
                                kb.op('pe', lambda e: e.transpose(out=pb[:, kk * 128:(kk + 1) * 128], in_=xs[blk % 2][:, k8 * 128:(k8 + 1) * 128], identity=ident[:]), r=[xk, 'ident'], w=[pbk])
                            evac(xsT[blk % 3][:, hh * 4:hh * 4 + 4, :], pb[:, :].rearrange("p (k t) -> p k t", k=4), None, [pbk, xtk], [xtk])

                    def phB(blk):
                        wk, xtk, sgk = 'wgu%d' % (blk % NW), 'xsT%d' % (blk % 3), 'esg%d' % (blk % 3)
                        ph, phk = bk()
                        for kk in range(8):
                            kb.op('pe', lambda e: e.matmul(ph[:, :].rearrange("p (a f) -> p a f", a=2), lhsT=xsT[blk % 3][:, kk, :], rhs=wgu[blk % NW][:, :, kk, :],
                                                           start=(kk == 0), stop=(kk == 7)), r=[wk, xtk], w=[phk])
                        kb.op('act', lambda e: e.activation(out=sg_[blk % 3][:], in_=ph[:, 0:256], func=AF.Silu), r=[phk], w=[sgk])
                        kb.op('dve', lambda e: e.tensor_tensor(out=sg_[blk % 3][:], in0=ph[:, 256:512], in1=sg_[blk % 3][:], op=ALU.mult), r=[phk, sgk], w=[sgk])

                    def phC(blk):
                        sgk, ak = 'esg%d' % (blk % 3), 'actT%d' % (blk % 3)
                        pt_, ptk = bk()
                        for kk in range(2):
                            kb.op('pe', lambda e: e.transpose(out=pt_[:, kk * 128:(kk + 1) * 128], in_=sg_[blk % 3][:, kk * 128:(kk + 1) * 128], identity=ident[:]), r=[sgk, 'ident'], w=[ptk])
                        evac(actT[blk % 3][:].rearrange("p k t -> p (k t)"), pt_[:, 0:256], None, [ptk, ak], [ak])

                    def phD(blk):
                        dk, ak, yk = 'wdn%d' % (blk % 4), 'actT%d' % (blk % 3), 'ys%d' % (blk % 2)
                        for hf in range(2):
                            py, pyk = bk()
                            for kk in range(2):
                                kb.op('pe', lambda e: e.matmul(py[:, :], lhsT=actT[blk % 3][:, kk, :], rhs=wdn[blk % 4][:, kk, hf * 512:(hf + 1) * 512], start=(kk == 0), stop=(kk == 1)), r=[ak, dk], w=[pyk])
                            evac(ys[blk % 2][:, hf * 512:(hf + 1) * 512], py[:, :], None, [pyk, yk], [yk])
                        kb.dma('sp', lambda e: e.dma_start(out=YS[blk * 128:(blk + 1) * 128, :], in_=ys[blk % 2][:]), r=[yk], w=['YSw'])

                    for s_ in range(NBLK + 3):
                        if s_ < NBLK:
                            phA(s_)
                        if 0 <= s_ - 1 < NBLK:
                            phB(s_ - 1)
                        if 0 <= s_ - 2 < NBLK:
                            phC(s_ - 2)
                        if 0 <= s_ - 3 < NBLK:
                            phD(s_ - 3)
                    kb.barrier()
                    acc = [sbt(st, "cacc%d" % i, [128, D]) for i in range(2)]; gg = [sbt(st, "cg%d" % i, [128, D]) for i in range(3)]
                    gi_ = 0
                    for ti in range(NT):
                        b2 = ti % 2; ack = 'cacc%d' % b2
                        for k8 in range(8):
                            g3 = gi_ % 3; gi_ += 1; ggk = 'cg%d' % g3
                            kb.dma('pool', lambda e: e.indirect_dma_start(out=gg[g3][:], out_offset=None, in_=YS, in_offset=bass.IndirectOffsetOnAxis(ap=D8I[:, ti * 8 + k8:ti * 8 + k8 + 1], axis=0),
                                                                         bounds_check=RB_XS, oob_is_err=False), r=['YSw', 'D8I'], w=[ggk])
                            if k8 == 0:
                                kb.op('dve', lambda e: e.tensor_scalar(out=acc[b2][:], in0=gg[g3][:], scalar1=W8[:, ti, 0:1], scalar2=None, op0=ALU.mult), r=[ggk, 'W8', ack], w=[ack])
                            else:
                                kb.op('dve', lambda e: e.scalar_tensor_tensor(out=acc[b2][:], in0=gg[g3][:], scalar=W8[:, ti, k8:k8 + 1], in1=acc[b2][:], op0=ALU.mult, op1=ALU.add),
                                      r=[ggk, 'W8', ack], w=[ack])
                        kb.dma('sp', lambda e: e.dma_start(out=ROUTED[ti * 128:(ti + 1) * 128, :], in_=acc[b2][:]), r=[ack], w=['ROUTED'])

            with stage() as st:
                g2row = row_bcast(st, "g2row", MODROW[0:1, 5120:6144])
                fgrow = sbt(st, "fgrow", [128, D])
                kb.dma('sp', lambda e: e.dma_start(out=fgrow[:], in_=FING.rearrange("(o n) -> o n", o=1).to_broadcast([128, D])), w=['fgrow'])
                xa = [sbt(st, "xa%d" % i, [128, D]) for i in range(2)]; sa = [sbt(st, "sa%d" % i, [128, D]) for i in range(2)]
                ra = [sbt(st, "ra%d" % i, [128, D]) for i in range(2)]; stt = [sbt(st, "stt%d" % i, [128, 4]) for i in range(2)]
                junk = sbt(st, "fjunk", [128, D])
                for ti in range(OWN // 128):
                    b2 = ti % 2; xk, sk, rk, tk = 'xa%d' % b2, 'sa%d' % b2, 'ra%d' % b2, 'stt%d' % b2
                    rows = slice(ti * 128, (ti + 1) * 128)
                    kb.dma('sp', lambda e: e.dma_start(out=xa[b2][:], in_=X1D[rows, :]), r=['X1D'], w=[xk])
                    kb.dma('sp', lambda e: e.dma_start(out=sa[b2][:], in_=SHOUT[rows, :]), r=['SHOUT'], w=[sk])
                    kb.dma('sp', lambda e: e.dma_start(out=ra[b2][:], in_=ROUTED[rows, :]), r=['ROUTED'], w=[rk])
                    kb.op('pool', lambda e: e.tensor_tensor(out=sa[b2][:], in0=sa[b2][:], in1=ra[b2][:], op=ALU.add), r=[sk, rk], w=[sk])
                    kb.op('dve', lambda e: e.tensor_tensor(out=sa[b2][:], in0=sa[b2][:], in1=g2row[:], op=ALU.mult), r=[sk, 'g2row'], w=[sk])
                    kb.op('pool', lambda e: e.tensor_tensor(out=xa[b2][:], in0=xa[b2][:], in1=sa[b2][:], op=ALU.add), r=[xk, sk], w=[xk])
                    kb.op('act', lambda e: e.activation(out=junk[:], in_=xa[b2][:], func=AF.Square, accum_out=stt[b2][:, 0:1]), r=[xk], w=['fjunk', tk])
                    kb.op('dve', lambda e: e.tensor_scalar(out=stt[b2][:, 1:2], in0=stt[b2][:, 0:1], scalar1=1.0 / D, scalar2=EPS, op0=ALU.mult, op1=ALU.add), r=[tk], w=[tk])
                    kb.op('act', lambda e: e.activation(out=stt[b2][:, 2:3], in_=stt[b2][:, 1:2], func=AF.Sqrt), r=[tk], w=[tk])
                    kb.op('dve', lambda e: e.reciprocal(out=stt[b2][:, 3:4], in_=stt[b2][:, 2:3]), r=[tk], w=[tk])
                    kb.op('dve', lambda e: e.scalar_tensor_tensor(out=xa[b2][:], in0=xa[b2][:], scalar=stt[b2][:, 3:4], in1=fgrow[:], op0=ALU.mult, op1=ALU.mult),
                          r=[xk, tk, 'fgrow'], w=[xk])
                    kb.dma('pool', lambda e: e.dma_start(out=OUT[rows, :], in_=xa[b2][:]), r=[xk], w=['OUT'])

        kb.finish('sp')
        kb.finish('pool')
        pg.ninstr = kb.ninstr
    return pg


_PROG = None


def make_in_maps(pg, inputs):
    hc = host_consts()
    sq = lambda a: np.ascontiguousarray(a[0])
    in_maps = []
    shared = {}
    if 'EWGU' in pg.ins:
        wg = np.asarray(inputs['exp_w_gate'])[0].reshape(NE, 8, 128, 256)
        wu = np.asarray(inputs['exp_w_up'])[0].reshape(NE, 8, 128, 256)
        ew = np.empty((NE, 128, 2, 8, 256), np.float32)
        ew[:, :, 0] = wg.transpose(0, 2, 1, 3); ew[:, :, 1] = wu.transpose(0, 2, 1, 3)
        shared['EWGU'] = ew.reshape(NE * 128, 4096)
        shared['EWD'] = np.ascontiguousarray(np.asarray(inputs['exp_w_down'])[0].reshape(NE, 2, 128, D).transpose(0, 2, 1, 3)).reshape(NE * 128, 2048)
    for c in range(8):
        b, j = c // 4, c % 4
        own = slice(j * OWN, (j + 1) * OWN)
        idx = ((np.arange(4)[None, :] * 128 + np.arange(128)[:, None]) * 4 + j).astype(np.int32)
        full = {
            'x': inputs['x'][b], 'ctx': inputs['ctx'][b], 'xown': inputs['x'][b, own], 'posown': hc['POS'][own],
            'c': inputs['c'][b], 'c_ctx': inputs['c_ctx'], 'final_g': inputs['final_g'], 'OWNIDX': idx,
            'hg_lb_logits': np.asarray(inputs['hg_lb_logits']).reshape(2, 1024),
        }
        full.update(shared)
        for k in pg.ins:
            if k not in full and k not in hc:
                full[k] = sq(inputs[k])
        full.update(hc)
        in_maps.append({k: np.ascontiguousarray(np.asarray(full[k])) for k in pg.ins})
    return in_maps


def kernel(**inputs):
    global _PROG
    if _PROG is None:
        _PROG = build()
    pg = _PROG
    in_maps = make_in_maps(pg, inputs)
    res = run_bass_kernel_spmd(pg.nc, in_maps, core_ids=list(range(8)))
    out = np.zeros((2, L, D), np.float32)
    for c in range(8):
        b, j = c // 4, c % 4
        out[b, j * OWN:(j + 1) * OWN] = res.results[c]['out']
    return out
```

```python
import math
import numpy as np
from contextlib import ExitStack, contextmanager
import concourse.bass as bass
import concourse.mybir as mybir
from concourse.bass_utils import run_bass_kernel_spmd

F32 = mybir.dt.float32
I32 = mybir.dt.int32
U32 = mybir.dt.uint32
ALU = mybir.AluOpType
AF = mybir.ActivationFunctionType
AX = mybir.AxisListType

N_DMA_SEMS = 24
D = 1024
L = 8192
NCTX = 256
LT = L + NCTX
OWN = 2048
NE = 256
NBLK = 383
EPS = 1e-6
TWO_PI = 2.0 * math.pi


class KB:
    def __init__(self, nc, es):
        self.nc = nc
        self.engs = {'pe': nc.tensor, 'act': nc.scalar, 'dve': nc.vector, 'pool': nc.gpsimd, 'sp': nc.sync}
        self.sems = {}
        self.cnt = {}
        for e in self.engs:
            self.sems[e] = es.enter_context(nc.semaphore("s_" + e))
            self.cnt[e] = 0
        for i in range(N_DMA_SEMS):
            self.sems['d%d' % i] = es.enter_context(nc.semaphore("s_d%d" % i))
            self.cnt['d%d' % i] = 0
        self.dnext = 0
        self.waited = {e: {} for e in self.engs}
        self.res = {}
        self.ninstr = 0

    def _need(self, eng, toks):
        best = {}
        for t in toks:
            if t is None:
                continue
            sk, v = t
            if sk == eng and eng == 'pe':
                continue
            if best.get(sk, 0) < v:
                best[sk] = v
        for sk, v in best.items():
            if self.waited[eng].get(sk, 0) >= v:
                continue
            self.engs[eng].wait_ge(self.sems[sk], v)
            self.waited[eng][sk] = v

    def _deps(self, r, w):
        toks = []
        for k in r:
            st = self.res.get(k)
            if st is not None:
                toks.append(st[0])
        for k in w:
            st = self.res.get(k)
            if st is not None:
                toks.append(st[0])
                toks.extend(st[1])
        return toks

    def _commit(self, tok, r, w):
        for k in r:
            st = self.res.setdefault(k, [None, []])
            st[1].append(tok)
            if len(st[1]) > 32:
                best = {}
                for sk, v in st[1]:
                    if best.get(sk, 0) < v:
                        best[sk] = v
                st[1] = list(best.items())
        for k in w:
            self.res[k] = [tok, []]

    def op(self, eng, fn, r=(), w=()):
        self._need(eng, self._deps(r, w))
        ins = fn(self.engs[eng])
        self.cnt[eng] += 1
        ins.then_inc(self.sems[eng], 1)
        self._commit((eng, self.cnt[eng]), r, w)
        self.ninstr += 1

    def dma(self, q, fn, r=(), w=()):
        i = self.dnext
        self.dnext = (self.dnext + 1) % N_DMA_SEMS
        sk = 'd%d' % i
        toks = self._deps(r, w)
        if self.cnt[sk] > 0:
            toks.append((sk, self.cnt[sk]))
        self._need(q, toks)
        ins = fn(self.engs[q])
        self.cnt[sk] += 16
        ins.then_inc(self.sems[sk], 16)
        self._commit((sk, self.cnt[sk]), r, w)
        self.ninstr += 1

    def barrier(self):
        toks = [(sk, v) for sk, v in self.cnt.items() if v > 0]
        for e in self.engs:
            self._need(e, toks)

    def finish(self, eng):
        toks = []
        for st in self.res.values():
            toks.append(st[0])
            toks.extend(st[1])
        self._need(eng, toks)


_CONST = None


def host_consts():
    global _CONST
    if _CONST is not None:
        return _CONST
    c = {}
    quarter = D // 4
    omega = (1.0 / (np.float32(10000.0) ** (np.arange(quarter, dtype=np.float32) / np.float32(quarter)))).astype(np.float32)
    rows, cols = L // 64, 64
    ang_r = (np.arange(rows, dtype=np.float32)[:, None] * omega).astype(np.float32)
    ang_c = (np.arange(cols, dtype=np.float32)[:, None] * omega).astype(np.float32)
    emb_r = np.concatenate([np.sin(ang_r), np.cos(ang_r)], -1)
    emb_c = np.concatenate([np.sin(ang_c), np.cos(ang_c)], -1)
    emb = np.concatenate([np.broadcast_to(emb_r[:, None], (rows, cols, D // 2)),
                          np.broadcast_to(emb_c[None], (rows, cols, D // 2))], -1)
    c['POS'] = np.ascontiguousarray(emb.reshape(L, D).astype(np.float32))
    c['IDENT'] = np.eye(128, dtype=np.float32)
    si = np.arange(128)[:, None]; ti = np.arange(128)[None, :]
    same = (si // 64) == (ti // 64)
    c['TRIF'] = (same & (si <= ti)).astype(np.float32)
    c['TRIB'] = (same & (si >= ti)).astype(np.float32)
    c['ONES'] = np.ones((128, 128), np.float32)
    c['STRI'] = (si < ti).astype(np.float32)
    e1 = np.arange(128)[:, None]; e2 = np.arange(256)[None, :]
    c['SLT'] = np.concatenate([(e1 < e2), (e1 + 128 < e2)], 1).astype(np.float32)
    c['BLK128'] = np.broadcast_to((np.arange(NBLK, dtype=np.float32) * 128.0)[None, :], (128, NBLK)).copy()
    c['PIDX'] = np.arange(128, dtype=np.float32).reshape(128, 1).copy()
    NN = 16384
    a = np.arange(128, dtype=np.float64)
    ang = 2.0 * np.pi * np.outer(a, a) / 128.0
    Fre = np.cos(ang); Fim = -np.sin(ang)
    f32 = lambda v: np.ascontiguousarray(v.astype(np.float32))
    c['FA'] = f32(np.concatenate([Fre, Fim], 1)); c['FAH'] = f32(np.concatenate([Fre, Fim], 1)[64:128])
    c['FRE'] = f32(Fre); c['FIM'] = f32(Fim); c['NFIM'] = f32(-Fim)
    c['CA'] = f32(np.concatenate([Fre, -Fim], 1)); c['CB'] = f32(np.concatenate([Fim, Fre], 1))
    c['FREN'] = f32(Fre[:, :64] / NN); c['FIMN'] = f32(Fim[:, :64] / NN)
    angT = 2.0 * np.pi * np.outer(a, a) / NN
    c['TRE2'] = f32(np.concatenate([np.cos(angT), np.cos(angT)], 1)); c['TIM2'] = f32(np.concatenate([-np.sin(angT), -np.sin(angT)], 1))
    bands = np.linspace(1e-4, 15.0, 16, dtype=np.float32)
    def zfeat(t):
        t = t.astype(np.float32)
        tn = (t / np.float32(L - 1)).astype(np.float32)
        an = (np.float32(2 * math.pi / L) * t[:, None] * bands).astype(np.float32)
        return np.concatenate([tn[:, None], np.cos(an), -np.sin(an)], -1).astype(np.float32), tn
    zf, tnf = zfeat(np.arange(L)); zb, tnb = zfeat(L - np.arange(L))
    c['ZT'] = np.ascontiguousarray(np.concatenate([zf, zb], 1).T)
    lo_ = math.log(1e-2) / 1.5; hi_ = math.log(1e-2) / 0.3
    deltas = np.abs(np.linspace(lo_, hi_, 512, dtype=np.float32))
    decf = np.exp(-tnf[:, None] * deltas).astype(np.float32)
    decb = np.exp(-tnb[:, None] * deltas).astype(np.float32); decb[0] = 0.0
    c['DEC'] = np.ascontiguousarray(np.stack([decf.reshape(64, 128, 512).transpose(0, 2, 1), decb.reshape(64, 128, 512).transpose(0, 2, 1)]))
    _CONST = c
    return c


class Prog:
    def __init__(self, debug=None):
        self.debug = debug or ()
        self.nc = bass.Bass("TRN2", target_bir_lowering=False)
        self.ins = {}
        self.outs = {}

    def inp(self, name, shape, dt=F32):
        t = self.nc.dram_tensor(name, list(shape), dt, kind="ExternalInput").ap()
        self.ins[name] = t
        return t

    def scratch(self, name, shape, dt=F32):
        if name in self.debug:
            t = self.nc.dram_tensor(name, list(shape), dt, kind="ExternalOutput").ap()
            self.outs[name] = t
        else:
            t = self.nc.dram_tensor(name, list(shape), dt, kind="Internal").ap()
        return t


def build(stages=99, debug=None, dummy_mix=False):
    pg = Prog(debug)
    nc = pg.nc
    X = pg.inp("x", [L, D]); CTX = pg.inp("ctx", [NCTX, D]); XOWN = pg.inp("xown", [OWN, D]); POSOWN = pg.inp("posown", [OWN, D])
    CV = pg.inp("c", [D]); CCTX = pg.inp("c_ctx", [D])
    N1G = pg.inp("norm1_g", [D]); N2G = pg.inp("norm2_g", [D])
    ADAW = pg.inp("ada_w", [D, 6 * D]); ADAB = pg.inp("ada_b", [6 * D])
    WIN = pg.inp("w_in", [D, 6144])
    POS = pg.inp("POS", [L, D]); IDENT = pg.inp("IDENT", [128, 128])
    WHY = pg.inp("w_hy_out", [512, D]); WHG = pg.inp("w_hg_out", [512, D]); WOUT = pg.inp("w_out", [D, D])
    SHG = pg.inp("sh_w_gate", [D, 256]); SHU = pg.inp("sh_w_up", [D, 256]); SHD = pg.inp("sh_w_down", [256, D])
    FING = pg.inp("final_g", [D]); OWNIDX = pg.inp("OWNIDX", [128, 4], I32)
    CONVW = pg.inp("hy_conv_w", [3, 1536]); CONVB = pg.inp("hy_conv_b", [1536])
    TRIFd = pg.inp("TRIF", [128, 128]); TRIBd = pg.inp("TRIB", [128, 128]); ONESd = pg.inp("ONES", [128, 128])
    LBL = pg.inp("hg_lb_logits", [2, 1024]); HGNG = pg.inp("hg_norm_g", [128])
    STRId = pg.inp("STRI", [128, 128]); SLTd = pg.inp("SLT", [128, 512]); BLKd = pg.inp("BLK128", [128, NBLK]); PIDXd = pg.inp("PIDX", [128, 1])
    RW = pg.inp("router_w", [D, NE]); RB = pg.inp("router_bias", [NE])
    EWGU = pg.inp("EWGU", [NE * 128, 4096]); EWD = pg.inp("EWD", [NE * 128, 2048])
    FAd = pg.inp("FA", [128, 256]); FAHd = pg.inp("FAH", [64, 256]); FREd = pg.inp("FRE", [128, 128]); FIMd = pg.inp("FIM", [128, 128]); NFIMd = pg.inp("NFIM", [128, 128])
    CAd = pg.inp("CA", [128, 256]); CBd = pg.inp("CB", [128, 256]); FRENd = pg.inp("FREN", [128, 64]); FIMNd = pg.inp("FIMN", [128, 64])
    TRE2d = pg.inp("TRE2", [128, 256]); TIM2d = pg.inp("TIM2", [128, 256]); ZTd = pg.inp("ZT", [66, L]); DECd = pg.inp("DEC", [2, 64, 512, 128])
    FW1 = pg.inp("hy_f_w1", [33, 64]); FB1 = pg.inp("hy_f_b1", [64]); FW2 = pg.inp("hy_f_w2", [64, 64]); FB2 = pg.inp("hy_f_b2", [64])
    FW3 = pg.inp("hy_f_w3", [64, 64]); FB3 = pg.inp("hy_f_b3", [64]); FW4 = pg.inp("hy_f_w4", [64, 2048]); FFQ = pg.inp("hy_f_freq", [64])
    HSKIP = pg.inp("hy_skip", [1024])
    HSPEC = pg.scratch("HSPEC", [1024, 128, 2, 128])
    OUT = pg.nc.dram_tensor("out", [OWN, D], F32, kind="ExternalOutput").ap()
    pg.outs["out"] = OUT
    XNT = pg.scratch("XNT", [D, LT]); XNTOWN = pg.scratch("XNTOWN", [D, OWN])
    MODROW = pg.scratch("MODROW", [2, 6 * D])
    HYRAW = pg.scratch("HYRAW", [1536, L])
    QT = pg.scratch("QT", [512, L]); GT = pg.scratch("GT", [512, L])
    IFF = pg.scratch("IFF", [LT, 1536])
    SGT = pg.scratch("SGT", [2048, OWN])
    HYC = pg.scratch("HYC", [1536, L])
    YHYT = pg.scratch("YHYT", [512, L]); YHGT = pg.scratch("YHGT", [512, L])
    YOWN = pg.scratch("YOWN", [1024, OWN])
    X1D = pg.scratch("X1D", [OWN, D]); H2T = pg.scratch("H2T", [D, OWN])
    SGA = pg.scratch("SGA", [256, OWN]); SUA = pg.scratch("SUA", [256, OWN]); ACTT = pg.scratch("ACTT", [256, OWN])
    SHOUT = pg.scratch("SHOUT", [OWN, D]); ROUTED = pg.scratch("ROUTED", [OWN, D])
    H2 = pg.scratch("H2", [OWN, D]); SCORES = pg.scratch("SCORES", [OWN, NE])
    XS = pg.scratch("XS", [NBLK * 128, D]); YS = pg.scratch("YS", [NBLK * 128, D])

    with ExitStack() as es:
        kb = KB(nc, es)
        sbt = lambda stack, name, shape, dt=F32: stack.enter_context(nc.sbuf_tensor(name, list(shape), dt))
        PA = es.enter_context(nc.psum_tensor("PA", [128, 2048], F32))
        PB = es.enter_context(nc.psum_tensor("PB", [128, 2048], F32))
        banks = [(PA[:, 512 * i:512 * (i + 1)], "PA%d" % i) for i in range(4)] + \
                [(PB[:, 512 * i:512 * (i + 1)], "PB%d" % i) for i in range(4)]
        RB_OWN = nc.gpsimd.alloc_register("bc_own"); nc.gpsimd.reg_mov(RB_OWN, 2047)
        RB_XS = nc.gpsimd.alloc_register("bc_xs"); nc.gpsimd.reg_mov(RB_XS, NBLK * 128 - 1)
        RB_W = nc.gpsimd.alloc_register("bc_w"); nc.gpsimd.reg_mov(RB_W, NE * 128 - 1)
        ident = sbt(es, "ident", [128, 128])
        kb.dma('sp', lambda e: e.dma_start(out=ident[:], in_=IDENT), w=['ident'])
        modT = sbt(es, "modT", [128, 48, 2])
        a1T = sbt(es, "a1T", [128, 8, 2]); sh1T = sbt(es, "sh1T", [128, 8, 2]); g2T = sbt(es, "g2T", [128, 8])
        @contextmanager
        def stage():
            with ExitStack() as st_:
                yield st_
                kb.barrier()

        def dbg(name, ap, shape):
            if name in pg.debug:
                t = nc.dram_tensor("D_" + name, list(shape), F32, kind="ExternalOutput").ap()
                pg.outs["D_" + name] = t
                kb.dma('pool', lambda e: e.dma_start(out=t, in_=ap), r=[name], w=['DBG' + name])

        with stage() as s0:
            sT = sbt(s0, "sT", [128, 8, 2]); vraw = sbt(s0, "vraw", [80, 128]); VT = sbt(s0, "VT", [128, 80])
            kb.dma('sp', lambda e: e.dma_start(out=vraw[0:8, :], in_=CV.rearrange("(q p) -> q p", p=128)), w=['vraw'])
            kb.dma('sp', lambda e: e.dma_start(out=vraw[8:16, :], in_=CCTX.rearrange("(q p) -> q p", p=128)), r=['vraw'], w=['vraw'])
            kb.dma('sp', lambda e: e.dma_start(out=vraw[16:24, :], in_=N1G.rearrange("(q p) -> q p", p=128)), r=['vraw'], w=['vraw'])
            kb.dma('sp', lambda e: e.dma_start(out=vraw[24:32, :], in_=N2G.rearrange("(q p) -> q p", p=128)), r=['vraw'], w=['vraw'])
            kb.dma('sp', lambda e: e.dma_start(out=vraw[32:80, :], in_=ADAB.rearrange("(q p) -> q p", p=128)), r=['vraw'], w=['vraw'])
            ps, psk = banks[0]
            kb.op('pe', lambda e: e.transpose(out=ps[:, 128:208], in_=vraw[:], identity=ident[0:80, 0:80]), r=['vraw', 'ident'], w=[psk])
            kb.op('dve', lambda e: e.tensor_copy(out=VT[:], in_=ps[:, 128:208]), r=[psk], w=['VT'])
            abT = VT[:, 32:80]; g1T = VT[:, 16:24]
            for r_ in range(2):
                kb.op('act', lambda e: e.activation(out=sT[:, :, r_], in_=VT[:, 8 * r_:8 * r_ + 8], func=AF.Silu), r=['VT', 'sT'], w=['sT'])
            kb.op('dve', lambda e: e.tensor_copy(out=g2T[:], in_=VT[:, 24:32]), r=['VT'], w=['g2T'])
            wbufs = [sbt(s0, "adaw%d" % i, [128, 8, 768]) for i in range(2)]
            mrow = sbt(s0, "mrow", [2, 6144]); brow = sbt(s0, "brow", [2, 6144])
            for r_ in range(2):
                kb.dma('sp', lambda e: e.dma_start(out=brow[r_:r_ + 1, :], in_=ADAB.rearrange("(o n) -> o n", o=1)), r=['brow'], w=['brow'])
            for cb in range(8):
                wb = wbufs[cb % 2]; wk = "adaw%d" % (cb % 2)
                kb.dma('sp', lambda e: e.dma_start(out=wb[:], in_=ADAW[:, cb * 768:(cb + 1) * 768].rearrange("(k p) c -> p k c", p=128)), w=[wk])
                for hh in range(2):
                    pr, prk = banks[1 + (2 * cb + hh) % 3]
                    for kk in range(8):
                        kb.op('pe', lambda e: e.matmul(pr[0:2, 0:384], lhsT=sT[:, kk, :], rhs=wb[:, kk, hh * 384:(hh + 1) * 384],
                                                       start=(kk == 0), stop=(kk == 7)), r=[wk, 'sT'], w=[prk])
                    c0 = cb * 768 + hh * 384
                    kb.op('dve', lambda e: e.tensor_tensor(out=mrow[:, c0:c0 + 384], in0=pr[0:2, 0:384], in1=brow[:, c0:c0 + 384], op=ALU.add),
                          r=[prk, 'brow'], w=['mrow'])
            kb.dma('pool', lambda e: e.dma_start(out=MODROW, in_=mrow[:]), r=['mrow'], w=['MODROW'])
            mq = sbt(s0, "mq", [96, 128])
            kb.dma('sp', lambda e: e.dma_start(out=mq[:], in_=MODROW.rearrange("r (q p) -> (r q) p", p=128)), r=['MODROW'], w=['mq'])
            kb.op('pe', lambda e: e.transpose(out=ps[:, 256:352], in_=mq[:], identity=ident[0:96, 0:96]), r=['mq', 'ident'], w=[psk])
            for r_ in range(2):
                kb.op('dve', lambda e: e.tensor_copy(out=modT[:, :, r_], in_=ps[:, 256 + 48 * r_:256 + 48 * r_ + 48]), r=[psk, 'modT'], w=['modT'])
            kb.op('dve', lambda e: e.tensor_scalar(out=a1T[:], in0=modT[:, 8:16, :], scalar1=1.0, scalar2=None, op0=ALU.add),
                  r=['modT'], w=['a1T'])
            for r_ in range(2):
                kb.op('dve', lambda e: e.tensor_tensor(out=a1T[:, :, r_], in0=a1T[:, :, r_], in1=g1T, op=ALU.mult),
                      r=['a1T', 'VT'], w=['a1T'])
            kb.op('dve', lambda e: e.tensor_copy(out=sh1T[:], in_=modT[:, 0:8, :]), r=['modT'], w=['sh1T'])

        dbg('modT', modT[:], [128, 48, 2]); dbg('a1T', a1T[:], [128, 8, 2])
        def norm_to_xt(stack, srcs, XT, a_ap, b_ap, tag, xtk):
            xin = [sbt(stack, "%s_xin%d" % (tag, i), [128, 1024]) for i in range(2)]
            pin = [sbt(stack, "%s_pin%d" % (tag, i), [128, 1024]) for i in range(2)]
            junk = sbt(stack, "%s_junk" % tag, [128, 1024])
            st = [sbt(stack, "%s_st%d" % (tag, i), [128, 4]) for i in range(2)]
            xo = [sbt(stack, "%s_xo%d" % (tag, i), [128, 8, 128]) for i in range(2)]
            for i, (xap, pap, c0) in enumerate(srcs):
                b = i % 2
                xk, pk, sk, ok = "%s_xin%d" % (tag, b), "%s_pin%d" % (tag, b), "%s_st%d" % (tag, b), "%s_xo%d" % (tag, b)
                kb.dma('sp', lambda e: e.dma_start(out=xin[b][:], in_=xap), w=[xk])
                if pap is not None:
                    kb.dma('sp', lambda e: e.dma_start(out=pin[b][:], in_=pap), w=[pk])
                    kb.op('pool', lambda e: e.tensor_tensor(out=xin[b][:], in0=xin[b][:], in1=pin[b][:], op=ALU.add), r=[xk, pk], w=[xk])
                kb.op('act', lambda e: e.activation(out=junk[:], in_=xin[b][:], func=AF.Square, accum_out=st[b][:, 0:1]),
                      r=[xk], w=[tag + '_junk', sk])
                kb.op('dve', lambda e: e.tensor_scalar(out=st[b][:, 1:2], in0=st[b][:, 0:1], scalar1=1.0 / D, scalar2=EPS, op0=ALU.mult, op1=ALU.add),
                      r=[sk], w=[sk])
                kb.op('act', lambda e: e.activation(out=st[b][:, 2:3], in_=st[b][:, 1:2], func=AF.Sqrt), r=[sk], w=[sk])
                kb.op('dve', lambda e: e.reciprocal(out=st[b][:, 3:4], in_=st[b][:, 2:3]), r=[sk], w=[sk])
                kb.op('dve', lambda e: e.tensor_scalar(out=xin[b][:], in0=xin[b][:], scalar1=st[b][:, 3:4], scalar2=None, op0=ALU.mult),
                      r=[xk, sk], w=[xk])
                for h in range(2):
                    pb, pbk = banks[(2 * i + h) % 4]
                    for kk in range(4):
                        k8 = h * 4 + kk
                        kb.op('pe', lambda e: e.transpose(out=pb[:, kk * 128:(kk + 1) * 128], in_=xin[b][:, k8 * 128:(k8 + 1) * 128], identity=ident[:]),
                              r=[xk, 'ident'], w=[pbk])
                    for kk in range(4):
                        k8 = h * 4 + kk
                        kb.op('act', lambda e: e.activation(out=xo[b][:, k8, :], in_=pb[:, kk * 128:(kk + 1) * 128], func=AF.Identity,
                                                            bias=b_ap[:, k8:k8 + 1], scale=a_ap[:, k8:k8 + 1]),
                              r=[pbk, 'a1T', 'sh1T', 'a2T'], w=[ok])
                kb.dma('pool', lambda e: e.dma_start(out=XT[:, c0:c0 + 128].rearrange("(k p) t -> p k t", p=128), in_=xo[b][:]), r=[ok], w=[xtk])

        if stages >= 1:
            with stage() as s1:
                srcs = [(X[i * 128:(i + 1) * 128, :], POS[i * 128:(i + 1) * 128, :], i * 128) for i in range(L // 128)]
                norm_to_xt(s1, srcs, XNT, a1T[:, :, 0], sh1T[:, :, 0], "n1", 'XNT')
            with stage() as s1:
                srcs = [(CTX[i * 128:(i + 1) * 128, :], None, L + i * 128) for i in range(NCTX // 128)]
                norm_to_xt(s1, srcs, XNT, a1T[:, :, 1], sh1T[:, :, 1], "n1c", 'XNT')
            with stage() as s1:
                srcs = [(XOWN[i * 128:(i + 1) * 128, :], POSOWN[i * 128:(i + 1) * 128, :], i * 128) for i in range(OWN // 128)]
                norm_to_xt(s1, srcs, XNTOWN, a1T[:, :, 0], sh1T[:, :, 0], "n1o", 'XNTOWN')

        cp_toggle = [0]

        def evac(out_ap, in_ap, func, r, w):
            if func is None:
                cp_toggle[0] ^= 1
                if cp_toggle[0]:
                    kb.op('dve', lambda e: e.tensor_copy(out=out_ap, in_=in_ap), r=r, w=w)
                else:
                    kb.op('act', lambda e: e.copy(out=out_ap, in_=in_ap), r=r, w=w)
            else:
                kb.op('act', lambda e: e.activation(out=out_ap, in_=in_ap, func=func), r=r, w=w)

        def gemm_group(tag, Wsrc, kch, ncols, XT, xtkey, tblocks, jobs):
            with stage() as st:
                Wsb = sbt(st, tag + "_W", [128, kch, ncols])
                kb.dma('sp', lambda e: e.dma_start(out=Wsb[:], in_=Wsrc.rearrange("(k p) c -> p k c", p=128)), w=[tag + '_W'])
                Xb = [sbt(st, "%s_X%d" % (tag, i), [128, kch, 512]) for i in range(2)]
                stg = [sbt(st, "%s_s%d" % (tag, i), [128, 512]) for i in range(4)]
                si = 0
                bi = 0
                for ti, (t0, tn) in enumerate(tblocks):
                    xb = Xb[ti % 2]; xk = "%s_X%d" % (tag, ti % 2)
                    kb.dma('sp', lambda e: e.dma_start(out=xb[:, :, 0:tn], in_=XT[:, t0:t0 + tn].rearrange("(k p) t -> p k t", p=128)),
                           r=[xtkey], w=[xk])
                    for jb in jobs:
                        if jb.get('tsel') is not None and not jb['tsel'](t0):
                            continue
                        c0, cn, tofs = jb['c0'], jb['cn'], jb.get('tofs', 0)
                        if jb['mode'] == 'fm':
                            for m in range(cn // 128):
                                pb, pbk = banks[bi % 8]; bi += 1
                                for kk in range(kch):
                                    kb.op('pe', lambda e: e.matmul(pb[:, 0:tn], lhsT=Wsb[:, kk, c0 + m * 128:c0 + (m + 1) * 128], rhs=xb[:, kk, 0:tn],
                                                                   start=(kk == 0), stop=(kk == kch - 1)), r=[tag + '_W', xk], w=[pbk])
                                sg = stg[si % 4]; sgk = "%s_s%d" % (tag, si % 4); si += 1
                                evac(sg[:, 0:tn], pb[:, 0:tn], jb['func'], [pbk], [sgk])
                                orow = jb.get('oc0', 0) + m * 128
                                kb.dma('pool', lambda e: e.dma_start(out=jb['out'][orow:orow + 128, t0 - tofs:t0 - tofs + tn], in_=sg[:, 0:tn]),
                                       r=[sgk], w=[jb['okey']])
                        else:
                            for tt in range(tn // 128):
                                pb, pbk = banks[bi % 8]; bi += 1
                                for kk in range(kch):
                                    kb.op('pe', lambda e: e.matmul(pb[:, 0:cn], lhsT=xb[:, kk, tt * 128:(tt + 1) * 128], rhs=Wsb[:, kk, c0:c0 + cn],
                                                                   start=(kk == 0), stop=(kk == kch - 1)), r=[tag + '_W', xk], w=[pbk])
                                sg = stg[si % 4]; sgk = "%s_s%d" % (tag, si % 4); si += 1
                                evac(sg[:, 0:cn], pb[:, 0:cn], jb['func'], [pbk], [sgk])
                                tr = t0 - tofs + tt * 128
                                oc0 = jb.get('oc0', 0)
                                kb.dma('pool', lambda e: e.dma_start(out=jb['out'][tr:tr + 128, oc0:oc0 + cn], in_=sg[:, 0:cn]),
                                       r=[sgk], w=[jb['okey']])

        if stages >= 2:
            lat_blocks = [(t, 512) for t in range(0, L, 512)]
            all_blocks = lat_blocks + [(L, 256)]
            gemm_group("g1", WIN[:, 0:1024], 8, 1024, XNT, 'XNT', lat_blocks,
                       [dict(c0=0, cn=1024, mode='fm', func=None, out=HYRAW, okey='HYRAW', oc0=0)])
            gemm_group("g2", WIN[:, 1024:2048], 8, 1024, XNT, 'XNT', lat_blocks,
                       [dict(c0=0, cn=512, mode='fm', func=None, out=HYRAW, okey='HYRAW', oc0=1024),
                        dict(c0=512, cn=512, mode='fm', func=AF.Silu, out=QT, okey='QT', oc0=0)])
        if stages >= 3:
            gemm_group("g3", WIN[:, 2048:3072], 8, 1024, XNT, 'XNT', all_blocks,
                       [dict(c0=0, cn=512, mode='tm', func=None, out=IFF, okey='IFF', oc0=0),
                        dict(c0=512, cn=512, mode='tm', func=None, out=IFF, okey='IFF', oc0=512)])
            gemm_group("g4", WIN[:, 3072:4096], 8, 1024, XNT, 'XNT', all_blocks,
                       [dict(c0=0, cn=512, mode='tm', func=None, out=IFF, okey='IFF', oc0=1024),
                        dict(c0=512, cn=512, mode='fm', func=AF.Silu, out=GT, okey='GT', oc0=0, tsel=lambda t0: t0 < L)])

        if stages >= 3:
            own_blocks = [(t, 512) for t in range(0, OWN, 512)]
            for gi in range(2):
                gemm_group("g%d" % (5 + gi), WIN[:, 4096 + 1024 * gi:5120 + 1024 * gi], 8, 1024, XNTOWN, 'XNTOWN', own_blocks,
                           [dict(c0=0, cn=1024, mode='fm', func=AF.Sigmoid, out=SGT, okey='SGT', oc0=1024 * gi)])

        if stages >= 4:
            with stage() as st:
                cwT = sbt(st, "cwT", [128, 12, 4]); craw = sbt(st, "craw", [48, 128])
                for j3 in range(3):
                    kb.dma('sp', lambda e: e.dma_start(out=craw[12 * j3:12 * j3 + 12, :], in_=CONVW[j3].rearrange("(q p) -> q p", p=128)), r=['craw'], w=['craw'])
                kb.dma('sp', lambda e: e.dma_start(out=craw[36:48, :], in_=CONVB.rearrange("(q p) -> q p", p=128)), r=['craw'], w=['craw'])
                pb, pbk = banks[0]
                kb.op('pe', lambda e: e.transpose(out=pb[:, 0:48], in_=craw[:], identity=ident[0:48, 0:48]), r=['craw', 'ident'], w=[pbk])
                kb.op('dve', lambda e: e.tensor_copy(out=cwT[:], in_=pb[:, 0:48].rearrange("p (j q) -> p q j", j=4)), r=[pbk], w=['cwT'])
                uin = [sbt(st, "uin%d" % i, [128, L + 2]) for i in range(2)]
                uo = [sbt(st, "uo%d" % i, [128, L]) for i in range(2)]
                for i in range(2):
                    kb.op('pool', lambda e: e.memset(uin[i][:, 0:1], 0.0), w=['uin%d' % i])
                    kb.op('pool', lambda e: e.memset(uin[i][:, L + 1:L + 2], 0.0), r=['uin%d' % i], w=['uin%d' % i])
                for q in range(12):
                    b2 = q % 2; uk = 'uin%d' % b2; ok = 'uo%d' % b2
                    kb.dma('sp', lambda e: e.dma_start(out=uin[b2][:, 1:L + 1], in_=HYRAW[q * 128:(q + 1) * 128, :]), r=['HYRAW', uk], w=[uk])
                    kb.op('dve', lambda e: e.tensor_scalar(out=uo[b2][:], in0=uin[b2][:, 0:L], scalar1=cwT[:, q, 0:1], scalar2=cwT[:, q, 3:4],
                                                           op0=ALU.mult, op1=ALU.add), r=[uk, 'cwT'], w=[ok])
                    kb.op('dve', lambda e: e.scalar_tensor_tensor(out=uo[b2][:], in0=uin[b2][:, 1:L + 1], scalar=cwT[:, q, 1:2], in1=uo[b2][:],
                                                                  op0=ALU.mult, op1=ALU.add), r=[uk, 'cwT', ok], w=[ok])
                    kb.op('dve', lambda e: e.scalar_tensor_tensor(out=uo[b2][:], in0=uin[b2][:, 2:L + 2], scalar=cwT[:, q, 2:3], in1=uo[b2][:],
                                                                  op0=ALU.mult, op1=ALU.add), r=[uk, 'cwT', ok], w=[ok])
                    kb.dma('pool', lambda e: e.dma_start(out=HYC[q * 128:(q + 1) * 128, :], in_=uo[b2][:]), r=[ok], w=['HYC'])

        if stages >= 5 and not dummy_mix:
          with stage() as st:
            tri = {0: sbt(st, "trif", [128, 128]), 1: sbt(st, "trib", [128, 128])}
            ones = sbt(st, "ones", [128, 128])
            kb.dma('sp', lambda e: e.dma_start(out=tri[0][:], in_=TRIFd), w=['tri'])
            kb.dma('sp', lambda e: e.dma_start(out=tri[1][:], in_=TRIBd), r=['tri'], w=['tri'])
            kb.dma('sp', lambda e: e.dma_start(out=ones[:], in_=ONESd), w=['ones'])
            l0 = sbt(st, "l0", [128, 1024]); l1 = sbt(st, "l1", [128, 1024]); oml = sbt(st, "oml", [128, 1024])
            kb.dma('sp', lambda e: e.dma_start(out=l0[:], in_=LBL[0:1, :].to_broadcast([128, 1024])), w=['l0'])
            kb.dma('sp', lambda e: e.dma_start(out=l1[:], in_=LBL[1:2, :].to_broadcast([128, 1024])), w=['l1'])
            kb.op('dve', lambda e: e.tensor_tensor(out=l0[:], in0=l0[:], in1=l1[:], op=ALU.subtract), r=['l0', 'l1'], w=['l0'])
            kb.op('act', lambda e: e.activation(out=l0[:], in_=l0[:], func=AF.Sigmoid), r=['l0'], w=['l0'])
            kb.op('dve', lambda e: e.tensor_scalar(out=oml[:], in0=l0[:], scalar1=-1.0, scalar2=1.0, op0=ALU.mult, op1=ALU.add), r=['l0'], w=['oml'])
            ngT = sbt(st, "ngT", [128, 1])
            with nc.allow_non_contiguous_dma(reason="128-element vector to partitions"):
                kb.dma('sp', lambda e: e.dma_start(out=ngT[:], in_=HGNG.rearrange("(p o) -> p o", o=1)), w=['ngT'])
            osum = sbt(st, "osum", [128, L])
            LB8 = sbt(st, "LB8", [128, 8, 128]); OML8 = sbt(st, "OML8", [128, 8, 128])
            vseg = [sbt(st, "vseg%d" % i, [128, 8, 128]) for i in range(2)]
            fseg = [sbt(st, "fseg%d" % i, [128, 8, 128]) for i in range(2)]
            lseg = [sbt(st, "lseg%d" % i, [128, 8, 128]) for i in range(2)]
            kseg = [sbt(st, "kseg%d" % i, [128, 8, 128]) for i in range(2)]
            qseg = [sbt(st, "qseg%d" % i, [128, 1024]) for i in range(2)]
            R3 = 3
            ekb = [sbt(st, "ekb%d" % i, [128, 128]) for i in range(R3)]
            kd = [sbt(st, "kd%d" % i, [128, 128]) for i in range(R3)]
            ebT = [sbt(st, "ebT%d" % i, [128, 128]) for i in range(R3)]
            qdT = [sbt(st, "qdT%d" % i, [128, 128]) for i in range(R3)]
            kdT = [sbt(st, "kdT%d" % i, [128, 128]) for i in range(R3)]
            attm = [sbt(st, "attm%d" % i, [128, 128]) for i in range(R3)]
            Sr = [sbt(st, "S%d" % i, [128, 128]) for i in range(4)]
            Se = [sbt(st, "Se%d" % i, [128, 128]) for i in range(2)]
            pbi = [0]

            def nb():
                b_ = banks[pbi[0] % 8]; pbi[0] += 1
                return b_

            for h in range(4):
                for dr in range(2):
                    T = tri[dr]
                    fcol = 512 + 512 * dr + h * 128
                    for n in range(8):
                        kb.op('pool', lambda e: e.tensor_copy(out=LB8[:, n, :], in_=l0[:, dr * 512 + h * 128:dr * 512 + (h + 1) * 128]), r=['l0', 'LB8'], w=['LB8'])
                        kb.op('pool', lambda e: e.tensor_copy(out=OML8[:, n, :], in_=oml[:, dr * 512 + h * 128:dr * 512 + (h + 1) * 128]), r=['oml', 'OML8'], w=['OML8'])
                    si_ = 0
                    kb.op('pool', lambda e: e.memset(Sr[0][:], 0.0), w=['S0'])
                    segs = [(L, 2, False)] + [(sg * 1024, 8, True) for sg in (range(8) if dr == 0 else range(7, -1, -1))]
                    tcount = 0
                    for sgi, (r0, nt, lat) in enumerate(segs):
                        b2 = sgi % 2
                        vk, fk, lk, kk_, qk = 'vseg%d' % b2, 'fseg%d' % b2, 'lseg%d' % b2, 'kseg%d' % b2, 'qseg%d' % b2
                        kb.dma('sp', lambda e: e.dma_start(out=vseg[b2][:, 0:nt, :], in_=IFF[r0:r0 + nt * 128, h * 128:(h + 1) * 128].rearrange("(n p) c -> p n c", p=128)),
                               r=['IFF'], w=[vk])
                        kb.dma('sp', lambda e: e.dma_start(out=fseg[b2][:, 0:nt, :], in_=IFF[r0:r0 + nt * 128, fcol:fcol + 128].rearrange("(n p) c -> p n c", p=128)),
                               r=['IFF'], w=[fk])
                        if lat:
                            kb.dma('sp', lambda e: e.dma_start(out=qseg[b2][:], in_=QT[h * 128:(h + 1) * 128, r0:r0 + 1024]), r=['QT'], w=[qk])
                        kb.op('act', lambda e: e.activation(out=fseg[b2][:, 0:nt, :], in_=fseg[b2][:, 0:nt, :], func=AF.Sigmoid), r=[fk], w=[fk])
                        kb.op('dve', lambda e: e.tensor_tensor(out=fseg[b2][:, 0:nt, :], in0=fseg[b2][:, 0:nt, :], in1=OML8[:, 0:nt, :], op=ALU.mult), r=[fk, 'OML8'], w=[fk])
                        kb.op('dve', lambda e: e.tensor_tensor(out=fseg[b2][:, 0:nt, :], in0=fseg[b2][:, 0:nt, :], in1=LB8[:, 0:nt, :], op=ALU.add), r=[fk, 'LB8'], w=[fk])
                        kb.op('act', lambda e: e.activation(out=lseg[b2][:, 0:nt, :], in_=fseg[b2][:, 0:nt, :], func=AF.Ln), r=[fk], w=[lk])
                        kb.op('dve', lambda e: e.tensor_scalar(out=kseg[b2][:, 0:nt, :], in0=fseg[b2][:, 0:nt, :], scalar1=-1.0, scalar2=1.0, op0=ALU.mult, op1=ALU.add),
                              r=[fk], w=[kk_])
                        order = list(range(nt)) if dr == 0 else list(range(nt - 1, -1, -1))
                        slot = {}

                        def prep(n, b2=b2, lat=lat, lk=lk, kk_=kk_, qk=qk):
                            nonlocal tcount
                            r3 = tcount % R3; tcount += 1
                            slot[n] = r3
                            ek, kdk, ebk, qdk, ktk = 'ekb%d' % r3, 'kd%d' % r3, 'ebT%d' % r3, 'qdT%d' % r3, 'kdT%d' % r3
                            p1, p1k = nb()
                            kb.op('pe', lambda e: e.matmul(p1[:, 0:128], lhsT=T[:], rhs=lseg[b2][:, n, :], start=True, stop=True), r=['tri', lk], w=[p1k])
                            p2, p2k = nb()
                            kb.op('pe', lambda e: e.matmul(p2[:, 0:128], lhsT=lseg[b2][:, n, :], rhs=T[:], start=True, stop=True), r=['tri', lk], w=[p2k])
                            kb.op('act', lambda e: e.activation(out=ekb[r3][:], in_=p1[:, 0:128], func=AF.Exp, scale=-1.0), r=[p1k], w=[ek])
                            kb.op('act', lambda e: e.activation(out=ebT[r3][:], in_=p2[:, 0:128], func=AF.Exp), r=[p2k], w=[ebk])
                            kb.op('dve', lambda e: e.tensor_tensor(out=kd[r3][:], in0=kseg[b2][:, n, :], in1=ekb[r3][:], op=ALU.mult), r=[kk_, ek], w=[kdk])
                            if lat:
                                kb.op('dve', lambda e: e.tensor_tensor(out=qdT[r3][:], in0=qseg[b2][:, n * 128:(n + 1) * 128], in1=ebT[r3][:], op=ALU.mult), r=[qk, ebk], w=[qdk])
                                p3, p3k = nb()
                                kb.op('pe', lambda e: e.transpose(out=p3[:, 0:128], in_=kd[r3][:], identity=ident[:]), r=[kdk, 'ident'], w=[p3k])
                                kb.op('act', lambda e: e.copy(out=kdT[r3][:], in_=p3[:, 0:128]), r=[p3k], w=[ktk])

                        def scan(n, b2=b2, lat=lat, vk=vk, r0=r0):
                            nonlocal si_
                            r3 = slot[n]
                            kdk, ebk, qdk, ktk, amk = 'kd%d' % r3, 'ebT%d' % r3, 'qdT%d' % r3, 'kdT%d' % r3, 'attm%d' % r3
                            halves = [(0, 64, 63), (64, 128, 127)] if dr == 0 else [(64, 128, 64), (0, 64, 0)]
                            if lat:
                                p4, p4k = nb()
                                kb.op('pe', lambda e: e.matmul(p4[:, 0:128], lhsT=kdT[r3][:], rhs=qdT[r3][:], start=True, stop=True), r=[ktk, qdk], w=[p4k])
                                kb.op('dve', lambda e: e.tensor_tensor(out=attm[r3][:], in0=p4[:, 0:128], in1=T[:], op=ALU.mult), r=[p4k, 'tri'], w=[amk])
                            pds = []
                            for (c0, c1, ce) in halves:
                                pd, pdk = nb()
                                kb.op('pe', lambda e: e.matmul(pd[:, 0:128], lhsT=kd[r3][c0:c1, :], rhs=vseg[b2][c0:c1, n, :], start=True, stop=True), r=[kdk, vk], w=[pdk])
                                pds.append((pd, pdk))
                            Ss = []
                            for hi_, (c0, c1, ce) in enumerate(halves):
                                Sc = Sr[si_ % 4]; Sck = 'S%d' % (si_ % 4)
                                Sn = Sr[(si_ + 1) % 4]; Snk = 'S%d' % ((si_ + 1) % 4)
                                sew = Se[si_ % 2]; sek = 'Se%d' % (si_ % 2)
                                si_ += 1
                                Ss.append((Sc, Sck))
                                pd, pdk = pds[hi_]
                                kb.op('act', lambda e: e.activation(out=sew[:], in_=Sc[:], func=AF.Copy, scale=ebT[r3][:, ce:ce + 1]), r=[Sck, ebk], w=[sek])
                                kb.op('dve', lambda e: e.scalar_tensor_tensor(out=Sn[:], in0=pd[:, 0:128], scalar=ebT[r3][:, ce:ce + 1], in1=sew[:], op0=ALU.mult, op1=ALU.add),
                                      r=[pdk, ebk, sek], w=[Snk])
                            if lat:
                                po, pok = nb()
                                kb.op('pe', lambda e: e.matmul(po[:, 0:128], lhsT=vseg[b2][:, n, :], rhs=attm[r3][:], start=True, stop=False), r=[vk, amk], w=[pok])
                                for hi_, (c0, c1, ce) in enumerate(halves):
                                    Sc, Sck = Ss[hi_]
                                    kb.op('pe', lambda e: e.matmul(po[:, c0:c1], lhsT=Sc[:], rhs=qdT[r3][:, c0:c1], start=False, stop=True), r=[Sck, qdk], w=[pok])
                                cols = slice(r0 + n * 128, r0 + (n + 1) * 128)
                                if dr == 0:
                                    kb.op('act', lambda e: e.copy(out=osum[:, cols], in_=po[:, 0:128]), r=[pok], w=['osum%d' % (r0 // 1024)])
                                else:
                                    kb.op('dve', lambda e: e.tensor_tensor(out=osum[:, cols], in0=po[:, 0:128], in1=osum[:, cols], op=ALU.add),
                                          r=[pok, 'osum%d' % (r0 // 1024)], w=['osum%d' % (r0 // 1024)])

                        prep(order[0])
                        for oi, n in enumerate(order):
                            if oi + 1 < len(order):
                                prep(order[oi + 1])
                            scan(n)
                    if si_ % 4 != 0:
                        pass
                    kb.res.pop('unused', None)
                sq = [sbt(st, "hsq%d_%d" % (h, i), [128, 512]) for i in range(2)] if h == 0 else sq
                gt_ = [sbt(st, "hgt%d_%d" % (h, i), [128, 512]) for i in range(2)] if h == 0 else gt_
                rs = [sbt(st, "hrs%d_%d" % (h, i), [128, 512]) for i in range(2)] if h == 0 else rs
                for blk in range(L // 512):
                    b2 = blk % 2; cols = slice(blk * 512, (blk + 1) * 512)
                    sqk, gk, rk = 'hsq%d' % b2, 'hgt%d' % b2, 'hrs%d' % b2
                    ok_ = 'osum%d' % (blk // 2)
                    kb.dma('sp', lambda e: e.dma_start(out=gt_[b2][:], in_=GT[h * 128:(h + 1) * 128, cols]), r=['GT'], w=[gk])
                    kb.op('act', lambda e: e.activation(out=sq[b2][:], in_=osum[:, cols], func=AF.Square), r=[ok_], w=[sqk])
                    pb, pbk = nb()
                    kb.op('pe', lambda e: e.matmul(pb[:, :], lhsT=ones[:], rhs=sq[b2][:], start=True, stop=True), r=['ones', sqk], w=[pbk])
                    kb.op('dve', lambda e: e.tensor_scalar(out=rs[b2][:], in0=pb[:, :], scalar1=1.0 / 128, scalar2=EPS, op0=ALU.mult, op1=ALU.add), r=[pbk], w=[rk])
                    kb.op('act', lambda e: e.activation(out=rs[b2][:], in_=rs[b2][:], func=AF.Sqrt), r=[rk], w=[rk])
                    kb.op('dve', lambda e: e.reciprocal(out=rs[b2][:], in_=rs[b2][:]), r=[rk], w=[rk])
                    kb.op('dve', lambda e: e.scalar_tensor_tensor(out=sq[b2][:], in0=osum[:, cols], scalar=ngT[:, 0:1], in1=rs[b2][:], op0=ALU.mult, op1=ALU.mult),
                          r=[ok_, 'ngT', rk, sqk], w=[sqk])
                    kb.op('pool', lambda e: e.tensor_tensor(out=sq[b2][:], in0=sq[b2][:], in1=gt_[b2][:], op=ALU.mult), r=[sqk, gk], w=[sqk])
                    kb.dma('pool', lambda e: e.dma_start(out=YHGT[h * 128:(h + 1) * 128, cols], in_=sq[b2][:]), r=[sqk], w=['YHGT'])

        if stages >= 5 and not dummy_mix:
          with stage() as st:
            cst = {}
            for nm, src, shp in (("FA", FAd, [128, 256]), ("FAH", FAHd, [64, 256]), ("FRE", FREd, [128, 128]), ("FIM", FIMd, [128, 128]), ("NFIM", NFIMd, [128, 128]),
                                 ("CA", CAd, [128, 256]), ("CB", CBd, [128, 256]), ("FREN", FRENd, [128, 64]), ("FIMN", FIMNd, [128, 64]),
                                 ("TRE2", TRE2d, [128, 256]), ("TIM2", TIM2d, [128, 256]), ("ONESH", ONESd, [128, 128])):
                cst[nm] = sbt(st, "c_" + nm, shp)
                kb.dma('sp', lambda e: e.dma_start(out=cst[nm][:], in_=src), w=['hconst'])
            skb = sbt(st, "skb", [128, 1024])
            kb.dma('sp', lambda e: e.dma_start(out=skb[:], in_=HSKIP.rearrange("(o n) -> o n", o=1).to_broadcast([128, 1024])), w=['skb'])
            tw = [sbt(st, "tw%d" % i, [128, 256]) for i in range(8)]
            hbi = [0]

            def hb():
                b_ = banks[hbi[0] % 8]; hbi[0] += 1
                return b_

            def twiddle(ps, psk, Bt, bkey, c0, conj):
                A = ps[:, :].rearrange("p (c r f) -> p c r f", c=2, r=2)
                Are, Aim = A[:, :, 0, :], A[:, :, 1, :]
                T2r = cst["TRE2"][:].rearrange("p (c f) -> p c f", c=2); T2i = cst["TIM2"][:].rearrange("p (c f) -> p c f", c=2)
                par = (c0 // 2) % 2
                t = [tw[4 * par + i][:].rearrange("p (c f) -> p c f", c=2) for i in range(4)]
                tk = ['tw%d' % (4 * par + i) for i in range(4)]
                bkey = '%sq%d' % (bkey, c0 // 4)
                kb.op('dve', lambda e: e.tensor_tensor(out=t[0], in0=Are, in1=T2r, op=ALU.mult), r=[psk, 'hconst', tk[0]], w=[tk[0]])
                kb.op('dve', lambda e: e.tensor_tensor(out=t[1], in0=Aim, in1=T2i, op=ALU.mult), r=[psk, 'hconst', tk[1]], w=[tk[1]])
                kb.op('dve', lambda e: e.tensor_tensor(out=t[2], in0=Are, in1=T2i, op=ALU.mult), r=[psk, 'hconst', tk[2]], w=[tk[2]])
                kb.op('dve', lambda e: e.tensor_tensor(out=t[3], in0=Aim, in1=T2r, op=ALU.mult), r=[psk, 'hconst', tk[3]], w=[tk[3]])
                if not conj:
                    kb.op('pool', lambda e: e.tensor_tensor(out=Bt[:, c0:c0 + 2, 0, :], in0=t[0], in1=t[1], op=ALU.subtract), r=[tk[0], tk[1], bkey], w=[bkey])
                    kb.op('pool', lambda e: e.tensor_tensor(out=Bt[:, c0:c0 + 2, 1, :], in0=t[2], in1=t[3], op=ALU.add), r=[tk[2], tk[3], bkey], w=[bkey])
                else:
                    kb.op('pool', lambda e: e.tensor_tensor(out=Bt[:, c0:c0 + 2, 0, :], in0=t[0], in1=t[1], op=ALU.add), r=[tk[0], tk[1], bkey], w=[bkey])
                    kb.op('pool', lambda e: e.tensor_tensor(out=Bt[:, c0:c0 + 2, 1, :], in0=t[3], in1=t[2], op=ALU.subtract), r=[tk[2], tk[3], bkey], w=[bkey])

            def stage2(Bt, bkey, c4):
                pr, prk = hb(); pi_, pik = hb()
                bkey = '%sq%d' % (bkey, c4 // 4)
                Bre, Bim = Bt[:, c4:c4 + 4, 0, :], Bt[:, c4:c4 + 4, 1, :]
                kb.op('pe', lambda e: e.matmul(pr[:, :], lhsT=cst["FRE"][:], rhs=Bre, start=True, stop=False), r=['hconst', bkey], w=[prk])
                kb.op('pe', lambda e: e.matmul(pr[:, :], lhsT=cst["NFIM"][:], rhs=Bim, start=False, stop=True), r=['hconst', bkey], w=[prk])
                kb.op('pe', lambda e: e.matmul(pi_[:, :], lhsT=cst["FIM"][:], rhs=Bre, start=True, stop=False), r=['hconst', bkey], w=[pik])
                kb.op('pe', lambda e: e.matmul(pi_[:, :], lhsT=cst["FRE"][:], rhs=Bim, start=False, stop=True), r=['hconst', bkey], w=[pik])
                return pr, prk, pi_, pik

            B16 = sbt(st, "B16", [128, 16, 2, 128])
            with stage() as sf:
                A3Z = sbt(sf, "A3Z", [128, 2 * L])
                kb.op('pool', lambda e: e.memset(A3Z[0:64, L:2 * L], 0.0), w=['A3Z'])
                kb.op('pool', lambda e: e.memset(A3Z[64:128, 0:L], 0.0), r=['A3Z'], w=['A3Z'])
                w4sb = sbt(sf, "w4sb", [128, 2048])
                kb.dma('sp', lambda e: e.dma_start(out=w4sb[0:64, :], in_=FW4), w=['w4sb'])
                kb.dma('sp', lambda e: e.dma_start(out=w4sb[64:128, :], in_=FW4), r=['w4sb'], w=['w4sb'])
                with stage() as sm:
                    wl = [sbt(sm, "w1bd", [66, 128]), sbt(sm, "w2bd", [128, 128]), sbt(sm, "w3bd", [128, 128])]
                    for i_, (wt, src, kin) in enumerate(zip(wl, (FW1, FW2, FW3), (33, 64, 64))):
                        kb.op('pool', lambda e: e.memset(wt[:], 0.0), w=['wbd%d' % i_])
                        kb.dma('sp', lambda e: e.dma_start(out=wt[0:kin, 0:64], in_=src), r=['wbd%d' % i_], w=['wbd%d' % i_])
                        kb.dma('sp', lambda e: e.dma_start(out=wt[kin:2 * kin, 64:128], in_=src), r=['wbd%d' % i_], w=['wbd%d' % i_])
                    fqb = sbt(sm, "fqb", [128, 4])
                    with nc.allow_non_contiguous_dma(reason="64-element vectors onto partitions"):
                        for j_, src in enumerate((FFQ, FB1, FB2, FB3)):
                            for hh in range(2):
                                kb.dma('sp', lambda e: e.dma_start(out=fqb[64 * hh:64 * hh + 64, j_:j_ + 1], in_=src.rearrange("(p o) -> p o", o=1)), r=['fqb'], w=['fqb'])
                    kb.op('dve', lambda e: e.tensor_scalar(out=fqb[:, 0:1], in0=fqb[:, 0:1], scalar1=1.0 / TWO_PI, scalar2=None, op0=ALU.mult), r=['fqb'], w=['fqb'])
                    kb.op('dve', lambda e: e.tensor_scalar(out=fqb[:, 1:4], in0=fqb[:, 1:4], scalar1=fqb[:, 0:1], scalar2=None, op0=ALU.mult), r=['fqb'], w=['fqb'])
                    zc = [sbt(sm, "zc%d" % i, [66, 2048]) for i in range(2)]
                    hid = [sbt(sm, "hid%d" % i, [128, 2048]) for i in range(2)]
                    uu = sbt(sm, "uu", [128, 2048]); ui = sbt(sm, "ui", [128, 2048], I32); uf = sbt(sm, "uf", [128, 2048])
                    pi_ = 0
                    for ch in range(4):
                        zk = 'zc%d' % (ch % 2)
                        kb.dma('sp', lambda e: e.dma_start(out=zc[ch % 2][:], in_=ZTd[:, ch * 2048:(ch + 1) * 2048]), w=[zk])
                        src_t, kdim, srck = zc[ch % 2], 66, zk
                        for l_ in range(3):
                            Pq, pkeys = (PA, ["PA0", "PA1", "PA2", "PA3"]) if pi_ % 2 == 0 else (PB, ["PB0", "PB1", "PB2", "PB3"])
                            pi_ += 1
                            for q in range(4):
                                kb.op('pe', lambda e: e.matmul(Pq[:, q * 512:(q + 1) * 512], lhsT=wl[l_][0:kdim, :], rhs=src_t[0:kdim, q * 512:(q + 1) * 512], start=True, stop=True),
                                      r=['wbd%d' % l_, srck], w=[pkeys[q]])
                            kb.op('act', lambda e: e.activation(out=uu[:], in_=Pq[:, :], func=AF.Identity, bias=fqb[:, 1 + l_:2 + l_], scale=fqb[:, 0:1]), r=pkeys + ['fqb'], w=['uu'])
                            kb.op('dve', lambda e: e.tensor_copy(out=ui[:], in_=uu[:]), r=['uu'], w=['ui'])
                            kb.op('dve', lambda e: e.tensor_copy(out=uf[:], in_=ui[:]), r=['ui'], w=['uf'])
                            kb.op('pool', lambda e: e.tensor_tensor(out=uu[:], in0=uu[:], in1=uf[:], op=ALU.subtract), r=['uu', 'uf'], w=['uu'])
                            kb.op('dve', lambda e: e.scalar_tensor_tensor(out=uf[:], in0=uu[:], scalar=0.5, in1=uu[:], op0=ALU.is_gt, op1=ALU.subtract), r=['uu', 'uf'], w=['uf'])
                            kb.op('dve', lambda e: e.scalar_tensor_tensor(out=uu[:], in0=uf[:], scalar=0.5, in1=uf[:], op0=ALU.is_gt, op1=ALU.subtract), r=['uu', 'uf'], w=['uu'])
                            if l_ < 2:
                                hk = 'hid%d' % l_
                                kb.op('act', lambda e: e.activation(out=hid[l_][:], in_=uu[:], func=AF.Sin, scale=6.283185), r=['uu', hk], w=[hk])
                                src_t, kdim, srck = hid[l_], 128, hk
                            else:
                                kb.op('act', lambda e: e.activation(out=A3Z[0:64, ch * 2048:(ch + 1) * 2048], in_=uu[0:64, :], func=AF.Sin, scale=6.283185), r=['uu', 'A3Z'], w=['A3Z'])
                                kb.op('act', lambda e: e.activation(out=A3Z[64:128, L + ch * 2048:L + (ch + 1) * 2048], in_=uu[64:128, :], func=AF.Sin, scale=6.283185), r=['uu', 'A3Z'], w=['A3Z'])
                Kt = sbt(sf, "Kt", [128, 64, 128])
                dec = sbt(sf, "dec", [128, 64, 128]); rab = sbt(sf, "rab", [128, 64]); rn = sbt(sf, "rn", [128, 64]); Hst = sbt(sf, "Hst", [128, 16, 2, 128])
                w4c = sbt(sf, "w4c", [128, 64])
                DEC2 = DECd.rearrange("h n c f -> (h n) c f")
                for o in range(2):
                    for cg in range(8):
                        kb.op('pool', lambda e: e.tensor_copy(out=w4c[0:64, :], in_=w4sb[0:64, o * 1024 + cg * 64:o * 1024 + cg * 64 + 64]), r=['w4sb', 'w4c'], w=['w4c'])
                        kb.op('pool', lambda e: e.tensor_copy(out=w4c[64:128, :], in_=w4sb[64:128, o * 1024 + 512 + cg * 64:o * 1024 + 512 + cg * 64 + 64]), r=['w4sb', 'w4c'], w=['w4c'])
                        kb.dma('sp', lambda e: e.dma_start(out=dec[:], in_=DEC2[:, cg * 64:(cg + 1) * 64, :]), r=['dec'], w=['dec'])
                        for nbk in range(16):
                            ps, psk = hb()
                            for j_ in range(8):
                                n2 = nbk * 8 + j_
                                kb.op('pe', lambda e: e.matmul(ps[:, j_ * 64:(j_ + 1) * 64], lhsT=A3Z[:, n2:2 * L:128], rhs=w4c[:], start=True, stop=True), r=['A3Z', 'w4c'], w=[psk])
                            evac(Kt[:, :, nbk * 8:(nbk + 1) * 8], ps[:, :].rearrange("p (n c) -> p c n", c=64), None, [psk, 'Kt'], ['Kt'])
                        kb.op('pool', lambda e: e.tensor_tensor(out=Kt[:], in0=Kt[:], in1=dec[:], op=ALU.mult), r=['Kt', 'dec'], w=['Kt'])
                        kb.op('dve', lambda e: e.tensor_reduce(out=rab[:], in_=Kt[:], axis=AX.X, op=ALU.add, apply_absolute_value=True), r=['Kt', 'rab'], w=['rab'])
                        ps, psk = hb()
                        kb.op('pe', lambda e: e.matmul(ps[:, 0:64], lhsT=cst["ONESH"][:], rhs=rab[:], start=True, stop=True), r=['hconst', 'rab'], w=[psk])
                        kb.op('dve', lambda e: e.reciprocal(out=rn[:], in_=ps[:, 0:64]), r=[psk], w=['rn'])
                        for sb4 in range(4):
                            for c2 in range(8):
                                ps, psk = hb()
                                for j_ in range(2):
                                    cc = sb4 * 16 + c2 * 2 + j_
                                    kb.op('pe', lambda e: e.matmul(ps[:, j_ * 256:(j_ + 1) * 256], lhsT=Kt[:, cc, :], rhs=cst["FA"][:], start=True, stop=True), r=['Kt', 'hconst'], w=[psk])
                                twiddle(ps, psk, B16, 'B16', c2 * 2, False)
                            for c4 in range(0, 16, 4):
                                pr, prk, pi_, pik = stage2(B16, 'B16', c4)
                                for j_ in range(4):
                                    cc = sb4 * 16 + c4 + j_; gc = cg * 64 + cc
                                    kb.op('dve', lambda e: e.tensor_scalar(out=Hst[:, c4 + j_, 0, :], in0=pr[:, j_ * 128:(j_ + 1) * 128], scalar1=rn[:, cc:cc + 1], scalar2=skb[:, o * 512 + gc:o * 512 + gc + 1],
                                                                           op0=ALU.mult, op1=ALU.add), r=[prk, 'rn', 'skb', 'Hst'], w=['Hst'])
                                    kb.op('act', lambda e: e.activation(out=Hst[:, c4 + j_, 1, :], in_=pi_[:, j_ * 128:(j_ + 1) * 128], func=AF.Copy, scale=rn[:, cc:cc + 1]), r=[pik, 'rn', 'Hst'], w=['Hst'])
                            g0 = o * 512 + cg * 64 + sb4 * 16
                            kb.dma('sp', lambda e: e.dma_start(out=HSPEC[g0:g0 + 16].rearrange("c k r f -> k c r f"), in_=Hst[:]), r=['Hst'], w=['HSPEC'])
            with stage() as sc:
                v16 = sbt(sc, "v16", [64, 16, 128]); x116 = sbt(sc, "x116", [64, 16, 128]); x216 = sbt(sc, "x216", [64, 16, 128]); z16 = sbt(sc, "z16", [64, 16, 128])
                y16 = sbt(sc, "y16", [64, 16, 128])
                H1 = sbt(sc, "H1", [128, 16, 2, 128]); H2s = sbt(sc, "H2s", [128, 16, 2, 128]); Y16 = sbt(sc, "Y16", [128, 16, 2, 128]); G16 = sbt(sc, "G16", [128, 16, 2, 128])
                hm = [sbt(sc, "hm%d" % i, [128, 4, 128]) for i in range(8)]

                def conv16(Din, dkey, Hs, hkey, Xmul, xkey, Out, okey):
                    for c2 in range(8):
                        ps, psk = hb()
                        for j_ in range(2):
                            kb.op('pe', lambda e: e.matmul(ps[:, j_ * 256:(j_ + 1) * 256], lhsT=Din[:, c2 * 2 + j_, :], rhs=cst["FA"][0:64, :], start=True, stop=True), r=[dkey, 'hconst'], w=[psk])
                        twiddle(ps, psk, B16, 'B16', c2 * 2, False)
                    for c4 in range(0, 16, 4):
                        pr, prk, pi_, pik = stage2(B16, 'B16', c4)
                        Xr = pr[:, :].rearrange("p (c f) -> p c f", c=4); Xi = pi_[:, :].rearrange("p (c f) -> p c f", c=4)
                        Hr, Hi = Hs[:, c4:c4 + 4, 0, :], Hs[:, c4:c4 + 4, 1, :]
                        hp = 4 * ((c4 // 4) % 2)
                        hmk = ['hm%d' % (hp + i) for i in range(4)]
                        yk_ = 'Y16q%d' % (c4 // 4)
                        kb.op('dve', lambda e: e.tensor_tensor(out=hm[hp + 0][:], in0=Xr, in1=Hr, op=ALU.mult), r=[prk, hkey, hmk[0]], w=[hmk[0]])
                        kb.op('dve', lambda e: e.tensor_tensor(out=hm[hp + 1][:], in0=Xi, in1=Hi, op=ALU.mult), r=[pik, hkey, hmk[1]], w=[hmk[1]])
                        kb.op('dve', lambda e: e.tensor_tensor(out=hm[hp + 2][:], in0=Xr, in1=Hi, op=ALU.mult), r=[prk, hkey, hmk[2]], w=[hmk[2]])
                        kb.op('dve', lambda e: e.tensor_tensor(out=hm[hp + 3][:], in0=Xi, in1=Hr, op=ALU.mult), r=[pik, hkey, hmk[3]], w=[hmk[3]])
                        kb.op('pool', lambda e: e.tensor_tensor(out=Y16[:, c4:c4 + 4, 0, :], in0=hm[hp + 0][:], in1=hm[hp + 1][:], op=ALU.subtract), r=[hmk[0], hmk[1], yk_], w=[yk_])
                        kb.op('pool', lambda e: e.tensor_tensor(out=Y16[:, c4:c4 + 4, 1, :], in0=hm[hp + 2][:], in1=hm[hp + 3][:], op=ALU.add), r=[hmk[2], hmk[3], yk_], w=[yk_])
                    for c2 in range(8):
                        ps, psk = hb()
                        for j_ in range(2):
                            cc = c2 * 2 + j_
                            kb.op('pe', lambda e: e.matmul(ps[:, j_ * 256:(j_ + 1) * 256], lhsT=Y16[:, cc, 0, :], rhs=cst["CA"][:], start=True, stop=False), r=['Y16q%d' % (cc // 4), 'hconst'], w=[psk])
                            kb.op('pe', lambda e: e.matmul(ps[:, j_ * 256:(j_ + 1) * 256], lhsT=Y16[:, cc, 1, :], rhs=cst["CB"][:], start=False, stop=True), r=['Y16q%d' % (cc // 4), 'hconst'], w=[psk])
                        twiddle(ps, psk, G16, 'G16', c2 * 2, True)
                    for c4 in range(0, 16, 4):
                        py, pyk = hb()
                        kb.op('pe', lambda e: e.matmul(py[0:64, :], lhsT=cst["FREN"][:], rhs=G16[:, c4:c4 + 4, 0, :], start=True, stop=False), r=['hconst', 'G16q%d' % (c4 // 4)], w=[pyk])
                        kb.op('pe', lambda e: e.matmul(py[0:64, :], lhsT=cst["FIMN"][:], rhs=G16[:, c4:c4 + 4, 1, :], start=False, stop=True), r=['hconst', 'G16q%d' % (c4 // 4)], w=[pyk])
                        kb.op('dve', lambda e: e.tensor_tensor(out=Out[:, c4:c4 + 4, :], in0=py[0:64, :].rearrange("p (c f) -> p c f", c=4), in1=Xmul[:, c4:c4 + 4, :], op=ALU.mult),
                              r=[pyk, xkey, okey], w=[okey])

                for g in range(32):
                    gc0 = g * 16
                    lh = lambda r0: HYC[r0 + gc0:r0 + gc0 + 16, :].rearrange("c (n1 n2) -> n1 c n2", n2=128)
                    kb.dma('sp', lambda e: e.dma_start(out=v16[:], in_=lh(0)), r=['HYC'], w=['v16'])
                    kb.dma('sp', lambda e: e.dma_start(out=x116[:], in_=lh(512)), r=['HYC'], w=['x116'])
                    kb.dma('sp', lambda e: e.dma_start(out=x216[:], in_=lh(1024)), r=['HYC'], w=['x216'])
                    kb.dma('sp', lambda e: e.dma_start(out=H1[:], in_=HSPEC[gc0:gc0 + 16].rearrange("c k r f -> k c r f")), r=['HSPEC'], w=['H1'])
                    kb.dma('sp', lambda e: e.dma_start(out=H2s[:], in_=HSPEC[512 + gc0:512 + gc0 + 16].rearrange("c k r f -> k c r f")), r=['HSPEC'], w=['H2s'])
                    conv16(v16, 'v16', H1, 'H1', x116, 'x116', z16, 'z16')
                    conv16(z16, 'z16', H2s, 'H2s', x216, 'x216', y16, 'y16')
                    kb.dma('pool', lambda e: e.dma_start(out=YHYT[gc0:gc0 + 16, :].rearrange("c (n1 n2) -> n1 c n2", n2=128), in_=y16[:]), r=['y16'], w=['YHYT'])

        if stages >= 5:
            with stage() as st:
                z = sbt(st, "zt", [128, L])
                kb.op('pool', lambda e: e.memset(z[:], 0.0), w=['zt'])
                for q in range(4):
                    if dummy_mix:
                        kb.dma('sp', lambda e: e.dma_start(out=z[:], in_=QT[q * 128:(q + 1) * 128, :]), r=['zt', 'QT'], w=['zt'])
                    if dummy_mix:
                        kb.dma('sp', lambda e: e.dma_start(out=YHYT[q * 128:(q + 1) * 128, :], in_=z[:]), r=['zt'], w=['YHYT'])
                    if dummy_mix:
                        kb.dma('sp', lambda e: e.dma_start(out=z[:], in_=GT[q * 128:(q + 1) * 128, :]), r=['zt', 'GT'], w=['zt'])
                    if dummy_mix:
                        kb.dma('sp', lambda e: e.dma_start(out=YHGT[q * 128:(q + 1) * 128, :], in_=z[:]), r=['zt'], w=['YHGT'])
                zr = sbt(st, "zr", [128, D])
                kb.op('pool', lambda e: e.memset(zr[:], 0.0), w=['zr'])
                if dummy_mix or stages < 7:
                    for i in range(OWN // 128):
                        kb.dma('sp', lambda e: e.dma_start(out=ROUTED[i * 128:(i + 1) * 128, :], in_=zr[:]), r=['zr'], w=['ROUTED'])

        def row_bcast(stack, name, src_row_ap):
            t = sbt(stack, name, [128, D])
            kb.dma('sp', lambda e: e.dma_start(out=t[:], in_=src_row_ap.to_broadcast([128, D])), r=['MODROW'], w=[name])
            return t

        if stages >= 5:
            with stage() as st:
                oidx = sbt(st, "oidx", [128, 4], I32)
                kb.dma('sp', lambda e: e.dma_start(out=oidx[:], in_=OWNIDX), w=['oidx'])
                yg = [sbt(st, "yg%d" % i, [128, OWN]) for i in range(2)]
                n = 0
                for src, skey, r0 in ((YHYT, 'YHYT', 0), (YHGT, 'YHGT', 512)):
                    v = src.rearrange("c (j t) -> (c j) t", j=4)
                    for cc in range(4):
                        g = yg[n % 2]; gk = "yg%d" % (n % 2); n += 1
                        kb.dma('pool', lambda e: e.indirect_dma_start(out=g[:], out_offset=None, in_=v,
                                                                     in_offset=bass.IndirectOffsetOnAxis(ap=oidx[:, cc:cc + 1], axis=0),
                                                                     bounds_check=RB_OWN, oob_is_err=False), r=[skey, 'oidx'], w=[gk])
                        kb.dma('sp', lambda e: e.dma_start(out=YOWN[r0 + cc * 128:r0 + (cc + 1) * 128, :], in_=g[:]), r=[gk], w=['YOWN'])
            with stage() as st:
                Wy = sbt(st, "Wy", [128, 8, D]); Wo = sbt(st, "Wo", [128, 8, D])
                kb.dma('sp', lambda e: e.dma_start(out=Wy[:, 0:4, :], in_=WHY.rearrange("(k p) c -> p k c", p=128)), w=['Wy'])
                kb.dma('sp', lambda e: e.dma_start(out=Wy[:, 4:8, :], in_=WHG.rearrange("(k p) c -> p k c", p=128)), r=['Wy'], w=['Wy'])
                kb.dma('sp', lambda e: e.dma_start(out=Wo[:], in_=WOUT.rearrange("(k p) c -> p k c", p=128)), w=['Wo'])
                g1row = row_bcast(st, "g1row", MODROW[0:1, 2048:3072])
                yb = sbt(st, "yb", [128, 8, 512]); sgb = sbt(st, "sgb", [128, 16, 512]); mT = sbt(st, "mT", [128, 8, 512])
                t1 = [sbt(st, "t1_%d" % i, [128, 512]) for i in range(2)]
                xt = [sbt(st, "xt%d" % i, [128, D]) for i in range(2)]; pt = [sbt(st, "pt%d" % i, [128, D]) for i in range(2)]
                bi = 0
                for blk in range(OWN // 512):
                    t0 = blk * 512
                    kb.dma('sp', lambda e: e.dma_start(out=yb[:], in_=YOWN[:, t0:t0 + 512].rearrange("(k p) t -> p k t", p=128)), r=['YOWN'], w=['yb'])
                    kb.dma('sp', lambda e: e.dma_start(out=sgb[:], in_=SGT[:, t0:t0 + 512].rearrange("(k p) t -> p k t", p=128)), r=['SGT'], w=['sgb'])
                    for dm in range(8):
                        for br in range(2):
                            pb, pbk = banks[bi % 8]; bi += 1
                            for cc in range(4):
                                kb.op('pe', lambda e: e.matmul(pb[:, :], lhsT=Wy[:, br * 4 + cc, dm * 128:(dm + 1) * 128], rhs=yb[:, br * 4 + cc, :],
                                                               start=(cc == 0), stop=(cc == 3)), r=['Wy', 'yb'], w=[pbk])
                            if br == 0:
                                kb.op('dve', lambda e: e.tensor_tensor(out=t1[dm % 2][:], in0=pb[:, :], in1=sgb[:, dm, :], op=ALU.mult),
                                      r=[pbk, 'sgb'], w=['t1_%d' % (dm % 2)])
                            else:
                                kb.op('dve', lambda e: e.tensor_tensor(out=mT[:, dm, :], in0=pb[:, :], in1=sgb[:, 8 + dm, :], op=ALU.mult),
                                      r=[pbk, 'sgb', 'mT'], w=['mT'])
                                kb.op('pool', lambda e: e.tensor_tensor(out=mT[:, dm, :], in0=mT[:, dm, :], in1=t1[dm % 2][:], op=ALU.add),
                                      r=['mT', 't1_%d' % (dm % 2)], w=['mT'])
                    for tt in range(4):
                        ti = blk * 4 + tt; b2 = ti % 2
                        kb.dma('sp', lambda e: e.dma_start(out=xt[b2][:], in_=XOWN[ti * 128:(ti + 1) * 128, :]), w=['xt%d' % b2])
                        kb.dma('sp', lambda e: e.dma_start(out=pt[b2][:], in_=POSOWN[ti * 128:(ti + 1) * 128, :]), w=['pt%d' % b2])
                        kb.op('pool', lambda e: e.tensor_tensor(out=xt[b2][:], in0=xt[b2][:], in1=pt[b2][:], op=ALU.add), r=['xt%d' % b2, 'pt%d' % b2], w=['xt%d' % b2])
                        for hf in range(2):
                            pb, pbk = banks[bi % 8]; bi += 1
                            for kk in range(8):
                                kb.op('pe', lambda e: e.matmul(pb[:, :], lhsT=mT[:, kk, tt * 128:(tt + 1) * 128], rhs=Wo[:, kk, hf * 512:(hf + 1) * 512],
                                                               start=(kk == 0), stop=(kk == 7)), r=['mT', 'Wo'], w=[pbk])
                            kb.op('dve', lambda e: e.tensor_tensor(out=pt[b2][:, hf * 512:(hf + 1) * 512], in0=pb[:, :], in1=g1row[:, hf * 512:(hf + 1) * 512], op=ALU.mult),
                                  r=[pbk, 'g1row', 'pt%d' % b2], w=['pt%d' % b2])
                        kb.op('pool', lambda e: e.tensor_tensor(out=xt[b2][:], in0=xt[b2][:], in1=pt[b2][:], op=ALU.add), r=['xt%d' % b2, 'pt%d' % b2], w=['xt%d' % b2])
                        kb.dma('pool', lambda e: e.dma_start(out=X1D[ti * 128:(ti + 1) * 128, :], in_=xt[b2][:]), r=['xt%d' % b2], w=['X1D'])

        if stages >= 6:
            a2T = sbt(es, "a2T", [128, 8]); sh2T = sbt(es, "sh2T", [128, 8])
            kb.op('dve', lambda e: e.tensor_scalar(out=a2T[:], in0=modT[:, 32:40, 0], scalar1=1.0, scalar2=None, op0=ALU.add), r=['modT'], w=['a2T'])
            kb.op('dve', lambda e: e.tensor_tensor(out=a2T[:], in0=a2T[:], in1=g2T[:], op=ALU.mult), r=['a2T', 'g2T'], w=['a2T'])
            kb.op('dve', lambda e: e.tensor_copy(out=sh2T[:], in_=modT[:, 24:32, 0]), r=['modT'], w=['sh2T'])
            with stage() as s1:
                srcs = [(X1D[i * 128:(i + 1) * 128, :], None, i * 128) for i in range(OWN // 128)]
                kb.res.setdefault('a1T', [None, []])
                norm_to_xt(s1, srcs, H2T, a2T, sh2T, "n2", 'H2T')
            own_blocks = [(t, 512) for t in range(0, OWN, 512)]
            gemm_group("sg", SHG, 8, 256, H2T, 'H2T', own_blocks, [dict(c0=0, cn=256, mode='fm', func=AF.Silu, out=SGA, okey='SGA', oc0=0)])
            gemm_group("su", SHU, 8, 256, H2T, 'H2T', own_blocks, [dict(c0=0, cn=256, mode='fm', func=None, out=SUA, okey='SUA', oc0=0)])
            with stage() as st:
                ga = sbt(st, "ga", [128, 2, OWN]); ua = sbt(st, "ua", [128, 2, OWN])
                kb.dma('sp', lambda e: e.dma_start(out=ga[:], in_=SGA.rearrange("(k p) t -> p k t", p=128)), r=['SGA'], w=['ga'])
                kb.dma('sp', lambda e: e.dma_start(out=ua[:], in_=SUA.rearrange("(k p) t -> p k t", p=128)), r=['SUA'], w=['ua'])
                kb.op('dve', lambda e: e.tensor_tensor(out=ga[:], in0=ga[:], in1=ua[:], op=ALU.mult), r=['ga', 'ua'], w=['ga'])
                kb.dma('pool', lambda e: e.dma_start(out=ACTT.rearrange("(k p) t -> p k t", p=128), in_=ga[:]), r=['ga'], w=['ACTT'])
            gemm_group("sd", SHD, 2, 1024, ACTT, 'ACTT', own_blocks,
                       [dict(c0=0, cn=512, mode='tm', func=None, out=SHOUT, okey='SHOUT', oc0=0),
                        dict(c0=512, cn=512, mode='tm', func=None, out=SHOUT, okey='SHOUT', oc0=512)])
            if stages >= 7 and not dummy_mix:
                with stage() as st:
                    a2row = sbt(st, "a2row", [128, D]); g2nrow = sbt(st, "g2nrow", [128, D])
                    kb.dma('sp', lambda e: e.dma_start(out=a2row[:], in_=MODROW[0:1, 4096:5120].to_broadcast([128, D])), r=['MODROW'], w=['a2row'])
                    kb.dma('sp', lambda e: e.dma_start(out=g2nrow[:], in_=N2G.rearrange("(o n) -> o n", o=1).to_broadcast([128, D])), w=['g2nrow'])
                    kb.op('dve', lambda e: e.scalar_tensor_tensor(out=a2row[:], in0=a2row[:], scalar=1.0, in1=g2nrow[:], op0=ALU.add, op1=ALU.mult),
                          r=['a2row', 'g2nrow'], w=['a2row'])
                    sh2row = row_bcast(st, "sh2row", MODROW[0:1, 3072:4096])
                    xa = [sbt(st, "hxa%d" % i, [128, D]) for i in range(2)]; stt = [sbt(st, "hst%d" % i, [128, 4]) for i in range(2)]
                    junk = sbt(st, "hjunk", [128, D])
                    for ti in range(OWN // 128):
                        b2 = ti % 2; xk, tk = 'hxa%d' % b2, 'hst%d' % b2
                        rows = slice(ti * 128, (ti + 1) * 128)
                        kb.dma('sp', lambda e: e.dma_start(out=xa[b2][:], in_=X1D[rows, :]), r=['X1D'], w=[xk])
                        kb.op('act', lambda e: e.activation(out=junk[:], in_=xa[b2][:], func=AF.Square, accum_out=stt[b2][:, 0:1]), r=[xk], w=['hjunk', tk])
                        kb.op('dve', lambda e: e.tensor_scalar(out=stt[b2][:, 1:2], in0=stt[b2][:, 0:1], scalar1=1.0 / D, scalar2=EPS, op0=ALU.mult, op1=ALU.add), r=[tk], w=[tk])
                        kb.op('act', lambda e: e.activation(out=stt[b2][:, 2:3], in_=stt[b2][:, 1:2], func=AF.Sqrt), r=[tk], w=[tk])
                        kb.op('dve', lambda e: e.reciprocal(out=stt[b2][:, 3:4], in_=stt[b2][:, 2:3]), r=[tk], w=[tk])
                        kb.op('dve', lambda e: e.scalar_tensor_tensor(out=xa[b2][:], in0=xa[b2][:], scalar=stt[b2][:, 3:4], in1=a2row[:], op0=ALU.mult, op1=ALU.mult),
                              r=[xk, tk, 'a2row'], w=[xk])
                        kb.op('pool', lambda e: e.tensor_tensor(out=xa[b2][:], in0=xa[b2][:], in1=sh2row[:], op=ALU.add), r=[xk, 'sh2row'], w=[xk])
                        kb.dma('pool', lambda e: e.dma_start(out=H2[rows, :], in_=xa[b2][:]), r=[xk], w=['H2'])
                gemm_group("rt", RW, 8, NE, H2T, 'H2T', own_blocks, [dict(c0=0, cn=NE, mode='tm', func=AF.Sigmoid, out=SCORES, okey='SCORES', oc0=0)])
                with stage() as st:
                    NT = OWN // 128
                    onesm = sbt(st, "onesm", [128, 128]); stri = sbt(st, "stri", [128, 128]); slt = sbt(st, "slt", [128, 512])
                    blk128 = sbt(st, "blk128", [128, NBLK]); pidx = sbt(st, "pidx", [128, 1]); brow_ = sbt(st, "rbrow", [128, NE])
                    kb.dma('sp', lambda e: e.dma_start(out=onesm[:], in_=ONESd), w=['onesm'])
                    kb.dma('sp', lambda e: e.dma_start(out=stri[:], in_=STRId), w=['stri'])
                    kb.dma('sp', lambda e: e.dma_start(out=slt[:], in_=SLTd), w=['slt'])
                    kb.dma('sp', lambda e: e.dma_start(out=blk128[:], in_=BLKd), w=['blk128'])
                    kb.dma('sp', lambda e: e.dma_start(out=pidx[:], in_=PIDXd), w=['pidx'])
                    kb.dma('sp', lambda e: e.dma_start(out=brow_[:], in_=RB.rearrange("(o n) -> o n", o=1).to_broadcast([128, NE])), w=['rbrow'])
                    D8F = sbt(st, "D8F", [128, NT, 8]); W8 = sbt(st, "W8", [128, NT, 8]); D8I = sbt(st, "D8I", [128, NT * 8], I32); GI = sbt(st, "GI", [128, NBLK], I32)
                    with stage() as sr:
                        MSK = sbt(sr, "MSK", [128, NT, NE]); SEL = sbt(sr, "SEL", [128, NT, NE]); WD = sbt(sr, "WDm", [128, NT, NE]); DST = sbt(sr, "DST", [128, NT, NE])
                        V8 = sbt(sr, "V8", [128, NT, 8])
                        sc_ = [sbt(sr, "rsc%d" % i, [128, NE]) for i in range(2)]; bs = sbt(sr, "rbs", [128, NE])
                        M8 = sbt(sr, "M8", [128, 8, 8]); gs = sbt(sr, "rgs", [128, 8]); g8 = sbt(sr, "rg8", [128, 8]); gm = sbt(sr, "rgm", [128, 8]); pen = sbt(sr, "rpen", [128, 8])
                        den = sbt(sr, "rden", [128, 2]); base = sbt(sr, "rbase", [128, NE]); tmpq = sbt(sr, "rtmpq", [128, NE])
                        kb.op('pool', lambda e: e.memset(base[:], 0.0), w=['rbase'])
                        for ti in range(NT):
                            b2 = ti % 2; sk_ = 'rsc%d' % b2
                            kb.dma('sp', lambda e: e.dma_start(out=sc_[b2][:], in_=SCORES[ti * 128:(ti + 1) * 128, :]), r=['SCORES'], w=[sk_])
                            kb.op('dve', lambda e: e.tensor_tensor(out=bs[:], in0=sc_[b2][:], in1=brow_[:], op=ALU.add), r=[sk_, 'rbrow'], w=['rbs'])
                            for g in range(8):
                                kb.op('dve', lambda e: e.max(out=M8[:, g, :], in_=bs[:, 32 * g:32 * g + 32]), r=['rbs', 'M8'], w=['M8'])
                            kb.op('dve', lambda e: e.tensor_tensor(out=gs[:], in0=M8[:, :, 0], in1=M8[:, :, 1], op=ALU.add), r=['M8'], w=['rgs'])
                            kb.op('dve', lambda e: e.max(out=g8[:], in_=gs[:]), r=['rgs'], w=['rg8'])
                            kb.op('dve', lambda e: e.tensor_scalar(out=gm[:], in0=gs[:], scalar1=g8[:, 3:4], scalar2=None, op0=ALU.is_ge), r=['rgs', 'rg8'], w=['rgm'])
                            kb.op('dve', lambda e: e.tensor_scalar(out=pen[:], in0=gm[:], scalar1=-1.0, scalar2=1e30, op0=ALU.add, op1=ALU.mult), r=['rgm'], w=['rpen'])
                            for g in range(8):
                                kb.op('dve', lambda e: e.tensor_scalar(out=MSK[:, ti, 32 * g:32 * g + 32], in0=bs[:, 32 * g:32 * g + 32], scalar1=gm[:, g:g + 1], scalar2=pen[:, g:g + 1],
                                                                       op0=ALU.mult, op1=ALU.add), r=['rbs', 'rgm', 'rpen', 'MSK'], w=['MSK'])
                            kb.op('dve', lambda e: e.max(out=V8[:, ti, :], in_=MSK[:, ti, :]), r=['MSK', 'V8'], w=['V8'])
                            kb.op('dve', lambda e: e.tensor_scalar(out=SEL[:, ti, :], in0=MSK[:, ti, :], scalar1=V8[:, ti, 7:8], scalar2=None, op0=ALU.is_ge), r=['MSK', 'V8', 'SEL'], w=['SEL'])
                            kb.op('dve', lambda e: e.tensor_tensor(out=WD[:, ti, :], in0=SEL[:, ti, :], in1=sc_[b2][:], op=ALU.mult), r=['SEL', sk_, 'WDm'], w=['WDm'])
                            kb.op('dve', lambda e: e.tensor_reduce(out=den[:, 0:1], in_=WD[:, ti, :], axis=AX.X, op=ALU.add), r=['WDm', 'rden'], w=['rden'])
                            kb.op('dve', lambda e: e.reciprocal(out=den[:, 1:2], in_=den[:, 0:1]), r=['rden'], w=['rden'])
                            kb.op('dve', lambda e: e.tensor_scalar(out=WD[:, ti, :], in0=WD[:, ti, :], scalar1=den[:, 1:2], scalar2=2.5, op0=ALU.mult, op1=ALU.mult), r=['WDm', 'rden'], w=['WDm'])
                            p1, p1k = banks[(2 * ti) % 8]; p2, p2k = banks[(2 * ti + 1) % 8]
                            kb.op('pe', lambda e: e.matmul(p1[:, 0:NE], lhsT=stri[:], rhs=SEL[:, ti, :], start=True, stop=True), r=['stri', 'SEL'], w=[p1k])
                            kb.op('pe', lambda e: e.matmul(p2[:, 0:NE], lhsT=onesm[:], rhs=SEL[:, ti, :], start=True, stop=True), r=['onesm', 'SEL'], w=[p2k])
                            kb.op('dve', lambda e: e.tensor_tensor(out=DST[:, ti, :], in0=p1[:, 0:NE], in1=base[:], op=ALU.add), r=[p1k, 'rbase', 'DST'], w=['DST'])
                            kb.op('dve', lambda e: e.tensor_tensor(out=base[:], in0=p2[:, 0:NE], in1=base[:], op=ALU.add), r=[p2k, 'rbase'], w=['rbase'])
                        ci = sbt(sr, "rci", [128, NE], I32); padded = sbt(sr, "rpad", [128, NE]); pstart = sbt(sr, "rpst", [128, NE]); pend = sbt(sr, "rpend", [128, NE])
                        kb.op('dve', lambda e: e.tensor_scalar(out=tmpq[:], in0=base[:], scalar1=127.0, scalar2=None, op0=ALU.add), r=['rbase'], w=['rtmpq'])
                        kb.op('dve', lambda e: e.tensor_copy(out=ci[:], in_=tmpq[:]), r=['rtmpq'], w=['rci'])
                        kb.op('dve', lambda e: e.tensor_scalar(out=ci[:], in0=ci[:], scalar1=7, scalar2=None, op0=ALU.arith_shift_right), r=['rci'], w=['rci'])
                        kb.op('dve', lambda e: e.tensor_scalar(out=ci[:], in0=ci[:], scalar1=7, scalar2=None, op0=ALU.logical_shift_left), r=['rci'], w=['rci'])
                        kb.op('dve', lambda e: e.tensor_copy(out=padded[:], in_=ci[:]), r=['rci'], w=['rpad'])
                        padT = sbt(sr, "rpadT", [128, 2, 128]); pendT = sbt(sr, "rpendT", [128, 2, 128])
                        pa, pak = banks[0]
                        for hh in range(2):
                            kb.op('pe', lambda e: e.transpose(out=pa[:, hh * 128:(hh + 1) * 128], in_=padded[:, hh * 128:(hh + 1) * 128], identity=ident[:]), r=['rpad', 'ident'], w=[pak])
                        kb.op('dve', lambda e: e.tensor_copy(out=padT[:], in_=pa[:, 0:256].rearrange("p (h c) -> p h c", h=2)), r=[pak], w=['rpadT'])
                        pb_, pbk_ = banks[1]
                        for hh in range(2):
                            kb.op('pe', lambda e: e.matmul(pb_[:, 0:NE], lhsT=padT[:, hh, :], rhs=slt[:, hh * 256:(hh + 1) * 256], start=(hh == 0), stop=(hh == 1)), r=['rpadT', 'slt'], w=[pbk_])
                        kb.op('dve', lambda e: e.tensor_copy(out=pstart[:], in_=pb_[:, 0:NE]), r=[pbk_], w=['rpst'])
                        kb.op('dve', lambda e: e.tensor_tensor(out=pend[:], in0=pstart[:], in1=padded[:], op=ALU.add), r=['rpst', 'rpad'], w=['rpend'])
                        pc_, pck_ = banks[2]
                        for hh in range(2):
                            kb.op('pe', lambda e: e.transpose(out=pc_[:, hh * 128:(hh + 1) * 128], in_=pend[:, hh * 128:(hh + 1) * 128], identity=ident[:]), r=['rpend', 'ident'], w=[pck_])
                        kb.op('dve', lambda e: e.tensor_copy(out=pendT[:], in_=pc_[:, 0:256].rearrange("p (h c) -> p h c", h=2)), r=[pck_], w=['rpendT'])
                        cmpT = sbt(sr, "rcmpT", [128, 2, NBLK]); bef = sbt(sr, "rbef", [128, NBLK])
                        for hh in range(2):
                            kb.op('dve', lambda e: e.tensor_scalar(out=cmpT[:, hh, :], in0=blk128[:], scalar1=pendT[:, hh, 0:1], scalar2=None, op0=ALU.is_ge), r=['blk128', 'rpendT', 'rcmpT'], w=['rcmpT'])
                        pd_, pdk_ = banks[3]
                        for hh in range(2):
                            kb.op('pe', lambda e: e.matmul(pd_[:, 0:NBLK], lhsT=onesm[:], rhs=cmpT[:, hh, :], start=(hh == 0), stop=(hh == 1)), r=['onesm', 'rcmpT'], w=[pdk_])
                        kb.op('dve', lambda e: e.tensor_scalar(out=bef[:], in0=pd_[:, 0:NBLK], scalar1=255.0, scalar2=128.0, op0=ALU.min, op1=ALU.mult), r=[pdk_], w=['rbef'])
                        kb.op('dve', lambda e: e.tensor_scalar(out=bef[:], in0=bef[:], scalar1=pidx[:, 0:1], scalar2=None, op0=ALU.add), r=['rbef', 'pidx'], w=['rbef'])
                        kb.op('dve', lambda e: e.tensor_copy(out=GI[:], in_=bef[:]), r=['rbef'], w=['GI'])
                        eqj = sbt(sr, "reqj", [128, NE])
                        for ti in range(NT):
                            kb.op('dve', lambda e: e.tensor_tensor(out=DST[:, ti, :], in0=DST[:, ti, :], in1=pstart[:], op=ALU.add), r=['DST', 'rpst'], w=['DST'])
                            for k8 in range(8):
                                kb.op('dve', lambda e: e.scalar_tensor_tensor(out=eqj[:], in0=MSK[:, ti, :], scalar=V8[:, ti, k8:k8 + 1], in1=DST[:, ti, :], op0=ALU.is_equal, op1=ALU.mult),
                                      r=['MSK', 'V8', 'DST', 'reqj'], w=['reqj'])
                                kb.op('dve', lambda e: e.tensor_reduce(out=D8F[:, ti, k8:k8 + 1], in_=eqj[:], axis=AX.X, op=ALU.add), r=['reqj', 'D8F'], w=['D8F'])
                                kb.op('dve', lambda e: e.scalar_tensor_tensor(out=eqj[:], in0=MSK[:, ti, :], scalar=V8[:, ti, k8:k8 + 1], in1=WD[:, ti, :], op0=ALU.is_equal, op1=ALU.mult),
                                      r=['MSK', 'V8', 'WDm', 'reqj'], w=['reqj'])
                                kb.op('dve', lambda e: e.tensor_reduce(out=W8[:, ti, k8:k8 + 1], in_=eqj[:], axis=AX.X, op=ALU.add), r=['reqj', 'W8'], w=['W8'])
                        kb.op('dve', lambda e: e.tensor_copy(out=D8I[:], in_=D8F[:].rearrange("p t k -> p (t k)")), r=['D8F'], w=['D8I'])
                    dbg('D8F', D8F[:], [128, NT, 8]); dbg('W8', W8[:], [128, NT, 8])
                    ht = [sbt(st, "dht%d" % i, [128, D]) for i in range(2)]
                    for ti in range(NT):
                        b2 = ti % 2; hk = 'dht%d' % b2
                        kb.dma('sp', lambda e: e.dma_start(out=ht[b2][:], in_=H2[ti * 128:(ti + 1) * 128, :]), r=['H2'], w=[hk])
                        for k8 in range(8):
                            kb.dma('pool', lambda e: e.indirect_dma_start(out=XS, out_offset=bass.IndirectOffsetOnAxis(ap=D8I[:, ti * 8 + k8:ti * 8 + k8 + 1], axis=0), in_=ht[b2][:], in_offset=None,
                                                                         bounds_check=RB_XS, oob_is_err=False), r=[hk, 'D8I'], w=['XSw%d_%d' % (ti, k8)])
                    kb.barrier()
                    NW = 3
                    wgu = [sbt(st, "wgu%d" % i, [128, 2, 8, 256]) for i in range(NW)]; wdn = [sbt(st, "wdn%d" % i, [128, 2, D]) for i in range(4)]
                    xs = [sbt(st, "xs%d" % i, [128, D]) for i in range(4)]; xsT = [sbt(st, "xsT%d" % i, [128, 8, 128]) for i in range(3)]
                    actT = [sbt(st, "actT%d" % i, [128, 2, 128]) for i in range(3)]; sg_ = [sbt(st, "esg%d" % i, [128, 256]) for i in range(3)]
                    ys = [sbt(st, "ys%d" % i, [128, D]) for i in range(2)]
                    bctr = [0]

                    def bk():
                        b_ = banks[bctr[0] % 8]; bctr[0] += 1
                        return b_

                    def phA(blk):
                        wk, dk, xk, xtk = 'wgu%d' % (blk % NW), 'wdn%d' % (blk % 4), 'xs%d' % (blk % 4), 'xsT%d' % (blk % 3)
                        kb.dma('pool', lambda e: e.indirect_dma_start(out=wgu[blk % NW][:].rearrange("p a k f -> p (a k f)"), out_offset=None, in_=EWGU,
                                                                     in_offset=bass.IndirectOffsetOnAxis(ap=GI[:, blk:blk + 1], axis=0),
                                                                     bounds_check=RB_W, oob_is_err=False), r=['GI'], w=[wk])
                        kb.dma('pool', lambda e: e.indirect_dma_start(out=wdn[blk % 4][:].rearrange("p k f -> p (k f)"), out_offset=None, in_=EWD,
                                                                     in_offset=bass.IndirectOffsetOnAxis(ap=GI[:, blk:blk + 1], axis=0),
                                                                     bounds_check=RB_W, oob_is_err=False), r=['GI'], w=[dk])
                        for hh in range(2):
                            pb, pbk = bk()
                            for kk in range(4):
                                k8 = hh * 4 + kk
                                kb.op('pe', lambda e: e.transpose(out=pb[:, kk * 128:(kk + 1) * 128], in_=xs[blk % 4][:, k8 * 128:(k8 + 1) * 128], identity=ident[:]), r=[xk, 'ident'], w=[pbk])
                            evac(xsT[blk % 3][:, hh * 4:hh * 4 + 4, :], pb[:, :].rearrange("p (k t) -> p k t", k=4), None, [pbk, xtk], [xtk])

                    def phB(blk):
                        wk, xtk, sgk = 'wgu%d' % (blk % NW), 'xsT%d' % (blk % 3), 'esg%d' % (blk % 3)
                        ph, phk = bk()
                        for kk in range(8):
                            kb.op('pe', lambda e: e.matmul(ph[:, :].rearrange("p (a f) -> p a f", a=2), lhsT=xsT[blk % 3][:, kk, :], rhs=wgu[blk % NW][:, :, kk, :],
                                                           start=(kk == 0), stop=(kk == 7)), r=[wk, xtk], w=[phk])
                        kb.op('act', lambda e: e.activation(out=sg_[blk % 3][:], in_=ph[:, 0:256], func=AF.Silu), r=[phk], w=[sgk])
                        kb.op('dve', lambda e: e.tensor_tensor(out=sg_[blk % 3][:], in0=ph[:, 256:512], in1=sg_[blk % 3][:], op=ALU.mult), r=[phk, sgk], w=[sgk])

                    def phC(blk):
                        sgk, ak = 'esg%d' % (blk % 3), 'actT%d' % (blk % 3)
                        pt_, ptk = bk()
                        for kk in range(2):
                            kb.op('pe', lambda e: e.transpose(out=pt_[:, kk * 128:(kk + 1) * 128], in_=sg_[blk % 3][:, kk * 128:(kk + 1) * 128], identity=ident[:]), r=[sgk, 'ident'], w=[ptk])
                        evac(actT[blk % 3][:].rearrange("p k t -> p (k t)"), pt_[:, 0:256], None, [ptk, ak], [ak])

                    def phD(blk):
                        dk, ak, yk = 'wdn%d' % (blk % 4), 'actT%d' % (blk % 3), 'ys%d' % (blk % 2)
                        for hf in range(2):
                            py, pyk = bk()
                            for kk in range(2):
                                kb.op('pe', lambda e: e.matmul(py[:, :], lhsT=actT[blk % 3][:, kk, :], rhs=wdn[blk % 4][:, kk, hf * 512:(hf + 1) * 512], start=(kk == 0), stop=(kk == 1)), r=[ak, dk], w=[pyk])
                            evac(ys[blk % 2][:, hf * 512:(hf + 1) * 512], py[:, :], None, [pyk, yk], [yk])
                        kb.dma('sp', lambda e: e.dma_start(out=YS[blk * 128:(blk + 1) * 128, :], in_=ys[blk % 2][:]), r=[yk], w=['YSw%d' % blk])

                    def ldx(blk):
                        kb.dma('sp', lambda e: e.dma_start(out=xs[blk % 4][:], in_=XS[blk * 128:(blk + 1) * 128, :]), w=['xs%d' % (blk % 4)])

                    ldx(0); ldx(1)
                    for s_ in range(NBLK + 3):
                        if s_ + 2 < NBLK:
                            ldx(s_ + 2)
                        if s_ < NBLK:
                            phA(s_)
                        if 0 <= s_ - 1 < NBLK:
                            phB(s_ - 1)
                        if 0 <= s_ - 2 < NBLK:
                            phC(s_ - 2)
                        if 0 <= s_ - 3 < NBLK:
                            phD(s_ - 3)
                    kb.barrier()
                    acc = [sbt(st, "cacc%d" % i, [128, D]) for i in range(2)]; gg = [sbt(st, "cg%d" % i, [128, D]) for i in range(3)]
                    gi_ = 0
                    for ti in range(NT):
                        b2 = ti % 2; ack = 'cacc%d' % b2
                        for k8 in range(8):
                            g3 = gi_ % 3; gi_ += 1; ggk = 'cg%d' % g3
                            kb.dma('pool', lambda e: e.indirect_dma_start(out=gg[g3][:], out_offset=None, in_=YS, in_offset=bass.IndirectOffsetOnAxis(ap=D8I[:, ti * 8 + k8:ti * 8 + k8 + 1], axis=0),
                                                                         bounds_check=RB_XS, oob_is_err=False), r=['D8I'], w=[ggk])
                            if k8 == 0:
                                kb.op('dve', lambda e: e.tensor_scalar(out=acc[b2][:], in0=gg[g3][:], scalar1=W8[:, ti, 0:1], scalar2=None, op0=ALU.mult), r=[ggk, 'W8', ack], w=[ack])
                            else:
                                kb.op('dve', lambda e: e.scalar_tensor_tensor(out=acc[b2][:], in0=gg[g3][:], scalar=W8[:, ti, k8:k8 + 1], in1=acc[b2][:], op0=ALU.mult, op1=ALU.add),
                                      r=[ggk, 'W8', ack], w=[ack])
                        kb.dma('sp', lambda e: e.dma_start(out=ROUTED[ti * 128:(ti + 1) * 128, :], in_=acc[b2][:]), r=[ack], w=['ROUTED'])

            with stage() as st:
                g2row = row_bcast(st, "g2row", MODROW[0:1, 5120:6144])
                fgrow = sbt(st, "fgrow", [128, D])
                kb.dma('sp', lambda e: e.dma_start(out=fgrow[:], in_=FING.rearrange("(o n) -> o n", o=1).to_broadcast([128, D])), w=['fgrow'])
                xa = [sbt(st, "xa%d" % i, [128, D]) for i in range(2)]; sa = [sbt(st, "sa%d" % i, [128, D]) for i in range(2)]
                ra = [sbt(st, "ra%d" % i, [128, D]) for i in range(2)]; stt = [sbt(st, "stt%d" % i, [128, 4]) for i in range(2)]
                junk = sbt(st, "fjunk", [128, D])
                for ti in range(OWN // 128):
                    b2 = ti % 2; xk, sk, rk, tk = 'xa%d' % b2, 'sa%d' % b2, 'ra%d' % b2, 'stt%d' % b2
                    rows = slice(ti * 128, (ti + 1) * 128)
                    kb.dma('sp', lambda e: e.dma_start(out=xa[b2][:], in_=X1D[rows, :]), r=['X1D'], w=[xk])
                    kb.dma('sp', lambda e: e.dma_start(out=sa[b2][:], in_=SHOUT[rows, :]), r=['SHOUT'], w=[sk])
                    kb.dma('sp', lambda e: e.dma_start(out=ra[b2][:], in_=ROUTED[rows, :]), r=['ROUTED'], w=[rk])
                    kb.op('pool', lambda e: e.tensor_tensor(out=sa[b2][:], in0=sa[b2][:], in1=ra[b2][:], op=ALU.add), r=[sk, rk], w=[sk])
                    kb.op('dve', lambda e: e.tensor_tensor(out=sa[b2][:], in0=sa[b2][:], in1=g2row[:], op=ALU.mult), r=[sk, 'g2row'], w=[sk])
                    kb.op('pool', lambda e: e.tensor_tensor(out=xa[b2][:], in0=xa[b2][:], in1=sa[b2][:], op=ALU.add), r=[xk, sk], w=[xk])
                    kb.op('act', lambda e: e.activation(out=junk[:], in_=xa[b2][:], func=AF.Square, accum_out=stt[b2][:, 0:1]), r=[xk], w=['fjunk', tk])
                    kb.op('dve', lambda e: e.tensor_scalar(out=stt[b2][:, 1:2], in0=stt[b2][:, 0:1], scalar1=1.0 / D, scalar2=EPS, op0=ALU.mult, op1=ALU.add), r=[tk], w=[tk])
                    kb.op('act', lambda e: e.activation(out=stt[b2][:, 2:3], in_=stt[b2][:, 1:2], func=AF.Sqrt), r=[tk], w=[tk])
                    kb.op('dve', lambda e: e.reciprocal(out=stt[b2][:, 3:4], in_=stt[b2][:, 2:3]), r=[tk], w=[tk])
                    kb.op('dve', lambda e: e.scalar_tensor_tensor(out=xa[b2][:], in0=xa[b2][:], scalar=stt[b2][:, 3:4], in1=fgrow[:], op0=ALU.mult, op1=ALU.mult),
                          r=[xk, tk, 'fgrow'], w=[xk])
                    kb.dma('pool', lambda e: e.dma_start(out=OUT[rows, :], in_=xa[b2][:]), r=[xk], w=['OUT'])

        kb.finish('sp')
        kb.finish('pool')
        pg.ninstr = kb.ninstr
    return pg


_PROG = None


def make_in_maps(pg, inputs):
    hc = host_consts()
    sq = lambda a: np.ascontiguousarray(a[0])
    in_maps = []
    shared = {}
    if 'EWGU' in pg.ins:
        wg = np.asarray(inputs['exp_w_gate'])[0].reshape(NE, 8, 128, 256)
        wu = np.asarray(inputs['exp_w_up'])[0].reshape(NE, 8, 128, 256)
        ew = np.empty((NE, 128, 2, 8, 256), np.float32)
        ew[:, :, 0] = wg.transpose(0, 2, 1, 3); ew[:, :, 1] = wu.transpose(0, 2, 1, 3)
        shared['EWGU'] = ew.reshape(NE * 128, 4096)
        shared['EWD'] = np.ascontiguousarray(np.asarray(inputs['exp_w_down'])[0].reshape(NE, 2, 128, D).transpose(0, 2, 1, 3)).reshape(NE * 128, 2048)
    for c in range(8):
        b, j = c // 4, c % 4
        own = slice(j * OWN, (j + 1) * OWN)
        idx = ((np.arange(4)[None, :] * 128 + np.arange(128)[:, None]) * 4 + j).astype(np.int32)
        full = {
            'x': inputs['x'][b], 'ctx': inputs['ctx'][b], 'xown': inputs['x'][b, own], 'posown': hc['POS'][own],
            'c': inputs['c'][b], 'c_ctx': inputs['c_ctx'], 'final_g': inputs['final_g'], 'OWNIDX': idx,
            'hg_lb_logits': np.asarray(inputs['hg_lb_logits']).reshape(2, 1024),
        }
        full.update(shared)
        for k in pg.ins:
            if k not in full and k not in hc:
                full[k] = sq(inputs[k])
        full.update(hc)
        in_maps.append({k: np.ascontiguousarray(np.asarray(full[k])) for k in pg.ins})
    return in_maps


def kernel(**inputs):
    global _PROG
    if _PROG is None:
        _PROG = build()
    pg = _PROG
    in_maps = make_in_maps(pg, inputs)
    res = run_bass_kernel_spmd(pg.nc, in_maps, core_ids=list(range(8)))
    out = np.zeros((2, L, D), np.float32)
    for c in range(8):
        b, j = c // 4, c % 4
        out[b, j * OWN:(j + 1) * OWN] = res.results[c]['out']
    return out
```
